# Optimizing a Trainium2 kernel written in Bass

```python
import jax
import jax.numpy as jnp
from jax import lax
import numpy as np

D_MODEL = 1024
BATCH = 8
SEQ = 4096
DEPTH = 4

CTX_LEN = 256
GRID_W = 64
EPS = 1e-6

MLA_HEADS = 8
MLA_Q_RANK = 256
MLA_KV_RANK = 128
MLA_NOPE = 64
MLA_ROPE = 32
MLA_V = 64
MLA_SCALE = (MLA_NOPE + MLA_ROPE) ** -0.5
ROPE_AXIS = MLA_ROPE // 2
ROPE_BASE = 10000.0
Q_BLOCK = 128

FNET_GROUPS = 4
FNET_GROUP_W = 128

GLA_HEADS = 4
GLA_DK = 64
GLA_DV = 128
GLA_GATE_RANK = 16
GLA_TAU = 16.0
GLA_CHUNK = 64
GLA_SCALE = GLA_DK ** -0.5

N_EXPERTS = 32
TOP_K = 4
D_EXPERT = D_MODEL
SWIGLU_ALPHA = 1.702
SWIGLU_LIMIT = 7.0
MOE_BLOCK = 256

MLA_W = MLA_HEADS * MLA_V
FNET_W = FNET_GROUPS * FNET_GROUP_W
GLA_W = GLA_HEADS * GLA_DV
N_BRANCH = 3
IN_SPLITS = (MLA_Q_RANK, MLA_KV_RANK + MLA_ROPE, FNET_W, GLA_HEADS * GLA_DK, GLA_HEADS * GLA_DK, GLA_W, GLA_W, GLA_GATE_RANK, GLA_GATE_RANK, N_BRANCH * D_MODEL)
IN_WIDTH = sum(IN_SPLITS)

kernel_name = 'hybrid_mla_fourier_gla_moe_dit'


def rms_norm(x, g):
    xf = x.astype(jnp.float32)
    y = xf * lax.rsqrt(jnp.mean(xf * xf, axis=-1, keepdims=True) + EPS)
    return (y * g.astype(jnp.float32)).astype(x.dtype)


def modulate(h, shift, scale):
    return h * (1 + scale) + shift


def split_in(u):
    idx = [int(i) for i in np.cumsum(IN_SPLITS)[:-1]]
    return jnp.split(u, idx, axis=-1)


def axial_rope_angles(rows, dtype):
    row = jnp.repeat(jnp.arange(rows, dtype=jnp.float32), GRID_W)
    col = jnp.tile(jnp.arange(GRID_W, dtype=jnp.float32), rows)
    inv_freq = ROPE_BASE ** (-jnp.arange(0, ROPE_AXIS, 2, dtype=jnp.float32) / ROPE_AXIS)
    ang_r = (row[:, None] * inv_freq)[:, None, :]
    ang_c = (col[:, None] * inv_freq)[:, None, :]
    return (jnp.cos(ang_r).astype(dtype), jnp.sin(ang_r).astype(dtype),
            jnp.cos(ang_c).astype(dtype), jnp.sin(ang_c).astype(dtype))


def rope_axis(x, cos, sin):
    x1, x2 = jnp.split(x, 2, axis=-1)
    return jnp.concatenate([x1 * cos - x2 * sin, x2 * cos + x1 * sin], axis=-1)


def rope_2d(x, rope):
    cos_r, sin_r, cos_c, sin_c = rope
    xr, xc = jnp.split(x, 2, axis=-1)
    return jnp.concatenate([rope_axis(xr, cos_r, sin_r), rope_axis(xc, cos_c, sin_c)], axis=-1)


def mla_qkv(u_q, u_kv, q_norm_g, w_uq, kv_norm_g, w_ukv, rope):
    b, t, _ = u_q.shape
    q = (rms_norm(u_q, q_norm_g) @ w_uq).reshape(b, t, MLA_HEADS, MLA_NOPE + MLA_ROPE)
    c_kv, k_rope = jnp.split(u_kv, [MLA_KV_RANK], axis=-1)
    kv = (rms_norm(c_kv, kv_norm_g) @ w_ukv).reshape(b, t, MLA_HEADS, MLA_NOPE + MLA_V)
    k_nope, v = jnp.split(kv, [MLA_NOPE], axis=-1)
    q_nope, q_rope = jnp.split(q, [MLA_NOPE], axis=-1)
    k_rope = k_rope[:, :, None, :]
    if rope is not None:
        q_rope = rope_2d(q_rope, rope)
        k_rope = rope_2d(k_rope, rope)
    q = jnp.concatenate([q_nope, q_rope], axis=-1)
    k = jnp.concatenate([k_nope, jnp.broadcast_to(k_rope, (b, t, MLA_HEADS, MLA_ROPE))], axis=-1)
    return q, k, v


def attend(q, k, v):
    s = jnp.einsum('bqhd,bkhd->bhqk', q, k).astype(jnp.float32) * MLA_SCALE
    p = jax.nn.softmax(s, axis=-1).astype(v.dtype)
    return jnp.einsum('bhqk,bkhd->bqhd', p, v)


def attend_latent_blocks(q, k, v):
    b, t, h, dk = q.shape
    nb = t // Q_BLOCK
    qb = jnp.moveaxis(q.reshape(b, nb, Q_BLOCK, h, dk), 1, 0)
    o = lax.map(lambda qq: attend(qq, k, v), qb)
    return jnp.moveaxis(o, 0, 1).reshape(b, t, h * v.shape[-1])


def fourier_mix(u):
    b, t, _ = u.shape
    uf = u.astype(jnp.float32).reshape(b, t, FNET_GROUPS, FNET_GROUP_W)
    y = jnp.fft.fft2(uf, axes=(1, 3), norm='ortho').real
    return y.reshape(b, t, FNET_W).astype(u.dtype)


def gla_prepare(u_q, u_k, u_v, u_gf, u_gb, w_gf, b_gf, w_gb, b_gb):
    b, t, _ = u_q.shape
    q = u_q.reshape(b, t, GLA_HEADS, GLA_DK) * GLA_SCALE
    k = u_k.reshape(b, t, GLA_HEADS, GLA_DK)
    v = u_v.reshape(b, t, GLA_HEADS, GLA_DV)
    lf = (jax.nn.log_sigmoid((u_gf @ w_gf + b_gf).astype(jnp.float32)) / GLA_TAU).reshape(b, t, GLA_HEADS, GLA_DK)
    lb = (jax.nn.log_sigmoid((u_gb @ w_gb + b_gb).astype(jnp.float32)) / GLA_TAU).reshape(b, t, GLA_HEADS, GLA_DK)
    return q, k, v, lf, lb


def gla_scan(q, k, v, logf, s0):
    b, t, h, _ = q.shape
    nc = t // GLA_CHUNK

    def chunks(a):
        return a.astype(jnp.float32).reshape(b, nc, GLA_CHUNK, h, a.shape[-1]).transpose(1, 0, 3, 2, 4)

    lower = jnp.tril(jnp.ones((GLA_CHUNK, GLA_CHUNK), dtype=bool))[:, :, None]

    def step(state, xs):
        qc, kc, vc, fc = xs
        g = jnp.cumsum(fc, axis=2)
        o_inter = jnp.einsum('bhld,bhde->bhle', qc * jnp.exp(g), state)
        diff = jnp.where(lower, g[:, :, :, None, :] - g[:, :, None, :, :], -jnp.inf)
        a = jnp.einsum('bhtd,bhsd,bhtsd->bhts', qc, kc, jnp.exp(diff))
        o_intra = jnp.einsum('bhts,bhse->bhte', a, vc)
        g_last = g[:, :, -1:, :]
        new_state = (jnp.exp(g_last[:, :, 0, :])[..., None] * state
                     + jnp.einsum('bhsd,bhse->bhde', kc * jnp.exp(g_last - g), vc))
        return new_state, o_inter + o_intra

    s_fin, o = lax.scan(step, s0, (chunks(q), chunks(k), chunks(v), chunks(logf)))
    o = o.transpose(1, 0, 3, 2, 4).reshape(b, t, h, v.shape[-1])
    return o, s_fin


def gla_output(o, u_og, norm_g):
    b, t = u_og.shape[:2]
    o = rms_norm(o.astype(u_og.dtype), norm_g.reshape(GLA_HEADS, GLA_DV))
    return (o * jax.nn.silu(u_og.reshape(b, t, GLA_HEADS, GLA_DV))).reshape(b, t, GLA_W)


def merge_branches(o_mla, o_fn, o_gla, gate_pre, w_br_mla, w_br_fnet, w_br_gla, w_o):
    ga, gf, gg = jnp.split(jax.nn.sigmoid(gate_pre), N_BRANCH, axis=-1)
    y = ga * (o_mla @ w_br_mla) + gf * (o_fn @ w_br_fnet) + gg * (o_gla @ w_br_gla)
    return y @ w_o


def flip(a):
    return a[:, ::-1]


def token_mixers(h_ctx, h_lat, w_in, mla_q_norm_g, mla_w_uq, mla_kv_norm_g, mla_w_ukv,
                 gla_w_gate_f, gla_b_gate_f, gla_w_gate_b, gla_b_gate_b, gla_norm_g,
                 w_br_mla, w_br_fnet, w_br_gla, w_o, rope, need_ctx):
    b, n_ctx, _ = h_ctx.shape
    lq, lkv, lfn, lgq, lgk, lgv, log_, lgf, lgb, lgate = split_in(h_lat @ w_in)
    cq, ckv, cfn, cgq, cgk, cgv, cog, cgf, cgb, cgate = split_in(h_ctx @ w_in)

    q_l, k_l, v_l = mla_qkv(lq, lkv, mla_q_norm_g, mla_w_uq, mla_kv_norm_g, mla_w_ukv, rope)
    q_c, k_c, v_c = mla_qkv(cq, ckv, mla_q_norm_g, mla_w_uq, mla_kv_norm_g, mla_w_ukv, None)
    o_mla_l = attend_latent_blocks(q_l, jnp.concatenate([k_l, k_c], axis=1), jnp.concatenate([v_l, v_c], axis=1))

    o_fn_l = fourier_mix(lfn)

    gq_l, gk_l, gv_l, lf_l, lb_l = gla_prepare(lgq, lgk, lgv, lgf, lgb, gla_w_gate_f, gla_b_gate_f, gla_w_gate_b, gla_b_gate_b)
    gq_c, gk_c, gv_c, lf_c, lb_c = gla_prepare(cgq, cgk, cgv, cgf, cgb, gla_w_gate_f, gla_b_gate_f, gla_w_gate_b, gla_b_gate_b)
    zero = jnp.zeros((b, GLA_HEADS, GLA_DK, GLA_DV), jnp.float32)
    o_cf, s_f = gla_scan(gq_c, gk_c, gv_c, lf_c, zero)
    o_cb, s_b = gla_scan(flip(gq_c), flip(gk_c), flip(gv_c), flip(lb_c), zero)
    o_lf, _ = gla_scan(gq_l, gk_l, gv_l, lf_l, s_f)
    o_lb, _ = gla_scan(flip(gq_l), flip(gk_l), flip(gv_l), flip(lb_l), s_b)
    o_gla_l = gla_output(o_lf + flip(o_lb), log_, gla_norm_g)

    y_lat = merge_branches(o_mla_l, o_fn_l, o_gla_l, lgate, w_br_mla, w_br_fnet, w_br_gla, w_o)
    y_ctx = None
    if need_ctx:
        o_mla_c = attend(q_c, k_c, v_c).reshape(b, n_ctx, MLA_W)
        o_fn_c = fourier_mix(cfn)
        o_gla_c = gla_output(o_cf + flip(o_cb), cog, gla_norm_g)
        y_ctx = merge_branches(o_mla_c, o_fn_c, o_gla_c, cgate, w_br_mla, w_br_fnet, w_br_gla, w_o)
    return y_ctx, y_lat


def moe_ffn(h, router_w, router_b, w_up, b_up, w_down, b_down):
    n, d = h.shape
    logits = (h @ router_w).astype(jnp.float32) + router_b.astype(jnp.float32)
    top_val, top_idx = lax.top_k(logits, TOP_K)
    gate = jax.nn.softmax(top_val, axis=-1)
    n_assign = n * TOP_K
    flat_e = top_idx.reshape(n_assign)
    order = jnp.argsort(flat_e)
    sorted_e = flat_e[order]
    counts = jnp.bincount(flat_e, length=N_EXPERTS)
    padded = ((counts + MOE_BLOCK - 1) // MOE_BLOCK) * MOE_BLOCK
    pad_end = jnp.cumsum(padded)
    pad_start = pad_end - padded
    grp_start = jnp.cumsum(counts) - counts
    dest = pad_start[sorted_e] + (jnp.arange(n_assign) - grp_start[sorted_e])
    n_blocks = -(-n_assign // MOE_BLOCK) + N_EXPERTS
    n_rows = n_blocks * MOE_BLOCK
    row_tok = jnp.full((n_rows,), n, jnp.int32).at[dest].set((order // TOP_K).astype(jnp.int32))
    row_gate = jnp.zeros((n_rows,), jnp.float32).at[dest].set(gate.reshape(n_assign)[order])
    blk_e = jnp.minimum(jnp.searchsorted(pad_end, jnp.arange(n_blocks) * MOE_BLOCK, side='right'), N_EXPERTS - 1)
    h_pad = jnp.concatenate([h, jnp.zeros((1, d), h.dtype)], axis=0)

    def step(acc, xs):
        tok, gw, e = xs
        xb = h_pad[tok]
        up = xb @ w_up[e] + b_up[e]
        glu, lin = jnp.split(up, 2, axis=-1)
        glu = jnp.minimum(glu, SWIGLU_LIMIT)
        lin = jnp.clip(lin, -SWIGLU_LIMIT, SWIGLU_LIMIT)
        act = glu * jax.nn.sigmoid(SWIGLU_ALPHA * glu) * (lin + 1)
        yb = act @ w_down[e] + b_down[e]
        return acc.at[tok].add(yb * gw[:, None].astype(yb.dtype)), None

    acc0 = jnp.zeros((n + 1, d), h.dtype)
    acc, _ = lax.scan(step, acc0, (row_tok.reshape(n_blocks, MOE_BLOCK), row_gate.reshape(n_blocks, MOE_BLOCK), blk_e))
    return acc[:n]


def setup_inputs(seed: int = 0) -> dict:
    key = jax.random.key(seed)
    ks = jax.random.split(key, 29)
    L, D = DEPTH, D_MODEL

    def nrm(i, shape, scale):
        return jax.random.normal(ks[i], shape, jnp.float32) * scale

    return {
        'x': nrm(0, (BATCH, SEQ, D), 1.0),
        'c': nrm(1, (BATCH, D), 1.0),
        'ctx': nrm(2, (BATCH, CTX_LEN, D), 1.0),
        'c_ctx': nrm(3, (D,), 1.0),
        'w_mod': nrm(4, (L, D, 6 * D), 0.5 * D ** -0.5),
        'b_mod': nrm(5, (L, 6 * D), 0.02),
        'norm1_g': 1.0 + nrm(6, (L, D), 0.02),
        'w_in': nrm(7, (L, D, IN_WIDTH), D ** -0.5),
        'mla_q_norm_g': 1.0 + nrm(8, (L, MLA_Q_RANK), 0.02),
        'mla_w_uq': nrm(9, (L, MLA_Q_RANK, MLA_HEADS * (MLA_NOPE + MLA_ROPE)), MLA_Q_RANK ** -0.5),
        'mla_kv_norm_g': 1.0 + nrm(10, (L, MLA_KV_RANK), 0.02),
        'mla_w_ukv': nrm(11, (L, MLA_KV_RANK, MLA_HEADS * (MLA_NOPE + MLA_V)), MLA_KV_RANK ** -0.5),
        'gla_w_gate_f': nrm(12, (L, GLA_GATE_RANK, GLA_HEADS * GLA_DK), GLA_GATE_RANK ** -0.5),
        'gla_b_gate_f': nrm(13, (L, GLA_HEADS * GLA_DK), 0.1),
        'gla_w_gate_b': nrm(14, (L, GLA_GATE_RANK, GLA_HEADS * GLA_DK), GLA_GATE_RANK ** -0.5),
        'gla_b_gate_b': nrm(15, (L, GLA_HEADS * GLA_DK), 0.1),
        'gla_norm_g': 1.0 + nrm(16, (L, GLA_W), 0.02),
        'w_br_mla': nrm(17, (L, MLA_W, D), MLA_W ** -0.5),
        'w_br_fnet': nrm(18, (L, FNET_W, D), FNET_W ** -0.5),
        'w_br_gla': nrm(19, (L, GLA_W, D), GLA_W ** -0.5),
        'w_o': nrm(20, (L, D, D), D ** -0.5),
        'norm2_g': 1.0 + nrm(21, (L, D), 0.02),
        'router_w': nrm(22, (L, D, N_EXPERTS), D ** -0.5),
        'router_b': nrm(23, (L, N_EXPERTS), 0.01),
        'exp_w_up': nrm(24, (L, N_EXPERTS, D, 2 * D_EXPERT), D ** -0.5),
        'exp_b_up': nrm(25, (L, N_EXPERTS, 2 * D_EXPERT), 0.02),
        'exp_w_down': nrm(26, (L, N_EXPERTS, D_EXPERT, D), D_EXPERT ** -0.5),
        'exp_b_down': nrm(27, (L, N_EXPERTS, D), 0.02),
        'final_norm_g': 1.0 + nrm(28, (D,), 0.02),
    }


def reference(x, c, ctx, c_ctx, w_mod, b_mod, norm1_g, w_in, mla_q_norm_g, mla_w_uq, mla_kv_norm_g, mla_w_ukv,
              gla_w_gate_f, gla_b_gate_f, gla_w_gate_b, gla_b_gate_b, gla_norm_g, w_br_mla, w_br_fnet, w_br_gla,
              w_o, norm2_g, router_w, router_b, exp_w_up, exp_b_up, exp_w_down, exp_b_down, final_norm_g):
    b, n_lat, d = x.shape
    rows = n_lat // GRID_W
    rope = axial_rope_angles(rows, x.dtype)
    xc = ctx
    silu_c = jax.nn.silu(c)
    silu_cc = jax.nn.silu(c_ctx)
    for l in range(DEPTH):
        need_ctx = l < DEPTH - 1
        mod = (silu_c @ w_mod[l] + b_mod[l])[:, None, :]
        mod_c = (silu_cc @ w_mod[l] + b_mod[l])[None, None, :]
        sh1, sc1, g1, sh2, sc2, g2 = jnp.split(mod, 6, axis=-1)
        csh1, csc1, cg1, csh2, csc2, cg2 = jnp.split(mod_c, 6, axis=-1)

        h_lat = modulate(rms_norm(x, norm1_g[l]), sh1, sc1)
        h_ctx = modulate(rms_norm(xc, norm1_g[l]), csh1, csc1)
        y_ctx, y_lat = token_mixers(h_ctx, h_lat, w_in[l], mla_q_norm_g[l], mla_w_uq[l], mla_kv_norm_g[l], mla_w_ukv[l],
                                    gla_w_gate_f[l], gla_b_gate_f[l], gla_w_gate_b[l], gla_b_gate_b[l], gla_norm_g[l],
                                    w_br_mla[l], w_br_fnet[l], w_br_gla[l], w_o[l], rope, need_ctx)
        x = x + g1 * y_lat
        h_lat = modulate(rms_norm(x, norm2_g[l]), sh2, sc2)
        moe_params = (router_w[l], router_b[l], exp_w_up[l], exp_b_up[l], exp_w_down[l], exp_b_down[l])
        if need_ctx:
            xc = xc + cg1 * y_ctx
            h_ctx = modulate(rms_norm(xc, norm2_g[l]), csh2, csc2)
            n_ctx_tok = b * xc.shape[1]
            y = moe_ffn(jnp.concatenate([h_ctx.reshape(-1, d), h_lat.reshape(-1, d)], axis=0), *moe_params)
            xc = xc + cg2 * y[:n_ctx_tok].reshape(xc.shape)
            x = x + g2 * y[n_ctx_tok:].reshape(x.shape)
        else:
            x = x + g2 * moe_ffn(h_lat.reshape(-1, d), *moe_params).reshape(x.shape)
    return rms_norm(x, final_norm_g)
```

```python
import contextlib
import math
import numpy as np
import ml_dtypes
import concourse.bass as bass
import concourse.mybir as mybir
from concourse.bass_utils import run_bass_kernel_spmd

F32 = mybir.dt.float32
BF16 = mybir.dt.bfloat16
I32 = mybir.dt.int32
AF = mybir.ActivationFunctionType
ALU = mybir.AluOpType
AX = mybir.AxisListType

D = 1024
SEQ = 4096
CTX = 256
T = SEQ + CTX
NT = T // 128
DEPTH = 4
INW = 5568
NE = 32
BS = 256
SUB = BS // 128
NBLK = (T * 4) // BS + NE
NROWS = NBLK * BS
EPS = 1e-6
MLA_SCALE = 96 ** -0.5
GLA_SCALE = 64 ** -0.5
O_UQ, O_KV, O_FN, O_GQ, O_GK, O_GV, O_OG, O_GF, O_GB, O_GATE = 0, 256, 416, 928, 1184, 1440, 1952, 2464, 2480, 2496


class Tok:
    __slots__ = ("w", "rs")

    def __init__(self):
        self.w = None
        self.rs = {}


class Sched:
    NDMA = 32

    def __init__(self, nc, st):
        self.nc = nc
        self.engs = ["pe", "act", "dve", "pool", "sp"]
        self.ops = {k: [] for k in self.engs}
        self.cnt = {k: 0 for k in self.engs}
        self.seen = {k: {} for k in self.engs}
        self.dma_k = {"d": 0, "g": 0}
        self.dma_last = {}
        self.n_ops = 0
        self.sems = {}
        for k in ["e_" + e for e in self.engs] + ["d%d" % i for i in range(self.NDMA)] + ["g%d" % i for i in range(self.NDMA)]:
            self.sems[k] = st.enter_context(nc.semaphore(k))

    def _need(self, eng, ev, waits):
        if ev is None:
            return
        src, key, val = ev
        if src == eng and eng == "pe":
            return
        if self.seen[eng].get(key, 0) >= val:
            return
        self.seen[eng][key] = val
        waits.append((key, val))

    def op(self, eng, fn, r=(), w=(), dma=False):
        waits = []
        for t in r:
            self._need(eng, t.w, waits)
        for t in w:
            self._need(eng, t.w, waits)
            for ev in t.rs.values():
                self._need(eng, ev, waits)
        if dma:
            pre = "g" if eng == "pool" else "d"
            k = self.dma_k[pre]
            self.dma_k[pre] += 1
            key = "%s%d" % (pre, k % self.NDMA)
            val = 16 * (k // self.NDMA + 1)
            if val > 16:
                self._need(eng, ("dma", key, val - 16), waits)
            ev = ("dma", key, val)
            inc = (key, 16)
            self.dma_last[key] = val
        else:
            self.cnt[eng] += 1
            key = "e_" + eng
            ev = (eng, key, self.cnt[eng])
            inc = (key, 1)
        self.ops[eng].append((waits, fn, inc))
        self.n_ops += 1
        for t in w:
            t.w = ev
            t.rs = {}
        for t in r:
            if t not in w:
                t.rs[ev[1]] = ev
        return ev

    def barrier(self):
        evs = [(e, "e_" + e, self.cnt[e]) for e in self.engs if self.cnt[e] > 0]
        evs += [("dma", k, v) for k, v in self.dma_last.items()]
        for eng in self.engs:
            waits = []
            for ev in evs:
                if ev[0] == eng:
                    continue
                self._need(eng, ev, waits)
            if waits:
                self.ops[eng].append((waits, None, None))

    def flush(self):
        self.barrier()
        nc = self.nc
        sems = self.sems
        ops = self.ops
        with nc.Block() as block:
            def mk(engname):
                def body(e):
                    for waits, fn, inc in ops[engname]:
                        for key, val in waits:
                            e.wait_ge(sems[key], val)
                        if fn is not None:
                            fn(e).then_inc(sems[inc[0]], inc[1])
                return body
            block.tensor(mk("pe"))
            block.scalar(mk("act"))
            block.vector(mk("dve"))
            block.gpsimd(mk("pool"))
            block.sync(mk("sp"))
        self.ops = {k: [] for k in self.engs}


class B:
    __slots__ = ("t", "k", "psum")

    def __init__(self, t, psum=False):
        self.t = t
        self.k = Tok()
        self.psum = psum


def _toks(xs):
    return [x.k if isinstance(x, B) else x for x in xs]


class KB:
    def __init__(self, nc, st):
        self.nc = nc
        self.S = Sched(nc, st)
        self.dq = 0
        self.bregs = {}

    def sb(self, st, name, shape, dt):
        self.dq += 1
        return B(st.enter_context(self.nc.sbuf_tensor("%s_u%d" % (name, self.dq), shape, dt)))

    def op(self, eng, fn, r, w, dma=False):
        r2 = [x for x in r if not (isinstance(x, B) and x.psum)]
        w2 = list(w) + [x for x in r if isinstance(x, B) and x.psum and x not in w]
        return self.S.op(eng, fn, r=_toks(r2), w=_toks(w2), dma=dma)

    def mm(self, out, lhsT, rhs, start, stop, r, w):
        self.op("pe", lambda e: e.matmul(out, lhsT, rhs, start=start, stop=stop), r, w)

    def tr(self, out, in_, ident, r, w):
        self.op("pe", lambda e: e.transpose(out, in_, ident), r, w)

    def act(self, out, in_, func, r, w, bias=None, scale=None, accum=None):
        kw = {}
        if bias is not None:
            kw["bias"] = bias
        if scale is not None:
            kw["scale"] = scale
        if accum is not None:
            kw["accum_out"] = accum
        self.op("act", lambda e: e.activation(out=out, in_=in_, func=func, **kw), r, w)

    def cp(self, eng, out, in_, r, w):
        if eng == "act":
            self.op("act", lambda e: e.copy(out=out, in_=in_), r, w)
        else:
            self.op(eng, lambda e: e.tensor_copy(out=out, in_=in_), r, w)

    def tt(self, eng, out, in0, in1, op, r, w):
        self.op(eng, lambda e: e.tensor_tensor(out=out, in0=in0, in1=in1, op=op), r, w)

    def ts(self, eng, out, in0, s1, s2, op0, op1, r, w):
        if op1 is None:
            self.op(eng, lambda e: e.tensor_scalar(out=out, in0=in0, scalar1=s1, scalar2=None, op0=op0), r, w)
        else:
            self.op(eng, lambda e: e.tensor_scalar(out=out, in0=in0, scalar1=s1, scalar2=s2, op0=op0, op1=op1), r, w)

    def stt(self, eng, out, in0, scalar, in1, op0, op1, r, w):
        self.op(eng, lambda e: e.scalar_tensor_tensor(out=out, in0=in0, scalar=scalar, in1=in1, op0=op0, op1=op1), r, w)

    def red(self, eng, out, in_, op, r, w, axis=AX.X):
        self.op(eng, lambda e: e.tensor_reduce(out=out, in_=in_, axis=axis, op=op), r, w)

    def recip(self, out, in_, r, w):
        self.op("dve", lambda e: e.reciprocal(out=out, in_=in_), r, w)

    def memset(self, eng, out, val, r, w):
        self.op(eng, lambda e: e.memset(out, val), r, w)

    def dma(self, eng, out, in_, r, w, slow=False):
        if slow:
            self.op(eng, lambda e: e.dma_start(out=out, in_=in_, allow_slow_non_contiguous=True), r, w, dma=True)
        else:
            self.op(eng, lambda e: e.dma_start(out=out, in_=in_), r, w, dma=True)

    def _breg(self, e, bound):
        if bound not in self.bregs:
            self.bregs[bound] = e.to_reg(bound)
        return self.bregs[bound]

    def gather(self, out, in_, idx, r, w, bound):
        self.op("pool", lambda e: e.indirect_dma_start(
            out=out, out_offset=None, in_=in_, in_offset=bass.IndirectOffsetOnAxis(ap=idx, axis=0),
            bounds_check=self._breg(e, bound), oob_is_err=False), r, w, dma=True)

    def scatter(self, out, in_, idx, r, w, bound):
        self.op("pool", lambda e: e.indirect_dma_start(
            out=out, out_offset=bass.IndirectOffsetOnAxis(ap=idx, axis=0), in_=in_, in_offset=None,
            bounds_check=self._breg(e, bound), oob_is_err=False), r, w, dma=True)


def host_consts():
    c = {}
    bf = ml_dtypes.bfloat16
    i = np.arange(128)
    c["ident_f"] = np.eye(128, dtype=np.float32)
    c["ident_b"] = np.eye(128).astype(bf)
    trif = (i[:, None] <= i[None, :]).astype(np.float32)
    trib = (i[:, None] >= i[None, :]).astype(np.float32)
    c["tri_f"] = np.stack([trif, trib], 0)
    c["mask4"] = np.stack([np.repeat(trif[:, None, :], 4, 1), np.repeat(trib[:, None, :], 4, 1)], 0).astype(np.float32)
    c["tri_x"] = (i[:, None] < i[None, :]).astype(bf)
    c["ones_f"] = np.ones((128, 128), np.float32)
    c["ones_b"] = np.ones((128, 128)).astype(bf)
    inv = 10000.0 ** (-np.arange(0, 16, 2, dtype=np.float32) / 16)
    row = np.repeat(np.arange(64, dtype=np.float32), 64)
    col = np.tile(np.arange(64, dtype=np.float32), 64)
    ar = row[:, None] * inv
    ac = col[:, None] * inv
    cr, sr, cc, sc = (np.cos(ar).astype(np.float32), np.sin(ar).astype(np.float32),
                      np.cos(ac).astype(np.float32), np.sin(ac).astype(np.float32))
    cos32 = np.concatenate([cr, cr, cc, cc], 1)
    sin32 = np.concatenate([-sr, sr, -sc, sc], 1)
    cos32 = np.concatenate([np.ones((CTX, 32), np.float32), cos32], 0)
    sin32 = np.concatenate([np.zeros((CTX, 32), np.float32), sin32], 0)
    c["rope_k"] = np.stack([cos32, sin32], 1).astype(np.float32)
    rq = np.stack([np.tile(cos32, (1, 8)), np.tile(sin32, (1, 8))], 1) * np.float32(MLA_SCALE)
    c["rope_q"] = rq.astype(np.float32)
    ang = 2 * np.pi * np.outer(i, i) / 128.0
    c["cs128"] = (np.concatenate([np.cos(ang), np.sin(ang)], 1) / math.sqrt(128)).astype(bf)
    n = np.arange(SEQ, dtype=np.int64)
    kt = (np.outer(n, n) % SEQ).astype(np.float64) * (2 * np.pi / SEQ)
    ctm = (np.cos(kt) / 64.0).astype(np.float32).reshape(32, 128, 32, 128)
    stm = (-np.sin(kt) / 64.0).astype(np.float32).reshape(32, 128, 32, 128)
    c["dft_c"] = np.ascontiguousarray(ctm.transpose(2, 1, 0, 3)).astype(bf)
    c["dft_s"] = np.ascontiguousarray(stm.transpose(2, 1, 0, 3)).astype(bf)
    del kt, ctm, stm
    m = np.arange(CTX, dtype=np.int64)
    k2 = (np.outer(m, m) % CTX).astype(np.float64) * (2 * np.pi / CTX)
    c2 = (np.cos(k2) / 16.0).astype(np.float32).reshape(2, 128, 2, 128)
    s2 = (-np.sin(k2) / 16.0).astype(np.float32).reshape(2, 128, 2, 128)
    c["dft_c2"] = np.ascontiguousarray(c2.transpose(2, 1, 0, 3)).astype(bf)
    c["dft_s2"] = np.ascontiguousarray(s2.transpose(2, 1, 0, 3)).astype(bf)
    c["jv"] = np.tile((np.arange(NBLK, dtype=np.float32) * BS)[None, :], (128, 1))
    c["kp"] = (np.arange(8, dtype=np.float32)[None, :] * 128 + i[:, None]).astype(np.float32)
    return c


CONST_SPECS = {
    "ident_f": ([128, 128], F32), "ident_b": ([128, 128], BF16), "tri_f": ([2, 128, 128], F32),
    "mask4": ([2, 128, 4, 128], F32), "tri_x": ([128, 128], BF16), "ones_f": ([128, 128], F32),
    "ones_b": ([128, 128], BF16), "rope_k": ([T, 2, 32], F32), "rope_q": ([T, 2, 256], F32),
    "cs128": ([128, 256], BF16), "dft_c": ([32, 128, 32, 128], BF16), "dft_s": ([32, 128, 32, 128], BF16),
    "dft_c2": ([2, 128, 2, 128], BF16), "dft_s2": ([2, 128, 2, 128], BF16),
    "jv": ([128, NBLK], F32), "kp": ([128, 8], F32),
}

WEIGHT_SPECS = {
    "w_mod": [DEPTH, D, 6 * D], "b_mod": [DEPTH, 6 * D], "norm1_g": [DEPTH, D], "w_in": [DEPTH, D, INW],
    "mla_q_norm_g": [DEPTH, 256], "mla_w_uq": [DEPTH, 256, 768], "mla_kv_norm_g": [DEPTH, 128],
    "mla_w_ukv": [DEPTH, 128, 1024], "gla_w_gate_f": [DEPTH, 16, 256], "gla_b_gate_f": [DEPTH, 256],
    "gla_w_gate_b": [DEPTH, 16, 256], "gla_b_gate_b": [DEPTH, 256], "gla_norm_g": [DEPTH, 512],
    "w_br_mla": [DEPTH, 512, D], "w_br_fnet": [DEPTH, 512, D], "w_br_gla": [DEPTH, 512, D],
    "w_o": [DEPTH, D, D], "norm2_g": [DEPTH, D], "router_w": [DEPTH, D, NE], "router_b": [DEPTH, NE],
    "exp_w_up": [DEPTH, NE, D, 2 * D], "exp_b_up": [DEPTH, NE, 2 * D], "exp_w_down": [DEPTH, NE, D, D],
    "exp_b_down": [DEPTH, NE, D], "final_norm_g": [D],
}


class Prog:
    def __init__(self, nl=DEPTH, dbg=(), stop_after=None, wdepth=DEPTH):
        self.nl = nl
        self.dbg = set(dbg)
        self.stop_after = stop_after
        self.nc = bass.Bass("TRN2", target_bir_lowering=False)
        self.st = contextlib.ExitStack()
        self.kb = KB(self.nc, self.st)
        nc = self.nc
        self.x_in = nc.dram_tensor("x", [SEQ, D], F32, kind="ExternalInput").ap()
        self.ctx_in = nc.dram_tensor("ctx", [CTX, D], F32, kind="ExternalInput").ap()
        self.c_in = nc.dram_tensor("c", [D], F32, kind="ExternalInput").ap()
        self.cc_in = nc.dram_tensor("c_ctx", [D], F32, kind="ExternalInput").ap()
        self.W = {k: nc.dram_tensor(k, ([wdepth] + shp[1:]) if (len(shp) > 1 or k in ('norm1_g',)) and shp[0] == DEPTH and k != 'final_norm_g' else shp, F32, kind="ExternalInput").ap() for k, shp in WEIGHT_SPECS.items()}
        self.C = {k: nc.dram_tensor(k, shp, dt, kind="ExternalInput").ap() for k, (shp, dt) in CONST_SPECS.items()}
        self.out = nc.dram_tensor("out", [SEQ, D], F32, kind="ExternalOutput").ap()
        self.tout = Tok()
        self.Dr = {}
        self.Dk = {}
        for name, shp, dt in [
            ("XR", [T, D], F32), ("KT", [8, 96, T], BF16), ("QT", [8, 96, T], BF16), ("VA", [T, 8, 65], BF16),
            ("UFT", [4, 128, T], BF16), ("GQT", [4, 64, T], BF16), ("GKT", [4, 64, T], BF16),
            ("GKVO", [T, 1280], BF16), ("LFB", [T, 512], F32), ("GATE", [T, 3072], BF16),
            ("OMT", [8, 64, T], BF16), ("OFT", [4, 128, T], BF16), ("OGT", [4, 128, T], BF16),
            ("OFW", [T, 512], F32), ("XS", [NROWS, D], BF16), ("YB", [NROWS, D], F32),
            ("H2B", [T, D], BF16), ("DBG_DST", [128, NT * 4], I32), ("DBG_G4", [128, NT * 4], F32),
            ("DBG_EB", [128, NBLK], I32), ("DBG_LG", [128, NT * NE], F32), ("DBG_CNT", [128, NE], F32),
        ]:
            kind = "ExternalOutput" if name in self.dbg else "Internal"
            self.Dr[name] = nc.dram_tensor(name, shp, dt, kind=kind).ap()
            self.Dk[name] = Tok()

    def build(self):
        kb = self.kb
        nc = self.nc
        with self.st:
            P = self.P = {}
            for name, shp, dt in [
                ("ident_f", [128, 128], F32), ("ident_b", [128, 128], BF16), ("ones_f", [128, 128], F32),
                ("ones_b", [128, 128], BF16), ("modT", [128, 48, 2], F32), ("nb", [128, 8], F32),
                ("scT", [128, 8, 2], F32),
            ]:
                P[name] = kb.sb(self.st, "p_" + name, shp, dt)
            self.ps = [B(self.st.enter_context(nc.psum_tensor("ps%d" % i, [128, 512], F32)), psum=True) for i in range(8)]
            self.psi = 0
            self.pinned = set()
            for nm in ["ident_f", "ident_b", "ones_f", "ones_b"]:
                kb.dma("sp", P[nm].t[:], self.C[nm], [], [P[nm]])
            kb.dma("sp", self.Dr["XR"][0:CTX, :], self.ctx_in, [], [self.Dk["XR"]])
            kb.dma("pool", self.Dr["XR"][CTX:T, :], self.x_in, [], [self.Dk["XR"]])
            kb.S.flush()
            for l in range(self.nl):
                import os
                skip = os.environ.get("SKIP_PH", "").split(",")
                for ph in [self.phase_mod, self.phase_a, self.phase_attn, self.phase_fnet, self.phase_gla,
                           self.phase_merge, self.phase_moe]:
                    if ph.__name__ in skip:
                        continue
                    ph(l)
                    kb.S.flush()
                    if self.stop_after == (l, ph.__name__):
                        break
                else:
                    continue
                break
            kb.S.flush()
        return nc

    def psum(self, pin=False):
        while True:
            idx = self.psi % 8
            self.psi += 1
            if idx not in self.pinned:
                break
        if pin:
            self.pinned.add(idx)
        return self.ps[idx]

    def unpin(self, p):
        self.pinned.discard(self.ps.index(p))

    def phase_mod(self, l):
        kb, P, W = self.kb, self.P, self.W
        with contextlib.ExitStack() as st:
            cT = kb.sb(st, "cT", [128, 8, 2], F32)
            sT = kb.sb(st, "sT", [128, 8, 2], F32)
            bT = kb.sb(st, "bT", [128, 48], F32)
            wm = [kb.sb(st, "wm%d" % i, [128, 8, 1024], F32) for i in range(2)]
            kb.dma("sp", cT.t[:, :, 0], self.c_in.rearrange("(k p) -> p k", p=128), [], [cT], slow=True)
            kb.dma("sp", cT.t[:, :, 1], self.cc_in.rearrange("(k p) -> p k", p=128), [], [cT], slow=True)
            kb.dma("sp", bT.t[:], W["b_mod"][l].rearrange("(j p) -> p j", p=128), [], [bT], slow=True)
            kb.act(sT.t[:], cT.t[:], AF.Silu, [cT], [sT])
            for sec in range(6):
                w = wm[sec % 2]
                kb.dma("sp" if sec % 2 == 0 else "pool", w.t[:],
                       W["w_mod"][l][:, sec * 1024:(sec + 1) * 1024].rearrange("(k p) n -> p k n", p=128), [], [w])
                ps = self.psum()
                for j in range(8):
                    for kc in range(8):
                        kb.mm(ps.t[:, j * 2:j * 2 + 2], w.t[:, kc, j * 128:(j + 1) * 128], sT.t[:, kc, :],
                              kc == 0, kc == 7, [w, sT], [ps])
                for j in range(8):
                    jj = sec * 8 + j
                    kb.ts("dve", P["modT"].t[:, jj, :], ps.t[:, j * 2:j * 2 + 2], bT.t[:, jj:jj + 1], None, ALU.add, None,
                          [ps, bT], [P["modT"]])
            kb.S.flush()

    def bcast_rows(self, st, name, colT_ap_fn, r):
        kb, P = self.kb, self.P
        out = kb.sb(st, name, [128, 1024], F32)
        tmp = kb.sb(st, name + "_t", [128, 128], F32)
        for half in range(2):
            ps = self.psum()
            for k4 in range(4):
                kc = half * 4 + k4
                kb.ts("dve", tmp.t[:], P["ones_f"].t[:], colT_ap_fn(kc), None, ALU.mult, None, r + [P["ones_f"]], [tmp])
                kb.mm(ps.t[:, k4 * 128:(k4 + 1) * 128], tmp.t[:], P["ident_f"].t[:], True, True, [tmp, P["ident_f"]], [ps])
            kb.cp("act", out.t[:, half * 512:(half + 1) * 512], ps.t[:], [ps], [out])
        return out

    def mod_rows(self, st, l, which, names=("A", "S", "G")):
        kb, P, W = self.kb, self.P, self.W
        sec_sh, sec_sc, sec_g = (0, 1, 2) if which == 1 else (3, 4, 5)
        gT = kb.sb(st, "gT", [128, 8], F32)
        kb.dma("sp", gT.t[:], W["norm1_g" if which == 1 else "norm2_g"][l].rearrange("(k p) -> p k", p=128), [], [gT], slow=True)
        aT = kb.sb(st, "aT", [128, 8, 2], F32)
        m = P["modT"]
        for v in range(2):
            kb.stt("dve", aT.t[:, :, v], m.t[:, sec_sc * 8:(sec_sc + 1) * 8, v], 1.0, gT.t[:], ALU.add, ALU.mult, [m, gT], [aT])
        rows = {}
        for v, nm in ((0, "l"), (1, "c")):
            if "A" in names:
                rows["A" + nm] = self.bcast_rows(st, "rA" + nm, lambda kc, v=v: aT.t[:, kc, v:v + 1], [aT])
            if "S" in names:
                rows["S" + nm] = self.bcast_rows(st, "rS" + nm, lambda kc, v=v: m.t[:, sec_sh * 8 + kc, v:v + 1], [m])
            if "G" in names:
                rows["G" + nm] = self.bcast_rows(st, "rG" + nm, lambda kc, v=v: m.t[:, sec_g * 8 + kc, v:v + 1], [m])
        return rows

    def norm_mod(self, xt, rows, i, junk, ssq, h32, hb, full32=False):
        kb = self.kb
        nm = "c" if i < 2 else "l"
        kb.act(junk.t[:], xt.t[:], AF.Square, [xt], [junk, ssq], accum=ssq.t[:, 0:1])
        kb.ts("dve", ssq.t[:, 1:2], ssq.t[:, 0:1], 1.0 / D, EPS, ALU.mult, ALU.add, [ssq], [ssq])
        kb.act(ssq.t[:, 1:2], ssq.t[:, 1:2], AF.Sqrt, [ssq], [ssq])
        kb.recip(ssq.t[:, 2:3], ssq.t[:, 1:2], [ssq], [ssq])
        kb.stt("dve", h32.t[:], xt.t[:], ssq.t[:, 2:3], rows["A" + nm].t[:], ALU.mult, ALU.mult, [xt, ssq, rows["A" + nm]], [h32])
        if full32:
            kb.tt("pool", h32.t[:], h32.t[:], rows["S" + nm].t[:], ALU.add, [h32, rows["S" + nm]], [h32])
            kb.cp("dve", hb.t[:], h32.t[:], [h32], [hb])
        else:
            kb.tt("pool", hb.t[:], h32.t[:], rows["S" + nm].t[:], ALU.add, [h32, rows["S" + nm]], [hb])

    def phase_a(self, l):
        kb, P, W, C, Dr, Dk = self.kb, self.P, self.W, self.C, self.Dr, self.Dk
        with contextlib.ExitStack() as st:
            rows = self.mod_rows(st, l, 1, names=("A", "S"))
            win = kb.sb(st, "win", [128, 8, INW], BF16)
            for kc in range(8):
                kb.dma("pool", win.t[:, kc, :], W["w_in"][l][kc * 128:(kc + 1) * 128, :], [], [win])
            wuq32 = kb.sb(st, "wuq32", [128, 2, 768], F32)
            wuq = kb.sb(st, "wuq", [128, 2, 768], BF16)
            gq = kb.sb(st, "gq", [128, 2], F32)
            wkv32 = kb.sb(st, "wkv32", [128, 1024], F32)
            wkv = kb.sb(st, "wkv", [128, 1024], BF16)
            gkv = kb.sb(st, "gkv", [128, 1], F32)
            wg = kb.sb(st, "wg", [17, 2, 256], F32)
            kb.dma("sp", wuq32.t[:], W["mla_w_uq"][l].rearrange("(k p) n -> p k n", p=128), [], [wuq32])
            kb.dma("sp", gq.t[:], W["mla_q_norm_g"][l].rearrange("(k p) -> p k", p=128), [], [gq], slow=True)
            kb.dma("sp", wkv32.t[:], W["mla_w_ukv"][l], [], [wkv32])
            kb.dma("sp", gkv.t[:], W["mla_kv_norm_g"][l].rearrange("(k p) -> p k", p=128), [], [gkv], slow=True)
            kb.dma("sp", wg.t[0:16, 0, :], W["gla_w_gate_f"][l], [], [wg])
            kb.dma("sp", wg.t[0:16, 1, :], W["gla_w_gate_b"][l], [], [wg])
            kb.dma("sp", wg.t[16:17, 0, :], W["gla_b_gate_f"][l].rearrange("(o n) -> o n", o=1), [], [wg])
            kb.dma("sp", wg.t[16:17, 1, :], W["gla_b_gate_b"][l].rearrange("(o n) -> o n", o=1), [], [wg])
            for kc in range(2):
                kb.ts("dve", wuq.t[:, kc, :], wuq32.t[:, kc, :], gq.t[:, kc:kc + 1], None, ALU.mult, None, [wuq32, gq], [wuq])
            kb.ts("dve", wkv.t[:], wkv32.t[:], gkv.t[:, 0:1], None, ALU.mult, None, [wkv32, gkv], [wkv])
            m16 = kb.sb(st, "m16", [128, 16], F32)
            kb.memset("dve", m16.t[:], 0.0, [], [m16])
            NB2 = 1
            def mk(name, shp, dt):
                return [kb.sb(st, "%s%d" % (name, j), shp, dt) for j in range(NB2)]
            xt = mk("xt", [128, D], F32)
            junk = mk("junk", [128, D], F32)
            ssq = mk("ssq", [128, 8], F32)
            h32 = mk("h32", [128, D], F32)
            hb = mk("hb", [128, D], BF16)
            hT = mk("hT", [128, 8, 128], BF16)
            uqn = mk("uqn", [128, 384], BF16)
            kr = mk("kr", [128, 4, 32], F32)
            uT = mk("uT", [128, 3, 128], BF16)
            rq = mk("rq", [128, 2, 256], F32)
            rk = mk("rk", [128, 2, 32], F32)
            qo = mk("qo", [128, 8, 96], BF16)
            ko = mk("ko", [128, 8, 96], BF16)
            vo = mk("vo", [128, 8, 65], BF16)
            qtmp = mk("qtmp", [128, 3, 256], F32)
            sq = mk("sq", [128, 768], F32)
            n2 = mk("n2", [128, 16], F32)
            qkT = mk("qkT", [96, 16, 128], BF16)
            gkvo = mk("gkvo", [128, 1280], BF16)
            gate = mk("gate", [128, 3072], BF16)
            fT = mk("fT", [128, 4, 128], BF16)
            gqkT = mk("gqkT", [64, 8, 128], BF16)
            ugT = mk("ugT", [17, 2, 128], F32)
            lfb = mk("lfb", [128, 512], F32)
            lfe = mk("lfe", [128, 512], F32)
            for j in range(NB2):
                kb.memset("pool", vo[j].t[:], 1.0, [], [vo[j]])
                kb.memset("pool", ugT[j].t[:], 1.0, [], [ugT[j]])
            idb = P["ident_b"]
            for i in range(NT):
                b = i % NB2
                X, J, SS, H32, HB, HT = xt[b], junk[b], ssq[b], h32[b], hb[b], hT[b]
                kb.dma("sp", X.t[:], Dr["XR"][i * 128:(i + 1) * 128, :], [Dk["XR"]], [X])
                kb.dma("sp", rq[b].t[:], C["rope_q"][i * 128:(i + 1) * 128], [], [rq[b]])
                kb.dma("sp", rk[b].t[:], C["rope_k"][i * 128:(i + 1) * 128], [], [rk[b]])
                kb.memset("pool", SS.t[:], 0.0, [], [SS])
                self.norm_mod(X, rows, i, J, SS, H32, HB)
                pT = self.psum()
                pTb = pT.t[:].bitcast(BF16)
                for kc in range(8):
                    kb.tr(pTb[:, kc * 128:(kc + 1) * 128], HB.t[:, kc * 128:(kc + 1) * 128], idb.t[:], [HB, idb], [pT])
                kb.cp("act", HT.t[:].rearrange("p k t -> p (k t)"), pTb, [pT], [HT])

                def tm(c0, cw):
                    ps = self.psum()
                    for kc in range(8):
                        kb.mm(ps.t[:, 0:cw], HT.t[:, kc, :], win.t[:, kc, c0:c0 + cw], kc == 0, kc == 7, [HT, win], [ps])
                    return ps

                def fm(c0, cw, ps, o0):
                    for kc in range(8):
                        kb.mm(ps.t[0:cw, o0:o0 + 128], win.t[:, kc, c0:c0 + cw], HT.t[:, kc, :], kc == 0, kc == 7, [HT, win], [ps])

                ps1 = tm(0, 416)
                UQ, KR, UT, QO, KO, VO, QTMP, SQ, N2 = uqn[b], kr[b], uT[b], qo[b], ko[b], vo[b], qtmp[b], sq[b], n2[b]
                kb.act(J.t[:, 0:256], ps1.t[:, 0:256], AF.Square, [ps1], [J, SS], accum=SS.t[:, 3:4])
                kb.act(J.t[:, 256:384], ps1.t[:, 256:384], AF.Square, [ps1], [J, SS], accum=SS.t[:, 4:5])
                kb.ts("dve", SS.t[:, 5:6], SS.t[:, 3:4], 1.0 / 256, EPS, ALU.mult, ALU.add, [SS], [SS])
                kb.ts("dve", SS.t[:, 6:7], SS.t[:, 4:5], 1.0 / 128, EPS, ALU.mult, ALU.add, [SS], [SS])
                kb.act(SS.t[:, 5:7], SS.t[:, 5:7], AF.Sqrt, [SS], [SS])
                kb.recip(SS.t[:, 5:7], SS.t[:, 5:7], [SS], [SS])
                kb.ts("dve", UQ.t[:, 0:256], ps1.t[:, 0:256], SS.t[:, 5:6], None, ALU.mult, None, [ps1, SS], [UQ])
                kb.ts("dve", UQ.t[:, 256:384], ps1.t[:, 256:384], SS.t[:, 6:7], None, ALU.mult, None, [ps1, SS], [UQ])
                kb.cp("act", KR.t[:, 0, :], ps1.t[:, 384:416], [ps1], [KR])
                krv = KR.t[:, 0, :].rearrange("p (a f e) -> p a f e", a=2, f=2)
                krs = KR.t[:, 1, :].rearrange("p (a f e) -> p a f e", a=2, f=2)
                kb.cp("pool", krs[:, :, 0, :], krv[:, :, 1, :], [KR], [KR])
                kb.cp("pool", krs[:, :, 1, :], krv[:, :, 0, :], [KR], [KR])
                kb.tt("dve", KR.t[:, 2, :], KR.t[:, 0, :], rk[b].t[:, 0, :], ALU.mult, [KR, rk[b]], [KR])
                kb.tt("dve", KR.t[:, 3, :], KR.t[:, 1, :], rk[b].t[:, 1, :], ALU.mult, [KR, rk[b]], [KR])
                kb.tt("dve", KR.t[:, 0, :], KR.t[:, 2, :], KR.t[:, 3, :], ALU.add, [KR], [KR])
                p2 = self.psum()
                p2b = p2.t[:].bitcast(BF16)
                for j in range(3):
                    kb.tr(p2b[:, j * 128:(j + 1) * 128], UQ.t[:, j * 128:(j + 1) * 128], idb.t[:], [UQ, idb], [p2])
                kb.cp("act", UT.t[:].rearrange("p k t -> p (k t)"), p2b[:, 0:384], [p2], [UT])
                for hh in range(2):
                    pq = self.psum()
                    for kc in range(2):
                        kb.mm(pq.t[:, 0:384], UT.t[:, kc, :], wuq.t[:, kc, hh * 384:(hh + 1) * 384], kc == 0, kc == 1, [UT, wuq], [pq])
                    pqv = pq.t[:, 0:384].rearrange("p (h d) -> p h d", h=4)
                    qov = QO.t[:, hh * 4:(hh + 1) * 4, :]
                    kb.act(qov[:, :, 0:64], pqv[:, :, 0:64], AF.Identity, [pq], [QO], scale=MLA_SCALE)
                    t0 = QTMP.t[:, 0, hh * 128:(hh + 1) * 128].rearrange("p (h d) -> p h d", h=4)
                    t1 = QTMP.t[:, 1, hh * 128:(hh + 1) * 128].rearrange("p (h d) -> p h d", h=4)
                    kb.cp("act", t0, pqv[:, :, 64:96], [pq], [QTMP])
                    t0v = t0.rearrange("p h (a f e) -> p h a f e", a=2, f=2)
                    t1v = t1.rearrange("p h (a f e) -> p h a f e", a=2, f=2)
                    for a in range(2):
                        kb.cp("pool", t1v[:, :, a, 0, :], t0v[:, :, a, 1, :], [QTMP], [QTMP])
                        kb.cp("pool", t1v[:, :, a, 1, :], t0v[:, :, a, 0, :], [QTMP], [QTMP])
                    rc = rq[b].t[:, 0, hh * 128:(hh + 1) * 128].rearrange("p (h d) -> p h d", h=4)
                    rs = rq[b].t[:, 1, hh * 128:(hh + 1) * 128].rearrange("p (h d) -> p h d", h=4)
                    kb.tt("dve", t0, t0, rc, ALU.mult, [QTMP, rq[b]], [QTMP])
                    kb.tt("dve", t1, t1, rs, ALU.mult, [QTMP, rq[b]], [QTMP])
                    kb.tt("dve", qov[:, :, 64:96], t0, t1, ALU.add, [QTMP], [QO])
                for hh in range(2):
                    pk = self.psum()
                    kb.mm(pk.t[:], UT.t[:, 2, :], wkv.t[:, hh * 512:(hh + 1) * 512], True, True, [UT, wkv], [pk])
                    pkv = pk.t[:].rearrange("p (h d) -> p h d", h=4)
                    kb.cp("act", KO.t[:, hh * 4:(hh + 1) * 4, 0:64], pkv[:, :, 0:64], [pk], [KO])
                    kb.cp("dve", VO.t[:, hh * 4:(hh + 1) * 4, 0:64], pkv[:, :, 64:128], [pk], [VO])
                for h in range(8):
                    kb.cp("pool", KO.t[:, h, 64:96], KR.t[:, 0, :], [KR], [KO])
                kb.tt("dve", SQ.t[:], QO.t[:].rearrange("p h d -> p (h d)"), QO.t[:].rearrange("p h d -> p (h d)"), ALU.mult, [QO], [SQ])
                kb.red("dve", N2.t[:, 8:16], SQ.t[:].rearrange("p (h d) -> p h d", h=8), ALU.add, [SQ], [N2])
                kb.tt("dve", SQ.t[:], KO.t[:].rearrange("p h d -> p (h d)"), KO.t[:].rearrange("p h d -> p (h d)"), ALU.mult, [KO], [SQ])
                kb.red("dve", N2.t[:, 0:8], SQ.t[:].rearrange("p (h d) -> p h d", h=8), ALU.add, [SQ], [N2])
                kb.tt("dve", m16.t[:], m16.t[:], N2.t[:], ALU.max, [N2, m16], [m16])
                QKT = qkT[b]
                for which, src in ((0, KO), (1, QO)):
                    p3 = self.psum()
                    p3b = p3.t[:].bitcast(BF16)
                    for h in range(8):
                        kb.tr(p3b[0:96, h * 128:(h + 1) * 128], src.t[:, h, :], idb.t[:], [src, idb], [p3])
                    kb.cp("act" if which == 0 else "dve", QKT.t[:, which * 8:(which + 1) * 8, :].rearrange("p h t -> p (h t)"),
                          p3b[0:96, :], [p3], [QKT])
                kb.dma("sp", Dr["KT"][:, :, i * 128:(i + 1) * 128].rearrange("h d t -> d h t"), QKT.t[:, 0:8, :], [QKT], [Dk["KT"]])
                kb.dma("sp", Dr["QT"][:, :, i * 128:(i + 1) * 128].rearrange("h d t -> d h t"), QKT.t[:, 8:16, :], [QKT], [Dk["QT"]])
                kb.dma("sp", Dr["VA"][i * 128:(i + 1) * 128], VO.t[:], [VO], [Dk["VA"]])
                G = gkvo[b]
                for gi, (c0, cw) in enumerate(((O_GK, 512), (O_GK + 512, 512), (O_GK + 1024, 256))):
                    ps = tm(c0, cw)
                    o0 = c0 - O_GK
                    if gi == 0:
                        kb.cp("dve", G.t[:, 0:512], ps.t[:, 0:512], [ps], [G])
                    elif gi == 1:
                        kb.cp("dve", G.t[:, 512:768], ps.t[:, 0:256], [ps], [G])
                        kb.act(G.t[:, 768:1024], ps.t[:, 256:512], AF.Silu, [ps], [G])
                    else:
                        kb.act(G.t[:, 1024:1280], ps.t[:, 0:256], AF.Silu, [ps], [G])
                kb.dma("sp", Dr["GKVO"][i * 128:(i + 1) * 128, :], G.t[:], [G], [Dk["GKVO"]])
                GA = gate[b]
                for gi in range(6):
                    ps = tm(O_GATE + gi * 512, 512)
                    kb.act(GA.t[:, gi * 512:(gi + 1) * 512], ps.t[:], AF.Sigmoid, [ps], [GA])
                kb.dma("sp", Dr["GATE"][i * 128:(i + 1) * 128, :], GA.t[:], [GA], [Dk["GATE"]])
                ps = self.psum()
                for g in range(4):
                    fm(O_FN + g * 128, 128, ps, g * 128)
                kb.cp("dve", fT[b].t[:].rearrange("p g t -> p (g t)"), ps.t[:], [ps], [fT[b]])
                kb.dma("sp", Dr["UFT"][:, :, i * 128:(i + 1) * 128].rearrange("g c t -> c g t"), fT[b].t[:], [fT[b]], [Dk["UFT"]])
                psq = self.psum()
                psk = self.psum()
                for h in range(4):
                    fm(O_GQ + h * 64, 64, psq, h * 128)
                    fm(O_GK + h * 64, 64, psk, h * 128)
                kb.act(gqkT[b].t[:, 0:4, :].rearrange("p h t -> p (h t)"), psq.t[0:64, :], AF.Identity, [psq], [gqkT[b]], scale=GLA_SCALE)
                kb.cp("dve", gqkT[b].t[:, 4:8, :].rearrange("p h t -> p (h t)"), psk.t[0:64, :], [psk], [gqkT[b]])
                kb.dma("sp", Dr["GQT"][:, :, i * 128:(i + 1) * 128].rearrange("h d t -> d h t"), gqkT[b].t[:, 0:4, :], [gqkT[b]], [Dk["GQT"]])
                kb.dma("sp", Dr["GKT"][:, :, i * 128:(i + 1) * 128].rearrange("h d t -> d h t"), gqkT[b].t[:, 4:8, :], [gqkT[b]], [Dk["GKT"]])
                psg = self.psum()
                fm(O_GF, 16, psg, 0)
                fm(O_GB, 16, psg, 128)
                kb.cp("act", ugT[b].t[0:16, :, :].rearrange("p d t -> p (d t)"), psg.t[0:16, 0:256], [psg], [ugT[b]])
                psl = self.psum()
                for d in range(2):
                    kb.mm(psl.t[:, d * 256:(d + 1) * 256], ugT[b].t[:, d, :], wg.t[:, d, :], True, True, [ugT[b], wg], [psl])
                kb.act(lfe[b].t[:], psl.t[:], AF.Exp, [psl], [lfe[b]], scale=-1.0)
                kb.act(lfe[b].t[:], lfe[b].t[:], AF.Ln, [lfe[b]], [lfe[b]], bias=1.0)
                kb.ts("dve", lfb[b].t[:], lfe[b].t[:], -1.0 / 16.0, None, ALU.mult, None, [lfe[b]], [lfb[b]])
                kb.dma("sp", Dr["LFB"][i * 128:(i + 1) * 128, :], lfb[b].t[:], [lfb[b]], [Dk["LFB"]])
            pm = self.psum()
            kb.tr(pm.t[0:16, 0:128], m16.t[:], P["ident_f"].t[:], [m16, P["ident_f"]], [pm])
            mcol = kb.sb(st, "mcol", [16, 1], F32)
            mb = kb.sb(st, "mb", [16, 128], F32)
            kb.red("dve", mcol.t[:], pm.t[0:16, 0:128], ALU.max, [pm], [mcol])
            kb.ts("dve", mb.t[:], P["ones_f"].t[0:16, :], mcol.t[:, 0:1], None, ALU.mult, None, [mcol, P["ones_f"]], [mb])
            pm2 = self.psum()
            kb.mm(pm2.t[:, 0:16], mb.t[:], P["ident_f"].t[0:16, 0:16], True, True, [mb, P["ident_f"]], [pm2])
            nbt = kb.sb(st, "nbt", [128, 16], F32)
            kb.cp("act", nbt.t[:], pm2.t[:, 0:16], [pm2], [nbt])
            kb.tt("dve", nbt.t[:, 0:8], nbt.t[:, 0:8], nbt.t[:, 8:16], ALU.mult, [nbt], [nbt])
            kb.act(nbt.t[:, 0:8], nbt.t[:, 0:8], AF.Sqrt, [nbt], [nbt])
            kb.ts("dve", P["nb"].t[:], nbt.t[:, 0:8], -1.0, None, ALU.mult, None, [nbt], [P["nb"]])
            kb.S.flush()

    def phase_attn(self, l):
        kb, P, Dr, Dk = self.kb, self.P, self.Dr, self.Dk
        with contextlib.ExitStack() as st:
            va = kb.sb(st, "va", [128, NT, 520], BF16)
            kb.dma("pool", va.t[:], Dr["VA"].rearrange("(n p) h e -> p n (h e)", p=128), [Dk["VA"]], [va])
            kt_ = [kb.sb(st, "ktb%d" % j, [96, T], BF16) for j in range(2)]
            qt_ = [kb.sb(st, "qtb%d" % j, [96, T], BF16) for j in range(2)]
            pt = [kb.sb(st, "pt%d" % j, [128, 512], BF16) for j in range(4)]
            osb = [kb.sb(st, "osb%d" % j, [65, 512], F32) for j in range(2)]
            rec = [kb.sb(st, "rec%d" % j, [64, 512], F32) for j in range(2)]
            om = [kb.sb(st, "om%d" % j, [64, 512], BF16) for j in range(2)]
            ones = P["ones_f"]
            nb = P["nb"]
            cnt = 0
            blk = 0
            for h in range(8):
                KT, QT = kt_[h % 2], qt_[h % 2]
                kb.dma("sp", KT.t[:], Dr["KT"][h], [Dk["KT"]], [KT])
                kb.dma("sp", QT.t[:], Dr["QT"][h], [Dk["QT"]], [QT])
                blocks = [(0, 256, [0, 1])] + [(CTX + qb * 512, 512, list(range(NT))) for qb in range(8)]
                for (q0, qn, keys) in blocks:
                    po = self.psum(pin=True)
                    pend = None
                    seq = []
                    for kt in keys:
                        ps = self.psum()
                        kb.mm(ps.t[:, 0:qn], KT.t[:, kt * 128:(kt + 1) * 128], QT.t[:, q0:q0 + qn], True, True, [KT, QT], [ps])
                        seq.append((kt, ps))
                        if len(seq) >= 2:
                            self._attn_pv(seq.pop(0), keys, po, pt, va, nb, h, qn, cnt)
                            cnt += 1
                    while seq:
                        self._attn_pv(seq.pop(0), keys, po, pt, va, nb, h, qn, cnt)
                        cnt += 1
                    O, R, OM = osb[blk % 2], rec[blk % 2], om[blk % 2]
                    blk += 1
                    kb.cp("act", O.t[:, 0:qn], po.t[0:65, 0:qn], [po], [O])
                    self.unpin(po)
                    pb = self.psum()
                    kb.mm(pb.t[0:64, 0:qn], ones.t[64:65, 0:64], O.t[64:65, 0:qn], True, True, [ones, O], [pb])
                    kb.recip(R.t[:, 0:qn], pb.t[0:64, 0:qn], [pb], [R])
                    kb.tt("pool", OM.t[:, 0:qn], O.t[0:64, 0:qn], R.t[:, 0:qn], ALU.mult, [O, R], [OM])
                    kb.dma("sp", Dr["OMT"][h, :, q0:q0 + qn], OM.t[:, 0:qn], [OM], [Dk["OMT"]])
            kb.S.flush()

    def _attn_pv(self, item, keys, po, pt, va, nb, h, qn, cnt):
        kb = self.kb
        kt, ps = item
        PT = pt[cnt % 4]
        kb.act(PT.t[:, 0:qn], ps.t[:, 0:qn], AF.Exp, [ps, nb], [PT], bias=nb.t[:, h:h + 1], scale=1.0)
        kb.mm(po.t[0:65, 0:qn], va.t[:, kt, h * 65:(h + 1) * 65], PT.t[:, 0:qn], kt == keys[0], kt == keys[-1], [va, PT], [po])

    def phase_fnet(self, l):
        kb, P, C, Dr, Dk = self.kb, self.P, self.C, self.Dr, self.Dk
        with contextlib.ExitStack() as st:
            A = kb.sb(st, "fA", [128, NT, 512], BF16)
            Bm = kb.sb(st, "fB", [128, NT, 512], BF16)
            cs = kb.sb(st, "cs", [128, 256], BF16)
            kb.dma("sp", cs.t[:], C["cs128"], [], [cs])
            uf = [kb.sb(st, "uf%d" % j, [128, 4, 128], BF16) for j in range(2)]
            import os
            for i in range(int(os.environ.get("FN_TILES", NT))):
                U = uf[i % 2]
                kb.dma("sp", U.t[:], Dr["UFT"][:, :, i * 128:(i + 1) * 128].rearrange("g c t -> c g t"), [Dk["UFT"]], [U])
                for half in range(2):
                    ps = self.psum()
                    for g2 in range(2):
                        g = half * 2 + g2
                        kb.mm(ps.t[:, g2 * 256:(g2 + 1) * 256], U.t[:, g, :], cs.t[:], True, True, [U, cs], [ps])
                    psv = ps.t[:].rearrange("p (g x m) -> p g x m", g=2, x=2)
                    kb.cp("act", A.t[:, i, half * 256:(half + 1) * 256].rearrange("p (g m) -> p g m", g=2), psv[:, :, 0, :], [ps], [A])
                    kb.cp("dve", Bm.t[:, i, half * 256:(half + 1) * 256].rearrange("p (g m) -> p g m", g=2), psv[:, :, 1, :], [ps], [Bm])
            dc = [kb.sb(st, "dc%d" % j, [128, 32, 128], BF16) for j in range(2)]
            ds = [kb.sb(st, "ds%d" % j, [128, 32, 128], BF16) for j in range(2)]
            y = [kb.sb(st, "fy%d" % j, [128, 512], BF16) for j in range(2)]
            oft = [kb.sb(st, "oft%d" % j, [128, 4, 128], BF16) for j in range(2)]
            idb = P["ident_b"]
            jobs = [("c", kt) for kt in range(2)] + [("l", kt) for kt in range(32)]
            import os
            if os.environ.get("FN_PART") == "1":
                jobs = []
            if os.environ.get("FN_PART") == "2":
                jobs = jobs[:2]
            for n, (kind, kt) in enumerate(jobs):
                DC, DS, Y, OF = dc[n % 2], ds[n % 2], y[n % 2], oft[n % 2]
                if kind == "c":
                    ntt, t0, tok0 = 2, 0, kt * 128
                    kb.dma("sp", DC.t[:, 0:2, :], C["dft_c2"][kt], [], [DC])
                    kb.dma("pool", DS.t[:, 0:2, :], C["dft_s2"][kt], [], [DS])
                else:
                    ntt, t0, tok0 = 32, 2, CTX + kt * 128
                    kb.dma("sp", DC.t[:], C["dft_c"][kt], [], [DC])
                    kb.dma("pool", DS.t[:], C["dft_s"][kt], [], [DS])
                ps = self.psum()
                for tt in range(ntt):
                    kb.mm(ps.t[:], DC.t[:, tt, :], A.t[:, t0 + tt, :], tt == 0, False, [DC, A], [ps])
                    kb.mm(ps.t[:], DS.t[:, tt, :], Bm.t[:, t0 + tt, :], False, tt == ntt - 1, [DS, Bm], [ps])
                kb.cp("act", Y.t[:], ps.t[:], [ps], [Y])
                p2 = self.psum()
                p2b = p2.t[:].bitcast(BF16)
                for g in range(4):
                    kb.tr(p2b[:, g * 128:(g + 1) * 128], Y.t[:, g * 128:(g + 1) * 128], idb.t[:], [Y, idb], [p2])
                kb.cp("dve", OF.t[:].rearrange("p g t -> p (g t)"), p2b[:, 0:512], [p2], [OF])
                kb.dma("sp", Dr["OFT"][:, :, tok0:tok0 + 128].rearrange("g m t -> m g t"), OF.t[:], [OF], [Dk["OFT"]])
            kb.S.flush()

    def phase_gla(self, l):
        kb, P, C, W, Dr, Dk = self.kb, self.P, self.C, self.W, self.Dr, self.Dk
        with contextlib.ExitStack() as st:
            tri = kb.sb(st, "tri", [128, 2, 128], F32)
            mask4 = kb.sb(st, "mask4", [128, 2, 512], F32)
            gn = kb.sb(st, "gn", [128, 512], F32)
            for d in range(2):
                kb.dma("sp", tri.t[:, d, :], C["tri_f"][d], [], [tri])
                kb.dma("sp", mask4.t[:, d, :], C["mask4"][d].rearrange("p h t -> p (h t)"), [], [mask4])
            kb.dma("sp", gn.t[:], W["gla_norm_g"][l].partition_broadcast(128), [], [gn])
            S32 = kb.sb(st, "S32", [64, 4, 128], F32)
            Sb = kb.sb(st, "Sb", [64, 4, 128], BF16)
            NB2 = 2
            def mk(name, shp, dt):
                return [kb.sb(st, "%s%d" % (name, j), shp, dt) for j in range(NB2)]
            qT = mk("gqT", [64, 4, 128], BF16)
            kT = mk("gkT", [64, 4, 128], BF16)
            gk = mk("ggk", [128, 1280], BF16)
            lf = mk("glf", [128, 256], F32)
            gtok = mk("gtok", [128, 256], F32)
            e1 = mk("ge1", [128, 256], F32)
            khat = mk("khat", [128, 256], BF16)
            eq = mk("geq", [64, 4, 128], F32)
            ek = mk("gek", [64, 4, 128], F32)
            qtl = mk("qtl", [64, 4, 128], BF16)
            ktl = mk("ktl", [64, 4, 128], BF16)
            at = mk("gat", [128, 512], BF16)
            ofw = mk("gofw", [128, 512], F32)
            o32 = mk("go32", [128, 512], F32)
            sqo = mk("gsq", [128, 512], F32)
            st4 = mk("gst4", [128, 8], F32)
            ob = mk("gob", [128, 512], BF16)
            ogt = mk("gogt", [128, 4, 128], BF16)
            idb = P["ident_b"]
            for d in range(2):
                order = list(range(NT)) if d == 0 else [1, 0] + list(range(NT - 1, 1, -1))
                last = 127 if d == 0 else 0
                kb.memset("dve", S32.t[:], 0.0, [], [S32])
                kb.memset("pool", Sb.t[:], 0.0, [], [Sb])
                for n, i in enumerate(order):
                    b = n % NB2
                    sl = slice(i * 128, (i + 1) * 128)
                    kb.dma("sp", qT[b].t[:], Dr["GQT"][:, :, sl].rearrange("h d t -> d h t"), [Dk["GQT"]], [qT[b]])
                    kb.dma("sp", kT[b].t[:], Dr["GKT"][:, :, sl].rearrange("h d t -> d h t"), [Dk["GKT"]], [kT[b]])
                    kb.dma("sp", gk[b].t[:], Dr["GKVO"][sl, :], [Dk["GKVO"]], [gk[b]])
                    kb.dma("sp", lf[b].t[:], Dr["LFB"][sl, d * 256:(d + 1) * 256], [Dk["LFB"]], [lf[b]])
                    if d == 1:
                        kb.dma("sp", ofw[b].t[:], Dr["OFW"][sl, :], [Dk["OFW"]], [ofw[b]])
                    LF = lf[b]
                    pg = self.psum()
                    kb.mm(pg.t[:, 0:256], tri.t[:, d, :], LF.t[:], True, True, [tri, LF], [pg])
                    kb.mm(pg.t[:, 256:512], P["ones_f"].t[:], LF.t[:], True, True, [P["ones_f"], LF], [pg])
                    kb.cp("act", gtok[b].t[:], pg.t[:, 0:256], [pg], [gtok[b]])
                    kb.tt("dve", e1[b].t[:], pg.t[:, 256:512], gtok[b].t[:], ALU.subtract, [pg, gtok[b]], [e1[b]])
                    kb.act(e1[b].t[:], e1[b].t[:], AF.Exp, [e1[b]], [e1[b]])
                    kb.tt("dve", khat[b].t[:], gk[b].t[:, 0:256], e1[b].t[:], ALU.mult, [gk[b], e1[b]], [khat[b]])
                    pf = self.psum()
                    for h in range(4):
                        kb.mm(pf.t[0:64, h * 128:(h + 1) * 128], LF.t[:, h * 64:(h + 1) * 64], tri.t[:, d, :], True, True, [LF, tri], [pf])
                    kb.act(eq[b].t[:].rearrange("p h t -> p (h t)"), pf.t[0:64, :], AF.Exp, [pf], [eq[b]])
                    kb.act(ek[b].t[:].rearrange("p h t -> p (h t)"), pf.t[0:64, :], AF.Exp, [pf], [ek[b]], scale=-1.0)
                    kb.tt("dve", qtl[b].t[:], qT[b].t[:], eq[b].t[:], ALU.mult, [qT[b], eq[b]], [qtl[b]])
                    kb.tt("pool", ktl[b].t[:], kT[b].t[:], ek[b].t[:], ALU.mult, [kT[b], ek[b]], [ktl[b]])
                    pa = self.psum()
                    for h in range(4):
                        kb.mm(pa.t[:, h * 128:(h + 1) * 128], ktl[b].t[:, h, :], qtl[b].t[:, h, :], True, True, [ktl[b], qtl[b]], [pa])
                    kb.tt("dve", at[b].t[:], pa.t[:], mask4.t[:, d, :], ALU.mult, [pa, mask4], [at[b]])
                    po = self.psum()
                    for h in range(4):
                        kb.mm(po.t[:, h * 128:(h + 1) * 128], qtl[b].t[:, h, :], Sb.t[:, h, :], True, False, [qtl[b], Sb], [po])
                        kb.mm(po.t[:, h * 128:(h + 1) * 128], at[b].t[:, h * 128:(h + 1) * 128],
                              gk[b].t[:, 256 + h * 128:256 + (h + 1) * 128], False, True, [at[b], gk[b]], [po])
                    pS = self.psum()
                    for h in range(4):
                        kb.mm(pS.t[0:64, h * 128:(h + 1) * 128], khat[b].t[:, h * 64:(h + 1) * 64],
                              gk[b].t[:, 256 + h * 128:256 + (h + 1) * 128], True, True, [khat[b], gk[b]], [pS])
                    for h in range(4):
                        kb.stt("dve", S32.t[:, h, :], S32.t[:, h, :], eq[b].t[:, h, last:last + 1], pS.t[0:64, h * 128:(h + 1) * 128],
                               ALU.mult, ALU.add, [S32, eq[b], pS], [S32])
                    kb.cp("pool", Sb.t[:], S32.t[:], [S32], [Sb])
                    if d == 0:
                        kb.cp("act", o32[b].t[:], po.t[:], [po], [o32[b]])
                        kb.dma("sp", Dr["OFW"][sl, :], o32[b].t[:], [o32[b]], [Dk["OFW"]])
                    else:
                        O, SQ, S4 = o32[b], sqo[b], st4[b]
                        kb.tt("dve", O.t[:], po.t[:], ofw[b].t[:], ALU.add, [po, ofw[b]], [O])
                        kb.tt("pool", SQ.t[:], O.t[:], O.t[:], ALU.mult, [O], [SQ])
                        kb.red("dve", S4.t[:, 0:4], SQ.t[:].rearrange("p (h e) -> p h e", h=4), ALU.add, [SQ], [S4])
                        kb.ts("dve", S4.t[:, 0:4], S4.t[:, 0:4], 1.0 / 128, EPS, ALU.mult, ALU.add, [S4], [S4])
                        kb.act(S4.t[:, 0:4], S4.t[:, 0:4], AF.Sqrt, [S4], [S4])
                        kb.recip(S4.t[:, 4:8], S4.t[:, 0:4], [S4], [S4])
                        for h in range(4):
                            kb.stt("dve", O.t[:, h * 128:(h + 1) * 128], O.t[:, h * 128:(h + 1) * 128], S4.t[:, 4 + h:5 + h],
                                   gn.t[:, h * 128:(h + 1) * 128], ALU.mult, ALU.mult, [O, S4, gn], [O])
                        kb.tt("pool", ob[b].t[:], O.t[:], gk[b].t[:, 768:1280], ALU.mult, [O, gk[b]], [ob[b]])
                        p2 = self.psum()
                        p2b = p2.t[:].bitcast(BF16)
                        for g in range(4):
                            kb.tr(p2b[:, g * 128:(g + 1) * 128], ob[b].t[:, g * 128:(g + 1) * 128], idb.t[:], [ob[b], idb], [p2])
                        kb.cp("act", ogt[b].t[:].rearrange("p g t -> p (g t)"), p2b[:, 0:512], [p2], [ogt[b]])
                        kb.dma("sp", Dr["OGT"][:, :, sl].rearrange("g m t -> m g t"), ogt[b].t[:], [ogt[b]], [Dk["OGT"]])
            kb.S.flush()

    def phase_merge(self, l):
        kb, P, W, Dr, Dk = self.kb, self.P, self.W, self.Dr, self.Dk
        with contextlib.ExitStack() as st:
            rows = self.mod_rows(st, l, 1, names=("G",))
            wbm = kb.sb(st, "wbm", [64, 8, D], BF16)
            wbf = kb.sb(st, "wbf", [128, 4, D], BF16)
            wbg = kb.sb(st, "wbg", [128, 4, D], BF16)
            wo = kb.sb(st, "wo", [128, 8, D], BF16)
            kb.dma("pool", wbm.t[:], W["w_br_mla"][l].rearrange("(h d) n -> d h n", d=64), [], [wbm])
            kb.dma("pool", wbf.t[:], W["w_br_fnet"][l].rearrange("(k p) n -> p k n", p=128), [], [wbf])
            kb.dma("pool", wbg.t[:], W["w_br_gla"][l].rearrange("(k p) n -> p k n", p=128), [], [wbg])
            kb.dma("pool", wo.t[:], W["w_o"][l].rearrange("(k p) n -> p k n", p=128), [], [wo])
            NB2 = 2
            def mk(name, shp, dt):
                return [kb.sb(st, "%s%d" % (name, j), shp, dt) for j in range(NB2)]
            omT = mk("momT", [64, 8, 128], BF16)
            ofT = mk("mofT", [128, 4, 128], BF16)
            ogT = mk("mogT", [128, 4, 128], BF16)
            ga = mk("mga", [128, 3072], BF16)
            xt = mk("mxt", [128, D], F32)
            y32 = mk("my32", [128, D], F32)
            t32 = mk("mt32", [128, D], F32)
            yb = mk("myb", [128, D], BF16)
            yT = mk("myT", [128, 8, 128], BF16)
            xn = mk("mxn", [128, D], F32)
            idb = P["ident_b"]
            for i in range(NT):
                b = i % NB2
                sl = slice(i * 128, (i + 1) * 128)
                G = rows["Gc" if i < 2 else "Gl"]
                kb.dma("sp", omT[b].t[:], Dr["OMT"][:, :, sl].rearrange("h d t -> d h t"), [Dk["OMT"]], [omT[b]])
                kb.dma("sp", ofT[b].t[:], Dr["OFT"][:, :, sl].rearrange("g m t -> m g t"), [Dk["OFT"]], [ofT[b]])
                kb.dma("sp", ogT[b].t[:], Dr["OGT"][:, :, sl].rearrange("g m t -> m g t"), [Dk["OGT"]], [ogT[b]])
                kb.dma("sp", ga[b].t[:], Dr["GATE"][sl, :], [Dk["GATE"]], [ga[b]])
                kb.dma("sp", xt[b].t[:], Dr["XR"][sl, :], [Dk["XR"]], [xt[b]])
                for half in range(2):
                    cs_ = slice(half * 512, (half + 1) * 512)
                    pm = self.psum()
                    for h in range(8):
                        kb.mm(pm.t[:], omT[b].t[:, h, :], wbm.t[:, h, cs_], h == 0, h == 7, [omT[b], wbm], [pm])
                    pf = self.psum()
                    for k in range(4):
                        kb.mm(pf.t[:], ofT[b].t[:, k, :], wbf.t[:, k, cs_], k == 0, k == 3, [ofT[b], wbf], [pf])
                    pg = self.psum()
                    for k in range(4):
                        kb.mm(pg.t[:], ogT[b].t[:, k, :], wbg.t[:, k, cs_], k == 0, k == 3, [ogT[b], wbg], [pg])
                    Y, T32 = y32[b], t32[b]
                    kb.tt("dve", Y.t[:, cs_], pm.t[:], ga[b].t[:, half * 512:(half + 1) * 512], ALU.mult, [pm, ga[b]], [Y])
                    kb.tt("dve", T32.t[:, cs_], pf.t[:], ga[b].t[:, 1024 + half * 512:1024 + (half + 1) * 512], ALU.mult, [pf, ga[b]], [T32])
                    kb.tt("pool", Y.t[:, cs_], Y.t[:, cs_], T32.t[:, cs_], ALU.add, [Y, T32], [Y])
                    kb.tt("dve", T32.t[:, cs_], pg.t[:], ga[b].t[:, 2048 + half * 512:2048 + (half + 1) * 512], ALU.mult, [pg, ga[b]], [T32])
                    kb.tt("pool", yb[b].t[:, cs_], Y.t[:, cs_], T32.t[:, cs_], ALU.add, [Y, T32], [yb[b]])
                p2 = self.psum()
                p2b = p2.t[:].bitcast(BF16)
                for kc in range(8):
                    kb.tr(p2b[:, kc * 128:(kc + 1) * 128], yb[b].t[:, kc * 128:(kc + 1) * 128], idb.t[:], [yb[b], idb], [p2])
                kb.cp("act", yT[b].t[:].rearrange("p k t -> p (k t)"), p2b, [p2], [yT[b]])
                for half in range(2):
                    cs_ = slice(half * 512, (half + 1) * 512)
                    pz = self.psum()
                    for kc in range(8):
                        kb.mm(pz.t[:], yT[b].t[:, kc, :], wo.t[:, kc, cs_], kc == 0, kc == 7, [yT[b], wo], [pz])
                    kb.tt("dve", xn[b].t[:, cs_], pz.t[:], G.t[:, cs_], ALU.mult, [pz, G], [xn[b]])
                    kb.tt("pool", xn[b].t[:, cs_], xn[b].t[:, cs_], xt[b].t[:, cs_], ALU.add, [xn[b], xt[b]], [xn[b]])
                kb.dma("sp", Dr["XR"][sl, :], xn[b].t[:], [xn[b]], [Dk["XR"]])
            kb.S.flush()

    def phase_moe(self, l):
        kb, P, W, C, Dr, Dk = self.kb, self.P, self.W, self.C, self.Dr, self.Dk
        is_last = (l == self.nl - 1)
        idb = P["ident_b"]
        with contextlib.ExitStack() as st0:
            DSTI = kb.sb(st0, "DSTI", [128, NT, 4], I32)
            G4 = kb.sb(st0, "G4", [128, NT, 4], F32)
            IDXW = kb.sb(st0, "IDXW", [128, NBLK, 8], I32)
            EBI = kb.sb(st0, "EBI", [128, NBLK], I32)
            with contextlib.ExitStack() as st:
                rows = self.mod_rows(st, l, 2, names=("A", "S"))
                rw = kb.sb(st, "rw", [128, 8, NE], F32)
                rb = kb.sb(st, "rb", [1, NE], F32)
                trx = kb.sb(st, "trx", [128, 128], BF16)
                jv = kb.sb(st, "jv", [128, NBLK], F32)
                kp = kb.sb(st, "kp", [128, 8], F32)
                kb.dma("sp", rw.t[:], W["router_w"][l].rearrange("(k p) e -> p k e", p=128), [], [rw])
                kb.dma("sp", rb.t[:], W["router_b"][l].rearrange("(o e) -> o e", o=1), [], [rb])
                kb.dma("sp", trx.t[:], C["tri_x"], [], [trx])
                kb.dma("sp", jv.t[:], C["jv"], [], [jv])
                kb.dma("sp", kp.t[:], C["kp"], [], [kp])
                LG = kb.sb(st, "LG", [128, NT, NE], F32)
                GF = kb.sb(st, "GF", [128, NT, NE], F32)
                POS = kb.sb(st, "POS", [128, NT, NE], F32)
                TOP = kb.sb(st, "TOP", [128, NT, 8], F32)
                cnt = kb.sb(st, "cnt", [128, NE], F32)
                kb.memset("dve", cnt.t[:], 0.0, [], [cnt])
                NB2 = 2
                def mk(name, shp, dt):
                    return [kb.sb(st, "%s%d" % (name, j), shp, dt) for j in range(NB2)]
                xt = mk("ext", [128, D], F32)
                junk = kb.sb(st, "ejunk", [128, D], F32)
                ssq = mk("essq", [128, 8], F32)
                h32 = mk("eh32", [128, D], F32)
                hb = mk("ehb", [128, D], BF16)
                h2T = mk("eh2T", [128, 8, 128], F32)
                sm = mk("esm", [128, 4, NE], F32)
                sc = mk("esc", [128, 8], F32)
                mkb = mk("emkb", [128, NE], BF16)
                for i in range(NT):
                    b = i % NB2
                    sl = slice(i * 128, (i + 1) * 128)
                    kb.dma("sp", xt[b].t[:], Dr["XR"][sl, :], [Dk["XR"]], [xt[b]])
                    kb.memset("pool", ssq[b].t[:], 0.0, [], [ssq[b]])
                    self.norm_mod(xt[b], rows, i, junk, ssq[b], h32[b], hb[b], full32=True)
                    kb.dma("sp", Dr["H2B"][sl, :], hb[b].t[:], [hb[b]], [Dk["H2B"]])
                    for half in range(2):
                        pt_ = self.psum()
                        for k4 in range(4):
                            kc = half * 4 + k4
                            kb.tr(pt_.t[:, k4 * 128:(k4 + 1) * 128], h32[b].t[:, kc * 128:(kc + 1) * 128], P["ident_f"].t[:],
                                  [h32[b], P["ident_f"]], [pt_])
                        kb.cp("act" if half == 0 else "dve", h2T[b].t[:, half * 4:(half + 1) * 4, :].rearrange("p k t -> p (k t)"),
                              pt_.t[:], [pt_], [h2T[b]])
                    pl = self.psum()
                    for kc in range(8):
                        kb.mm(pl.t[:, 0:NE], h2T[b].t[:, kc, :], rw.t[:, kc, :], kc == 0, False, [h2T[b], rw], [pl])
                    kb.mm(pl.t[:, 0:NE], P["ones_f"].t[0:1, :], rb.t[0:1, :], False, True, [P["ones_f"], rb], [pl])
                    kb.cp("act", LG.t[:, i, :], pl.t[:, 0:NE], [pl], [LG])
                    kb.op("dve", lambda e, o=TOP.t[:, i, :], a=LG.t[:, i, :]: e.max(out=o, in_=a), [LG], [TOP])
                    SM, SC = sm[b], sc[b]
                    kb.ts("dve", SM.t[:, 0, :], LG.t[:, i, :], TOP.t[:, i, 3:4], None, ALU.is_ge, None, [LG, TOP], [SM])
                    kb.ts("dve", SC.t[:, 0:1], TOP.t[:, i, 0:1], -1.0, None, ALU.mult, None, [TOP], [SC])
                    kb.act(SM.t[:, 1, :], LG.t[:, i, :], AF.Exp, [LG, SC], [SM], bias=SC.t[:, 0:1], scale=1.0)
                    kb.tt("dve", SM.t[:, 2, :], SM.t[:, 1, :], SM.t[:, 0, :], ALU.mult, [SM], [SM])
                    kb.red("dve", SC.t[:, 1:2], SM.t[:, 2, :], ALU.add, [SM], [SC])
                    kb.recip(SC.t[:, 2:3], SC.t[:, 1:2], [SC], [SC])
                    kb.ts("dve", GF.t[:, i, :], SM.t[:, 2, :], SC.t[:, 2:3], None, ALU.mult, None, [SM, SC], [GF])
                    kb.cp("pool", mkb[b].t[:], SM.t[:, 0, :], [SM], [mkb[b]])
                    pp = self.psum()
                    kb.mm(pp.t[:, 0:NE], trx.t[:], mkb[b].t[:], True, True, [trx, mkb[b]], [pp])
                    kb.mm(pp.t[:, NE:2 * NE], P["ones_b"].t[:], mkb[b].t[:], True, True, [P["ones_b"], mkb[b]], [pp])
                    kb.tt("dve", POS.t[:, i, :], pp.t[:, 0:NE], cnt.t[:], ALU.add, [pp, cnt], [POS])
                    kb.tt("dve", cnt.t[:], pp.t[:, NE:2 * NE], cnt.t[:], ALU.add, [pp, cnt], [cnt])
                nbk = kb.sb(st, "nbk", [128, NE], F32)
                pend = kb.sb(st, "pend", [128, NE], F32)
                pstart = kb.sb(st, "pstart", [128, NE], F32)
                eb = kb.sb(st, "eb", [128, NBLK], F32)
                idxf = kb.sb(st, "idxf", [128, NBLK, 8], F32)
                kb.memset("dve", nbk.t[:], 0.0, [], [nbk])
                for j in range(T // BS + 1):
                    kb.stt("dve", nbk.t[:], cnt.t[:], float(j * BS), nbk.t[:], ALU.is_gt, ALU.add, [cnt, nbk], [nbk])
                kb.ts("dve", nbk.t[:], nbk.t[:], float(BS), None, ALU.mult, None, [nbk], [nbk])
                kb.cp("dve", pend.t[:, 0:1], nbk.t[:, 0:1], [nbk], [pend])
                for e_ in range(1, NE):
                    kb.tt("dve", pend.t[:, e_:e_ + 1], pend.t[:, e_ - 1:e_], nbk.t[:, e_:e_ + 1], ALU.add, [pend, nbk], [pend])
                kb.tt("dve", pstart.t[:], pend.t[:], nbk.t[:], ALU.subtract, [pend, nbk], [pstart])
                kb.memset("dve", eb.t[:], 0.0, [], [eb])
                for e_ in range(NE):
                    kb.stt("dve", eb.t[:], jv.t[:], pend.t[:, e_:e_ + 1], eb.t[:], ALU.is_ge, ALU.add, [jv, pend, eb], [eb])
                kb.ts("dve", eb.t[:], eb.t[:], float(NE - 1), None, ALU.min, None, [eb], [eb])
                kb.ts("dve", kp.t[:], kp.t[:], float(l * NE * D), None, ALU.add, None, [kp], [kp])
                for kc in range(8):
                    kb.ts("dve", idxf.t[:, :, kc], eb.t[:], 1024.0, kp.t[:, kc:kc + 1], ALU.mult, ALU.add, [eb, kp], [idxf])
                kb.cp("dve", IDXW.t[:], idxf.t[:], [idxf], [IDXW])
                kb.ts("dve", eb.t[:], eb.t[:], float(l * NE), None, ALU.add, None, [eb], [eb])
                kb.cp("dve", EBI.t[:], eb.t[:], [eb], [EBI])
                dstf = mk("edstf", [128, 4], F32)
                hs = mk("ehs", [128, D], BF16)
                for i in range(NT):
                    b = i % NB2
                    sl = slice(i * 128, (i + 1) * 128)
                    SM = sm[b]
                    kb.tt("dve", SM.t[:, 3, :], POS.t[:, i, :], pstart.t[:], ALU.add, [POS, pstart], [SM])
                    for k in range(4):
                        kb.ts("dve", SM.t[:, 0, :], LG.t[:, i, :], TOP.t[:, i, k:k + 1], None, ALU.is_equal, None, [LG, TOP], [SM])
                        kb.tt("dve", SM.t[:, 1, :], SM.t[:, 0, :], SM.t[:, 3, :], ALU.mult, [SM], [SM])
                        kb.red("dve", dstf[b].t[:, k:k + 1], SM.t[:, 1, :], ALU.add, [SM], [dstf[b]])
                        kb.tt("dve", SM.t[:, 2, :], SM.t[:, 0, :], GF.t[:, i, :], ALU.mult, [SM, GF], [SM])
                        kb.red("dve", G4.t[:, i, k:k + 1], SM.t[:, 2, :], ALU.add, [SM], [G4])
                    kb.cp("dve", DSTI.t[:, i, :], dstf[b].t[:], [dstf[b]], [DSTI])
                    kb.dma("sp", hs[b].t[:], Dr["H2B"][sl, :], [Dk["H2B"]], [hs[b]])
                    for k in range(4):
                        kb.scatter(Dr["XS"], hs[b].t[:], DSTI.t[:, i, k:k + 1], [hs[b], DSTI], [Dk["XS"]], NROWS - 1)
                if "DBG_DST" in self.dbg:
                    kb.dma("sp", Dr["DBG_DST"], DSTI.t[:].rearrange("p n k -> p (n k)"), [DSTI], [Dk["DBG_DST"]])
                    kb.dma("sp", Dr["DBG_G4"], G4.t[:].rearrange("p n k -> p (n k)"), [G4], [Dk["DBG_G4"]])
                    kb.dma("sp", Dr["DBG_EB"], EBI.t[:], [EBI], [Dk["DBG_EB"]])
                    kb.dma("sp", Dr["DBG_LG"], LG.t[:].rearrange("p n k -> p (n k)"), [LG], [Dk["DBG_LG"]])
                    kb.dma("sp", Dr["DBG_CNT"], cnt.t[:], [cnt], [Dk["DBG_CNT"]])
                kb.S.flush()
            with contextlib.ExitStack() as st:
                wup = [kb.sb(st, "wup%d" % j, [128, 8, 2 * D], BF16) for j in range(2)]
                wdn = [kb.sb(st, "wdn%d" % j, [128, 8, D], BF16) for j in range(1)]
                stg = [kb.sb(st, "stg%d" % j, [128, 2 * D], F32) for j in range(2)]
                bu = kb.sb(st, "bu", [2, 2 * D], F32)
                bd = kb.sb(st, "bd", [2, D], F32)
                NB2 = 2
                def mk(name, shp, dt):
                    return [kb.sb(st, "%s%d" % (name, j), shp, dt) for j in range(NB2)]
                xs = mk("xs", [128, D], BF16)
                xT = mk("xT", [128, 8, 128], BF16)
                gl = mk("gl", [128, 512], F32)
                li = mk("li", [128, 512], F32)
                sg = mk("sg", [128, 512], F32)
                ab = mk("ab", [128, D], BF16)
                aT = mk("aT", [128, 8, 128], BF16)
                yb = mk("yb", [128, D], F32)
                wup_src = W["exp_w_up"].rearrange("l e k n -> (l e k) n")
                wdn_src = W["exp_w_down"].rearrange("l e k n -> (l e k) n")
                bup_src = W["exp_b_up"].rearrange("l e n -> (l e) n")
                bdn_src = W["exp_b_down"].rearrange("l e n -> (l e) n")
                nlw = W["exp_w_up"].shape[0]
                ones1 = P["ones_f"].t[0:1, :]
                ns = 0
                nq = 0
                ceng = ["act", "dve", "pool"]
                for j in range(NBLK):
                    WU, WD = wup[j % 2], wdn[0]
                    for kc in range(8):
                        S_ = stg[ns % 2]
                        ns += 1
                        kb.gather(S_.t[:], wup_src, IDXW.t[:, j, kc:kc + 1], [IDXW], [S_], nlw * NE * D - 1)
                        for hh in range(2):
                            kb.cp(ceng[nq % 3], WU.t[:, kc, hh * D:(hh + 1) * D], S_.t[:, hh * D:(hh + 1) * D], [S_], [WU])
                            nq += 1
                    for kc in range(8):
                        S_ = stg[ns % 2]
                        ns += 1
                        kb.gather(S_.t[:, 0:D], wdn_src, IDXW.t[:, j, kc:kc + 1], [IDXW], [S_], nlw * NE * D - 1)
                        kb.cp(ceng[nq % 3], WD.t[:, kc, :], S_.t[:, 0:D], [S_], [WD])
                        nq += 1
                    kb.gather(bu.t[:], bup_src, EBI.t[0:2, j:j + 1], [EBI], [bu], nlw * NE - 1)
                    kb.gather(bd.t[:], bdn_src, EBI.t[0:2, j:j + 1], [EBI], [bd], nlw * NE - 1)
                    for sub in range(SUB):
                        b = (j * SUB + sub) % NB2
                        r0 = j * BS + sub * 128
                        kb.dma("sp", xs[b].t[:], Dr["XS"][r0:r0 + 128, :], [Dk["XS"]], [xs[b]])
                        p2 = self.psum()
                        p2b = p2.t[:].bitcast(BF16)
                        for kc in range(8):
                            kb.tr(p2b[:, kc * 128:(kc + 1) * 128], xs[b].t[:, kc * 128:(kc + 1) * 128], idb.t[:], [xs[b], idb], [p2])
                        kb.cp("act", xT[b].t[:].rearrange("p k t -> p (k t)"), p2b, [p2], [xT[b]])
                        for pair in range(2):
                            pgl = self.psum()
                            pli = self.psum()
                            for (pp_, c0) in ((pgl, pair * 512), (pli, D + pair * 512)):
                                for kc in range(8):
                                    kb.mm(pp_.t[:], xT[b].t[:, kc, :], WU.t[:, kc, c0:c0 + 512], kc == 0, False, [xT[b], WU], [pp_])
                                kb.mm(pp_.t[:], ones1, bu.t[0:1, c0:c0 + 512], False, True, [P["ones_f"], bu], [pp_])
                            GL, LI, SG = gl[pair], li[pair], sg[pair]
                            kb.ts("dve", GL.t[:], pgl.t[:], 7.0, None, ALU.min, None, [pgl], [GL])
                            kb.act(SG.t[:], GL.t[:], AF.Sigmoid, [GL], [SG], scale=1.702)
                            kb.ts("dve", LI.t[:], pli.t[:], 7.0, -7.0, ALU.min, ALU.max, [pli], [LI])
                            kb.stt("dve", LI.t[:], LI.t[:], 1.0, GL.t[:], ALU.add, ALU.mult, [LI, GL], [LI])
                            kb.tt("pool", ab[b].t[:, pair * 512:(pair + 1) * 512], LI.t[:], SG.t[:], ALU.mult, [LI, SG], [ab[b]])
                        p3 = self.psum()
                        p3b = p3.t[:].bitcast(BF16)
                        for kc in range(8):
                            kb.tr(p3b[:, kc * 128:(kc + 1) * 128], ab[b].t[:, kc * 128:(kc + 1) * 128], idb.t[:], [ab[b], idb], [p3])
                        kb.cp("act", aT[b].t[:].rearrange("p k t -> p (k t)"), p3b, [p3], [aT[b]])
                        for half in range(2):
                            pd = self.psum()
                            for kc in range(8):
                                kb.mm(pd.t[:], aT[b].t[:, kc, :], WD.t[:, kc, half * 512:(half + 1) * 512], kc == 0, False, [aT[b], WD], [pd])
                            kb.mm(pd.t[:], ones1, bd.t[0:1, half * 512:(half + 1) * 512], False, True, [P["ones_f"], bd], [pd])
                            kb.cp("act" if half == 0 else "dve", yb[b].t[:, half * 512:(half + 1) * 512], pd.t[:], [pd], [yb[b]])
                        kb.dma("sp", Dr["YB"][r0:r0 + 128, :], yb[b].t[:], [yb[b]], [Dk["YB"]])
                kb.S.flush()
            with contextlib.ExitStack() as st:
                rows = self.mod_rows(st, l, 2, names=("G",))
                NB2 = 2
                def mk(name, shp, dt):
                    return [kb.sb(st, "%s%d" % (name, j), shp, dt) for j in range(NB2)]
                xt = mk("cxt", [128, D], F32)
                yk = [kb.sb(st, "cyk%d" % j, [128, D], F32) for j in range(4)]
                acc = mk("cacc", [128, D], F32)
                xn = mk("cxn", [128, D], F32)
                if is_last:
                    fg = kb.sb(st, "fg", [128, D], F32)
                    kb.dma("sp", fg.t[:], W["final_norm_g"].partition_broadcast(128), [], [fg])
                    junk = kb.sb(st, "cjunk", [128, D], F32)
                    ssq = mk("cssq", [128, 8], F32)
                    ot = mk("cot", [128, D], F32)
                for i in range(NT):
                    b = i % NB2
                    sl = slice(i * 128, (i + 1) * 128)
                    G = rows["Gc" if i < 2 else "Gl"]
                    kb.dma("sp", xt[b].t[:], Dr["XR"][sl, :], [Dk["XR"]], [xt[b]])
                    for k in range(4):
                        kb.gather(yk[k].t[:], Dr["YB"], DSTI.t[:, i, k:k + 1], [DSTI, Dk["YB"]], [yk[k]], NROWS - 1)
                    A_ = acc[b]
                    kb.ts("dve", A_.t[:], yk[0].t[:], G4.t[:, i, 0:1], None, ALU.mult, None, [yk[0], G4], [A_])
                    for k in range(1, 4):
                        kb.stt("dve", A_.t[:], yk[k].t[:], G4.t[:, i, k:k + 1], A_.t[:], ALU.mult, ALU.add, [yk[k], G4, A_], [A_])
                    kb.tt("pool", A_.t[:], A_.t[:], G.t[:], ALU.mult, [A_, G], [A_])
                    kb.tt("dve", xn[b].t[:], A_.t[:], xt[b].t[:], ALU.add, [A_, xt[b]], [xn[b]])
                    kb.dma("sp", Dr["XR"][sl, :], xn[b].t[:], [xn[b]], [Dk["XR"]])
                    if is_last and i >= 2:
                        SS = ssq[b]
                        kb.memset("pool", SS.t[:], 0.0, [], [SS])
                        kb.act(junk.t[:], xn[b].t[:], AF.Square, [xn[b]], [junk, SS], accum=SS.t[:, 0:1])
                        kb.ts("dve", SS.t[:, 1:2], SS.t[:, 0:1], 1.0 / D, EPS, ALU.mult, ALU.add, [SS], [SS])
                        kb.act(SS.t[:, 1:2], SS.t[:, 1:2], AF.Sqrt, [SS], [SS])
                        kb.recip(SS.t[:, 2:3], SS.t[:, 1:2], [SS], [SS])
                        kb.stt("dve", ot[b].t[:], xn[b].t[:], SS.t[:, 2:3], fg.t[:], ALU.mult, ALU.mult, [xn[b], SS, fg], [ot[b]])
                        kb.dma("sp", self.out[(i - 2) * 128:(i - 1) * 128, :], ot[b].t[:], [ot[b]], [self.tout])
                kb.S.flush()


_CACHE = {}


def kernel(**inputs):
    n_cores = 8
    if "nc" not in _CACHE:
        pg = Prog(nl=DEPTH)
        _CACHE["nc"] = pg.build()
        _CACHE["consts"] = host_consts()
    nc = _CACHE["nc"]
    consts = _CACHE["consts"]
    shared = {k: np.ascontiguousarray(np.asarray(inputs[k], dtype=np.float32)) for k in WEIGHT_SPECS}
    shared["c_ctx"] = np.ascontiguousarray(np.asarray(inputs["c_ctx"], dtype=np.float32))
    shared.update(consts)
    x = np.asarray(inputs["x"], dtype=np.float32)
    c = np.asarray(inputs["c"], dtype=np.float32)
    ctx = np.asarray(inputs["ctx"], dtype=np.float32)
    in_maps = []
    for b in range(n_cores):
        m = dict(shared)
        m["x"] = np.ascontiguousarray(x[b])
        m["ctx"] = np.ascontiguousarray(ctx[b])
        m["c"] = np.ascontiguousarray(c[b])
        in_maps.append(m)
    res = run_bass_kernel_spmd(nc, in_maps, core_ids=list(range(n_cores)))
    out = np.stack([np.asarray(res.results[b]["out"], dtype=np.float32) for b in range(n_cores)], axis=0)
    return out
```

```python
import contextlib
import math
import numpy as np
import ml_dtypes
import concourse.bass as bass
import concourse.mybir as mybir
from concourse.bass_utils import run_bass_kernel_spmd

F32 = mybir.dt.float32
BF16 = mybir.dt.bfloat16
I32 = mybir.dt.int32
AF = mybir.ActivationFunctionType
ALU = mybir.AluOpType
AX = mybir.AxisListType

D = 1024
SEQ = 4096
CTX = 256
T = SEQ + CTX
NT = T // 128
DEPTH = 4
INW = 5568
NE = 32
BS = 256
SUB = BS // 128
NBLK = (T * 4) // BS + NE
NROWS = NBLK * BS
EPS = 1e-6
MLA_SCALE = 96 ** -0.5
GLA_SCALE = 64 ** -0.5
O_UQ, O_KV, O_FN, O_GQ, O_GK, O_GV, O_OG, O_GF, O_GB, O_GATE = 0, 256, 416, 928, 1184, 1440, 1952, 2464, 2480, 2496


import os as _os
SAME_ENGINE_SYNC = _os.environ.get("NOSAME", "0") != "1"


class Tok:
    __slots__ = ("w", "rs")

    def __init__(self):
        self.w = None
        self.rs = {}


class Sched:
    NDMA = 32

    def __init__(self, nc, st):
        self.nc = nc
        self.engs = ["pe", "act", "dve", "pool", "sp"]
        self.ops = {k: [] for k in self.engs}
        self.cnt = {k: 0 for k in self.engs}
        self.seen = {k: {} for k in self.engs}
        self.dma_k = {"d": 0, "g": 0}
        self.dma_last = {}
        self.n_ops = 0
        self.sems = {}
        for k in ["e_" + e for e in self.engs] + ["d%d" % i for i in range(self.NDMA)] + ["g%d" % i for i in range(self.NDMA)]:
            self.sems[k] = st.enter_context(nc.semaphore(k))

    def _need(self, eng, ev, waits):
        if ev is None:
            return
        src, key, val = ev
        if src == eng and (eng == "pe" or not SAME_ENGINE_SYNC):
            return
        if self.seen[eng].get(key, 0) >= val:
            return
        self.seen[eng][key] = val
        waits.append((key, val))

    def op(self, eng, fn, r=(), w=(), dma=False):
        waits = []
        for t in r:
            self._need(eng, t.w, waits)
        for t in w:
            self._need(eng, t.w, waits)
            for ev in t.rs.values():
                self._need(eng, ev, waits)
        if dma:
            pre = "g" if eng == "pool" else "d"
            k = self.dma_k[pre]
            self.dma_k[pre] += 1
            key = "%s%d" % (pre, k % self.NDMA)
            val = 16 * (k // self.NDMA + 1)
            if val > 16:
                self._need(eng, ("dma", key, val - 16), waits)
            ev = ("dma", key, val)
            inc = (key, 16)
            self.dma_last[key] = val
        else:
            self.cnt[eng] += 1
            key = "e_" + eng
            ev = (eng, key, self.cnt[eng])
            inc = (key, 1)
        self.ops[eng].append((waits, fn, inc))
        self.n_ops += 1
        for t in w:
            t.w = ev
            t.rs = {}
        for t in r:
            if t not in w:
                t.rs[ev[1]] = ev
        return ev

    def barrier(self):
        evs = [(e, "e_" + e, self.cnt[e]) for e in self.engs if self.cnt[e] > 0]
        evs += [("dma", k, v) for k, v in self.dma_last.items()]
        for eng in self.engs:
            waits = []
            for ev in evs:
                if ev[0] == eng:
                    continue
                self._need(eng, ev, waits)
            if waits:
                self.ops[eng].append((waits, None, None))

    def flush(self):
        self.barrier()
        nc = self.nc
        sems = self.sems
        ops = self.ops
        with nc.Block() as block:
            def mk(engname):
                def body(e):
                    for waits, fn, inc in ops[engname]:
                        for key, val in waits:
                            e.wait_ge(sems[key], val)
                        if fn is not None:
                            fn(e).then_inc(sems[inc[0]], inc[1])
                return body
            block.tensor(mk("pe"))
            block.scalar(mk("act"))
            block.vector(mk("dve"))
            block.gpsimd(mk("pool"))
            block.sync(mk("sp"))
        self.ops = {k: [] for k in self.engs}


class B:
    __slots__ = ("t", "k", "psum")

    def __init__(self, t, psum=False):
        self.t = t
        self.k = Tok()
        self.psum = psum


def _toks(xs):
    return [x.k if isinstance(x, B) else x for x in xs]


class KB:
    def __init__(self, nc, st):
        self.nc = nc
        self.S = Sched(nc, st)
        self.dq = 0
        self.bregs = {}

    def sb(self, st, name, shape, dt):
        self.dq += 1
        return B(st.enter_context(self.nc.sbuf_tensor("%s_u%d" % (name, self.dq), shape, dt)))

    def op(self, eng, fn, r, w, dma=False):
        r2 = [x for x in r if not (isinstance(x, B) and x.psum)]
        w2 = list(w) + [x for x in r if isinstance(x, B) and x.psum and x not in w]
        return self.S.op(eng, fn, r=_toks(r2), w=_toks(w2), dma=dma)

    def mm(self, out, lhsT, rhs, start, stop, r, w):
        self.op("pe", lambda e: e.matmul(out, lhsT, rhs, start=start, stop=stop), r, w)

    def tr(self, out, in_, ident, r, w):
        self.op("pe", lambda e: e.transpose(out, in_, ident), r, w)

    def act(self, out, in_, func, r, w, bias=None, scale=None, accum=None):
        kw = {}
        if bias is not None:
            kw["bias"] = bias
        if scale is not None:
            kw["scale"] = scale
        if accum is not None:
            kw["accum_out"] = accum
        self.op("act", lambda e: e.activation(out=out, in_=in_, func=func, **kw), r, w)

    def cp(self, eng, out, in_, r, w):
        if eng == "act":
            self.op("act", lambda e: e.copy(out=out, in_=in_), r, w)
        else:
            self.op(eng, lambda e: e.tensor_copy(out=out, in_=in_), r, w)

    def tt(self, eng, out, in0, in1, op, r, w):
        self.op(eng, lambda e: e.tensor_tensor(out=out, in0=in0, in1=in1, op=op), r, w)

    def ts(self, eng, out, in0, s1, s2, op0, op1, r, w):
        if op1 is None:
            self.op(eng, lambda e: e.tensor_scalar(out=out, in0=in0, scalar1=s1, scalar2=None, op0=op0), r, w)
        else:
            self.op(eng, lambda e: e.tensor_scalar(out=out, in0=in0, scalar1=s1, scalar2=s2, op0=op0, op1=op1), r, w)

    def stt(self, eng, out, in0, scalar, in1, op0, op1, r, w):
        self.op(eng, lambda e: e.scalar_tensor_tensor(out=out, in0=in0, scalar=scalar, in1=in1, op0=op0, op1=op1), r, w)

    def red(self, eng, out, in_, op, r, w, axis=AX.X):
        self.op(eng, lambda e: e.tensor_reduce(out=out, in_=in_, axis=axis, op=op), r, w)

    def recip(self, out, in_, r, w):
        self.op("dve", lambda e: e.reciprocal(out=out, in_=in_), r, w)

    def memset(self, eng, out, val, r, w):
        self.op(eng, lambda e: e.memset(out, val), r, w)

    def dma(self, eng, out, in_, r, w, slow=False):
        if slow:
            self.op(eng, lambda e: e.dma_start(out=out, in_=in_, allow_slow_non_contiguous=True), r, w, dma=True)
        else:
            self.op(eng, lambda e: e.dma_start(out=out, in_=in_), r, w, dma=True)

    def _breg(self, e, bound):
        if bound not in self.bregs:
            self.bregs[bound] = e.to_reg(bound)
        return self.bregs[bound]

    def gather(self, out, in_, idx, r, w, bound):
        self.op("pool", lambda e: e.indirect_dma_start(
            out=out, out_offset=None, in_=in_, in_offset=bass.IndirectOffsetOnAxis(ap=idx, axis=0),
            bounds_check=self._breg(e, bound), oob_is_err=False), r, w, dma=True)

    def scatter(self, out, in_, idx, r, w, bound):
        self.op("pool", lambda e: e.indirect_dma_start(
            out=out, out_offset=bass.IndirectOffsetOnAxis(ap=idx, axis=0), in_=in_, in_offset=None,
            bounds_check=self._breg(e, bound), oob_is_err=False), r, w, dma=True)


def host_consts():
    c = {}
    bf = ml_dtypes.bfloat16
    i = np.arange(128)
    c["ident_f"] = np.eye(128, dtype=np.float32)
    c["ident_b"] = np.eye(128).astype(bf)
    trif = (i[:, None] <= i[None, :]).astype(np.float32)
    trib = (i[:, None] >= i[None, :]).astype(np.float32)
    c["tri_f"] = np.stack([trif, trib], 0)
    c["mask4"] = np.stack([np.repeat(trif[:, None, :], 4, 1), np.repeat(trib[:, None, :], 4, 1)], 0).astype(np.float32)
    c["tri_x"] = (i[:, None] < i[None, :]).astype(bf)
    c["ones_f"] = np.ones((128, 128), np.float32)
    c["ones_b"] = np.ones((128, 128)).astype(bf)
    inv = 10000.0 ** (-np.arange(0, 16, 2, dtype=np.float32) / 16)
    row = np.repeat(np.arange(64, dtype=np.float32), 64)
    col = np.tile(np.arange(64, dtype=np.float32), 64)
    ar = row[:, None] * inv
    ac = col[:, None] * inv
    cr, sr, cc, sc = (np.cos(ar).astype(np.float32), np.sin(ar).astype(np.float32),
                      np.cos(ac).astype(np.float32), np.sin(ac).astype(np.float32))
    cos32 = np.concatenate([cr, cr, cc, cc], 1)
    sin32 = np.concatenate([-sr, sr, -sc, sc], 1)
    cos32 = np.concatenate([np.ones((CTX, 32), np.float32), cos32], 0)
    sin32 = np.concatenate([np.zeros((CTX, 32), np.float32), sin32], 0)
    c["rope_k"] = np.stack([cos32, sin32], 1).astype(np.float32)
    rq = np.stack([np.tile(cos32, (1, 8)), np.tile(sin32, (1, 8))], 1) * np.float32(MLA_SCALE)
    c["rope_q"] = rq.astype(np.float32)
    ang = 2 * np.pi * np.outer(i, i) / 128.0
    c["cs128"] = (np.concatenate([np.cos(ang), np.sin(ang)], 1) / math.sqrt(128)).astype(bf)
    n = np.arange(SEQ, dtype=np.int64)
    kt = (np.outer(n, n) % SEQ).astype(np.float64) * (2 * np.pi / SEQ)
    ctm = (np.cos(kt) / 64.0).astype(np.float32).reshape(32, 128, 32, 128)
    stm = (-np.sin(kt) / 64.0).astype(np.float32).reshape(32, 128, 32, 128)
    c["dft_c"] = np.ascontiguousarray(ctm.transpose(2, 1, 0, 3)).astype(bf)
    c["dft_s"] = np.ascontiguousarray(stm.transpose(2, 1, 0, 3)).astype(bf)
    del kt, ctm, stm
    m = np.arange(CTX, dtype=np.int64)
    k2 = (np.outer(m, m) % CTX).astype(np.float64) * (2 * np.pi / CTX)
    c2 = (np.cos(k2) / 16.0).astype(np.float32).reshape(2, 128, 2, 128)
    s2 = (-np.sin(k2) / 16.0).astype(np.float32).reshape(2, 128, 2, 128)
    c["dft_c2"] = np.ascontiguousarray(c2.transpose(2, 1, 0, 3)).astype(bf)
    c["dft_s2"] = np.ascontiguousarray(s2.transpose(2, 1, 0, 3)).astype(bf)
    c["jv"] = np.tile((np.arange(NBLK, dtype=np.float32) * BS)[None, :], (128, 1))
    c["kp"] = (np.arange(8, dtype=np.float32)[None, :] * 128 + i[:, None]).astype(np.float32)
    return c


CONST_SPECS = {
    "ident_f": ([128, 128], F32), "ident_b": ([128, 128], BF16), "tri_f": ([2, 128, 128], F32),
    "mask4": ([2, 128, 4, 128], F32), "tri_x": ([128, 128], BF16), "ones_f": ([128, 128], F32),
    "ones_b": ([128, 128], BF16), "rope_k": ([T, 2, 32], F32), "rope_q": ([T, 2, 256], F32),
    "cs128": ([128, 256], BF16), "dft_c": ([32, 128, 32, 128], BF16), "dft_s": ([32, 128, 32, 128], BF16),
    "dft_c2": ([2, 128, 2, 128], BF16), "dft_s2": ([2, 128, 2, 128], BF16),
    "jv": ([128, NBLK], F32), "kp": ([128, 8], F32),
}

WEIGHT_SPECS = {
    "w_mod": [DEPTH, D, 6 * D], "b_mod": [DEPTH, 6 * D], "norm1_g": [DEPTH, D], "w_in": [DEPTH, D, INW],
    "mla_q_norm_g": [DEPTH, 256], "mla_w_uq": [DEPTH, 256, 768], "mla_kv_norm_g": [DEPTH, 128],
    "mla_w_ukv": [DEPTH, 128, 1024], "gla_w_gate_f": [DEPTH, 16, 256], "gla_b_gate_f": [DEPTH, 256],
    "gla_w_gate_b": [DEPTH, 16, 256], "gla_b_gate_b": [DEPTH, 256], "gla_norm_g": [DEPTH, 512],
    "w_br_mla": [DEPTH, 512, D], "w_br_fnet": [DEPTH, 512, D], "w_br_gla": [DEPTH, 512, D],
    "w_o": [DEPTH, D, D], "norm2_g": [DEPTH, D], "router_w": [DEPTH, D, NE], "router_b": [DEPTH, NE],
    "exp_w_up": [DEPTH, NE, D, 2 * D], "exp_b_up": [DEPTH, NE, 2 * D], "exp_w_down": [DEPTH, NE, D, D],
    "exp_b_down": [DEPTH, NE, D], "final_norm_g": [D],
}


class Prog:
    def __init__(self, nl=DEPTH, dbg=(), stop_after=None, wdepth=DEPTH):
        self.nl = nl
        self.dbg = set(dbg)
        self.stop_after = stop_after
        self.nc = bass.Bass("TRN2", target_bir_lowering=False)
        self.st = contextlib.ExitStack()
        self.kb = KB(self.nc, self.st)
        nc = self.nc
        self.x_in = nc.dram_tensor("x", [SEQ, D], F32, kind="ExternalInput").ap()
        self.ctx_in = nc.dram_tensor("ctx", [CTX, D], F32, kind="ExternalInput").ap()
        self.c_in = nc.dram_tensor("c", [D], F32, kind="ExternalInput").ap()
        self.cc_in = nc.dram_tensor("c_ctx", [D], F32, kind="ExternalInput").ap()
        self.W = {k: nc.dram_tensor(k, ([wdepth] + shp[1:]) if (len(shp) > 1 or k in ('norm1_g',)) and shp[0] == DEPTH and k != 'final_norm_g' else shp, F32, kind="ExternalInput").ap() for k, shp in WEIGHT_SPECS.items()}
        self.C = {k: nc.dram_tensor(k, shp, dt, kind="ExternalInput").ap() for k, (shp, dt) in CONST_SPECS.items()}
        self.out = nc.dram_tensor("out", [SEQ, D], F32, kind="ExternalOutput").ap()
        self.tout = Tok()
        self.Dr = {}
        self.Dk = {}
        for name, shp, dt in [
            ("XR", [T, D], F32), ("KT", [8, 96, T], BF16), ("QT", [8, 96, T], BF16), ("VA", [T, 8, 65], BF16),
            ("UFT", [4, 128, T], BF16), ("GQT", [4, 64, T], BF16), ("GKT", [4, 64, T], BF16),
            ("GKVO", [T, 1280], BF16), ("LFB", [T, 512], F32), ("GATE", [T, 3072], BF16),
            ("OMT", [8, 64, T], BF16), ("OFT", [4, 128, T], BF16), ("OGT", [4, 128, T], BF16),
            ("OFW", [T, 512], F32), ("XS", [NROWS, D], BF16), ("YB", [NROWS, D], F32),
            ("H2B", [T, D], BF16), ("DBG_DST", [128, NT * 4], I32), ("DBG_G4", [128, NT * 4], F32),
            ("DBG_EB", [128, NBLK], I32), ("DBG_LG", [128, NT * NE], F32), ("DBG_CNT", [128, NE], F32),
        ]:
            kind = "ExternalOutput" if name in self.dbg else "Internal"
            self.Dr[name] = nc.dram_tensor(name, shp, dt, kind=kind).ap()
            self.Dk[name] = Tok()

    def build(self):
        kb = self.kb
        nc = self.nc
        with self.st:
            P = self.P = {}
            for name, shp, dt in [
                ("ident_f", [128, 128], F32), ("ident_b", [128, 128], BF16), ("ones_f", [128, 128], F32),
                ("ones_b", [128, 128], BF16), ("modT", [128, 48, 2], F32), ("nb", [128, 8], F32),
                ("scT", [128, 8, 2], F32),
            ]:
                P[name] = kb.sb(self.st, "p_" + name, shp, dt)
            self.ps = [B(self.st.enter_context(nc.psum_tensor("ps%d" % i, [128, 512], F32)), psum=True) for i in range(8)]
            self.psi = 0
            self.pinned = set()
            for nm in ["ident_f", "ident_b", "ones_f", "ones_b"]:
                kb.dma("sp", P[nm].t[:], self.C[nm], [], [P[nm]])
            kb.dma("sp", self.Dr["XR"][0:CTX, :], self.ctx_in, [], [self.Dk["XR"]])
            kb.dma("pool", self.Dr["XR"][CTX:T, :], self.x_in, [], [self.Dk["XR"]])
            kb.S.flush()
            for l in range(self.nl):
                import os
                skip = os.environ.get("SKIP_PH", "").split(",")
                for ph in [self.phase_mod, self.phase_a, self.phase_attn, self.phase_fnet, self.phase_gla,
                           self.phase_merge, self.phase_moe]:
                    if ph.__name__ in skip:
                        continue
                    ph(l)
                    kb.S.flush()
                    if self.stop_after == (l, ph.__name__):
                        break
                else:
                    continue
                break
            kb.S.flush()
        return nc

    def psum(self, pin=False):
        while True:
            idx = self.psi % 8
            self.psi += 1
            if idx not in self.pinned:
                break
        if pin:
            self.pinned.add(idx)
        return self.ps[idx]

    def unpin(self, p):
        self.pinned.discard(self.ps.index(p))

    def phase_mod(self, l):
        kb, P, W = self.kb, self.P, self.W
        with contextlib.ExitStack() as st:
            cT = kb.sb(st, "cT", [128, 8, 2], F32)
            sT = kb.sb(st, "sT", [128, 8, 2], F32)
            bT = kb.sb(st, "bT", [128, 48], F32)
            wm = [kb.sb(st, "wm%d" % i, [128, 8, 1024], F32) for i in range(2)]
            kb.dma("sp", cT.t[:, :, 0], self.c_in.rearrange("(k p) -> p k", p=128), [], [cT], slow=True)
            kb.dma("sp", cT.t[:, :, 1], self.cc_in.rearrange("(k p) -> p k", p=128), [], [cT], slow=True)
            kb.dma("sp", bT.t[:], W["b_mod"][l].rearrange("(j p) -> p j", p=128), [], [bT], slow=True)
            kb.act(sT.t[:], cT.t[:], AF.Silu, [cT], [sT])
            for sec in range(6):
                w = wm[sec % 2]
                kb.dma("sp" if sec % 2 == 0 else "pool", w.t[:],
                       W["w_mod"][l][:, sec * 1024:(sec + 1) * 1024].rearrange("(k p) n -> p k n", p=128), [], [w])
                ps = self.psum()
                for j in range(8):
                    for kc in range(8):
                        kb.mm(ps.t[:, j * 2:j * 2 + 2], w.t[:, kc, j * 128:(j + 1) * 128], sT.t[:, kc, :],
                              kc == 0, kc == 7, [w, sT], [ps])
                for j in range(8):
                    jj = sec * 8 + j
                    kb.ts("dve", P["modT"].t[:, jj, :], ps.t[:, j * 2:j * 2 + 2], bT.t[:, jj:jj + 1], None, ALU.add, None,
                          [ps, bT], [P["modT"]])
            kb.S.flush()

    def bcast_rows(self, st, name, colT_ap_fn, r):
        kb, P = self.kb, self.P
        out = kb.sb(st, name, [128, 1024], F32)
        tmp = kb.sb(st, name + "_t", [128, 128], F32)
        for half in range(2):
            ps = self.psum()
            for k4 in range(4):
                kc = half * 4 + k4
                kb.ts("dve", tmp.t[:], P["ones_f"].t[:], colT_ap_fn(kc), None, ALU.mult, None, r + [P["ones_f"]], [tmp])
                kb.mm(ps.t[:, k4 * 128:(k4 + 1) * 128], tmp.t[:], P["ident_f"].t[:], True, True, [tmp, P["ident_f"]], [ps])
            kb.cp("act", out.t[:, half * 512:(half + 1) * 512], ps.t[:], [ps], [out])
        return out

    def mod_rows(self, st, l, which, names=("A", "S", "G")):
        kb, P, W = self.kb, self.P, self.W
        sec_sh, sec_sc, sec_g = (0, 1, 2) if which == 1 else (3, 4, 5)
        gT = kb.sb(st, "gT", [128, 8], F32)
        kb.dma("sp", gT.t[:], W["norm1_g" if which == 1 else "norm2_g"][l].rearrange("(k p) -> p k", p=128), [], [gT], slow=True)
        aT = kb.sb(st, "aT", [128, 8, 2], F32)
        m = P["modT"]
        for v in range(2):
            kb.stt("dve", aT.t[:, :, v], m.t[:, sec_sc * 8:(sec_sc + 1) * 8, v], 1.0, gT.t[:], ALU.add, ALU.mult, [m, gT], [aT])
        rows = {}
        for v, nm in ((0, "l"), (1, "c")):
            if "A" in names:
                rows["A" + nm] = self.bcast_rows(st, "rA" + nm, lambda kc, v=v: aT.t[:, kc, v:v + 1], [aT])
            if "S" in names:
                rows["S" + nm] = self.bcast_rows(st, "rS" + nm, lambda kc, v=v: m.t[:, sec_sh * 8 + kc, v:v + 1], [m])
            if "G" in names:
                rows["G" + nm] = self.bcast_rows(st, "rG" + nm, lambda kc, v=v: m.t[:, sec_g * 8 + kc, v:v + 1], [m])
        return rows

    def norm_mod(self, xt, rows, i, junk, ssq, h32, hb, full32=False):
        kb = self.kb
        nm = "c" if i < 2 else "l"
        kb.act(junk.t[:], xt.t[:], AF.Square, [xt], [junk, ssq], accum=ssq.t[:, 0:1])
        kb.ts("dve", ssq.t[:, 1:2], ssq.t[:, 0:1], 1.0 / D, EPS, ALU.mult, ALU.add, [ssq], [ssq])
        kb.act(ssq.t[:, 1:2], ssq.t[:, 1:2], AF.Sqrt, [ssq], [ssq])
        kb.recip(ssq.t[:, 2:3], ssq.t[:, 1:2], [ssq], [ssq])
        kb.stt("dve", h32.t[:], xt.t[:], ssq.t[:, 2:3], rows["A" + nm].t[:], ALU.mult, ALU.mult, [xt, ssq, rows["A" + nm]], [h32])
        if full32:
            kb.tt("pool", h32.t[:], h32.t[:], rows["S" + nm].t[:], ALU.add, [h32, rows["S" + nm]], [h32])
            kb.cp("dve", hb.t[:], h32.t[:], [h32], [hb])
        else:
            kb.tt("pool", hb.t[:], h32.t[:], rows["S" + nm].t[:], ALU.add, [h32, rows["S" + nm]], [hb])

    def phase_a(self, l):
        kb, P, W, C, Dr, Dk = self.kb, self.P, self.W, self.C, self.Dr, self.Dk
        with contextlib.ExitStack() as st:
            rows = self.mod_rows(st, l, 1, names=("A", "S"))
            win = kb.sb(st, "win", [128, 8, INW], BF16)
            for kc in range(8):
                kb.dma("pool", win.t[:, kc, :], W["w_in"][l][kc * 128:(kc + 1) * 128, :], [], [win])
            wuq32 = kb.sb(st, "wuq32", [128, 2, 768], F32)
            wuq = kb.sb(st, "wuq", [128, 2, 768], BF16)
            gq = kb.sb(st, "gq", [128, 2], F32)
            wkv32 = kb.sb(st, "wkv32", [128, 1024], F32)
            wkv = kb.sb(st, "wkv", [128, 1024], BF16)
            gkv = kb.sb(st, "gkv", [128, 1], F32)
            wg = kb.sb(st, "wg", [17, 2, 256], F32)
            kb.dma("sp", wuq32.t[:], W["mla_w_uq"][l].rearrange("(k p) n -> p k n", p=128), [], [wuq32])
            kb.dma("sp", gq.t[:], W["mla_q_norm_g"][l].rearrange("(k p) -> p k", p=128), [], [gq], slow=True)
            kb.dma("sp", wkv32.t[:], W["mla_w_ukv"][l], [], [wkv32])
            kb.dma("sp", gkv.t[:], W["mla_kv_norm_g"][l].rearrange("(k p) -> p k", p=128), [], [gkv], slow=True)
            kb.dma("sp", wg.t[0:16, 0, :], W["gla_w_gate_f"][l], [], [wg])
            kb.dma("sp", wg.t[0:16, 1, :], W["gla_w_gate_b"][l], [], [wg])
            kb.dma("sp", wg.t[16:17, 0, :], W["gla_b_gate_f"][l].rearrange("(o n) -> o n", o=1), [], [wg])
            kb.dma("sp", wg.t[16:17, 1, :], W["gla_b_gate_b"][l].rearrange("(o n) -> o n", o=1), [], [wg])
            for kc in range(2):
                kb.ts("dve", wuq.t[:, kc, :], wuq32.t[:, kc, :], gq.t[:, kc:kc + 1], None, ALU.mult, None, [wuq32, gq], [wuq])
            kb.ts("dve", wkv.t[:], wkv32.t[:], gkv.t[:, 0:1], None, ALU.mult, None, [wkv32, gkv], [wkv])
            m16 = kb.sb(st, "m16", [128, 16], F32)
            kb.memset("dve", m16.t[:], 0.0, [], [m16])
            NB2 = 1
            def mk(name, shp, dt):
                return [kb.sb(st, "%s%d" % (name, j), shp, dt) for j in range(NB2)]
            xt = mk("xt", [128, D], F32)
            junk = mk("junk", [128, D], F32)
            ssq = mk("ssq", [128, 8], F32)
            h32 = mk("h32", [128, D], F32)
            hb = mk("hb", [128, D], BF16)
            hT = mk("hT", [128, 8, 128], BF16)
            uqn = mk("uqn", [128, 384], BF16)
            kr = mk("kr", [128, 4, 32], F32)
            uT = mk("uT", [128, 3, 128], BF16)
            rq = mk("rq", [128, 2, 256], F32)
            rk = mk("rk", [128, 2, 32], F32)
            qo = mk("qo", [128, 8, 96], BF16)
            ko = mk("ko", [128, 8, 96], BF16)
            vo = mk("vo", [128, 8, 65], BF16)
            qtmp = mk("qtmp", [128, 3, 256], F32)
            sq = mk("sq", [128, 768], F32)
            n2 = mk("n2", [128, 16], F32)
            qkT = mk("qkT", [96, 16, 128], BF16)
            gkvo = mk("gkvo", [128, 1280], BF16)
            gate = mk("gate", [128, 3072], BF16)
            fT = mk("fT", [128, 4, 128], BF16)
            gqkT = mk("gqkT", [64, 8, 128], BF16)
            ugT = mk("ugT", [17, 2, 128], F32)
            lfb = mk("lfb", [128, 512], F32)
            lfe = mk("lfe", [128, 512], F32)
            for j in range(NB2):
                kb.memset("pool", vo[j].t[:], 1.0, [], [vo[j]])
                kb.memset("pool", ugT[j].t[:], 1.0, [], [ugT[j]])
            idb = P["ident_b"]
            for i in range(NT):
                b = i % NB2
                X, J, SS, H32, HB, HT = xt[b], junk[b], ssq[b], h32[b], hb[b], hT[b]
                kb.dma("sp", X.t[:], Dr["XR"][i * 128:(i + 1) * 128, :], [Dk["XR"]], [X])
                kb.dma("sp", rq[b].t[:], C["rope_q"][i * 128:(i + 1) * 128], [], [rq[b]])
                kb.dma("sp", rk[b].t[:], C["rope_k"][i * 128:(i + 1) * 128], [], [rk[b]])
                kb.memset("pool", SS.t[:], 0.0, [], [SS])
                self.norm_mod(X, rows, i, J, SS, H32, HB)
                pT = self.psum()
                pTb = pT.t[:].bitcast(BF16)
                for kc in range(8):
                    kb.tr(pTb[:, kc * 128:(kc + 1) * 128], HB.t[:, kc * 128:(kc + 1) * 128], idb.t[:], [HB, idb], [pT])
                kb.cp("act", HT.t[:].rearrange("p k t -> p (k t)"), pTb, [pT], [HT])

                def tm(c0, cw):
                    ps = self.psum()
                    for kc in range(8):
                        kb.mm(ps.t[:, 0:cw], HT.t[:, kc, :], win.t[:, kc, c0:c0 + cw], kc == 0, kc == 7, [HT, win], [ps])
                    return ps

                def fm(c0, cw, ps, o0):
                    for kc in range(8):
                        kb.mm(ps.t[0:cw, o0:o0 + 128], win.t[:, kc, c0:c0 + cw], HT.t[:, kc, :], kc == 0, kc == 7, [HT, win], [ps])

                ps1 = tm(0, 416)
                UQ, KR, UT, QO, KO, VO, QTMP, SQ, N2 = uqn[b], kr[b], uT[b], qo[b], ko[b], vo[b], qtmp[b], sq[b], n2[b]
                kb.act(J.t[:, 0:256], ps1.t[:, 0:256], AF.Square, [ps1], [J, SS], accum=SS.t[:, 3:4])
                kb.act(J.t[:, 256:384], ps1.t[:, 256:384], AF.Square, [ps1], [J, SS], accum=SS.t[:, 4:5])
                kb.ts("dve", SS.t[:, 5:6], SS.t[:, 3:4], 1.0 / 256, EPS, ALU.mult, ALU.add, [SS], [SS])
                kb.ts("dve", SS.t[:, 6:7], SS.t[:, 4:5], 1.0 / 128, EPS, ALU.mult, ALU.add, [SS], [SS])
                kb.act(SS.t[:, 5:7], SS.t[:, 5:7], AF.Sqrt, [SS], [SS])
                kb.recip(SS.t[:, 5:7], SS.t[:, 5:7], [SS], [SS])
                kb.ts("dve", UQ.t[:, 0:256], ps1.t[:, 0:256], SS.t[:, 5:6], None, ALU.mult, None, [ps1, SS], [UQ])
                kb.ts("dve", UQ.t[:, 256:384], ps1.t[:, 256:384], SS.t[:, 6:7], None, ALU.mult, None, [ps1, SS], [UQ])
                kb.cp("act", KR.t[:, 0, :], ps1.t[:, 384:416], [ps1], [KR])
                krv = KR.t[:, 0, :].rearrange("p (a f e) -> p a f e", a=2, f=2)
                krs = KR.t[:, 1, :].rearrange("p (a f e) -> p a f e", a=2, f=2)
                kb.cp("pool", krs[:, :, 0, :], krv[:, :, 1, :], [KR], [KR])
                kb.cp("pool", krs[:, :, 1, :], krv[:, :, 0, :], [KR], [KR])
                kb.tt("dve", KR.t[:, 2, :], KR.t[:, 0, :], rk[b].t[:, 0, :], ALU.mult, [KR, rk[b]], [KR])
                kb.tt("dve", KR.t[:, 3, :], KR.t[:, 1, :], rk[b].t[:, 1, :], ALU.mult, [KR, rk[b]], [KR])
                kb.tt("dve", KR.t[:, 0, :], KR.t[:, 2, :], KR.t[:, 3, :], ALU.add, [KR], [KR])
                p2 = self.psum()
                p2b = p2.t[:].bitcast(BF16)
                for j in range(3):
                    kb.tr(p2b[:, j * 128:(j + 1) * 128], UQ.t[:, j * 128:(j + 1) * 128], idb.t[:], [UQ, idb], [p2])
                kb.cp("act", UT.t[:].rearrange("p k t -> p (k t)"), p2b[:, 0:384], [p2], [UT])
                for hh in range(2):
                    pq = self.psum()
                    for kc in range(2):
                        kb.mm(pq.t[:, 0:384], UT.t[:, kc, :], wuq.t[:, kc, hh * 384:(hh + 1) * 384], kc == 0, kc == 1, [UT, wuq], [pq])
                    pqv = pq.t[:, 0:384].rearrange("p (h d) -> p h d", h=4)
                    qov = QO.t[:, hh * 4:(hh + 1) * 4, :]
                    kb.act(qov[:, :, 0:64], pqv[:, :, 0:64], AF.Identity, [pq], [QO], scale=MLA_SCALE)
                    t0 = QTMP.t[:, 0, hh * 128:(hh + 1) * 128].rearrange("p (h d) -> p h d", h=4)
                    t1 = QTMP.t[:, 1, hh * 128:(hh + 1) * 128].rearrange("p (h d) -> p h d", h=4)
                    kb.cp("act", t0, pqv[:, :, 64:96], [pq], [QTMP])
                    t0v = t0.rearrange("p h (a f e) -> p h a f e", a=2, f=2)
                    t1v = t1.rearrange("p h (a f e) -> p h a f e", a=2, f=2)
                    for a in range(2):
                        kb.cp("pool", t1v[:, :, a, 0, :], t0v[:, :, a, 1, :], [QTMP], [QTMP])
                        kb.cp("pool", t1v[:, :, a, 1, :], t0v[:, :, a, 0, :], [QTMP], [QTMP])
                    rc = rq[b].t[:, 0, hh * 128:(hh + 1) * 128].rearrange("p (h d) -> p h d", h=4)
                    rs = rq[b].t[:, 1, hh * 128:(hh + 1) * 128].rearrange("p (h d) -> p h d", h=4)
                    kb.tt("dve", t0, t0, rc, ALU.mult, [QTMP, rq[b]], [QTMP])
                    kb.tt("dve", t1, t1, rs, ALU.mult, [QTMP, rq[b]], [QTMP])
                    kb.tt("dve", qov[:, :, 64:96], t0, t1, ALU.add, [QTMP], [QO])
                for hh in range(2):
                    pk = self.psum()
                    kb.mm(pk.t[:], UT.t[:, 2, :], wkv.t[:, hh * 512:(hh + 1) * 512], True, True, [UT, wkv], [pk])
                    pkv = pk.t[:].rearrange("p (h d) -> p h d", h=4)
                    kb.cp("act", KO.t[:, hh * 4:(hh + 1) * 4, 0:64], pkv[:, :, 0:64], [pk], [KO])
                    kb.cp("dve", VO.t[:, hh * 4:(hh + 1) * 4, 0:64], pkv[:, :, 64:128], [pk], [VO])
                for h in range(8):
                    kb.cp("pool", KO.t[:, h, 64:96], KR.t[:, 0, :], [KR], [KO])
                kb.tt("dve", SQ.t[:], QO.t[:].rearrange("p h d -> p (h d)"), QO.t[:].rearrange("p h d -> p (h d)"), ALU.mult, [QO], [SQ])
                kb.red("dve", N2.t[:, 8:16], SQ.t[:].rearrange("p (h d) -> p h d", h=8), ALU.add, [SQ], [N2])
                kb.tt("dve", SQ.t[:], KO.t[:].rearrange("p h d -> p (h d)"), KO.t[:].rearrange("p h d -> p (h d)"), ALU.mult, [KO], [SQ])
                kb.red("dve", N2.t[:, 0:8], SQ.t[:].rearrange("p (h d) -> p h d", h=8), ALU.add, [SQ], [N2])
                kb.tt("dve", m16.t[:], m16.t[:], N2.t[:], ALU.max, [N2, m16], [m16])
                QKT = qkT[b]
                for which, src in ((0, KO), (1, QO)):
                    p3 = self.psum()
                    p3b = p3.t[:].bitcast(BF16)
                    for h in range(8):
                        kb.tr(p3b[0:96, h * 128:(h + 1) * 128], src.t[:, h, :], idb.t[:], [src, idb], [p3])
                    kb.cp("act" if which == 0 else "dve", QKT.t[:, which * 8:(which + 1) * 8, :].rearrange("p h t -> p (h t)"),
                          p3b[0:96, :], [p3], [QKT])
                kb.dma("sp", Dr["KT"][:, :, i * 128:(i + 1) * 128].rearrange("h d t -> d h t"), QKT.t[:, 0:8, :], [QKT], [Dk["KT"]])
                kb.dma("sp", Dr["QT"][:, :, i * 128:(i + 1) * 128].rearrange("h d t -> d h t"), QKT.t[:, 8:16, :], [QKT], [Dk["QT"]])
                kb.dma("sp", Dr["VA"][i * 128:(i + 1) * 128], VO.t[:], [VO], [Dk["VA"]])
                G = gkvo[b]
                for gi, (c0, cw) in enumerate(((O_GK, 512), (O_GK + 512, 512), (O_GK + 1024, 256))):
                    ps = tm(c0, cw)
                    o0 = c0 - O_GK
                    if gi == 0:
                        kb.cp("dve", G.t[:, 0:512], ps.t[:, 0:512], [ps], [G])
                    elif gi == 1:
                        kb.cp("dve", G.t[:, 512:768], ps.t[:, 0:256], [ps], [G])
                        kb.act(G.t[:, 768:1024], ps.t[:, 256:512], AF.Silu, [ps], [G])
                    else:
                        kb.act(G.t[:, 1024:1280], ps.t[:, 0:256], AF.Silu, [ps], [G])
                kb.dma("sp", Dr["GKVO"][i * 128:(i + 1) * 128, :], G.t[:], [G], [Dk["GKVO"]])
                GA = gate[b]
                for gi in range(6):
                    ps = tm(O_GATE + gi * 512, 512)
                    kb.act(GA.t[:, gi * 512:(gi + 1) * 512], ps.t[:], AF.Sigmoid, [ps], [GA])
                kb.dma("sp", Dr["GATE"][i * 128:(i + 1) * 128, :], GA.t[:], [GA], [Dk["GATE"]])
                ps = self.psum()
                for g in range(4):
                    fm(O_FN + g * 128, 128, ps, g * 128)
                kb.cp("dve", fT[b].t[:].rearrange("p g t -> p (g t)"), ps.t[:], [ps], [fT[b]])
                kb.dma("sp", Dr["UFT"][:, :, i * 128:(i + 1) * 128].rearrange("g c t -> c g t"), fT[b].t[:], [fT[b]], [Dk["UFT"]])
                psq = self.psum()
                psk = self.psum()
                for h in range(4):
                    fm(O_GQ + h * 64, 64, psq, h * 128)
                    fm(O_GK + h * 64, 64, psk, h * 128)
                kb.act(gqkT[b].t[:, 0:4, :].rearrange("p h t -> p (h t)"), psq.t[0:64, :], AF.Identity, [psq], [gqkT[b]], scale=GLA_SCALE)
                kb.cp("dve", gqkT[b].t[:, 4:8, :].rearrange("p h t -> p (h t)"), psk.t[0:64, :], [psk], [gqkT[b]])
                kb.dma("sp", Dr["GQT"][:, :, i * 128:(i + 1) * 128].rearrange("h d t -> d h t"), gqkT[b].t[:, 0:4, :], [gqkT[b]], [Dk["GQT"]])
                kb.dma("sp", Dr["GKT"][:, :, i * 128:(i + 1) * 128].rearrange("h d t -> d h t"), gqkT[b].t[:, 4:8, :], [gqkT[b]], [Dk["GKT"]])
                psg = self.psum()
                fm(O_GF, 16, psg, 0)
                fm(O_GB, 16, psg, 128)
                kb.cp("act", ugT[b].t[0:16, :, :].rearrange("p d t -> p (d t)"), psg.t[0:16, 0:256], [psg], [ugT[b]])
                psl = self.psum()
                for d in range(2):
                    kb.mm(psl.t[:, d * 256:(d + 1) * 256], ugT[b].t[:, d, :], wg.t[:, d, :], True, True, [ugT[b], wg], [psl])
                kb.act(lfe[b].t[:], psl.t[:], AF.Exp, [psl], [lfe[b]], scale=-1.0)
                kb.act(lfe[b].t[:], lfe[b].t[:], AF.Ln, [lfe[b]], [lfe[b]], bias=1.0)
                kb.ts("dve", lfb[b].t[:], lfe[b].t[:], -1.0 / 16.0, None, ALU.mult, None, [lfe[b]], [lfb[b]])
                kb.dma("sp", Dr["LFB"][i * 128:(i + 1) * 128, :], lfb[b].t[:], [lfb[b]], [Dk["LFB"]])
            pm = self.psum()
            kb.tr(pm.t[0:16, 0:128], m16.t[:], P["ident_f"].t[:], [m16, P["ident_f"]], [pm])
            mcol = kb.sb(st, "mcol", [16, 1], F32)
            mb = kb.sb(st, "mb", [16, 128], F32)
            kb.red("dve", mcol.t[:], pm.t[0:16, 0:128], ALU.max, [pm], [mcol])
            kb.ts("dve", mb.t[:], P["ones_f"].t[0:16, :], mcol.t[:, 0:1], None, ALU.mult, None, [mcol, P["ones_f"]], [mb])
            pm2 = self.psum()
            kb.mm(pm2.t[:, 0:16], mb.t[:], P["ident_f"].t[0:16, 0:16], True, True, [mb, P["ident_f"]], [pm2])
            nbt = kb.sb(st, "nbt", [128, 16], F32)
            kb.cp("act", nbt.t[:], pm2.t[:, 0:16], [pm2], [nbt])
            kb.tt("dve", nbt.t[:, 0:8], nbt.t[:, 0:8], nbt.t[:, 8:16], ALU.mult, [nbt], [nbt])
            kb.act(nbt.t[:, 0:8], nbt.t[:, 0:8], AF.Sqrt, [nbt], [nbt])
            kb.ts("dve", P["nb"].t[:], nbt.t[:, 0:8], -1.0, None, ALU.mult, None, [nbt], [P["nb"]])
            kb.S.flush()

    def phase_attn(self, l):
        kb, P, Dr, Dk = self.kb, self.P, self.Dr, self.Dk
        with contextlib.ExitStack() as st:
            va = kb.sb(st, "va", [128, NT, 520], BF16)
            kb.dma("pool", va.t[:], Dr["VA"].rearrange("(n p) h e -> p n (h e)", p=128), [Dk["VA"]], [va])
            kt_ = [kb.sb(st, "ktb%d" % j, [96, T], BF16) for j in range(2)]
            qt_ = [kb.sb(st, "qtb%d" % j, [96, T], BF16) for j in range(2)]
            pt = [kb.sb(st, "pt%d" % j, [128, 512], BF16) for j in range(4)]
            osb = [kb.sb(st, "osb%d" % j, [65, 512], F32) for j in range(2)]
            rec = [kb.sb(st, "rec%d" % j, [64, 512], F32) for j in range(2)]
            om = [kb.sb(st, "om%d" % j, [64, 512], BF16) for j in range(2)]
            ones = P["ones_f"]
            nb = P["nb"]
            cnt = 0
            blk = 0
            for h in range(8):
                KT, QT = kt_[h % 2], qt_[h % 2]
                kb.dma("sp", KT.t[:], Dr["KT"][h], [Dk["KT"]], [KT])
                kb.dma("sp", QT.t[:], Dr["QT"][h], [Dk["QT"]], [QT])
                blocks = [(0, 256, [0, 1])] + [(CTX + qb * 512, 512, list(range(NT))) for qb in range(8)]
                for (q0, qn, keys) in blocks:
                    po = self.psum(pin=True)
                    pend = None
                    seq = []
                    for kt in keys:
                        ps = self.psum()
                        kb.mm(ps.t[:, 0:qn], KT.t[:, kt * 128:(kt + 1) * 128], QT.t[:, q0:q0 + qn], True, True, [KT, QT], [ps])
                        seq.append((kt, ps))
                        if len(seq) >= 2:
                            self._attn_pv(seq.pop(0), keys, po, pt, va, nb, h, qn, cnt)
                            cnt += 1
                    while seq:
                        self._attn_pv(seq.pop(0), keys, po, pt, va, nb, h, qn, cnt)
                        cnt += 1
                    O, R, OM = osb[blk % 2], rec[blk % 2], om[blk % 2]
                    blk += 1
                    kb.cp("act", O.t[:, 0:qn], po.t[0:65, 0:qn], [po], [O])
                    self.unpin(po)
                    pb = self.psum()
                    kb.mm(pb.t[0:64, 0:qn], ones.t[64:65, 0:64], O.t[64:65, 0:qn], True, True, [ones, O], [pb])
                    kb.recip(R.t[:, 0:qn], pb.t[0:64, 0:qn], [pb], [R])
                    kb.tt("pool", OM.t[:, 0:qn], O.t[0:64, 0:qn], R.t[:, 0:qn], ALU.mult, [O, R], [OM])
                    kb.dma("sp", Dr["OMT"][h, :, q0:q0 + qn], OM.t[:, 0:qn], [OM], [Dk["OMT"]])
            kb.S.flush()

    def _attn_pv(self, item, keys, po, pt, va, nb, h, qn, cnt):
        kb = self.kb
        kt, ps = item
        PT = pt[cnt % 4]
        kb.act(PT.t[:, 0:qn], ps.t[:, 0:qn], AF.Exp, [ps, nb], [PT], bias=nb.t[:, h:h + 1], scale=1.0)
        kb.mm(po.t[0:65, 0:qn], va.t[:, kt, h * 65:(h + 1) * 65], PT.t[:, 0:qn], kt == keys[0], kt == keys[-1], [va, PT], [po])

    def phase_fnet(self, l):
        kb, P, C, Dr, Dk = self.kb, self.P, self.C, self.Dr, self.Dk
        with contextlib.ExitStack() as st:
            A = kb.sb(st, "fA", [128, NT, 512], BF16)
            Bm = kb.sb(st, "fB", [128, NT, 512], BF16)
            cs = kb.sb(st, "cs", [128, 256], BF16)
            kb.dma("sp", cs.t[:], C["cs128"], [], [cs])
            uf = [kb.sb(st, "uf%d" % j, [128, 4, 128], BF16) for j in range(2)]
            import os
            for i in range(int(os.environ.get("FN_TILES", NT))):
                U = uf[i % 2]
                kb.dma("sp", U.t[:], Dr["UFT"][:, :, i * 128:(i + 1) * 128].rearrange("g c t -> c g t"), [Dk["UFT"]], [U])
                for half in range(2):
                    ps = self.psum()
                    for g2 in range(2):
                        g = half * 2 + g2
                        kb.mm(ps.t[:, g2 * 256:(g2 + 1) * 256], U.t[:, g, :], cs.t[:], True, True, [U, cs], [ps])
                    psv = ps.t[:].rearrange("p (g x m) -> p g x m", g=2, x=2)
                    kb.cp("act", A.t[:, i, half * 256:(half + 1) * 256].rearrange("p (g m) -> p g m", g=2), psv[:, :, 0, :], [ps], [A])
                    kb.cp("dve", Bm.t[:, i, half * 256:(half + 1) * 256].rearrange("p (g m) -> p g m", g=2), psv[:, :, 1, :], [ps], [Bm])
            dc = [kb.sb(st, "dc%d" % j, [128, 32, 128], BF16) for j in range(2)]
            ds = [kb.sb(st, "ds%d" % j, [128, 32, 128], BF16) for j in range(2)]
            y = [kb.sb(st, "fy%d" % j, [128, 512], BF16) for j in range(2)]
            oft = [kb.sb(st, "oft%d" % j, [128, 4, 128], BF16) for j in range(2)]
            idb = P["ident_b"]
            jobs = [("c", kt) for kt in range(2)] + [("l", kt) for kt in range(32)]
            import os
            if os.environ.get("FN_PART") == "1":
                jobs = []
            if os.environ.get("FN_PART") == "2":
                jobs = jobs[:2]
            for n, (kind, kt) in enumerate(jobs):
                DC, DS, Y, OF = dc[n % 2], ds[n % 2], y[n % 2], oft[n % 2]
                if kind == "c":
                    ntt, t0, tok0 = 2, 0, kt * 128
                    kb.dma("sp", DC.t[:, 0:2, :], C["dft_c2"][kt], [], [DC])
                    kb.dma("pool", DS.t[:, 0:2, :], C["dft_s2"][kt], [], [DS])
                else:
                    ntt, t0, tok0 = 32, 2, CTX + kt * 128
                    kb.dma("sp", DC.t[:], C["dft_c"][kt], [], [DC])
                    kb.dma("pool", DS.t[:], C["dft_s"][kt], [], [DS])
                ps = self.psum()
                for tt in range(ntt):
                    kb.mm(ps.t[:], DC.t[:, tt, :], A.t[:, t0 + tt, :], tt == 0, False, [DC, A], [ps])
                    kb.mm(ps.t[:], DS.t[:, tt, :], Bm.t[:, t0 + tt, :], False, tt == ntt - 1, [DS, Bm], [ps])
                kb.cp("act", Y.t[:], ps.t[:], [ps], [Y])
                p2 = self.psum()
                p2b = p2.t[:].bitcast(BF16)
                for g in range(4):
                    kb.tr(p2b[:, g * 128:(g + 1) * 128], Y.t[:, g * 128:(g + 1) * 128], idb.t[:], [Y, idb], [p2])
                kb.cp("dve", OF.t[:].rearrange("p g t -> p (g t)"), p2b[:, 0:512], [p2], [OF])
                kb.dma("sp", Dr["OFT"][:, :, tok0:tok0 + 128].rearrange("g m t -> m g t"), OF.t[:], [OF], [Dk["OFT"]])
            kb.S.flush()

    def phase_gla(self, l):
        kb, P, C, W, Dr, Dk = self.kb, self.P, self.C, self.W, self.Dr, self.Dk
        with contextlib.ExitStack() as st:
            tri = kb.sb(st, "tri", [128, 2, 128], F32)
            mask4 = kb.sb(st, "mask4", [128, 2, 512], F32)
            gn = kb.sb(st, "gn", [128, 512], F32)
            for d in range(2):
                kb.dma("sp", tri.t[:, d, :], C["tri_f"][d], [], [tri])
                kb.dma("sp", mask4.t[:, d, :], C["mask4"][d].rearrange("p h t -> p (h t)"), [], [mask4])
            kb.dma("sp", gn.t[:], W["gla_norm_g"][l].partition_broadcast(128), [], [gn])
            S32 = kb.sb(st, "S32", [64, 4, 128], F32)
            Sb = kb.sb(st, "Sb", [64, 4, 128], BF16)
            NB2 = 2
            def mk(name, shp, dt):
                return [kb.sb(st, "%s%d" % (name, j), shp, dt) for j in range(NB2)]
            qT = mk("gqT", [64, 4, 128], BF16)
            kT = mk("gkT", [64, 4, 128], BF16)
            gk = mk("ggk", [128, 1280], BF16)
            lf = mk("glf", [128, 256], F32)
            gtok = mk("gtok", [128, 256], F32)
            e1 = mk("ge1", [128, 256], F32)
            khat = mk("khat", [128, 256], BF16)
            eq = mk("geq", [64, 4, 128], F32)
            ek = mk("gek", [64, 4, 128], F32)
            qtl = mk("qtl", [64, 4, 128], BF16)
            ktl = mk("ktl", [64, 4, 128], BF16)
            at = mk("gat", [128, 512], BF16)
            ofw = mk("gofw", [128, 512], F32)
            o32 = mk("go32", [128, 512], F32)
            sqo = mk("gsq", [128, 512], F32)
            st4 = mk("gst4", [128, 8], F32)
            ob = mk("gob", [128, 512], BF16)
            ogt = mk("gogt", [128, 4, 128], BF16)
            idb = P["ident_b"]
            for d in range(2):
                order = list(range(NT)) if d == 0 else [1, 0] + list(range(NT - 1, 1, -1))
                last = 127 if d == 0 else 0
                kb.memset("dve", S32.t[:], 0.0, [], [S32])
                kb.memset("pool", Sb.t[:], 0.0, [], [Sb])
                for n, i in enumerate(order):
                    b = n % NB2
                    sl = slice(i * 128, (i + 1) * 128)
                    kb.dma("sp", qT[b].t[:], Dr["GQT"][:, :, sl].rearrange("h d t -> d h t"), [Dk["GQT"]], [qT[b]])
                    kb.dma("sp", kT[b].t[:], Dr["GKT"][:, :, sl].rearrange("h d t -> d h t"), [Dk["GKT"]], [kT[b]])
                    kb.dma("sp", gk[b].t[:], Dr["GKVO"][sl, :], [Dk["GKVO"]], [gk[b]])
                    kb.dma("sp", lf[b].t[:], Dr["LFB"][sl, d * 256:(d + 1) * 256], [Dk["LFB"]], [lf[b]])
                    if d == 1:
                        kb.dma("sp", ofw[b].t[:], Dr["OFW"][sl, :], [Dk["OFW"]], [ofw[b]])
                    LF = lf[b]
                    pg = self.psum()
                    kb.mm(pg.t[:, 0:256], tri.t[:, d, :], LF.t[:], True, True, [tri, LF], [pg])
                    kb.mm(pg.t[:, 256:512], P["ones_f"].t[:], LF.t[:], True, True, [P["ones_f"], LF], [pg])
                    kb.cp("act", gtok[b].t[:], pg.t[:, 0:256], [pg], [gtok[b]])
                    kb.tt("dve", e1[b].t[:], pg.t[:, 256:512], gtok[b].t[:], ALU.subtract, [pg, gtok[b]], [e1[b]])
                    kb.act(e1[b].t[:], e1[b].t[:], AF.Exp, [e1[b]], [e1[b]])
                    kb.tt("dve", khat[b].t[:], gk[b].t[:, 0:256], e1[b].t[:], ALU.mult, [gk[b], e1[b]], [khat[b]])
                    pf = self.psum()
                    for h in range(4):
                        kb.mm(pf.t[0:64, h * 128:(h + 1) * 128], LF.t[:, h * 64:(h + 1) * 64], tri.t[:, d, :], True, True, [LF, tri], [pf])
                    kb.act(eq[b].t[:].rearrange("p h t -> p (h t)"), pf.t[0:64, :], AF.Exp, [pf], [eq[b]])
                    kb.act(ek[b].t[:].rearrange("p h t -> p (h t)"), pf.t[0:64, :], AF.Exp, [pf], [ek[b]], scale=-1.0)
                    kb.tt("dve", qtl[b].t[:], qT[b].t[:], eq[b].t[:], ALU.mult, [qT[b], eq[b]], [qtl[b]])
                    kb.tt("pool", ktl[b].t[:], kT[b].t[:], ek[b].t[:], ALU.mult, [kT[b], ek[b]], [ktl[b]])
                    pa = self.psum()
                    for h in range(4):
                        kb.mm(pa.t[:, h * 128:(h + 1) * 128], ktl[b].t[:, h, :], qtl[b].t[:, h, :], True, True, [ktl[b], qtl[b]], [pa])
                    kb.tt("dve", at[b].t[:], pa.t[:], mask4.t[:, d, :], ALU.mult, [pa, mask4], [at[b]])
                    po = self.psum()
                    for h in range(4):
                        kb.mm(po.t[:, h * 128:(h + 1) * 128], qtl[b].t[:, h, :], Sb.t[:, h, :], True, False, [qtl[b], Sb], [po])
                        kb.mm(po.t[:, h * 128:(h + 1) * 128], at[b].t[:, h * 128:(h + 1) * 128],
                              gk[b].t[:, 256 + h * 128:256 + (h + 1) * 128], False, True, [at[b], gk[b]], [po])
                    pS = self.psum()
                    for h in range(4):
                        kb.mm(pS.t[0:64, h * 128:(h + 1) * 128], khat[b].t[:, h * 64:(h + 1) * 64],
                              gk[b].t[:, 256 + h * 128:256 + (h + 1) * 128], True, True, [khat[b], gk[b]], [pS])
                    for h in range(4):
                        kb.stt("dve", S32.t[:, h, :], S32.t[:, h, :], eq[b].t[:, h, last:last + 1], pS.t[0:64, h * 128:(h + 1) * 128],
                               ALU.mult, ALU.add, [S32, eq[b], pS], [S32])
                    kb.cp("pool", Sb.t[:], S32.t[:], [S32], [Sb])
                    if d == 0:
                        kb.cp("act", o32[b].t[:], po.t[:], [po], [o32[b]])
                        kb.dma("sp", Dr["OFW"][sl, :], o32[b].t[:], [o32[b]], [Dk["OFW"]])
                    else:
                        O, SQ, S4 = o32[b], sqo[b], st4[b]
                        kb.tt("dve", O.t[:], po.t[:], ofw[b].t[:], ALU.add, [po, ofw[b]], [O])
                        kb.tt("pool", SQ.t[:], O.t[:], O.t[:], ALU.mult, [O], [SQ])
                        kb.red("dve", S4.t[:, 0:4], SQ.t[:].rearrange("p (h e) -> p h e", h=4), ALU.add, [SQ], [S4])
                        kb.ts("dve", S4.t[:, 0:4], S4.t[:, 0:4], 1.0 / 128, EPS, ALU.mult, ALU.add, [S4], [S4])
                        kb.act(S4.t[:, 0:4], S4.t[:, 0:4], AF.Sqrt, [S4], [S4])
                        kb.recip(S4.t[:, 4:8], S4.t[:, 0:4], [S4], [S4])
                        for h in range(4):
                            kb.stt("dve", O.t[:, h * 128:(h + 1) * 128], O.t[:, h * 128:(h + 1) * 128], S4.t[:, 4 + h:5 + h],
                                   gn.t[:, h * 128:(h + 1) * 128], ALU.mult, ALU.mult, [O, S4, gn], [O])
                        kb.tt("pool", ob[b].t[:], O.t[:], gk[b].t[:, 768:1280], ALU.mult, [O, gk[b]], [ob[b]])
                        p2 = self.psum()
                        p2b = p2.t[:].bitcast(BF16)
                        for g in range(4):
                            kb.tr(p2b[:, g * 128:(g + 1) * 128], ob[b].t[:, g * 128:(g + 1) * 128], idb.t[:], [ob[b], idb], [p2])
                        kb.cp("act", ogt[b].t[:].rearrange("p g t -> p (g t)"), p2b[:, 0:512], [p2], [ogt[b]])
                        kb.dma("sp", Dr["OGT"][:, :, sl].rearrange("g m t -> m g t"), ogt[b].t[:], [ogt[b]], [Dk["OGT"]])
            kb.S.flush()

    def phase_merge(self, l):
        kb, P, W, Dr, Dk = self.kb, self.P, self.W, self.Dr, self.Dk
        with contextlib.ExitStack() as st:
            rows = self.mod_rows(st, l, 1, names=("G",))
            wbm = kb.sb(st, "wbm", [64, 8, D], BF16)
            wbf = kb.sb(st, "wbf", [128, 4, D], BF16)
            wbg = kb.sb(st, "wbg", [128, 4, D], BF16)
            wo = kb.sb(st, "wo", [128, 8, D], BF16)
            kb.dma("pool", wbm.t[:], W["w_br_mla"][l].rearrange("(h d) n -> d h n", d=64), [], [wbm])
            kb.dma("pool", wbf.t[:], W["w_br_fnet"][l].rearrange("(k p) n -> p k n", p=128), [], [wbf])
            kb.dma("pool", wbg.t[:], W["w_br_gla"][l].rearrange("(k p) n -> p k n", p=128), [], [wbg])
            kb.dma("pool", wo.t[:], W["w_o"][l].rearrange("(k p) n -> p k n", p=128), [], [wo])
            NB2 = 2
            def mk(name, shp, dt):
                return [kb.sb(st, "%s%d" % (name, j), shp, dt) for j in range(NB2)]
            omT = mk("momT", [64, 8, 128], BF16)
            ofT = mk("mofT", [128, 4, 128], BF16)
            ogT = mk("mogT", [128, 4, 128], BF16)
            ga = mk("mga", [128, 3072], BF16)
            xt = mk("mxt", [128, D], F32)
            y32 = mk("my32", [128, D], F32)
            t32 = mk("mt32", [128, D], F32)
            yb = mk("myb", [128, D], BF16)
            yT = mk("myT", [128, 8, 128], BF16)
            xn = mk("mxn", [128, D], F32)
            idb = P["ident_b"]
            for i in range(NT):
                b = i % NB2
                sl = slice(i * 128, (i + 1) * 128)
                G = rows["Gc" if i < 2 else "Gl"]
                kb.dma("sp", omT[b].t[:], Dr["OMT"][:, :, sl].rearrange("h d t -> d h t"), [Dk["OMT"]], [omT[b]])
                kb.dma("sp", ofT[b].t[:], Dr["OFT"][:, :, sl].rearrange("g m t -> m g t"), [Dk["OFT"]], [ofT[b]])
                kb.dma("sp", ogT[b].t[:], Dr["OGT"][:, :, sl].rearrange("g m t -> m g t"), [Dk["OGT"]], [ogT[b]])
                kb.dma("sp", ga[b].t[:], Dr["GATE"][sl, :], [Dk["GATE"]], [ga[b]])
                kb.dma("sp", xt[b].t[:], Dr["XR"][sl, :], [Dk["XR"]], [xt[b]])
                for half in range(2):
                    cs_ = slice(half * 512, (half + 1) * 512)
                    pm = self.psum()
                    for h in range(8):
                        kb.mm(pm.t[:], omT[b].t[:, h, :], wbm.t[:, h, cs_], h == 0, h == 7, [omT[b], wbm], [pm])
                    pf = self.psum()
                    for k in range(4):
                        kb.mm(pf.t[:], ofT[b].t[:, k, :], wbf.t[:, k, cs_], k == 0, k == 3, [ofT[b], wbf], [pf])
                    pg = self.psum()
                    for k in range(4):
                        kb.mm(pg.t[:], ogT[b].t[:, k, :], wbg.t[:, k, cs_], k == 0, k == 3, [ogT[b], wbg], [pg])
                    Y, T32 = y32[b], t32[b]
                    kb.tt("dve", Y.t[:, cs_], pm.t[:], ga[b].t[:, half * 512:(half + 1) * 512], ALU.mult, [pm, ga[b]], [Y])
                    kb.tt("dve", T32.t[:, cs_], pf.t[:], ga[b].t[:, 1024 + half * 512:1024 + (half + 1) * 512], ALU.mult, [pf, ga[b]], [T32])
                    kb.tt("pool", Y.t[:, cs_], Y.t[:, cs_], T32.t[:, cs_], ALU.add, [Y, T32], [Y])
                    kb.tt("dve", T32.t[:, cs_], pg.t[:], ga[b].t[:, 2048 + half * 512:2048 + (half + 1) * 512], ALU.mult, [pg, ga[b]], [T32])
                    kb.tt("pool", yb[b].t[:, cs_], Y.t[:, cs_], T32.t[:, cs_], ALU.add, [Y, T32], [yb[b]])
                p2 = self.psum()
                p2b = p2.t[:].bitcast(BF16)
                for kc in range(8):
                    kb.tr(p2b[:, kc * 128:(kc + 1) * 128], yb[b].t[:, kc * 128:(kc + 1) * 128], idb.t[:], [yb[b], idb], [p2])
                kb.cp("act", yT[b].t[:].rearrange("p k t -> p (k t)"), p2b, [p2], [yT[b]])
                for half in range(2):
                    cs_ = slice(half * 512, (half + 1) * 512)
                    pz = self.psum()
                    for kc in range(8):
                        kb.mm(pz.t[:], yT[b].t[:, kc, :], wo.t[:, kc, cs_], kc == 0, kc == 7, [yT[b], wo], [pz])
                    kb.tt("dve", xn[b].t[:, cs_], pz.t[:], G.t[:, cs_], ALU.mult, [pz, G], [xn[b]])
                    kb.tt("pool", xn[b].t[:, cs_], xn[b].t[:, cs_], xt[b].t[:, cs_], ALU.add, [xn[b], xt[b]], [xn[b]])
                kb.dma("sp", Dr["XR"][sl, :], xn[b].t[:], [xn[b]], [Dk["XR"]])
            kb.S.flush()

    def phase_moe(self, l):
        kb, P, W, C, Dr, Dk = self.kb, self.P, self.W, self.C, self.Dr, self.Dk
        is_last = (l == self.nl - 1)
        idb = P["ident_b"]
        with contextlib.ExitStack() as st0:
            DSTI = kb.sb(st0, "DSTI", [128, NT, 4], I32)
            G4 = kb.sb(st0, "G4", [128, NT, 4], F32)
            IDXW = kb.sb(st0, "IDXW", [128, NBLK, 8], I32)
            EBI = kb.sb(st0, "EBI", [128, NBLK], I32)
            with contextlib.ExitStack() as st:
                rows = self.mod_rows(st, l, 2, names=("A", "S"))
                rw = kb.sb(st, "rw", [128, 8, NE], F32)
                rb = kb.sb(st, "rb", [1, NE], F32)
                trx = kb.sb(st, "trx", [128, 128], BF16)
                jv = kb.sb(st, "jv", [128, NBLK], F32)
                kp = kb.sb(st, "kp", [128, 8], F32)
                kb.dma("sp", rw.t[:], W["router_w"][l].rearrange("(k p) e -> p k e", p=128), [], [rw])
                kb.dma("sp", rb.t[:], W["router_b"][l].rearrange("(o e) -> o e", o=1), [], [rb])
                kb.dma("sp", trx.t[:], C["tri_x"], [], [trx])
                kb.dma("sp", jv.t[:], C["jv"], [], [jv])
                kb.dma("sp", kp.t[:], C["kp"], [], [kp])
                LG = kb.sb(st, "LG", [128, NT, NE], F32)
                GF = kb.sb(st, "GF", [128, NT, NE], F32)
                POS = kb.sb(st, "POS", [128, NT, NE], F32)
                TOP = kb.sb(st, "TOP", [128, NT, 8], F32)
                cnt = kb.sb(st, "cnt", [128, NE], F32)
                kb.memset("dve", cnt.t[:], 0.0, [], [cnt])
                NB2 = 2
                def mk(name, shp, dt):
                    return [kb.sb(st, "%s%d" % (name, j), shp, dt) for j in range(NB2)]
                xt = mk("ext", [128, D], F32)
                junk = kb.sb(st, "ejunk", [128, D], F32)
                ssq = mk("essq", [128, 8], F32)
                h32 = mk("eh32", [128, D], F32)
                hb = mk("ehb", [128, D], BF16)
                h2T = mk("eh2T", [128, 8, 128], F32)
                sm = mk("esm", [128, 4, NE], F32)
                sc = mk("esc", [128, 8], F32)
                mkb = mk("emkb", [128, NE], BF16)
                for i in range(NT):
                    b = i % NB2
                    sl = slice(i * 128, (i + 1) * 128)
                    kb.dma("sp", xt[b].t[:], Dr["XR"][sl, :], [Dk["XR"]], [xt[b]])
                    kb.memset("pool", ssq[b].t[:], 0.0, [], [ssq[b]])
                    self.norm_mod(xt[b], rows, i, junk, ssq[b], h32[b], hb[b], full32=True)
                    kb.dma("sp", Dr["H2B"][sl, :], hb[b].t[:], [hb[b]], [Dk["H2B"]])
                    for half in range(2):
                        pt_ = self.psum()
                        for k4 in range(4):
                            kc = half * 4 + k4
                            kb.tr(pt_.t[:, k4 * 128:(k4 + 1) * 128], h32[b].t[:, kc * 128:(kc + 1) * 128], P["ident_f"].t[:],
                                  [h32[b], P["ident_f"]], [pt_])
                        kb.cp("act" if half == 0 else "dve", h2T[b].t[:, half * 4:(half + 1) * 4, :].rearrange("p k t -> p (k t)"),
                              pt_.t[:], [pt_], [h2T[b]])
                    pl = self.psum()
                    for kc in range(8):
                        kb.mm(pl.t[:, 0:NE], h2T[b].t[:, kc, :], rw.t[:, kc, :], kc == 0, False, [h2T[b], rw], [pl])
                    kb.mm(pl.t[:, 0:NE], P["ones_f"].t[0:1, :], rb.t[0:1, :], False, True, [P["ones_f"], rb], [pl])
                    kb.cp("act", LG.t[:, i, :], pl.t[:, 0:NE], [pl], [LG])
                    kb.op("dve", lambda e, o=TOP.t[:, i, :], a=LG.t[:, i, :]: e.max(out=o, in_=a), [LG], [TOP])
                    SM, SC = sm[b], sc[b]
                    kb.ts("dve", SM.t[:, 0, :], LG.t[:, i, :], TOP.t[:, i, 3:4], None, ALU.is_ge, None, [LG, TOP], [SM])
                    kb.ts("dve", SC.t[:, 0:1], TOP.t[:, i, 0:1], -1.0, None, ALU.mult, None, [TOP], [SC])
                    kb.act(SM.t[:, 1, :], LG.t[:, i, :], AF.Exp, [LG, SC], [SM], bias=SC.t[:, 0:1], scale=1.0)
                    kb.tt("dve", SM.t[:, 2, :], SM.t[:, 1, :], SM.t[:, 0, :], ALU.mult, [SM], [SM])
                    kb.red("dve", SC.t[:, 1:2], SM.t[:, 2, :], ALU.add, [SM], [SC])
                    kb.recip(SC.t[:, 2:3], SC.t[:, 1:2], [SC], [SC])
                    kb.ts("dve", GF.t[:, i, :], SM.t[:, 2, :], SC.t[:, 2:3], None, ALU.mult, None, [SM, SC], [GF])
                    kb.cp("pool", mkb[b].t[:], SM.t[:, 0, :], [SM], [mkb[b]])
                    pp = self.psum()
                    kb.mm(pp.t[:, 0:NE], trx.t[:], mkb[b].t[:], True, True, [trx, mkb[b]], [pp])
                    kb.mm(pp.t[:, NE:2 * NE], P["ones_b"].t[:], mkb[b].t[:], True, True, [P["ones_b"], mkb[b]], [pp])
                    kb.tt("dve", POS.t[:, i, :], pp.t[:, 0:NE], cnt.t[:], ALU.add, [pp, cnt], [POS])
                    kb.tt("dve", cnt.t[:], pp.t[:, NE:2 * NE], cnt.t[:], ALU.add, [pp, cnt], [cnt])
                nbk = kb.sb(st, "nbk", [128, NE], F32)
                pend = kb.sb(st, "pend", [128, NE], F32)
                pstart = kb.sb(st, "pstart", [128, NE], F32)
                eb = kb.sb(st, "eb", [128, NBLK], F32)
                idxf = kb.sb(st, "idxf", [128, NBLK, 8], F32)
                kb.memset("dve", nbk.t[:], 0.0, [], [nbk])
                for j in range(T // BS + 1):
                    kb.stt("dve", nbk.t[:], cnt.t[:], float(j * BS), nbk.t[:], ALU.is_gt, ALU.add, [cnt, nbk], [nbk])
                kb.ts("dve", nbk.t[:], nbk.t[:], float(BS), None, ALU.mult, None, [nbk], [nbk])
                kb.cp("dve", pend.t[:, 0:1], nbk.t[:, 0:1], [nbk], [pend])
                for e_ in range(1, NE):
                    kb.tt("dve", pend.t[:, e_:e_ + 1], pend.t[:, e_ - 1:e_], nbk.t[:, e_:e_ + 1], ALU.add, [pend, nbk], [pend])
                kb.tt("dve", pstart.t[:], pend.t[:], nbk.t[:], ALU.subtract, [pend, nbk], [pstart])
                kb.memset("dve", eb.t[:], 0.0, [], [eb])
                for e_ in range(NE):
                    kb.stt("dve", eb.t[:], jv.t[:], pend.t[:, e_:e_ + 1], eb.t[:], ALU.is_ge, ALU.add, [jv, pend, eb], [eb])
                kb.ts("dve", eb.t[:], eb.t[:], float(NE - 1), None, ALU.min, None, [eb], [eb])
                kb.ts("dve", kp.t[:], kp.t[:], float(l * NE * D), None, ALU.add, None, [kp], [kp])
                for kc in range(8):
                    kb.ts("dve", idxf.t[:, :, kc], eb.t[:], 1024.0, kp.t[:, kc:kc + 1], ALU.mult, ALU.add, [eb, kp], [idxf])
                kb.cp("dve", IDXW.t[:], idxf.t[:], [idxf], [IDXW])
                kb.ts("dve", eb.t[:], eb.t[:], float(l * NE), None, ALU.add, None, [eb], [eb])
                kb.cp("dve", EBI.t[:], eb.t[:], [eb], [EBI])
                dstf = mk("edstf", [128, 4], F32)
                hs = mk("ehs", [128, D], BF16)
                for i in range(NT):
                    b = i % NB2
                    sl = slice(i * 128, (i + 1) * 128)
                    SM = sm[b]
                    kb.tt("dve", SM.t[:, 3, :], POS.t[:, i, :], pstart.t[:], ALU.add, [POS, pstart], [SM])
                    for k in range(4):
                        kb.ts("dve", SM.t[:, 0, :], LG.t[:, i, :], TOP.t[:, i, k:k + 1], None, ALU.is_equal, None, [LG, TOP], [SM])
                        kb.tt("dve", SM.t[:, 1, :], SM.t[:, 0, :], SM.t[:, 3, :], ALU.mult, [SM], [SM])
                        kb.red("dve", dstf[b].t[:, k:k + 1], SM.t[:, 1, :], ALU.add, [SM], [dstf[b]])
                        kb.tt("dve", SM.t[:, 2, :], SM.t[:, 0, :], GF.t[:, i, :], ALU.mult, [SM, GF], [SM])
                        kb.red("dve", G4.t[:, i, k:k + 1], SM.t[:, 2, :], ALU.add, [SM], [G4])
                    kb.cp("dve", DSTI.t[:, i, :], dstf[b].t[:], [dstf[b]], [DSTI])
                    kb.dma("sp", hs[b].t[:], Dr["H2B"][sl, :], [Dk["H2B"]], [hs[b]])
                    for k in range(4):
                        kb.scatter(Dr["XS"], hs[b].t[:], DSTI.t[:, i, k:k + 1], [hs[b], DSTI], [Dk["XS"]], NROWS - 1)
                if "DBG_DST" in self.dbg:
                    kb.dma("sp", Dr["DBG_DST"], DSTI.t[:].rearrange("p n k -> p (n k)"), [DSTI], [Dk["DBG_DST"]])
                    kb.dma("sp", Dr["DBG_G4"], G4.t[:].rearrange("p n k -> p (n k)"), [G4], [Dk["DBG_G4"]])
                    kb.dma("sp", Dr["DBG_EB"], EBI.t[:], [EBI], [Dk["DBG_EB"]])
                    kb.dma("sp", Dr["DBG_LG"], LG.t[:].rearrange("p n k -> p (n k)"), [LG], [Dk["DBG_LG"]])
                    kb.dma("sp", Dr["DBG_CNT"], cnt.t[:], [cnt], [Dk["DBG_CNT"]])
                kb.S.flush()
            with contextlib.ExitStack() as st:
                wup = [kb.sb(st, "wup%d" % j, [128, 8, 2 * D], BF16) for j in range(2)]
                wdn = [kb.sb(st, "wdn%d" % j, [128, 8, D], BF16) for j in range(2)]
                wupk = [[Tok() for _ in range(8)] for _ in range(2)]
                wdnk = [[Tok() for _ in range(8)] for _ in range(2)]
                bu = [kb.sb(st, "bu%d" % j, [2, 2 * D], F32) for j in range(2)]
                bd = [kb.sb(st, "bd%d" % j, [2, D], F32) for j in range(2)]
                NB2 = 2
                def mk(name, shp, dt):
                    return [kb.sb(st, "%s%d" % (name, j), shp, dt) for j in range(NB2)]
                xs = mk("xs", [128, D], BF16)
                xT = mk("xT", [128, 8, 128], BF16)
                gl = mk("gl", [128, 512], F32)
                li = mk("li", [128, 512], F32)
                sg = mk("sg", [128, 512], F32)
                ab = mk("ab", [128, D], BF16)
                aT = mk("aT", [128, 8, 128], BF16)
                yb = mk("yb", [128, D], F32)
                wup_src = W["exp_w_up"].rearrange("l e k n -> (l e k) n")
                wdn_src = W["exp_w_down"].rearrange("l e k n -> (l e k) n")
                bup_src = W["exp_b_up"].rearrange("l e n -> (l e) n")
                bdn_src = W["exp_b_down"].rearrange("l e n -> (l e) n")
                nlw = W["exp_w_up"].shape[0]
                ones1 = P["ones_f"].t[0:1, :]

                def load_w(j):
                    jb = j % 2
                    for kc in range(8):
                        kb.gather(wup[jb].t[:, kc, :], wup_src, IDXW.t[:, j, kc:kc + 1], [IDXW], [wupk[jb][kc]], nlw * NE * D - 1)
                    for kc in range(8):
                        kb.gather(wdn[jb].t[:, kc, :], wdn_src, IDXW.t[:, j, kc:kc + 1], [IDXW], [wdnk[jb][kc]], nlw * NE * D - 1)
                    kb.gather(bu[jb].t[:], bup_src, EBI.t[0:2, j:j + 1], [EBI], [bu[jb]], nlw * NE - 1)
                    kb.gather(bd[jb].t[:], bdn_src, EBI.t[0:2, j:j + 1], [EBI], [bd[jb]], nlw * NE - 1)

                def load_x(n):
                    kb.dma("sp", xs[n % NB2].t[:], Dr["XS"][n * 128:(n + 1) * 128, :], [Dk["XS"]], [xs[n % NB2]])

                load_w(0)
                load_x(0)
                for j in range(NBLK):
                    jb = j % 2
                    WU, WD, BU, BD = wup[jb], wdn[jb], bu[jb], bd[jb]
                    if j + 1 < NBLK:
                        load_w(j + 1)
                    for sub in range(SUB):
                        n = j * SUB + sub
                        b = n % NB2
                        r0 = n * 128
                        if n + 1 < NBLK * SUB:
                            load_x(n + 1)
                        p2 = self.psum()
                        p2b = p2.t[:].bitcast(BF16)
                        for kc in range(8):
                            kb.tr(p2b[:, kc * 128:(kc + 1) * 128], xs[b].t[:, kc * 128:(kc + 1) * 128], idb.t[:], [xs[b], idb], [p2])
                        kb.cp("act", xT[b].t[:].rearrange("p k t -> p (k t)"), p2b, [p2], [xT[b]])
                        for pair in range(2):
                            pgl = self.psum()
                            pli = self.psum()
                            for (pp_, c0) in ((pgl, pair * 512), (pli, D + pair * 512)):
                                for kc in range(8):
                                    kb.mm(pp_.t[:], xT[b].t[:, kc, :], WU.t[:, kc, c0:c0 + 512], kc == 0, False, [xT[b], wupk[jb][kc]], [pp_])
                                kb.mm(pp_.t[:], ones1, BU.t[0:1, c0:c0 + 512], False, True, [P["ones_f"], BU], [pp_])
                            GL, LI, SG = gl[pair], li[pair], sg[pair]
                            kb.ts("dve", GL.t[:], pgl.t[:], 7.0, None, ALU.min, None, [pgl], [GL])
                            kb.act(SG.t[:], GL.t[:], AF.Sigmoid, [GL], [SG], scale=1.702)
                            kb.ts("dve", LI.t[:], pli.t[:], 7.0, -7.0, ALU.min, ALU.max, [pli], [LI])
                            kb.stt("dve", LI.t[:], LI.t[:], 1.0, GL.t[:], ALU.add, ALU.mult, [LI, GL], [LI])
                            kb.tt("dve", ab[b].t[:, pair * 512:(pair + 1) * 512], LI.t[:], SG.t[:], ALU.mult, [LI, SG], [ab[b]])
                        p3 = self.psum()
                        p3b = p3.t[:].bitcast(BF16)
                        for kc in range(8):
                            kb.tr(p3b[:, kc * 128:(kc + 1) * 128], ab[b].t[:, kc * 128:(kc + 1) * 128], idb.t[:], [ab[b], idb], [p3])
                        kb.cp("act", aT[b].t[:].rearrange("p k t -> p (k t)"), p3b, [p3], [aT[b]])
                        for half in range(2):
                            pd = self.psum()
                            for kc in range(8):
                                kb.mm(pd.t[:], aT[b].t[:, kc, :], WD.t[:, kc, half * 512:(half + 1) * 512], kc == 0, False, [aT[b], wdnk[jb][kc]], [pd])
                            kb.mm(pd.t[:], ones1, BD.t[0:1, half * 512:(half + 1) * 512], False, True, [P["ones_f"], BD], [pd])
                            kb.cp("act" if half == 0 else "dve", yb[b].t[:, half * 512:(half + 1) * 512], pd.t[:], [pd], [yb[b]])
                        kb.dma("sp", Dr["YB"][r0:r0 + 128, :], yb[b].t[:], [yb[b]], [Dk["YB"]])
                kb.S.flush()
            with contextlib.ExitStack() as st:
                rows = self.mod_rows(st, l, 2, names=("G",))
                NB2 = 2
                def mk(name, shp, dt):
                    return [kb.sb(st, "%s%d" % (name, j), shp, dt) for j in range(NB2)]
                xt = mk("cxt", [128, D], F32)
                yk = [kb.sb(st, "cyk%d" % j, [128, D], F32) for j in range(4)]
                acc = mk("cacc", [128, D], F32)
                xn = mk("cxn", [128, D], F32)
                if is_last:
                    fg = kb.sb(st, "fg", [128, D], F32)
                    kb.dma("sp", fg.t[:], W["final_norm_g"].partition_broadcast(128), [], [fg])
                    junk = kb.sb(st, "cjunk", [128, D], F32)
                    ssq = mk("cssq", [128, 8], F32)
                    ot = mk("cot", [128, D], F32)
                for i in range(NT):
                    b = i % NB2
                    sl = slice(i * 128, (i + 1) * 128)
                    G = rows["Gc" if i < 2 else "Gl"]
                    kb.dma("sp", xt[b].t[:], Dr["XR"][sl, :], [Dk["XR"]], [xt[b]])
                    for k in range(4):
                        kb.gather(yk[k].t[:], Dr["YB"], DSTI.t[:, i, k:k + 1], [DSTI, Dk["YB"]], [yk[k]], NROWS - 1)
                    A_ = acc[b]
                    kb.ts("dve", A_.t[:], yk[0].t[:], G4.t[:, i, 0:1], None, ALU.mult, None, [yk[0], G4], [A_])
                    for k in range(1, 4):
                        kb.stt("dve", A_.t[:], yk[k].t[:], G4.t[:, i, k:k + 1], A_.t[:], ALU.mult, ALU.add, [yk[k], G4, A_], [A_])
                    kb.tt("pool", A_.t[:], A_.t[:], G.t[:], ALU.mult, [A_, G], [A_])
                    kb.tt("dve", xn[b].t[:], A_.t[:], xt[b].t[:], ALU.add, [A_, xt[b]], [xn[b]])
                    kb.dma("sp", Dr["XR"][sl, :], xn[b].t[:], [xn[b]], [Dk["XR"]])
                    if is_last and i >= 2:
                        SS = ssq[b]
                        kb.memset("pool", SS.t[:], 0.0, [], [SS])
                        kb.act(junk.t[:], xn[b].t[:], AF.Square, [xn[b]], [junk, SS], accum=SS.t[:, 0:1])
                        kb.ts("dve", SS.t[:, 1:2], SS.t[:, 0:1], 1.0 / D, EPS, ALU.mult, ALU.add, [SS], [SS])
                        kb.act(SS.t[:, 1:2], SS.t[:, 1:2], AF.Sqrt, [SS], [SS])
                        kb.recip(SS.t[:, 2:3], SS.t[:, 1:2], [SS], [SS])
                        kb.stt("dve", ot[b].t[:], xn[b].t[:], SS.t[:, 2:3], fg.t[:], ALU.mult, ALU.mult, [xn[b], SS, fg], [ot[b]])
                        kb.dma("sp", self.out[(i - 2) * 128:(i - 1) * 128, :], ot[b].t[:], [ot[b]], [self.tout])
                kb.S.flush()


_CACHE = {}


def kernel(**inputs):
    n_cores = 8
    if "nc" not in _CACHE:
        pg = Prog(nl=DEPTH)
        _CACHE["nc"] = pg.build()
        _CACHE["consts"] = host_consts()
    nc = _CACHE["nc"]
    consts = _CACHE["consts"]
    shared = {k: np.ascontiguousarray(np.asarray(inputs[k], dtype=np.float32)) for k in WEIGHT_SPECS}
    shared["c_ctx"] = np.ascontiguousarray(np.asarray(inputs["c_ctx"], dtype=np.float32))
    shared.update(consts)
    x = np.asarray(inputs["x"], dtype=np.float32)
    c = np.asarray(inputs["c"], dtype=np.float32)
    ctx = np.asarray(inputs["ctx"], dtype=np.float32)
    in_maps = []
    for b in range(n_cores):
        m = dict(shared)
        m["x"] = np.ascontiguousarray(x[b])
        m["ctx"] = np.ascontiguousarray(ctx[b])
        m["c"] = np.ascontiguousarray(c[b])
        in_maps.append(m)
    res = run_bass_kernel_spmd(nc, in_maps, core_ids=list(range(n_cores)))
    out = np.stack([np.asarray(res.results[b]["out"], dtype=np.float32) for b in range(n_cores)], axis=0)
    return out
```

```python
import contextlib
import math
import numpy as np
import ml_dtypes
import concourse.bass as bass
import concourse.mybir as mybir
from concourse.bass_utils import run_bass_kernel_spmd

F32 = mybir.dt.float32
BF16 = mybir.dt.bfloat16
I32 = mybir.dt.int32
AF = mybir.ActivationFunctionType
ALU = mybir.AluOpType
AX = mybir.AxisListType

D = 1024
SEQ = 4096
CTX = 256
T = SEQ + CTX
NT = T // 128
DEPTH = 4
INW = 5568
NE = 32
BS = 256
SUB = BS // 128
NBLK = (T * 4) // BS + NE
NROWS = NBLK * BS
EPS = 1e-6
MLA_SCALE = 96 ** -0.5
GLA_SCALE = 64 ** -0.5
O_UQ, O_KV, O_FN, O_GQ, O_GK, O_GV, O_OG, O_GF, O_GB, O_GATE = 0, 256, 416, 928, 1184, 1440, 1952, 2464, 2480, 2496


import os as _os
SAME_ENGINE_SYNC = _os.environ.get("NOSAME", "0") != "1"


class Tok:
    __slots__ = ("w", "rs")

    def __init__(self):
        self.w = None
        self.rs = {}


class Sched:
    NDMA = 32

    def __init__(self, nc, st):
        self.nc = nc
        self.engs = ["pe", "act", "dve", "pool", "sp"]
        self.ops = {k: [] for k in self.engs}
        self.cnt = {k: 0 for k in self.engs}
        self.seen = {k: {} for k in self.engs}
        self.dma_k = {"d": 0, "g": 0}
        self.dma_last = {}
        self.n_ops = 0
        self.sems = {}
        for k in ["e_" + e for e in self.engs] + ["d%d" % i for i in range(self.NDMA)] + ["g%d" % i for i in range(self.NDMA)]:
            self.sems[k] = st.enter_context(nc.semaphore(k))

    def _need(self, eng, ev, waits):
        if ev is None:
            return
        src, key, val = ev
        if src == eng and (eng == "pe" or not SAME_ENGINE_SYNC):
            return
        if self.seen[eng].get(key, 0) >= val:
            return
        self.seen[eng][key] = val
        waits.append((key, val))

    def op(self, eng, fn, r=(), w=(), dma=False):
        waits = []
        for t in r:
            self._need(eng, t.w, waits)
        for t in w:
            self._need(eng, t.w, waits)
            for ev in t.rs.values():
                self._need(eng, ev, waits)
        if dma:
            pre = "g" if eng == "pool" else "d"
            k = self.dma_k[pre]
            self.dma_k[pre] += 1
            key = "%s%d" % (pre, k % self.NDMA)
            val = 16 * (k // self.NDMA + 1)
            if val > 16:
                self._need(eng, ("dma", key, val - 16), waits)
            ev = ("dma", key, val)
            inc = (key, 16)
            self.dma_last[key] = val
        else:
            self.cnt[eng] += 1
            key = "e_" + eng
            ev = (eng, key, self.cnt[eng])
            inc = (key, 1)
        self.ops[eng].append((waits, fn, inc))
        self.n_ops += 1
        for t in w:
            t.w = ev
            t.rs = {}
        for t in r:
            if t not in w:
                t.rs[ev[1]] = ev
        return ev

    def barrier(self):
        evs = [(e, "e_" + e, self.cnt[e]) for e in self.engs if self.cnt[e] > 0]
        evs += [("dma", k, v) for k, v in self.dma_last.items()]
        for eng in self.engs:
            waits = []
            for ev in evs:
                if ev[0] == eng:
                    continue
                self._need(eng, ev, waits)
            if waits:
                self.ops[eng].append((waits, None, None))

    def flush(self):
        self.barrier()
        nc = self.nc
        sems = self.sems
        ops = self.ops
        with nc.Block() as block:
            def mk(engname):
                def body(e):
                    for waits, fn, inc in ops[engname]:
                        for key, val in waits:
                            e.wait_ge(sems[key], val)
                        if fn is not None:
                            fn(e).then_inc(sems[inc[0]], inc[1])
                return body
            block.tensor(mk("pe"))
            block.scalar(mk("act"))
            block.vector(mk("dve"))
            block.gpsimd(mk("pool"))
            block.sync(mk("sp"))
        self.ops = {k: [] for k in self.engs}


class B:
    __slots__ = ("t", "k", "psum")

    def __init__(self, t, psum=False):
        self.t = t
        self.k = Tok()
        self.psum = psum


def _toks(xs):
    return [x.k if isinstance(x, B) else x for x in xs]


class KB:
    def __init__(self, nc, st):
        self.nc = nc
        self.S = Sched(nc, st)
        self.dq = 0
        self.bregs = {}

    def sb(self, st, name, shape, dt):
        self.dq += 1
        return B(st.enter_context(self.nc.sbuf_tensor("%s_u%d" % (name, self.dq), shape, dt)))

    def op(self, eng, fn, r, w, dma=False):
        r2 = [x for x in r if not (isinstance(x, B) and x.psum)]
        w2 = list(w) + [x for x in r if isinstance(x, B) and x.psum and x not in w]
        return self.S.op(eng, fn, r=_toks(r2), w=_toks(w2), dma=dma)

    def mm(self, out, lhsT, rhs, start, stop, r, w):
        self.op("pe", lambda e: e.matmul(out, lhsT, rhs, start=start, stop=stop), r, w)

    def tr(self, out, in_, ident, r, w):
        self.op("pe", lambda e: e.transpose(out, in_, ident), r, w)

    def act(self, out, in_, func, r, w, bias=None, scale=None, accum=None):
        kw = {}
        if bias is not None:
            kw["bias"] = bias
        if scale is not None:
            kw["scale"] = scale
        if accum is not None:
            kw["accum_out"] = accum
        self.op("act", lambda e: e.activation(out=out, in_=in_, func=func, **kw), r, w)

    def cp(self, eng, out, in_, r, w):
        if eng == "act":
            self.op("act", lambda e: e.copy(out=out, in_=in_), r, w)
        else:
            self.op(eng, lambda e: e.tensor_copy(out=out, in_=in_), r, w)

    def tt(self, eng, out, in0, in1, op, r, w):
        self.op(eng, lambda e: e.tensor_tensor(out=out, in0=in0, in1=in1, op=op), r, w)

    def ts(self, eng, out, in0, s1, s2, op0, op1, r, w):
        if op1 is None:
            self.op(eng, lambda e: e.tensor_scalar(out=out, in0=in0, scalar1=s1, scalar2=None, op0=op0), r, w)
        else:
            self.op(eng, lambda e: e.tensor_scalar(out=out, in0=in0, scalar1=s1, scalar2=s2, op0=op0, op1=op1), r, w)

    def stt(self, eng, out, in0, scalar, in1, op0, op1, r, w):
        self.op(eng, lambda e: e.scalar_tensor_tensor(out=out, in0=in0, scalar=scalar, in1=in1, op0=op0, op1=op1), r, w)

    def red(self, eng, out, in_, op, r, w, axis=AX.X):
        self.op(eng, lambda e: e.tensor_reduce(out=out, in_=in_, axis=axis, op=op), r, w)

    def recip(self, out, in_, r, w):
        self.op("dve", lambda e: e.reciprocal(out=out, in_=in_), r, w)

    def memset(self, eng, out, val, r, w):
        self.op(eng, lambda e: e.memset(out, val), r, w)

    def dma(self, eng, out, in_, r, w, slow=False):
        if slow:
            self.op(eng, lambda e: e.dma_start(out=out, in_=in_, allow_slow_non_contiguous=True), r, w, dma=True)
        else:
            self.op(eng, lambda e: e.dma_start(out=out, in_=in_), r, w, dma=True)

    def _breg(self, e, bound):
        if bound not in self.bregs:
            self.bregs[bound] = e.to_reg(bound)
        return self.bregs[bound]

    def gather(self, out, in_, idx, r, w, bound):
        self.op("pool", lambda e: e.indirect_dma_start(
            out=out, out_offset=None, in_=in_, in_offset=bass.IndirectOffsetOnAxis(ap=idx, axis=0),
            bounds_check=self._breg(e, bound), oob_is_err=False), r, w, dma=True)

    def scatter(self, out, in_, idx, r, w, bound):
        self.op("pool", lambda e: e.indirect_dma_start(
            out=out, out_offset=bass.IndirectOffsetOnAxis(ap=idx, axis=0), in_=in_, in_offset=None,
            bounds_check=self._breg(e, bound), oob_is_err=False), r, w, dma=True)


def host_consts():
    c = {}
    bf = ml_dtypes.bfloat16
    i = np.arange(128)
    c["ident_f"] = np.eye(128, dtype=np.float32)
    c["ident_b"] = np.eye(128).astype(bf)
    trif = (i[:, None] <= i[None, :]).astype(np.float32)
    trib = (i[:, None] >= i[None, :]).astype(np.float32)
    c["tri_f"] = np.stack([trif, trib], 0)
    c["mask4"] = np.stack([np.repeat(trif[:, None, :], 4, 1), np.repeat(trib[:, None, :], 4, 1)], 0).astype(np.float32)
    c["tri_x"] = (i[:, None] < i[None, :]).astype(bf)
    c["ones_f"] = np.ones((128, 128), np.float32)
    c["ones_b"] = np.ones((128, 128)).astype(bf)
    inv = 10000.0 ** (-np.arange(0, 16, 2, dtype=np.float32) / 16)
    row = np.repeat(np.arange(64, dtype=np.float32), 64)
    col = np.tile(np.arange(64, dtype=np.float32), 64)
    ar = row[:, None] * inv
    ac = col[:, None] * inv
    cr, sr, cc, sc = (np.cos(ar).astype(np.float32), np.sin(ar).astype(np.float32),
                      np.cos(ac).astype(np.float32), np.sin(ac).astype(np.float32))
    cos32 = np.concatenate([cr, cr, cc, cc], 1)
    sin32 = np.concatenate([-sr, sr, -sc, sc], 1)
    cos32 = np.concatenate([np.ones((CTX, 32), np.float32), cos32], 0)
    sin32 = np.concatenate([np.zeros((CTX, 32), np.float32), sin32], 0)
    c["rope_k"] = np.stack([cos32, sin32], 1).astype(np.float32)
    rq = np.stack([np.tile(cos32, (1, 8)), np.tile(sin32, (1, 8))], 1) * np.float32(MLA_SCALE)
    c["rope_q"] = rq.astype(np.float32)
    ang = 2 * np.pi * np.outer(i, i) / 128.0
    c["cs128"] = (np.concatenate([np.cos(ang), np.sin(ang)], 1) / math.sqrt(128)).astype(bf)
    n = np.arange(SEQ, dtype=np.int64)
    kt = (np.outer(n, n) % SEQ).astype(np.float64) * (2 * np.pi / SEQ)
    ctm = (np.cos(kt) / 64.0).astype(np.float32).reshape(32, 128, 32, 128)
    stm = (-np.sin(kt) / 64.0).astype(np.float32).reshape(32, 128, 32, 128)
    c["dft_c"] = np.ascontiguousarray(ctm.transpose(2, 1, 0, 3)).astype(bf)
    c["dft_s"] = np.ascontiguousarray(stm.transpose(2, 1, 0, 3)).astype(bf)
    del kt, ctm, stm
    m = np.arange(CTX, dtype=np.int64)
    k2 = (np.outer(m, m) % CTX).astype(np.float64) * (2 * np.pi / CTX)
    c2 = (np.cos(k2) / 16.0).astype(np.float32).reshape(2, 128, 2, 128)
    s2 = (-np.sin(k2) / 16.0).astype(np.float32).reshape(2, 128, 2, 128)
    c["dft_c2"] = np.ascontiguousarray(c2.transpose(2, 1, 0, 3)).astype(bf)
    c["dft_s2"] = np.ascontiguousarray(s2.transpose(2, 1, 0, 3)).astype(bf)
    c["jv"] = np.tile((np.arange(NBLK, dtype=np.float32) * BS)[None, :], (128, 1))
    c["kp"] = (np.arange(8, dtype=np.float32)[None, :] * 128 + i[:, None]).astype(np.float32)
    return c


CONST_SPECS = {
    "ident_f": ([128, 128], F32), "ident_b": ([128, 128], BF16), "tri_f": ([2, 128, 128], F32),
    "mask4": ([2, 128, 4, 128], F32), "tri_x": ([128, 128], BF16), "ones_f": ([128, 128], F32),
    "ones_b": ([128, 128], BF16), "rope_k": ([T, 2, 32], F32), "rope_q": ([T, 2, 256], F32),
    "cs128": ([128, 256], BF16), "dft_c": ([32, 128, 32, 128], BF16), "dft_s": ([32, 128, 32, 128], BF16),
    "dft_c2": ([2, 128, 2, 128], BF16), "dft_s2": ([2, 128, 2, 128], BF16),
    "jv": ([128, NBLK], F32), "kp": ([128, 8], F32),
}

WEIGHT_SPECS = {
    "w_mod": [DEPTH, D, 6 * D], "b_mod": [DEPTH, 6 * D], "norm1_g": [DEPTH, D], "w_in": [DEPTH, D, INW],
    "mla_q_norm_g": [DEPTH, 256], "mla_w_uq": [DEPTH, 256, 768], "mla_kv_norm_g": [DEPTH, 128],
    "mla_w_ukv": [DEPTH, 128, 1024], "gla_w_gate_f": [DEPTH, 16, 256], "gla_b_gate_f": [DEPTH, 256],
    "gla_w_gate_b": [DEPTH, 16, 256], "gla_b_gate_b": [DEPTH, 256], "gla_norm_g": [DEPTH, 512],
    "w_br_mla": [DEPTH, 512, D], "w_br_fnet": [DEPTH, 512, D], "w_br_gla": [DEPTH, 512, D],
    "w_o": [DEPTH, D, D], "norm2_g": [DEPTH, D], "router_w": [DEPTH, D, NE], "router_b": [DEPTH, NE],
    "exp_w_up": [DEPTH, NE, D, 2 * D], "exp_b_up": [DEPTH, NE, 2 * D], "exp_w_down": [DEPTH, NE, D, D],
    "exp_b_down": [DEPTH, NE, D], "final_norm_g": [D],
}


class Prog:
    def __init__(self, nl=DEPTH, dbg=(), stop_after=None, wdepth=DEPTH):
        self.nl = nl
        self.dbg = set(dbg)
        self.stop_after = stop_after
        self.nc = bass.Bass("TRN2", target_bir_lowering=False)
        self.st = contextlib.ExitStack()
        self.kb = KB(self.nc, self.st)
        nc = self.nc
        self.x_in = nc.dram_tensor("x", [SEQ, D], F32, kind="ExternalInput").ap()
        self.ctx_in = nc.dram_tensor("ctx", [CTX, D], F32, kind="ExternalInput").ap()
        self.c_in = nc.dram_tensor("c", [D], F32, kind="ExternalInput").ap()
        self.cc_in = nc.dram_tensor("c_ctx", [D], F32, kind="ExternalInput").ap()
        self.W = {k: nc.dram_tensor(k, ([wdepth] + shp[1:]) if (len(shp) > 1 or k in ('norm1_g',)) and shp[0] == DEPTH and k != 'final_norm_g' else shp, F32, kind="ExternalInput").ap() for k, shp in WEIGHT_SPECS.items()}
        self.C = {k: nc.dram_tensor(k, shp, dt, kind="ExternalInput").ap() for k, (shp, dt) in CONST_SPECS.items()}
        self.out = nc.dram_tensor("out", [SEQ, D], F32, kind="ExternalOutput").ap()
        self.tout = Tok()
        self.Dr = {}
        self.Dk = {}
        for name, shp, dt in [
            ("XR", [T, D], F32), ("KT", [8, 96, T], BF16), ("QT", [8, 96, T], BF16), ("VA", [T, 8, 65], BF16),
            ("UFT", [4, 128, T], BF16), ("GQT", [4, 64, T], BF16), ("GKT", [4, 64, T], BF16),
            ("GKVO", [T, 1280], BF16), ("LFB", [T, 512], F32), ("GATE", [T, 3072], BF16),
            ("OMT", [8, 64, T], BF16), ("OFT", [4, 128, T], BF16), ("OGT", [4, 128, T], BF16),
            ("OFW", [T, 512], F32), ("XS", [NROWS, D], BF16), ("YB", [NROWS, D], F32),
            ("H2B", [T, D], BF16), ("DBG_DST", [128, NT * 4], I32), ("DBG_G4", [128, NT * 4], F32),
            ("DBG_EB", [128, NBLK], I32), ("DBG_LG", [128, NT * NE], F32), ("DBG_CNT", [128, NE], F32),
        ]:
            kind = "ExternalOutput" if name in self.dbg else "Internal"
            self.Dr[name] = nc.dram_tensor(name, shp, dt, kind=kind).ap()
            self.Dk[name] = Tok()

    def build(self):
        kb = self.kb
        nc = self.nc
        with self.st:
            P = self.P = {}
            for name, shp, dt in [
                ("ident_f", [128, 128], F32), ("ident_b", [128, 128], BF16), ("ones_f", [128, 128], F32),
                ("ones_b", [128, 128], BF16), ("modT", [128, 48, 2], F32), ("nb", [128, 8], F32),
                ("scT", [128, 8, 2], F32),
            ]:
                P[name] = kb.sb(self.st, "p_" + name, shp, dt)
            self.ps = [B(self.st.enter_context(nc.psum_tensor("ps%d" % i, [128, 512], F32)), psum=True) for i in range(8)]
            self.psi = 0
            self.pinned = set()
            for nm in ["ident_f", "ident_b", "ones_f", "ones_b"]:
                kb.dma("sp", P[nm].t[:], self.C[nm], [], [P[nm]])
            kb.dma("sp", self.Dr["XR"][0:CTX, :], self.ctx_in, [], [self.Dk["XR"]])
            kb.dma("pool", self.Dr["XR"][CTX:T, :], self.x_in, [], [self.Dk["XR"]])
            kb.S.flush()
            for l in range(self.nl):
                import os
                skip = os.environ.get("SKIP_PH", "").split(",")
                for ph in [self.phase_mod, self.phase_a, self.phase_attn, self.phase_fnet, self.phase_gla,
                           self.phase_merge, self.phase_moe]:
                    if ph.__name__ in skip:
                        continue
                    ph(l)
                    kb.S.flush()
                    if self.stop_after == (l, ph.__name__):
                        break
                else:
                    continue
                break
            kb.S.flush()
        return nc

    def psum(self, pin=False):
        while True:
            idx = self.psi % 8
            self.psi += 1
            if idx not in self.pinned:
                break
        if pin:
            self.pinned.add(idx)
        return self.ps[idx]

    def unpin(self, p):
        self.pinned.discard(self.ps.index(p))

    def phase_mod(self, l):
        kb, P, W = self.kb, self.P, self.W
        with contextlib.ExitStack() as st:
            cT = kb.sb(st, "cT", [128, 8, 2], F32)
            sT = kb.sb(st, "sT", [128, 8, 2], F32)
            bT = kb.sb(st, "bT", [128, 48], F32)
            wm = [kb.sb(st, "wm%d" % i, [128, 8, 1024], F32) for i in range(2)]
            kb.dma("sp", cT.t[:, :, 0], self.c_in.rearrange("(k p) -> p k", p=128), [], [cT], slow=True)
            kb.dma("sp", cT.t[:, :, 1], self.cc_in.rearrange("(k p) -> p k", p=128), [], [cT], slow=True)
            kb.dma("sp", bT.t[:], W["b_mod"][l].rearrange("(j p) -> p j", p=128), [], [bT], slow=True)
            kb.act(sT.t[:], cT.t[:], AF.Silu, [cT], [sT])
            for sec in range(6):
                w = wm[sec % 2]
                kb.dma("sp" if sec % 2 == 0 else "pool", w.t[:],
                       W["w_mod"][l][:, sec * 1024:(sec + 1) * 1024].rearrange("(k p) n -> p k n", p=128), [], [w])
                ps = self.psum()
                for j in range(8):
                    for kc in range(8):
                        kb.mm(ps.t[:, j * 2:j * 2 + 2], w.t[:, kc, j * 128:(j + 1) * 128], sT.t[:, kc, :],
                              kc == 0, kc == 7, [w, sT], [ps])
                for j in range(8):
                    jj = sec * 8 + j
                    kb.ts("dve", P["modT"].t[:, jj, :], ps.t[:, j * 2:j * 2 + 2], bT.t[:, jj:jj + 1], None, ALU.add, None,
                          [ps, bT], [P["modT"]])
            kb.S.flush()

    def bcast_rows(self, st, name, colT_ap_fn, r):
        kb, P = self.kb, self.P
        out = kb.sb(st, name, [128, 1024], F32)
        tmp = kb.sb(st, name + "_t", [128, 128], F32)
        for half in range(2):
            ps = self.psum()
            for k4 in range(4):
                kc = half * 4 + k4
                kb.ts("dve", tmp.t[:], P["ones_f"].t[:], colT_ap_fn(kc), None, ALU.mult, None, r + [P["ones_f"]], [tmp])
                kb.mm(ps.t[:, k4 * 128:(k4 + 1) * 128], tmp.t[:], P["ident_f"].t[:], True, True, [tmp, P["ident_f"]], [ps])
            kb.cp("act", out.t[:, half * 512:(half + 1) * 512], ps.t[:], [ps], [out])
        return out

    def mod_rows(self, st, l, which, names=("A", "S", "G")):
        kb, P, W = self.kb, self.P, self.W
        sec_sh, sec_sc, sec_g = (0, 1, 2) if which == 1 else (3, 4, 5)
        gT = kb.sb(st, "gT", [128, 8], F32)
        kb.dma("sp", gT.t[:], W["norm1_g" if which == 1 else "norm2_g"][l].rearrange("(k p) -> p k", p=128), [], [gT], slow=True)
        aT = kb.sb(st, "aT", [128, 8, 2], F32)
        m = P["modT"]
        for v in range(2):
            kb.stt("dve", aT.t[:, :, v], m.t[:, sec_sc * 8:(sec_sc + 1) * 8, v], 1.0, gT.t[:], ALU.add, ALU.mult, [m, gT], [aT])
        rows = {}
        for v, nm in ((0, "l"), (1, "c")):
            if "A" in names:
                rows["A" + nm] = self.bcast_rows(st, "rA" + nm, lambda kc, v=v: aT.t[:, kc, v:v + 1], [aT])
            if "S" in names:
                rows["S" + nm] = self.bcast_rows(st, "rS" + nm, lambda kc, v=v: m.t[:, sec_sh * 8 + kc, v:v + 1], [m])
            if "G" in names:
                rows["G" + nm] = self.bcast_rows(st, "rG" + nm, lambda kc, v=v: m.t[:, sec_g * 8 + kc, v:v + 1], [m])
        return rows

    def norm_mod(self, xt, rows, i, junk, ssq, h32, hb, full32=False):
        kb = self.kb
        nm = "c" if i < 2 else "l"
        kb.act(junk.t[:], xt.t[:], AF.Square, [xt], [junk, ssq], accum=ssq.t[:, 0:1])
        kb.ts("dve", ssq.t[:, 1:2], ssq.t[:, 0:1], 1.0 / D, EPS, ALU.mult, ALU.add, [ssq], [ssq])
        kb.act(ssq.t[:, 1:2], ssq.t[:, 1:2], AF.Sqrt, [ssq], [ssq])
        kb.recip(ssq.t[:, 2:3], ssq.t[:, 1:2], [ssq], [ssq])
        kb.stt("dve", h32.t[:], xt.t[:], ssq.t[:, 2:3], rows["A" + nm].t[:], ALU.mult, ALU.mult, [xt, ssq, rows["A" + nm]], [h32])
        if full32:
            kb.tt("pool", h32.t[:], h32.t[:], rows["S" + nm].t[:], ALU.add, [h32, rows["S" + nm]], [h32])
            kb.cp("dve", hb.t[:], h32.t[:], [h32], [hb])
        else:
            kb.tt("pool", hb.t[:], h32.t[:], rows["S" + nm].t[:], ALU.add, [h32, rows["S" + nm]], [hb])

    def phase_a(self, l):
        kb, P, W, C, Dr, Dk = self.kb, self.P, self.W, self.C, self.Dr, self.Dk
        with contextlib.ExitStack() as st:
            rows = self.mod_rows(st, l, 1, names=("A", "S"))
            win = kb.sb(st, "win", [128, 8, INW], BF16)
            for kc in range(8):
                kb.dma("pool", win.t[:, kc, :], W["w_in"][l][kc * 128:(kc + 1) * 128, :], [], [win])
            wuq32 = kb.sb(st, "wuq32", [128, 2, 768], F32)
            wuq = kb.sb(st, "wuq", [128, 2, 768], BF16)
            gq = kb.sb(st, "gq", [128, 2], F32)
            wkv32 = kb.sb(st, "wkv32", [128, 1024], F32)
            wkv = kb.sb(st, "wkv", [128, 1024], BF16)
            gkv = kb.sb(st, "gkv", [128, 1], F32)
            wg = kb.sb(st, "wg", [17, 2, 256], F32)
            kb.dma("sp", wuq32.t[:], W["mla_w_uq"][l].rearrange("(k p) n -> p k n", p=128), [], [wuq32])
            kb.dma("sp", gq.t[:], W["mla_q_norm_g"][l].rearrange("(k p) -> p k", p=128), [], [gq], slow=True)
            kb.dma("sp", wkv32.t[:], W["mla_w_ukv"][l], [], [wkv32])
            kb.dma("sp", gkv.t[:], W["mla_kv_norm_g"][l].rearrange("(k p) -> p k", p=128), [], [gkv], slow=True)
            kb.dma("sp", wg.t[0:16, 0, :], W["gla_w_gate_f"][l], [], [wg])
            kb.dma("sp", wg.t[0:16, 1, :], W["gla_w_gate_b"][l], [], [wg])
            kb.dma("sp", wg.t[16:17, 0, :], W["gla_b_gate_f"][l].rearrange("(o n) -> o n", o=1), [], [wg])
            kb.dma("sp", wg.t[16:17, 1, :], W["gla_b_gate_b"][l].rearrange("(o n) -> o n", o=1), [], [wg])
            for kc in range(2):
                kb.ts("dve", wuq.t[:, kc, :], wuq32.t[:, kc, :], gq.t[:, kc:kc + 1], None, ALU.mult, None, [wuq32, gq], [wuq])
            kb.ts("dve", wkv.t[:], wkv32.t[:], gkv.t[:, 0:1], None, ALU.mult, None, [wkv32, gkv], [wkv])
            m16 = kb.sb(st, "m16", [128, 16], F32)
            kb.memset("dve", m16.t[:], 0.0, [], [m16])
            NB2 = 1
            def mk(name, shp, dt):
                return [kb.sb(st, "%s%d" % (name, j), shp, dt) for j in range(NB2)]
            def mk2(name, shp, dt):
                return [kb.sb(st, "%s%d" % (name, j), shp, dt) for j in range(2)]
            xt = mk2("xt", [128, D], F32)
            junk = mk("junk", [128, D], F32)
            ssq = mk2("ssq", [128, 8], F32)
            h32 = mk("h32", [128, D], F32)
            hb = mk2("hb", [128, D], BF16)
            hT = mk2("hT", [128, 8, 128], BF16)
            uqn = mk("uqn", [128, 384], BF16)
            kr = mk("kr", [128, 4, 32], F32)
            uT = mk("uT", [128, 3, 128], BF16)
            rq = mk("rq", [128, 2, 256], F32)
            rk = mk("rk", [128, 2, 32], F32)
            qo = mk("qo", [128, 8, 96], BF16)
            ko = mk("ko", [128, 8, 96], BF16)
            vo = mk("vo", [128, 8, 65], BF16)
            qtmp = mk("qtmp", [128, 3, 256], F32)
            sq = mk("sq", [128, 768], F32)
            n2 = mk("n2", [128, 16], F32)
            qkT = mk("qkT", [96, 16, 128], BF16)
            gkvo = mk("gkvo", [128, 1280], BF16)
            gate = mk("gate", [128, 3072], BF16)
            fT = mk("fT", [128, 4, 128], BF16)
            gqkT = mk("gqkT", [64, 8, 128], BF16)
            ugT = mk("ugT", [17, 2, 128], F32)
            lfb = mk("lfb", [128, 512], F32)
            lfe = mk("lfe", [128, 512], F32)
            for j in range(NB2):
                kb.memset("pool", vo[j].t[:], 1.0, [], [vo[j]])
                kb.memset("pool", ugT[j].t[:], 1.0, [], [ugT[j]])
            idb = P["ident_b"]
            for i in range(NT):
                b = i % NB2
                b2 = i % 2
                X, J, SS, H32, HB, HT = xt[b2], junk[b], ssq[b2], h32[b], hb[b2], hT[b2]
                kb.dma("sp", X.t[:], Dr["XR"][i * 128:(i + 1) * 128, :], [Dk["XR"]], [X])
                kb.dma("sp", rq[b].t[:], C["rope_q"][i * 128:(i + 1) * 128], [], [rq[b]])
                kb.dma("sp", rk[b].t[:], C["rope_k"][i * 128:(i + 1) * 128], [], [rk[b]])
                kb.memset("pool", SS.t[:], 0.0, [], [SS])
                self.norm_mod(X, rows, i, J, SS, H32, HB)
                pT = self.psum()
                pTb = pT.t[:].bitcast(BF16)
                for kc in range(8):
                    kb.tr(pTb[:, kc * 128:(kc + 1) * 128], HB.t[:, kc * 128:(kc + 1) * 128], idb.t[:], [HB, idb], [pT])
                kb.cp("act", HT.t[:].rearrange("p k t -> p (k t)"), pTb, [pT], [HT])

                def tm(c0, cw):
                    ps = self.psum()
                    for kc in range(8):
                        kb.mm(ps.t[:, 0:cw], HT.t[:, kc, :], win.t[:, kc, c0:c0 + cw], kc == 0, kc == 7, [HT, win], [ps])
                    return ps

                def fm(c0, cw, ps, o0):
                    for kc in range(8):
                        kb.mm(ps.t[0:cw, o0:o0 + 128], win.t[:, kc, c0:c0 + cw], HT.t[:, kc, :], kc == 0, kc == 7, [HT, win], [ps])

                ps1 = tm(0, 416)
                UQ, KR, UT, QO, KO, VO, QTMP, SQ, N2 = uqn[b], kr[b], uT[b], qo[b], ko[b], vo[b], qtmp[b], sq[b], n2[b]
                kb.act(J.t[:, 0:256], ps1.t[:, 0:256], AF.Square, [ps1], [J, SS], accum=SS.t[:, 3:4])
                kb.act(J.t[:, 256:384], ps1.t[:, 256:384], AF.Square, [ps1], [J, SS], accum=SS.t[:, 4:5])
                kb.ts("dve", SS.t[:, 5:6], SS.t[:, 3:4], 1.0 / 256, EPS, ALU.mult, ALU.add, [SS], [SS])
                kb.ts("dve", SS.t[:, 6:7], SS.t[:, 4:5], 1.0 / 128, EPS, ALU.mult, ALU.add, [SS], [SS])
                kb.act(SS.t[:, 5:7], SS.t[:, 5:7], AF.Sqrt, [SS], [SS])
                kb.recip(SS.t[:, 5:7], SS.t[:, 5:7], [SS], [SS])
                kb.ts("dve", UQ.t[:, 0:256], ps1.t[:, 0:256], SS.t[:, 5:6], None, ALU.mult, None, [ps1, SS], [UQ])
                kb.ts("dve", UQ.t[:, 256:384], ps1.t[:, 256:384], SS.t[:, 6:7], None, ALU.mult, None, [ps1, SS], [UQ])
                kb.cp("act", KR.t[:, 0, :], ps1.t[:, 384:416], [ps1], [KR])
                krv = KR.t[:, 0, :].rearrange("p (a f e) -> p a f e", a=2, f=2)
                krs = KR.t[:, 1, :].rearrange("p (a f e) -> p a f e", a=2, f=2)
                kb.cp("pool", krs[:, :, 0, :], krv[:, :, 1, :], [KR], [KR])
                kb.cp("pool", krs[:, :, 1, :], krv[:, :, 0, :], [KR], [KR])
                kb.tt("dve", KR.t[:, 2, :], KR.t[:, 0, :], rk[b].t[:, 0, :], ALU.mult, [KR, rk[b]], [KR])
                kb.tt("dve", KR.t[:, 3, :], KR.t[:, 1, :], rk[b].t[:, 1, :], ALU.mult, [KR, rk[b]], [KR])
                kb.tt("dve", KR.t[:, 0, :], KR.t[:, 2, :], KR.t[:, 3, :], ALU.add, [KR], [KR])
                p2 = self.psum()
                p2b = p2.t[:].bitcast(BF16)
                for j in range(3):
                    kb.tr(p2b[:, j * 128:(j + 1) * 128], UQ.t[:, j * 128:(j + 1) * 128], idb.t[:], [UQ, idb], [p2])
                kb.cp("act", UT.t[:].rearrange("p k t -> p (k t)"), p2b[:, 0:384], [p2], [UT])
                for hh in range(2):
                    pq = self.psum()
                    for kc in range(2):
                        kb.mm(pq.t[:, 0:384], UT.t[:, kc, :], wuq.t[:, kc, hh * 384:(hh + 1) * 384], kc == 0, kc == 1, [UT, wuq], [pq])
                    pqv = pq.t[:, 0:384].rearrange("p (h d) -> p h d", h=4)
                    qov = QO.t[:, hh * 4:(hh + 1) * 4, :]
                    kb.act(qov[:, :, 0:64], pqv[:, :, 0:64], AF.Identity, [pq], [QO], scale=MLA_SCALE)
                    t0 = QTMP.t[:, 0, hh * 128:(hh + 1) * 128].rearrange("p (h d) -> p h d", h=4)
                    t1 = QTMP.t[:, 1, hh * 128:(hh + 1) * 128].rearrange("p (h d) -> p h d", h=4)
                    kb.cp("act", t0, pqv[:, :, 64:96], [pq], [QTMP])
                    t0v = t0.rearrange("p h (a f e) -> p h a f e", a=2, f=2)
                    t1v = t1.rearrange("p h (a f e) -> p h a f e", a=2, f=2)
                    for a in range(2):
                        kb.cp("pool", t1v[:, :, a, 0, :], t0v[:, :, a, 1, :], [QTMP], [QTMP])
                        kb.cp("pool", t1v[:, :, a, 1, :], t0v[:, :, a, 0, :], [QTMP], [QTMP])
                    rc = rq[b].t[:, 0, hh * 128:(hh + 1) * 128].rearrange("p (h d) -> p h d", h=4)
                    rs = rq[b].t[:, 1, hh * 128:(hh + 1) * 128].rearrange("p (h d) -> p h d", h=4)
                    kb.tt("dve", t0, t0, rc, ALU.mult, [QTMP, rq[b]], [QTMP])
                    kb.tt("dve", t1, t1, rs, ALU.mult, [QTMP, rq[b]], [QTMP])
                    kb.tt("dve", qov[:, :, 64:96], t0, t1, ALU.add, [QTMP], [QO])
                for hh in range(2):
                    pk = self.psum()
                    kb.mm(pk.t[:], UT.t[:, 2, :], wkv.t[:, hh * 512:(hh + 1) * 512], True, True, [UT, wkv], [pk])
                    pkv = pk.t[:].rearrange("p (h d) -> p h d", h=4)
                    kb.cp("act", KO.t[:, hh * 4:(hh + 1) * 4, 0:64], pkv[:, :, 0:64], [pk], [KO])
                    kb.cp("dve", VO.t[:, hh * 4:(hh + 1) * 4, 0:64], pkv[:, :, 64:128], [pk], [VO])
                for h in range(8):
                    kb.cp("pool", KO.t[:, h, 64:96], KR.t[:, 0, :], [KR], [KO])
                kb.tt("dve", SQ.t[:], QO.t[:].rearrange("p h d -> p (h d)"), QO.t[:].rearrange("p h d -> p (h d)"), ALU.mult, [QO], [SQ])
                kb.red("dve", N2.t[:, 8:16], SQ.t[:].rearrange("p (h d) -> p h d", h=8), ALU.add, [SQ], [N2])
                kb.tt("dve", SQ.t[:], KO.t[:].rearrange("p h d -> p (h d)"), KO.t[:].rearrange("p h d -> p (h d)"), ALU.mult, [KO], [SQ])
                kb.red("dve", N2.t[:, 0:8], SQ.t[:].rearrange("p (h d) -> p h d", h=8), ALU.add, [SQ], [N2])
                kb.tt("dve", m16.t[:], m16.t[:], N2.t[:], ALU.max, [N2, m16], [m16])
                QKT = qkT[b]
                for which, src in ((0, KO), (1, QO)):
                    p3 = self.psum()
                    p3b = p3.t[:].bitcast(BF16)
                    for h in range(8):
                        kb.tr(p3b[0:96, h * 128:(h + 1) * 128], src.t[:, h, :], idb.t[:], [src, idb], [p3])
                    kb.cp("act" if which == 0 else "dve", QKT.t[:, which * 8:(which + 1) * 8, :].rearrange("p h t -> p (h t)"),
                          p3b[0:96, :], [p3], [QKT])
                kb.dma("sp", Dr["KT"][:, :, i * 128:(i + 1) * 128].rearrange("h d t -> d h t"), QKT.t[:, 0:8, :], [QKT], [Dk["KT"]])
                kb.dma("sp", Dr["QT"][:, :, i * 128:(i + 1) * 128].rearrange("h d t -> d h t"), QKT.t[:, 8:16, :], [QKT], [Dk["QT"]])
                kb.dma("sp", Dr["VA"][i * 128:(i + 1) * 128], VO.t[:], [VO], [Dk["VA"]])
                G = gkvo[b]
                for gi, (c0, cw) in enumerate(((O_GK, 512), (O_GK + 512, 512), (O_GK + 1024, 256))):
                    ps = tm(c0, cw)
                    o0 = c0 - O_GK
                    if gi == 0:
                        kb.cp("dve", G.t[:, 0:512], ps.t[:, 0:512], [ps], [G])
                    elif gi == 1:
                        kb.cp("dve", G.t[:, 512:768], ps.t[:, 0:256], [ps], [G])
                        kb.act(G.t[:, 768:1024], ps.t[:, 256:512], AF.Silu, [ps], [G])
                    else:
                        kb.act(G.t[:, 1024:1280], ps.t[:, 0:256], AF.Silu, [ps], [G])
                kb.dma("sp", Dr["GKVO"][i * 128:(i + 1) * 128, :], G.t[:], [G], [Dk["GKVO"]])
                GA = gate[b]
                for gi in range(6):
                    ps = tm(O_GATE + gi * 512, 512)
                    kb.act(GA.t[:, gi * 512:(gi + 1) * 512], ps.t[:], AF.Sigmoid, [ps], [GA])
                kb.dma("sp", Dr["GATE"][i * 128:(i + 1) * 128, :], GA.t[:], [GA], [Dk["GATE"]])
                ps = self.psum()
                for g in range(4):
                    fm(O_FN + g * 128, 128, ps, g * 128)
                kb.cp("dve", fT[b].t[:].rearrange("p g t -> p (g t)"), ps.t[:], [ps], [fT[b]])
                kb.dma("sp", Dr["UFT"][:, :, i * 128:(i + 1) * 128].rearrange("g c t -> c g t"), fT[b].t[:], [fT[b]], [Dk["UFT"]])
                psq = self.psum()
                psk = self.psum()
                for h in range(4):
                    fm(O_GQ + h * 64, 64, psq, h * 128)
                    fm(O_GK + h * 64, 64, psk, h * 128)
                kb.act(gqkT[b].t[:, 0:4, :].rearrange("p h t -> p (h t)"), psq.t[0:64, :], AF.Identity, [psq], [gqkT[b]], scale=GLA_SCALE)
                kb.cp("dve", gqkT[b].t[:, 4:8, :].rearrange("p h t -> p (h t)"), psk.t[0:64, :], [psk], [gqkT[b]])
                kb.dma("sp", Dr["GQT"][:, :, i * 128:(i + 1) * 128].rearrange("h d t -> d h t"), gqkT[b].t[:, 0:4, :], [gqkT[b]], [Dk["GQT"]])
                kb.dma("sp", Dr["GKT"][:, :, i * 128:(i + 1) * 128].rearrange("h d t -> d h t"), gqkT[b].t[:, 4:8, :], [gqkT[b]], [Dk["GKT"]])
                psg = self.psum()
                fm(O_GF, 16, psg, 0)
                fm(O_GB, 16, psg, 128)
                kb.cp("act", ugT[b].t[0:16, :, :].rearrange("p d t -> p (d t)"), psg.t[0:16, 0:256], [psg], [ugT[b]])
                psl = self.psum()
                for d in range(2):
                    kb.mm(psl.t[:, d * 256:(d + 1) * 256], ugT[b].t[:, d, :], wg.t[:, d, :], True, True, [ugT[b], wg], [psl])
                kb.act(lfe[b].t[:], psl.t[:], AF.Exp, [psl], [lfe[b]], scale=-1.0)
                kb.act(lfe[b].t[:], lfe[b].t[:], AF.Ln, [lfe[b]], [lfe[b]], bias=1.0)
                kb.ts("dve", lfb[b].t[:], lfe[b].t[:], -1.0 / 16.0, None, ALU.mult, None, [lfe[b]], [lfb[b]])
                kb.dma("sp", Dr["LFB"][i * 128:(i + 1) * 128, :], lfb[b].t[:], [lfb[b]], [Dk["LFB"]])
            pm = self.psum()
            kb.tr(pm.t[0:16, 0:128], m16.t[:], P["ident_f"].t[:], [m16, P["ident_f"]], [pm])
            mcol = kb.sb(st, "mcol", [16, 1], F32)
            mb = kb.sb(st, "mb", [16, 128], F32)
            kb.red("dve", mcol.t[:], pm.t[0:16, 0:128], ALU.max, [pm], [mcol])
            kb.ts("dve", mb.t[:], P["ones_f"].t[0:16, :], mcol.t[:, 0:1], None, ALU.mult, None, [mcol, P["ones_f"]], [mb])
            pm2 = self.psum()
            kb.mm(pm2.t[:, 0:16], mb.t[:], P["ident_f"].t[0:16, 0:16], True, True, [mb, P["ident_f"]], [pm2])
            nbt = kb.sb(st, "nbt", [128, 16], F32)
            kb.cp("act", nbt.t[:], pm2.t[:, 0:16], [pm2], [nbt])
            kb.tt("dve", nbt.t[:, 0:8], nbt.t[:, 0:8], nbt.t[:, 8:16], ALU.mult, [nbt], [nbt])
            kb.act(nbt.t[:, 0:8], nbt.t[:, 0:8], AF.Sqrt, [nbt], [nbt])
            kb.ts("dve", P["nb"].t[:], nbt.t[:, 0:8], -1.0, None, ALU.mult, None, [nbt], [P["nb"]])
            kb.S.flush()

    def phase_attn(self, l):
        kb, P, Dr, Dk = self.kb, self.P, self.Dr, self.Dk
        with contextlib.ExitStack() as st:
            va = kb.sb(st, "va", [128, NT, 520], BF16)
            kb.dma("pool", va.t[:], Dr["VA"].rearrange("(n p) h e -> p n (h e)", p=128), [Dk["VA"]], [va])
            kt_ = [kb.sb(st, "ktb%d" % j, [96, T], BF16) for j in range(2)]
            qt_ = [kb.sb(st, "qtb%d" % j, [96, T], BF16) for j in range(2)]
            pt = [kb.sb(st, "pt%d" % j, [128, 512], BF16) for j in range(4)]
            osb = [kb.sb(st, "osb%d" % j, [65, 512], F32) for j in range(2)]
            rec = [kb.sb(st, "rec%d" % j, [64, 512], F32) for j in range(2)]
            om = [kb.sb(st, "om%d" % j, [64, 512], BF16) for j in range(2)]
            ones = P["ones_f"]
            nb = P["nb"]
            cnt = 0
            blk = 0
            for h in range(8):
                KT, QT = kt_[h % 2], qt_[h % 2]
                kb.dma("sp", KT.t[:], Dr["KT"][h], [Dk["KT"]], [KT])
                kb.dma("sp", QT.t[:], Dr["QT"][h], [Dk["QT"]], [QT])
                blocks = [(0, 256, [0, 1])] + [(CTX + qb * 512, 512, list(range(NT))) for qb in range(8)]
                for (q0, qn, keys) in blocks:
                    po = self.psum(pin=True)
                    pend = None
                    seq = []
                    for kt in keys:
                        ps = self.psum()
                        kb.mm(ps.t[:, 0:qn], KT.t[:, kt * 128:(kt + 1) * 128], QT.t[:, q0:q0 + qn], True, True, [KT, QT], [ps])
                        seq.append((kt, ps))
                        if len(seq) >= 2:
                            self._attn_pv(seq.pop(0), keys, po, pt, va, nb, h, qn, cnt)
                            cnt += 1
                    while seq:
                        self._attn_pv(seq.pop(0), keys, po, pt, va, nb, h, qn, cnt)
                        cnt += 1
                    O, R, OM = osb[blk % 2], rec[blk % 2], om[blk % 2]
                    blk += 1
                    kb.cp("act", O.t[:, 0:qn], po.t[0:65, 0:qn], [po], [O])
                    self.unpin(po)
                    pb = self.psum()
                    kb.mm(pb.t[0:64, 0:qn], ones.t[64:65, 0:64], O.t[64:65, 0:qn], True, True, [ones, O], [pb])
                    kb.recip(R.t[:, 0:qn], pb.t[0:64, 0:qn], [pb], [R])
                    kb.tt("pool", OM.t[:, 0:qn], O.t[0:64, 0:qn], R.t[:, 0:qn], ALU.mult, [O, R], [OM])
                    kb.dma("sp", Dr["OMT"][h, :, q0:q0 + qn], OM.t[:, 0:qn], [OM], [Dk["OMT"]])
            kb.S.flush()

    def _attn_pv(self, item, keys, po, pt, va, nb, h, qn, cnt):
        kb = self.kb
        kt, ps = item
        PT = pt[cnt % 4]
        kb.act(PT.t[:, 0:qn], ps.t[:, 0:qn], AF.Exp, [ps, nb], [PT], bias=nb.t[:, h:h + 1], scale=1.0)
        kb.mm(po.t[0:65, 0:qn], va.t[:, kt, h * 65:(h + 1) * 65], PT.t[:, 0:qn], kt == keys[0], kt == keys[-1], [va, PT], [po])

    def phase_fnet(self, l):
        kb, P, C, Dr, Dk = self.kb, self.P, self.C, self.Dr, self.Dk
        with contextlib.ExitStack() as st:
            A = kb.sb(st, "fA", [128, NT, 512], BF16)
            Bm = kb.sb(st, "fB", [128, NT, 512], BF16)
            cs = kb.sb(st, "cs", [128, 256], BF16)
            kb.dma("sp", cs.t[:], C["cs128"], [], [cs])
            uf = [kb.sb(st, "uf%d" % j, [128, 4, 128], BF16) for j in range(2)]
            import os
            for i in range(int(os.environ.get("FN_TILES", NT))):
                U = uf[i % 2]
                kb.dma("sp", U.t[:], Dr["UFT"][:, :, i * 128:(i + 1) * 128].rearrange("g c t -> c g t"), [Dk["UFT"]], [U])
                for half in range(2):
                    ps = self.psum()
                    for g2 in range(2):
                        g = half * 2 + g2
                        kb.mm(ps.t[:, g2 * 256:(g2 + 1) * 256], U.t[:, g, :], cs.t[:], True, True, [U, cs], [ps])
                    psv = ps.t[:].rearrange("p (g x m) -> p g x m", g=2, x=2)
                    kb.cp("act", A.t[:, i, half * 256:(half + 1) * 256].rearrange("p (g m) -> p g m", g=2), psv[:, :, 0, :], [ps], [A])
                    kb.cp("dve", Bm.t[:, i, half * 256:(half + 1) * 256].rearrange("p (g m) -> p g m", g=2), psv[:, :, 1, :], [ps], [Bm])
            dc = [kb.sb(st, "dc%d" % j, [128, 32, 128], BF16) for j in range(2)]
            ds = [kb.sb(st, "ds%d" % j, [128, 32, 128], BF16) for j in range(2)]
            y = [kb.sb(st, "fy%d" % j, [128, 512], BF16) for j in range(2)]
            oft = [kb.sb(st, "oft%d" % j, [128, 4, 128], BF16) for j in range(2)]
            idb = P["ident_b"]
            jobs = [("c", kt) for kt in range(2)] + [("l", kt) for kt in range(32)]
            import os
            if os.environ.get("FN_PART") == "1":
                jobs = []
            if os.environ.get("FN_PART") == "2":
                jobs = jobs[:2]
            for n, (kind, kt) in enumerate(jobs):
                DC, DS, Y, OF = dc[n % 2], ds[n % 2], y[n % 2], oft[n % 2]
                if kind == "c":
                    ntt, t0, tok0 = 2, 0, kt * 128
                    kb.dma("sp", DC.t[:, 0:2, :], C["dft_c2"][kt], [], [DC])
                    kb.dma("pool", DS.t[:, 0:2, :], C["dft_s2"][kt], [], [DS])
                else:
                    ntt, t0, tok0 = 32, 2, CTX + kt * 128
                    kb.dma("sp", DC.t[:], C["dft_c"][kt], [], [DC])
                    kb.dma("pool", DS.t[:], C["dft_s"][kt], [], [DS])
                ps = self.psum()
                for tt in range(ntt):
                    kb.mm(ps.t[:], DC.t[:, tt, :], A.t[:, t0 + tt, :], tt == 0, False, [DC, A], [ps])
                    kb.mm(ps.t[:], DS.t[:, tt, :], Bm.t[:, t0 + tt, :], False, tt == ntt - 1, [DS, Bm], [ps])
                kb.cp("act", Y.t[:], ps.t[:], [ps], [Y])
                p2 = self.psum()
                p2b = p2.t[:].bitcast(BF16)
                for g in range(4):
                    kb.tr(p2b[:, g * 128:(g + 1) * 128], Y.t[:, g * 128:(g + 1) * 128], idb.t[:], [Y, idb], [p2])
                kb.cp("dve", OF.t[:].rearrange("p g t -> p (g t)"), p2b[:, 0:512], [p2], [OF])
                kb.dma("sp", Dr["OFT"][:, :, tok0:tok0 + 128].rearrange("g m t -> m g t"), OF.t[:], [OF], [Dk["OFT"]])
            kb.S.flush()

    def phase_gla(self, l):
        kb, P, C, W, Dr, Dk = self.kb, self.P, self.C, self.W, self.Dr, self.Dk
        with contextlib.ExitStack() as st:
            tri = kb.sb(st, "tri", [128, 2, 128], F32)
            mask4 = kb.sb(st, "mask4", [128, 2, 512], F32)
            gn = kb.sb(st, "gn", [128, 512], F32)
            for d in range(2):
                kb.dma("sp", tri.t[:, d, :], C["tri_f"][d], [], [tri])
                kb.dma("sp", mask4.t[:, d, :], C["mask4"][d].rearrange("p h t -> p (h t)"), [], [mask4])
            kb.dma("sp", gn.t[:], W["gla_norm_g"][l].partition_broadcast(128), [], [gn])
            S32 = kb.sb(st, "S32", [64, 4, 128], F32)
            Sb = kb.sb(st, "Sb", [64, 4, 128], BF16)
            NB2 = 2
            def mk(name, shp, dt):
                return [kb.sb(st, "%s%d" % (name, j), shp, dt) for j in range(NB2)]
            qT = mk("gqT", [64, 4, 128], BF16)
            kT = mk("gkT", [64, 4, 128], BF16)
            gk = mk("ggk", [128, 1280], BF16)
            lf = mk("glf", [128, 256], F32)
            gtok = mk("gtok", [128, 256], F32)
            e1 = mk("ge1", [128, 256], F32)
            khat = mk("khat", [128, 256], BF16)
            eq = mk("geq", [64, 4, 128], F32)
            ek = mk("gek", [64, 4, 128], F32)
            qtl = mk("qtl", [64, 4, 128], BF16)
            ktl = mk("ktl", [64, 4, 128], BF16)
            at = mk("gat", [128, 512], BF16)
            ofw = mk("gofw", [128, 512], F32)
            o32 = mk("go32", [128, 512], F32)
            sqo = mk("gsq", [128, 512], F32)
            st4 = mk("gst4", [128, 8], F32)
            ob = mk("gob", [128, 512], BF16)
            ogt = mk("gogt", [128, 4, 128], BF16)
            idb = P["ident_b"]
            for d in range(2):
                order = list(range(NT)) if d == 0 else [1, 0] + list(range(NT - 1, 1, -1))
                last = 127 if d == 0 else 0
                kb.memset("dve", S32.t[:], 0.0, [], [S32])
                kb.memset("pool", Sb.t[:], 0.0, [], [Sb])
                for n, i in enumerate(order):
                    b = n % NB2
                    sl = slice(i * 128, (i + 1) * 128)
                    kb.dma("sp", qT[b].t[:], Dr["GQT"][:, :, sl].rearrange("h d t -> d h t"), [Dk["GQT"]], [qT[b]])
                    kb.dma("sp", kT[b].t[:], Dr["GKT"][:, :, sl].rearrange("h d t -> d h t"), [Dk["GKT"]], [kT[b]])
                    kb.dma("sp", gk[b].t[:], Dr["GKVO"][sl, :], [Dk["GKVO"]], [gk[b]])
                    kb.dma("sp", lf[b].t[:], Dr["LFB"][sl, d * 256:(d + 1) * 256], [Dk["LFB"]], [lf[b]])
                    if d == 1:
                        kb.dma("sp", ofw[b].t[:], Dr["OFW"][sl, :], [Dk["OFW"]], [ofw[b]])
                    LF = lf[b]
                    pg = self.psum()
                    kb.mm(pg.t[:, 0:256], tri.t[:, d, :], LF.t[:], True, True, [tri, LF], [pg])
                    kb.mm(pg.t[:, 256:512], P["ones_f"].t[:], LF.t[:], True, True, [P["ones_f"], LF], [pg])
                    kb.cp("act", gtok[b].t[:], pg.t[:, 0:256], [pg], [gtok[b]])
                    kb.tt("dve", e1[b].t[:], pg.t[:, 256:512], gtok[b].t[:], ALU.subtract, [pg, gtok[b]], [e1[b]])
                    kb.act(e1[b].t[:], e1[b].t[:], AF.Exp, [e1[b]], [e1[b]])
                    kb.tt("dve", khat[b].t[:], gk[b].t[:, 0:256], e1[b].t[:], ALU.mult, [gk[b], e1[b]], [khat[b]])
                    pf = self.psum()
                    for h in range(4):
                        kb.mm(pf.t[0:64, h * 128:(h + 1) * 128], LF.t[:, h * 64:(h + 1) * 64], tri.t[:, d, :], True, True, [LF, tri], [pf])
                    kb.act(eq[b].t[:].rearrange("p h t -> p (h t)"), pf.t[0:64, :], AF.Exp, [pf], [eq[b]])
                    kb.act(ek[b].t[:].rearrange("p h t -> p (h t)"), pf.t[0:64, :], AF.Exp, [pf], [ek[b]], scale=-1.0)
                    kb.tt("dve", qtl[b].t[:], qT[b].t[:], eq[b].t[:], ALU.mult, [qT[b], eq[b]], [qtl[b]])
                    kb.tt("pool", ktl[b].t[:], kT[b].t[:], ek[b].t[:], ALU.mult, [kT[b], ek[b]], [ktl[b]])
                    pa = self.psum()
                    for h in range(4):
                        kb.mm(pa.t[:, h * 128:(h + 1) * 128], ktl[b].t[:, h, :], qtl[b].t[:, h, :], True, True, [ktl[b], qtl[b]], [pa])
                    kb.tt("dve", at[b].t[:], pa.t[:], mask4.t[:, d, :], ALU.mult, [pa, mask4], [at[b]])
                    po = self.psum()
                    for h in range(4):
                        kb.mm(po.t[:, h * 128:(h + 1) * 128], qtl[b].t[:, h, :], Sb.t[:, h, :], True, False, [qtl[b], Sb], [po])
                        kb.mm(po.t[:, h * 128:(h + 1) * 128], at[b].t[:, h * 128:(h + 1) * 128],
                              gk[b].t[:, 256 + h * 128:256 + (h + 1) * 128], False, True, [at[b], gk[b]], [po])
                    pS = self.psum()
                    for h in range(4):
                        kb.mm(pS.t[0:64, h * 128:(h + 1) * 128], khat[b].t[:, h * 64:(h + 1) * 64],
                              gk[b].t[:, 256 + h * 128:256 + (h + 1) * 128], True, True, [khat[b], gk[b]], [pS])
                    for h in range(4):
                        kb.stt("dve", S32.t[:, h, :], S32.t[:, h, :], eq[b].t[:, h, last:last + 1], pS.t[0:64, h * 128:(h + 1) * 128],
                               ALU.mult, ALU.add, [S32, eq[b], pS], [S32])
                    kb.cp("pool", Sb.t[:], S32.t[:], [S32], [Sb])
                    if d == 0:
                        kb.cp("act", o32[b].t[:], po.t[:], [po], [o32[b]])
                        kb.dma("sp", Dr["OFW"][sl, :], o32[b].t[:], [o32[b]], [Dk["OFW"]])
                    else:
                        O, SQ, S4 = o32[b], sqo[b], st4[b]
                        kb.tt("dve", O.t[:], po.t[:], ofw[b].t[:], ALU.add, [po, ofw[b]], [O])
                        kb.tt("pool", SQ.t[:], O.t[:], O.t[:], ALU.mult, [O], [SQ])
                        kb.red("dve", S4.t[:, 0:4], SQ.t[:].rearrange("p (h e) -> p h e", h=4), ALU.add, [SQ], [S4])
                        kb.ts("dve", S4.t[:, 0:4], S4.t[:, 0:4], 1.0 / 128, EPS, ALU.mult, ALU.add, [S4], [S4])
                        kb.act(S4.t[:, 0:4], S4.t[:, 0:4], AF.Sqrt, [S4], [S4])
                        kb.recip(S4.t[:, 4:8], S4.t[:, 0:4], [S4], [S4])
                        for h in range(4):
                            kb.stt("dve", O.t[:, h * 128:(h + 1) * 128], O.t[:, h * 128:(h + 1) * 128], S4.t[:, 4 + h:5 + h],
                                   gn.t[:, h * 128:(h + 1) * 128], ALU.mult, ALU.mult, [O, S4, gn], [O])
                        kb.tt("pool", ob[b].t[:], O.t[:], gk[b].t[:, 768:1280], ALU.mult, [O, gk[b]], [ob[b]])
                        p2 = self.psum()
                        p2b = p2.t[:].bitcast(BF16)
                        for g in range(4):
                            kb.tr(p2b[:, g * 128:(g + 1) * 128], ob[b].t[:, g * 128:(g + 1) * 128], idb.t[:], [ob[b], idb], [p2])
                        kb.cp("act", ogt[b].t[:].rearrange("p g t -> p (g t)"), p2b[:, 0:512], [p2], [ogt[b]])
                        kb.dma("sp", Dr["OGT"][:, :, sl].rearrange("g m t -> m g t"), ogt[b].t[:], [ogt[b]], [Dk["OGT"]])
            kb.S.flush()

    def phase_merge(self, l):
        kb, P, W, Dr, Dk = self.kb, self.P, self.W, self.Dr, self.Dk
        with contextlib.ExitStack() as st:
            rows = self.mod_rows(st, l, 1, names=("G",))
            wbm = kb.sb(st, "wbm", [64, 8, D], BF16)
            wbf = kb.sb(st, "wbf", [128, 4, D], BF16)
            wbg = kb.sb(st, "wbg", [128, 4, D], BF16)
            wo = kb.sb(st, "wo", [128, 8, D], BF16)
            kb.dma("pool", wbm.t[:], W["w_br_mla"][l].rearrange("(h d) n -> d h n", d=64), [], [wbm])
            kb.dma("pool", wbf.t[:], W["w_br_fnet"][l].rearrange("(k p) n -> p k n", p=128), [], [wbf])
            kb.dma("pool", wbg.t[:], W["w_br_gla"][l].rearrange("(k p) n -> p k n", p=128), [], [wbg])
            kb.dma("pool", wo.t[:], W["w_o"][l].rearrange("(k p) n -> p k n", p=128), [], [wo])
            NB2 = 2
            def mk(name, shp, dt):
                return [kb.sb(st, "%s%d" % (name, j), shp, dt) for j in range(NB2)]
            omT = mk("momT", [64, 8, 128], BF16)
            ofT = mk("mofT", [128, 4, 128], BF16)
            ogT = mk("mogT", [128, 4, 128], BF16)
            ga = mk("mga", [128, 3072], BF16)
            xt = mk("mxt", [128, D], F32)
            y32 = mk("my32", [128, D], F32)
            t32 = mk("mt32", [128, D], F32)
            yb = mk("myb", [128, D], BF16)
            yT = mk("myT", [128, 8, 128], BF16)
            xn = mk("mxn", [128, D], F32)
            idb = P["ident_b"]
            for i in range(NT):
                b = i % NB2
                sl = slice(i * 128, (i + 1) * 128)
                G = rows["Gc" if i < 2 else "Gl"]
                kb.dma("sp", omT[b].t[:], Dr["OMT"][:, :, sl].rearrange("h d t -> d h t"), [Dk["OMT"]], [omT[b]])
                kb.dma("sp", ofT[b].t[:], Dr["OFT"][:, :, sl].rearrange("g m t -> m g t"), [Dk["OFT"]], [ofT[b]])
                kb.dma("sp", ogT[b].t[:], Dr["OGT"][:, :, sl].rearrange("g m t -> m g t"), [Dk["OGT"]], [ogT[b]])
                kb.dma("sp", ga[b].t[:], Dr["GATE"][sl, :], [Dk["GATE"]], [ga[b]])
                kb.dma("sp", xt[b].t[:], Dr["XR"][sl, :], [Dk["XR"]], [xt[b]])
                for half in range(2):
                    cs_ = slice(half * 512, (half + 1) * 512)
                    pm = self.psum()
                    for h in range(8):
                        kb.mm(pm.t[:], omT[b].t[:, h, :], wbm.t[:, h, cs_], h == 0, h == 7, [omT[b], wbm], [pm])
                    pf = self.psum()
                    for k in range(4):
                        kb.mm(pf.t[:], ofT[b].t[:, k, :], wbf.t[:, k, cs_], k == 0, k == 3, [ofT[b], wbf], [pf])
                    pg = self.psum()
                    for k in range(4):
                        kb.mm(pg.t[:], ogT[b].t[:, k, :], wbg.t[:, k, cs_], k == 0, k == 3, [ogT[b], wbg], [pg])
                    Y, T32 = y32[b], t32[b]
                    kb.tt("dve", Y.t[:, cs_], pm.t[:], ga[b].t[:, half * 512:(half + 1) * 512], ALU.mult, [pm, ga[b]], [Y])
                    kb.tt("dve", T32.t[:, cs_], pf.t[:], ga[b].t[:, 1024 + half * 512:1024 + (half + 1) * 512], ALU.mult, [pf, ga[b]], [T32])
                    kb.tt("pool", Y.t[:, cs_], Y.t[:, cs_], T32.t[:, cs_], ALU.add, [Y, T32], [Y])
                    kb.tt("dve", T32.t[:, cs_], pg.t[:], ga[b].t[:, 2048 + half * 512:2048 + (half + 1) * 512], ALU.mult, [pg, ga[b]], [T32])
                    kb.tt("pool", yb[b].t[:, cs_], Y.t[:, cs_], T32.t[:, cs_], ALU.add, [Y, T32], [yb[b]])
                p2 = self.psum()
                p2b = p2.t[:].bitcast(BF16)
                for kc in range(8):
                    kb.tr(p2b[:, kc * 128:(kc + 1) * 128], yb[b].t[:, kc * 128:(kc + 1) * 128], idb.t[:], [yb[b], idb], [p2])
                kb.cp("act", yT[b].t[:].rearrange("p k t -> p (k t)"), p2b, [p2], [yT[b]])
                for half in range(2):
                    cs_ = slice(half * 512, (half + 1) * 512)
                    pz = self.psum()
                    for kc in range(8):
                        kb.mm(pz.t[:], yT[b].t[:, kc, :], wo.t[:, kc, cs_], kc == 0, kc == 7, [yT[b], wo], [pz])
                    kb.tt("dve", xn[b].t[:, cs_], pz.t[:], G.t[:, cs_], ALU.mult, [pz, G], [xn[b]])
                    kb.tt("pool", xn[b].t[:, cs_], xn[b].t[:, cs_], xt[b].t[:, cs_], ALU.add, [xn[b], xt[b]], [xn[b]])
                kb.dma("sp", Dr["XR"][sl, :], xn[b].t[:], [xn[b]], [Dk["XR"]])
            kb.S.flush()

    def phase_moe(self, l):
        kb, P, W, C, Dr, Dk = self.kb, self.P, self.W, self.C, self.Dr, self.Dk
        is_last = (l == self.nl - 1)
        idb = P["ident_b"]
        with contextlib.ExitStack() as st0:
            DSTI = kb.sb(st0, "DSTI", [128, NT, 4], I32)
            G4 = kb.sb(st0, "G4", [128, NT, 4], F32)
            IDXW = kb.sb(st0, "IDXW", [128, NBLK, 8], I32)
            EBI = kb.sb(st0, "EBI", [128, NBLK], I32)
            with contextlib.ExitStack() as st:
                rows = self.mod_rows(st, l, 2, names=("A", "S"))
                rw = kb.sb(st, "rw", [128, 8, NE], F32)
                rb = kb.sb(st, "rb", [1, NE], F32)
                trx = kb.sb(st, "trx", [128, 128], BF16)
                jv = kb.sb(st, "jv", [128, NBLK], F32)
                kp = kb.sb(st, "kp", [128, 8], F32)
                kb.dma("sp", rw.t[:], W["router_w"][l].rearrange("(k p) e -> p k e", p=128), [], [rw])
                kb.dma("sp", rb.t[:], W["router_b"][l].rearrange("(o e) -> o e", o=1), [], [rb])
                kb.dma("sp", trx.t[:], C["tri_x"], [], [trx])
                kb.dma("sp", jv.t[:], C["jv"], [], [jv])
                kb.dma("sp", kp.t[:], C["kp"], [], [kp])
                LG = kb.sb(st, "LG", [128, NT, NE], F32)
                GF = kb.sb(st, "GF", [128, NT, NE], F32)
                POS = kb.sb(st, "POS", [128, NT, NE], F32)
                TOP = kb.sb(st, "TOP", [128, NT, 8], F32)
                cnt = kb.sb(st, "cnt", [128, NE], F32)
                kb.memset("dve", cnt.t[:], 0.0, [], [cnt])
                NB2 = 2
                def mk(name, shp, dt):
                    return [kb.sb(st, "%s%d" % (name, j), shp, dt) for j in range(NB2)]
                xt = mk("ext", [128, D], F32)
                junk = kb.sb(st, "ejunk", [128, D], F32)
                ssq = mk("essq", [128, 8], F32)
                h32 = mk("eh32", [128, D], F32)
                hb = mk("ehb", [128, D], BF16)
                h2T = mk("eh2T", [128, 8, 128], F32)
                sm = mk("esm", [128, 4, NE], F32)
                sc = mk("esc", [128, 8], F32)
                mkb = mk("emkb", [128, NE], BF16)
                for i in range(NT):
                    b = i % NB2
                    sl = slice(i * 128, (i + 1) * 128)
                    kb.dma("sp", xt[b].t[:], Dr["XR"][sl, :], [Dk["XR"]], [xt[b]])
                    kb.memset("pool", ssq[b].t[:], 0.0, [], [ssq[b]])
                    self.norm_mod(xt[b], rows, i, junk, ssq[b], h32[b], hb[b], full32=True)
                    kb.dma("sp", Dr["H2B"][sl, :], hb[b].t[:], [hb[b]], [Dk["H2B"]])
                    for half in range(2):
                        pt_ = self.psum()
                        for k4 in range(4):
                            kc = half * 4 + k4
                            kb.tr(pt_.t[:, k4 * 128:(k4 + 1) * 128], h32[b].t[:, kc * 128:(kc + 1) * 128], P["ident_f"].t[:],
                                  [h32[b], P["ident_f"]], [pt_])
                        kb.cp("act" if half == 0 else "dve", h2T[b].t[:, half * 4:(half + 1) * 4, :].rearrange("p k t -> p (k t)"),
                              pt_.t[:], [pt_], [h2T[b]])
                    pl = self.psum()
                    for kc in range(8):
                        kb.mm(pl.t[:, 0:NE], h2T[b].t[:, kc, :], rw.t[:, kc, :], kc == 0, False, [h2T[b], rw], [pl])
                    kb.mm(pl.t[:, 0:NE], P["ones_f"].t[0:1, :], rb.t[0:1, :], False, True, [P["ones_f"], rb], [pl])
                    kb.cp("act", LG.t[:, i, :], pl.t[:, 0:NE], [pl], [LG])
                    kb.op("dve", lambda e, o=TOP.t[:, i, :], a=LG.t[:, i, :]: e.max(out=o, in_=a), [LG], [TOP])
                    SM, SC = sm[b], sc[b]
                    kb.ts("dve", SM.t[:, 0, :], LG.t[:, i, :], TOP.t[:, i, 3:4], None, ALU.is_ge, None, [LG, TOP], [SM])
                    kb.ts("dve", SC.t[:, 0:1], TOP.t[:, i, 0:1], -1.0, None, ALU.mult, None, [TOP], [SC])
                    kb.act(SM.t[:, 1, :], LG.t[:, i, :], AF.Exp, [LG, SC], [SM], bias=SC.t[:, 0:1], scale=1.0)
                    kb.tt("dve", SM.t[:, 2, :], SM.t[:, 1, :], SM.t[:, 0, :], ALU.mult, [SM], [SM])
                    kb.red("dve", SC.t[:, 1:2], SM.t[:, 2, :], ALU.add, [SM], [SC])
                    kb.recip(SC.t[:, 2:3], SC.t[:, 1:2], [SC], [SC])
                    kb.ts("dve", GF.t[:, i, :], SM.t[:, 2, :], SC.t[:, 2:3], None, ALU.mult, None, [SM, SC], [GF])
                    kb.cp("pool", mkb[b].t[:], SM.t[:, 0, :], [SM], [mkb[b]])
                    pp = self.psum()
                    kb.mm(pp.t[:, 0:NE], trx.t[:], mkb[b].t[:], True, True, [trx, mkb[b]], [pp])
                    kb.mm(pp.t[:, NE:2 * NE], P["ones_b"].t[:], mkb[b].t[:], True, True, [P["ones_b"], mkb[b]], [pp])
                    kb.tt("dve", POS.t[:, i, :], pp.t[:, 0:NE], cnt.t[:], ALU.add, [pp, cnt], [POS])
                    kb.tt("dve", cnt.t[:], pp.t[:, NE:2 * NE], cnt.t[:], ALU.add, [pp, cnt], [cnt])
                nbk = kb.sb(st, "nbk", [128, NE], F32)
                pend = kb.sb(st, "pend", [128, NE], F32)
                pstart = kb.sb(st, "pstart", [128, NE], F32)
                eb = kb.sb(st, "eb", [128, NBLK], F32)
                idxf = kb.sb(st, "idxf", [128, NBLK, 8], F32)
                kb.memset("dve", nbk.t[:], 0.0, [], [nbk])
                for j in range(T // BS + 1):
                    kb.stt("dve", nbk.t[:], cnt.t[:], float(j * BS), nbk.t[:], ALU.is_gt, ALU.add, [cnt, nbk], [nbk])
                kb.ts("dve", nbk.t[:], nbk.t[:], float(BS), None, ALU.mult, None, [nbk], [nbk])
                kb.cp("dve", pend.t[:, 0:1], nbk.t[:, 0:1], [nbk], [pend])
                for e_ in range(1, NE):
                    kb.tt("dve", pend.t[:, e_:e_ + 1], pend.t[:, e_ - 1:e_], nbk.t[:, e_:e_ + 1], ALU.add, [pend, nbk], [pend])
                kb.tt("dve", pstart.t[:], pend.t[:], nbk.t[:], ALU.subtract, [pend, nbk], [pstart])
                kb.memset("dve", eb.t[:], 0.0, [], [eb])
                for e_ in range(NE):
                    kb.stt("dve", eb.t[:], jv.t[:], pend.t[:, e_:e_ + 1], eb.t[:], ALU.is_ge, ALU.add, [jv, pend, eb], [eb])
                kb.ts("dve", eb.t[:], eb.t[:], float(NE - 1), None, ALU.min, None, [eb], [eb])
                kb.ts("dve", kp.t[:], kp.t[:], float(l * NE * D), None, ALU.add, None, [kp], [kp])
                for kc in range(8):
                    kb.ts("dve", idxf.t[:, :, kc], eb.t[:], 1024.0, kp.t[:, kc:kc + 1], ALU.mult, ALU.add, [eb, kp], [idxf])
                kb.cp("dve", IDXW.t[:], idxf.t[:], [idxf], [IDXW])
                kb.ts("dve", eb.t[:], eb.t[:], float(l * NE), None, ALU.add, None, [eb], [eb])
                kb.cp("dve", EBI.t[:], eb.t[:], [eb], [EBI])
                dstf = mk("edstf", [128, 4], F32)
                hs = mk("ehs", [128, D], BF16)
                for i in range(NT):
                    b = i % NB2
                    sl = slice(i * 128, (i + 1) * 128)
                    SM = sm[b]
                    kb.tt("dve", SM.t[:, 3, :], POS.t[:, i, :], pstart.t[:], ALU.add, [POS, pstart], [SM])
                    for k in range(4):
                        kb.ts("dve", SM.t[:, 0, :], LG.t[:, i, :], TOP.t[:, i, k:k + 1], None, ALU.is_equal, None, [LG, TOP], [SM])
                        kb.tt("dve", SM.t[:, 1, :], SM.t[:, 0, :], SM.t[:, 3, :], ALU.mult, [SM], [SM])
                        kb.red("dve", dstf[b].t[:, k:k + 1], SM.t[:, 1, :], ALU.add, [SM], [dstf[b]])
                        kb.tt("dve", SM.t[:, 2, :], SM.t[:, 0, :], GF.t[:, i, :], ALU.mult, [SM, GF], [SM])
                        kb.red("dve", G4.t[:, i, k:k + 1], SM.t[:, 2, :], ALU.add, [SM], [G4])
                    kb.cp("dve", DSTI.t[:, i, :], dstf[b].t[:], [dstf[b]], [DSTI])
                    kb.dma("sp", hs[b].t[:], Dr["H2B"][sl, :], [Dk["H2B"]], [hs[b]])
                    for k in range(4):
                        kb.scatter(Dr["XS"], hs[b].t[:], DSTI.t[:, i, k:k + 1], [hs[b], DSTI], [Dk["XS"]], NROWS - 1)
                if "DBG_DST" in self.dbg:
                    kb.dma("sp", Dr["DBG_DST"], DSTI.t[:].rearrange("p n k -> p (n k)"), [DSTI], [Dk["DBG_DST"]])
                    kb.dma("sp", Dr["DBG_G4"], G4.t[:].rearrange("p n k -> p (n k)"), [G4], [Dk["DBG_G4"]])
                    kb.dma("sp", Dr["DBG_EB"], EBI.t[:], [EBI], [Dk["DBG_EB"]])
                    kb.dma("sp", Dr["DBG_LG"], LG.t[:].rearrange("p n k -> p (n k)"), [LG], [Dk["DBG_LG"]])
                    kb.dma("sp", Dr["DBG_CNT"], cnt.t[:], [cnt], [Dk["DBG_CNT"]])
                kb.S.flush()
            with contextlib.ExitStack() as st:
                wup = [kb.sb(st, "wup%d" % j, [128, 8, 2 * D], BF16) for j in range(2)]
                wdn = [kb.sb(st, "wdn%d" % j, [128, 8, D], BF16) for j in range(2)]
                wupk = [[Tok() for _ in range(8)] for _ in range(2)]
                wdnk = [[Tok() for _ in range(8)] for _ in range(2)]
                bu = [kb.sb(st, "bu%d" % j, [2, 2 * D], BF16) for j in range(2)]
                bd = [kb.sb(st, "bd%d" % j, [2, D], BF16) for j in range(2)]
                NB2 = 2
                def mk(name, shp, dt):
                    return [kb.sb(st, "%s%d" % (name, j), shp, dt) for j in range(NB2)]
                xs = mk("xs", [128, D], BF16)
                xT = mk("xT", [128, 8, 128], BF16)
                gl = mk("gl", [128, 512], F32)
                li = mk("li", [128, 512], F32)
                sg = mk("sg", [128, 512], F32)
                ab = mk("ab", [128, D], BF16)
                aT = mk("aT", [128, 8, 128], BF16)
                yb = mk("yb", [128, D], F32)
                wup_src = W["exp_w_up"].rearrange("l e k n -> (l e k) n")
                wdn_src = W["exp_w_down"].rearrange("l e k n -> (l e k) n")
                bup_src = W["exp_b_up"].rearrange("l e n -> (l e) n")
                bdn_src = W["exp_b_down"].rearrange("l e n -> (l e) n")
                nlw = W["exp_w_up"].shape[0]
                ones1 = P["ones_b"].t[0:1, :]

                def load_w(j):
                    jb = j % 2
                    for kc in range(8):
                        kb.gather(wup[jb].t[:, kc, :], wup_src, IDXW.t[:, j, kc:kc + 1], [IDXW], [wupk[jb][kc]], nlw * NE * D - 1)
                    for kc in range(8):
                        kb.gather(wdn[jb].t[:, kc, :], wdn_src, IDXW.t[:, j, kc:kc + 1], [IDXW], [wdnk[jb][kc]], nlw * NE * D - 1)
                    kb.gather(bu[jb].t[:], bup_src, EBI.t[0:2, j:j + 1], [EBI], [bu[jb]], nlw * NE - 1)
                    kb.gather(bd[jb].t[:], bdn_src, EBI.t[0:2, j:j + 1], [EBI], [bd[jb]], nlw * NE - 1)

                def load_x(n):
                    kb.dma("sp", xs[n % NB2].t[:], Dr["XS"][n * 128:(n + 1) * 128, :], [Dk["XS"]], [xs[n % NB2]])

                load_w(0)
                load_x(0)
                for j in range(NBLK):
                    jb = j % 2
                    WU, WD, BU, BD = wup[jb], wdn[jb], bu[jb], bd[jb]
                    if j + 1 < NBLK:
                        load_w(j + 1)
                    for sub in range(SUB):
                        n = j * SUB + sub
                        b = n % NB2
                        r0 = n * 128
                        if n + 1 < NBLK * SUB:
                            load_x(n + 1)
                        p2 = self.psum()
                        p2b = p2.t[:].bitcast(BF16)
                        for kc in range(8):
                            kb.tr(p2b[:, kc * 128:(kc + 1) * 128], xs[b].t[:, kc * 128:(kc + 1) * 128], idb.t[:], [xs[b], idb], [p2])
                        kb.cp("act", xT[b].t[:].rearrange("p k t -> p (k t)"), p2b, [p2], [xT[b]])
                        for pair in range(2):
                            pgl = self.psum()
                            pli = self.psum()
                            for (pp_, c0) in ((pgl, pair * 512), (pli, D + pair * 512)):
                                for kc in range(8):
                                    kb.mm(pp_.t[:], xT[b].t[:, kc, :], WU.t[:, kc, c0:c0 + 512], kc == 0, False, [xT[b], wupk[jb][kc]], [pp_])
                                kb.mm(pp_.t[:], ones1, BU.t[0:1, c0:c0 + 512], False, True, [P["ones_b"], BU], [pp_])
                            GL, LI, SG = gl[pair], li[pair], sg[pair]
                            kb.ts("dve", GL.t[:], pgl.t[:], 7.0, None, ALU.min, None, [pgl], [GL])
                            kb.act(SG.t[:], GL.t[:], AF.Sigmoid, [GL], [SG], scale=1.702)
                            kb.ts("dve", LI.t[:], pli.t[:], 7.0, -7.0, ALU.min, ALU.max, [pli], [LI])
                            kb.stt("dve", LI.t[:], LI.t[:], 1.0, GL.t[:], ALU.add, ALU.mult, [LI, GL], [LI])
                            kb.tt("dve", ab[b].t[:, pair * 512:(pair + 1) * 512], LI.t[:], SG.t[:], ALU.mult, [LI, SG], [ab[b]])
                        p3 = self.psum()
                        p3b = p3.t[:].bitcast(BF16)
                        for kc in range(8):
                            kb.tr(p3b[:, kc * 128:(kc + 1) * 128], ab[b].t[:, kc * 128:(kc + 1) * 128], idb.t[:], [ab[b], idb], [p3])
                        kb.cp("act", aT[b].t[:].rearrange("p k t -> p (k t)"), p3b, [p3], [aT[b]])
                        for half in range(2):
                            pd = self.psum()
                            for kc in range(8):
                                kb.mm(pd.t[:], aT[b].t[:, kc, :], WD.t[:, kc, half * 512:(half + 1) * 512], kc == 0, False, [aT[b], wdnk[jb][kc]], [pd])
                            kb.mm(pd.t[:], ones1, BD.t[0:1, half * 512:(half + 1) * 512], False, True, [P["ones_b"], BD], [pd])
                            kb.cp("act" if half == 0 else "dve", yb[b].t[:, half * 512:(half + 1) * 512], pd.t[:], [pd], [yb[b]])
                        kb.dma("sp", Dr["YB"][r0:r0 + 128, :], yb[b].t[:], [yb[b]], [Dk["YB"]])
                kb.S.flush()
            with contextlib.ExitStack() as st:
                rows = self.mod_rows(st, l, 2, names=("G",))
                NB2 = 2
                def mk(name, shp, dt):
                    return [kb.sb(st, "%s%d" % (name, j), shp, dt) for j in range(NB2)]
                xt = mk("cxt", [128, D], F32)
                yk = [kb.sb(st, "cyk%d" % j, [128, D], F32) for j in range(4)]
                acc = mk("cacc", [128, D], F32)
                xn = mk("cxn", [128, D], F32)
                if is_last:
                    fg = kb.sb(st, "fg", [128, D], F32)
                    kb.dma("sp", fg.t[:], W["final_norm_g"].partition_broadcast(128), [], [fg])
                    junk = kb.sb(st, "cjunk", [128, D], F32)
                    ssq = mk("cssq", [128, 8], F32)
                    ot = mk("cot", [128, D], F32)
                for i in range(NT):
                    b = i % NB2
                    sl = slice(i * 128, (i + 1) * 128)
                    G = rows["Gc" if i < 2 else "Gl"]
                    kb.dma("sp", xt[b].t[:], Dr["XR"][sl, :], [Dk["XR"]], [xt[b]])
                    for k in range(4):
                        kb.gather(yk[k].t[:], Dr["YB"], DSTI.t[:, i, k:k + 1], [DSTI, Dk["YB"]], [yk[k]], NROWS - 1)
                    A_ = acc[b]
                    kb.ts("dve", A_.t[:], yk[0].t[:], G4.t[:, i, 0:1], None, ALU.mult, None, [yk[0], G4], [A_])
                    for k in range(1, 4):
                        kb.stt("dve", A_.t[:], yk[k].t[:], G4.t[:, i, k:k + 1], A_.t[:], ALU.mult, ALU.add, [yk[k], G4, A_], [A_])
                    kb.tt("pool", A_.t[:], A_.t[:], G.t[:], ALU.mult, [A_, G], [A_])
                    kb.tt("dve", xn[b].t[:], A_.t[:], xt[b].t[:], ALU.add, [A_, xt[b]], [xn[b]])
                    kb.dma("sp", Dr["XR"][sl, :], xn[b].t[:], [xn[b]], [Dk["XR"]])
                    if is_last and i >= 2:
                        SS = ssq[b]
                        kb.memset("pool", SS.t[:], 0.0, [], [SS])
                        kb.act(junk.t[:], xn[b].t[:], AF.Square, [xn[b]], [junk, SS], accum=SS.t[:, 0:1])
                        kb.ts("dve", SS.t[:, 1:2], SS.t[:, 0:1], 1.0 / D, EPS, ALU.mult, ALU.add, [SS], [SS])
                        kb.act(SS.t[:, 1:2], SS.t[:, 1:2], AF.Sqrt, [SS], [SS])
                        kb.recip(SS.t[:, 2:3], SS.t[:, 1:2], [SS], [SS])
                        kb.stt("dve", ot[b].t[:], xn[b].t[:], SS.t[:, 2:3], fg.t[:], ALU.mult, ALU.mult, [xn[b], SS, fg], [ot[b]])
                        kb.dma("sp", self.out[(i - 2) * 128:(i - 1) * 128, :], ot[b].t[:], [ot[b]], [self.tout])
                kb.S.flush()


_CACHE = {}


def kernel(**inputs):
    n_cores = 8
    if "nc" not in _CACHE:
        pg = Prog(nl=DEPTH)
        _CACHE["nc"] = pg.build()
        _CACHE["consts"] = host_consts()
    nc = _CACHE["nc"]
    consts = _CACHE["consts"]
    shared = {k: np.ascontiguousarray(np.asarray(inputs[k], dtype=np.float32)) for k in WEIGHT_SPECS}
    shared["c_ctx"] = np.ascontiguousarray(np.asarray(inputs["c_ctx"], dtype=np.float32))
    shared.update(consts)
    x = np.asarray(inputs["x"], dtype=np.float32)
    c = np.asarray(inputs["c"], dtype=np.float32)
    ctx = np.asarray(inputs["ctx"], dtype=np.float32)
    in_maps = []
    for b in range(n_cores):
        m = dict(shared)
        m["x"] = np.ascontiguousarray(x[b])
        m["ctx"] = np.ascontiguousarray(ctx[b])
        m["c"] = np.ascontiguousarray(c[b])
        in_maps.append(m)
    res = run_bass_kernel_spmd(nc, in_maps, core_ids=list(range(n_cores)))
    out = np.stack([np.asarray(res.results[b]["out"], dtype=np.float32) for b in range(n_cores)], axis=0)
    return out
```

```python
import contextlib
import math
import numpy as np
import ml_dtypes
import concourse.bass as bass
import concourse.mybir as mybir
from concourse.bass_utils import run_bass_kernel_spmd

F32 = mybir.dt.float32
BF16 = mybir.dt.bfloat16
I32 = mybir.dt.int32
AF = mybir.ActivationFunctionType
ALU = mybir.AluOpType
AX = mybir.AxisListType

D = 1024
SEQ = 4096
CTX = 256
T = SEQ + CTX
NT = T // 128
DEPTH = 4
INW = 5568
NE = 32
BS = 256
SUB = BS // 128
NBLK = (T * 4) // BS + NE
NROWS = NBLK * BS
EPS = 1e-6
MLA_SCALE = 96 ** -0.5
GLA_SCALE = 64 ** -0.5
O_UQ, O_KV, O_FN, O_GQ, O_GK, O_GV, O_OG, O_GF, O_GB, O_GATE = 0, 256, 416, 928, 1184, 1440, 1952, 2464, 2480, 2496


import os as _os
SAME_ENGINE_SYNC = _os.environ.get("NOSAME", "0") != "1"


class Tok:
    __slots__ = ("w", "rs")

    def __init__(self):
        self.w = None
        self.rs = {}


class Sched:
    NDMA = 32

    def __init__(self, nc, st):
        self.nc = nc
        self.engs = ["pe", "act", "dve", "pool", "sp"]
        self.ops = {k: [] for k in self.engs}
        self.cnt = {k: 0 for k in self.engs}
        self.seen = {k: {} for k in self.engs}
        self.dma_k = {"d": 0, "g": 0}
        self.dma_last = {}
        self.n_ops = 0
        self.sems = {}
        for k in ["e_" + e for e in self.engs] + ["d%d" % i for i in range(self.NDMA)] + ["g%d" % i for i in range(self.NDMA)]:
            self.sems[k] = st.enter_context(nc.semaphore(k))

    def _need(self, eng, ev, waits):
        if ev is None:
            return
        src, key, val = ev
        if src == eng and (eng == "pe" or not SAME_ENGINE_SYNC):
            return
        if self.seen[eng].get(key, 0) >= val:
            return
        self.seen[eng][key] = val
        waits.append((key, val))

    def op(self, eng, fn, r=(), w=(), dma=False):
        waits = []
        for t in r:
            self._need(eng, t.w, waits)
        for t in w:
            self._need(eng, t.w, waits)
            for ev in t.rs.values():
                self._need(eng, ev, waits)
        if dma:
            pre = "g" if eng == "pool" else "d"
            k = self.dma_k[pre]
            self.dma_k[pre] += 1
            key = "%s%d" % (pre, k % self.NDMA)
            val = 16 * (k // self.NDMA + 1)
            if val > 16:
                self._need(eng, ("dma", key, val - 16), waits)
            ev = ("dma", key, val)
            inc = (key, 16)
            self.dma_last[key] = val
        else:
            self.cnt[eng] += 1
            key = "e_" + eng
            ev = (eng, key, self.cnt[eng])
            inc = (key, 1)
        self.ops[eng].append((waits, fn, inc))
        self.n_ops += 1
        for t in w:
            t.w = ev
            t.rs = {}
        for t in r:
            if t not in w:
                t.rs[ev[1]] = ev
        return ev

    def barrier(self):
        evs = [(e, "e_" + e, self.cnt[e]) for e in self.engs if self.cnt[e] > 0]
        evs += [("dma", k, v) for k, v in self.dma_last.items()]
        for eng in self.engs:
            waits = []
            for ev in evs:
                if ev[0] == eng:
                    continue
                self._need(eng, ev, waits)
            if waits:
                self.ops[eng].append((waits, None, None))

    def flush(self):
        self.barrier()
        nc = self.nc
        sems = self.sems
        ops = self.ops
        with nc.Block() as block:
            def mk(engname):
                def body(e):
                    for waits, fn, inc in ops[engname]:
                        for key, val in waits:
                            e.wait_ge(sems[key], val)
                        if fn is not None:
                            fn(e).then_inc(sems[inc[0]], inc[1])
                return body
            block.tensor(mk("pe"))
            block.scalar(mk("act"))
            block.vector(mk("dve"))
            block.gpsimd(mk("pool"))
            block.sync(mk("sp"))
        self.ops = {k: [] for k in self.engs}


class B:
    __slots__ = ("t", "k", "psum")

    def __init__(self, t, psum=False):
        self.t = t
        self.k = Tok()
        self.psum = psum


def _toks(xs):
    return [x.k if isinstance(x, B) else x for x in xs]


class KB:
    def __init__(self, nc, st):
        self.nc = nc
        self.S = Sched(nc, st)
        self.dq = 0
        self.bregs = {}

    def sb(self, st, name, shape, dt):
        self.dq += 1
        return B(st.enter_context(self.nc.sbuf_tensor("%s_u%d" % (name, self.dq), shape, dt)))

    def op(self, eng, fn, r, w, dma=False):
        r2 = [x for x in r if not (isinstance(x, B) and x.psum)]
        w2 = list(w) + [x for x in r if isinstance(x, B) and x.psum and x not in w]
        return self.S.op(eng, fn, r=_toks(r2), w=_toks(w2), dma=dma)

    def mm(self, out, lhsT, rhs, start, stop, r, w):
        self.op("pe", lambda e: e.matmul(out, lhsT, rhs, start=start, stop=stop), r, w)

    def tr(self, out, in_, ident, r, w):
        self.op("pe", lambda e: e.transpose(out, in_, ident), r, w)

    def act(self, out, in_, func, r, w, bias=None, scale=None, accum=None):
        kw = {}
        if bias is not None:
            kw["bias"] = bias
        if scale is not None:
            kw["scale"] = scale
        if accum is not None:
            kw["accum_out"] = accum
        self.op("act", lambda e: e.activation(out=out, in_=in_, func=func, **kw), r, w)

    def cp(self, eng, out, in_, r, w):
        if eng == "act":
            self.op("act", lambda e: e.copy(out=out, in_=in_), r, w)
        else:
            self.op(eng, lambda e: e.tensor_copy(out=out, in_=in_), r, w)

    def tt(self, eng, out, in0, in1, op, r, w):
        self.op(eng, lambda e: e.tensor_tensor(out=out, in0=in0, in1=in1, op=op), r, w)

    def ts(self, eng, out, in0, s1, s2, op0, op1, r, w):
        if op1 is None:
            self.op(eng, lambda e: e.tensor_scalar(out=out, in0=in0, scalar1=s1, scalar2=None, op0=op0), r, w)
        else:
            self.op(eng, lambda e: e.tensor_scalar(out=out, in0=in0, scalar1=s1, scalar2=s2, op0=op0, op1=op1), r, w)

    def stt(self, eng, out, in0, scalar, in1, op0, op1, r, w):
        self.op(eng, lambda e: e.scalar_tensor_tensor(out=out, in0=in0, scalar=scalar, in1=in1, op0=op0, op1=op1), r, w)

    def red(self, eng, out, in_, op, r, w, axis=AX.X):
        self.op(eng, lambda e: e.tensor_reduce(out=out, in_=in_, axis=axis, op=op), r, w)

    def recip(self, out, in_, r, w):
        self.op("dve", lambda e: e.reciprocal(out=out, in_=in_), r, w)

    def memset(self, eng, out, val, r, w):
        self.op(eng, lambda e: e.memset(out, val), r, w)

    def dma(self, eng, out, in_, r, w, slow=False):
        if slow:
            self.op(eng, lambda e: e.dma_start(out=out, in_=in_, allow_slow_non_contiguous=True), r, w, dma=True)
        else:
            self.op(eng, lambda e: e.dma_start(out=out, in_=in_), r, w, dma=True)

    def _breg(self, e, bound):
        if bound not in self.bregs:
            self.bregs[bound] = e.to_reg(bound)
        return self.bregs[bound]

    def gather(self, out, in_, idx, r, w, bound):
        self.op("pool", lambda e: e.indirect_dma_start(
            out=out, out_offset=None, in_=in_, in_offset=bass.IndirectOffsetOnAxis(ap=idx, axis=0),
            bounds_check=self._breg(e, bound), oob_is_err=False), r, w, dma=True)

    def scatter(self, out, in_, idx, r, w, bound):
        self.op("pool", lambda e: e.indirect_dma_start(
            out=out, out_offset=bass.IndirectOffsetOnAxis(ap=idx, axis=0), in_=in_, in_offset=None,
            bounds_check=self._breg(e, bound), oob_is_err=False), r, w, dma=True)


def host_consts():
    c = {}
    bf = ml_dtypes.bfloat16
    i = np.arange(128)
    c["ident_f"] = np.eye(128, dtype=np.float32)
    c["ident_b"] = np.eye(128).astype(bf)
    trif = (i[:, None] <= i[None, :]).astype(np.float32)
    trib = (i[:, None] >= i[None, :]).astype(np.float32)
    c["tri_f"] = np.stack([trif, trib], 0)
    c["mask4"] = np.stack([np.repeat(trif[:, None, :], 4, 1), np.repeat(trib[:, None, :], 4, 1)], 0).astype(np.float32)
    c["tri_x"] = (i[:, None] < i[None, :]).astype(bf)
    c["ones_f"] = np.ones((128, 128), np.float32)
    c["ones_b"] = np.ones((128, 128)).astype(bf)
    inv = 10000.0 ** (-np.arange(0, 16, 2, dtype=np.float32) / 16)
    row = np.repeat(np.arange(64, dtype=np.float32), 64)
    col = np.tile(np.arange(64, dtype=np.float32), 64)
    ar = row[:, None] * inv
    ac = col[:, None] * inv
    cr, sr, cc, sc = (np.cos(ar).astype(np.float32), np.sin(ar).astype(np.float32),
                      np.cos(ac).astype(np.float32), np.sin(ac).astype(np.float32))
    cos32 = np.concatenate([cr, cr, cc, cc], 1)
    sin32 = np.concatenate([-sr, sr, -sc, sc], 1)
    cos32 = np.concatenate([np.ones((CTX, 32), np.float32), cos32], 0)
    sin32 = np.concatenate([np.zeros((CTX, 32), np.float32), sin32], 0)
    c["rope_k"] = np.stack([cos32, sin32], 1).astype(np.float32)
    rq = np.stack([np.tile(cos32, (1, 8)), np.tile(sin32, (1, 8))], 1) * np.float32(MLA_SCALE)
    c["rope_q"] = rq.astype(np.float32)
    ang = 2 * np.pi * np.outer(i, i) / 128.0
    c["cs128"] = (np.concatenate([np.cos(ang), np.sin(ang)], 1) / math.sqrt(128)).astype(bf)
    n = np.arange(SEQ, dtype=np.int64)
    kt = (np.outer(n, n) % SEQ).astype(np.float64) * (2 * np.pi / SEQ)
    ctm = (np.cos(kt) / 64.0).astype(np.float32).reshape(32, 128, 32, 128)
    stm = (-np.sin(kt) / 64.0).astype(np.float32).reshape(32, 128, 32, 128)
    c["dft_c"] = np.ascontiguousarray(ctm.transpose(2, 1, 0, 3)).astype(bf)
    c["dft_s"] = np.ascontiguousarray(stm.transpose(2, 1, 0, 3)).astype(bf)
    del kt, ctm, stm
    m = np.arange(CTX, dtype=np.int64)
    k2 = (np.outer(m, m) % CTX).astype(np.float64) * (2 * np.pi / CTX)
    c2 = (np.cos(k2) / 16.0).astype(np.float32).reshape(2, 128, 2, 128)
    s2 = (-np.sin(k2) / 16.0).astype(np.float32).reshape(2, 128, 2, 128)
    c["dft_c2"] = np.ascontiguousarray(c2.transpose(2, 1, 0, 3)).astype(bf)
    c["dft_s2"] = np.ascontiguousarray(s2.transpose(2, 1, 0, 3)).astype(bf)
    c["jv"] = np.tile((np.arange(NBLK, dtype=np.float32) * BS)[None, :], (128, 1))
    c["kp"] = (np.arange(8, dtype=np.float32)[None, :] * 128 + i[:, None]).astype(np.float32)
    return c


CONST_SPECS = {
    "ident_f": ([128, 128], F32), "ident_b": ([128, 128], BF16), "tri_f": ([2, 128, 128], F32),
    "mask4": ([2, 128, 4, 128], F32), "tri_x": ([128, 128], BF16), "ones_f": ([128, 128], F32),
    "ones_b": ([128, 128], BF16), "rope_k": ([T, 2, 32], F32), "rope_q": ([T, 2, 256], F32),
    "cs128": ([128, 256], BF16), "dft_c": ([32, 128, 32, 128], BF16), "dft_s": ([32, 128, 32, 128], BF16),
    "dft_c2": ([2, 128, 2, 128], BF16), "dft_s2": ([2, 128, 2, 128], BF16),
    "jv": ([128, NBLK], F32), "kp": ([128, 8], F32),
}

WEIGHT_SPECS = {
    "w_mod": [DEPTH, D, 6 * D], "b_mod": [DEPTH, 6 * D], "norm1_g": [DEPTH, D], "w_in": [DEPTH, D, INW],
    "mla_q_norm_g": [DEPTH, 256], "mla_w_uq": [DEPTH, 256, 768], "mla_kv_norm_g": [DEPTH, 128],
    "mla_w_ukv": [DEPTH, 128, 1024], "gla_w_gate_f": [DEPTH, 16, 256], "gla_b_gate_f": [DEPTH, 256],
    "gla_w_gate_b": [DEPTH, 16, 256], "gla_b_gate_b": [DEPTH, 256], "gla_norm_g": [DEPTH, 512],
    "w_br_mla": [DEPTH, 512, D], "w_br_fnet": [DEPTH, 512, D], "w_br_gla": [DEPTH, 512, D],
    "w_o": [DEPTH, D, D], "norm2_g": [DEPTH, D], "router_w": [DEPTH, D, NE], "router_b": [DEPTH, NE],
    "exp_w_up": [DEPTH, NE, D, 2 * D], "exp_b_up": [DEPTH, NE, 2 * D], "exp_w_down": [DEPTH, NE, D, D],
    "exp_b_down": [DEPTH, NE, D], "final_norm_g": [D],
}


class Prog:
    def __init__(self, nl=DEPTH, dbg=(), stop_after=None, wdepth=DEPTH):
        self.nl = nl
        self.dbg = set(dbg)
        self.stop_after = stop_after
        self.nc = bass.Bass("TRN2", target_bir_lowering=False)
        self.st = contextlib.ExitStack()
        self.kb = KB(self.nc, self.st)
        nc = self.nc
        self.x_in = nc.dram_tensor("x", [SEQ, D], F32, kind="ExternalInput").ap()
        self.ctx_in = nc.dram_tensor("ctx", [CTX, D], F32, kind="ExternalInput").ap()
        self.c_in = nc.dram_tensor("c", [D], F32, kind="ExternalInput").ap()
        self.cc_in = nc.dram_tensor("c_ctx", [D], F32, kind="ExternalInput").ap()
        self.W = {k: nc.dram_tensor(k, ([wdepth] + shp[1:]) if (len(shp) > 1 or k in ('norm1_g',)) and shp[0] == DEPTH and k != 'final_norm_g' else shp, F32, kind="ExternalInput").ap() for k, shp in WEIGHT_SPECS.items()}
        self.C = {k: nc.dram_tensor(k, shp, dt, kind="ExternalInput").ap() for k, (shp, dt) in CONST_SPECS.items()}
        self.out = nc.dram_tensor("out", [SEQ, D], F32, kind="ExternalOutput").ap()
        self.tout = Tok()
        self.Dr = {}
        self.Dk = {}
        for name, shp, dt in [
            ("XR", [T, D], F32), ("KT", [8, 96, T], BF16), ("QT", [8, 96, T], BF16), ("VA", [T, 8, 65], BF16),
            ("UFT", [4, 128, T], BF16), ("GQT", [4, 64, T], BF16), ("GKT", [4, 64, T], BF16),
            ("GKVO", [T, 1280], BF16), ("LFB", [T, 512], F32), ("GATE", [T, 3072], BF16),
            ("OMT", [8, 64, T], BF16), ("OFT", [4, 128, T], BF16), ("OGT", [4, 128, T], BF16),
            ("OFW", [T, 512], F32), ("OBW", [T, 512], F32), ("XS", [NROWS, D], BF16), ("YB", [NROWS, D], F32),
            ("H2B", [T, D], BF16), ("DBG_DST", [128, NT * 4], I32), ("DBG_G4", [128, NT * 4], F32),
            ("DBG_EB", [128, NBLK], I32), ("DBG_LG", [128, NT * NE], F32), ("DBG_CNT", [128, NE], F32),
        ]:
            kind = "ExternalOutput" if name in self.dbg else "Internal"
            self.Dr[name] = nc.dram_tensor(name, shp, dt, kind=kind).ap()
            self.Dk[name] = Tok()

    def build(self):
        kb = self.kb
        nc = self.nc
        with self.st:
            P = self.P = {}
            for name, shp, dt in [
                ("ident_f", [128, 128], F32), ("ident_b", [128, 128], BF16), ("ones_f", [128, 128], F32),
                ("ones_b", [128, 128], BF16), ("modT", [128, 48, 2], F32), ("nb", [128, 8], F32),
                ("scT", [128, 8, 2], F32),
            ]:
                P[name] = kb.sb(self.st, "p_" + name, shp, dt)
            self.ps = [B(self.st.enter_context(nc.psum_tensor("ps%d" % i, [128, 512], F32)), psum=True) for i in range(8)]
            self.psi = 0
            self.pinned = set()
            for nm in ["ident_f", "ident_b", "ones_f", "ones_b"]:
                kb.dma("sp", P[nm].t[:], self.C[nm], [], [P[nm]])
            kb.dma("sp", self.Dr["XR"][0:CTX, :], self.ctx_in, [], [self.Dk["XR"]])
            kb.dma("pool", self.Dr["XR"][CTX:T, :], self.x_in, [], [self.Dk["XR"]])
            kb.S.flush()
            for l in range(self.nl):
                import os
                skip = os.environ.get("SKIP_PH", "").split(",")
                for ph in [self.phase_mod, self.phase_a, self.phase_attn, self.phase_fnet,
                           self.phase_merge, self.phase_moe]:
                    if ph.__name__ in skip:
                        continue
                    ph(l)
                    kb.S.flush()
                    if self.stop_after == (l, ph.__name__):
                        break
                else:
                    continue
                break
            kb.S.flush()
        return nc

    def psum(self, pin=False):
        while True:
            idx = self.psi % 8
            self.psi += 1
            if idx not in self.pinned:
                break
        if pin:
            self.pinned.add(idx)
        return self.ps[idx]

    def unpin(self, p):
        self.pinned.discard(self.ps.index(p))

    def phase_mod(self, l):
        kb, P, W = self.kb, self.P, self.W
        with contextlib.ExitStack() as st:
            cT = kb.sb(st, "cT", [128, 8, 2], F32)
            sT = kb.sb(st, "sT", [128, 8, 2], F32)
            bT = kb.sb(st, "bT", [128, 48], F32)
            wm = [kb.sb(st, "wm%d" % i, [128, 8, 1024], F32) for i in range(2)]
            kb.dma("sp", cT.t[:, :, 0], self.c_in.rearrange("(k p) -> p k", p=128), [], [cT], slow=True)
            kb.dma("sp", cT.t[:, :, 1], self.cc_in.rearrange("(k p) -> p k", p=128), [], [cT], slow=True)
            kb.dma("sp", bT.t[:], W["b_mod"][l].rearrange("(j p) -> p j", p=128), [], [bT], slow=True)
            kb.act(sT.t[:], cT.t[:], AF.Silu, [cT], [sT])
            for sec in range(6):
                w = wm[sec % 2]
                kb.dma("sp" if sec % 2 == 0 else "pool", w.t[:],
                       W["w_mod"][l][:, sec * 1024:(sec + 1) * 1024].rearrange("(k p) n -> p k n", p=128), [], [w])
                ps = self.psum()
                for j in range(8):
                    for kc in range(8):
                        kb.mm(ps.t[:, j * 2:j * 2 + 2], w.t[:, kc, j * 128:(j + 1) * 128], sT.t[:, kc, :],
                              kc == 0, kc == 7, [w, sT], [ps])
                for j in range(8):
                    jj = sec * 8 + j
                    kb.ts("dve", P["modT"].t[:, jj, :], ps.t[:, j * 2:j * 2 + 2], bT.t[:, jj:jj + 1], None, ALU.add, None,
                          [ps, bT], [P["modT"]])
            kb.S.flush()

    def bcast_rows(self, st, name, colT_ap_fn, r):
        kb, P = self.kb, self.P
        out = kb.sb(st, name, [128, 1024], F32)
        tmp = kb.sb(st, name + "_t", [128, 128], F32)
        for half in range(2):
            ps = self.psum()
            for k4 in range(4):
                kc = half * 4 + k4
                kb.ts("dve", tmp.t[:], P["ones_f"].t[:], colT_ap_fn(kc), None, ALU.mult, None, r + [P["ones_f"]], [tmp])
                kb.mm(ps.t[:, k4 * 128:(k4 + 1) * 128], tmp.t[:], P["ident_f"].t[:], True, True, [tmp, P["ident_f"]], [ps])
            kb.cp("act", out.t[:, half * 512:(half + 1) * 512], ps.t[:], [ps], [out])
        return out

    def mod_rows(self, st, l, which, names=("A", "S", "G")):
        kb, P, W = self.kb, self.P, self.W
        sec_sh, sec_sc, sec_g = (0, 1, 2) if which == 1 else (3, 4, 5)
        gT = kb.sb(st, "gT", [128, 8], F32)
        kb.dma("sp", gT.t[:], W["norm1_g" if which == 1 else "norm2_g"][l].rearrange("(k p) -> p k", p=128), [], [gT], slow=True)
        aT = kb.sb(st, "aT", [128, 8, 2], F32)
        m = P["modT"]
        for v in range(2):
            kb.stt("dve", aT.t[:, :, v], m.t[:, sec_sc * 8:(sec_sc + 1) * 8, v], 1.0, gT.t[:], ALU.add, ALU.mult, [m, gT], [aT])
        rows = {}
        for v, nm in ((0, "l"), (1, "c")):
            if "A" in names:
                rows["A" + nm] = self.bcast_rows(st, "rA" + nm, lambda kc, v=v: aT.t[:, kc, v:v + 1], [aT])
            if "S" in names:
                rows["S" + nm] = self.bcast_rows(st, "rS" + nm, lambda kc, v=v: m.t[:, sec_sh * 8 + kc, v:v + 1], [m])
            if "G" in names:
                rows["G" + nm] = self.bcast_rows(st, "rG" + nm, lambda kc, v=v: m.t[:, sec_g * 8 + kc, v:v + 1], [m])
        return rows

    def norm_mod(self, xt, rows, i, junk, ssq, h32, hb, full32=False):
        kb = self.kb
        nm = "c" if i < 2 else "l"
        kb.act(junk.t[:], xt.t[:], AF.Square, [xt], [junk, ssq], accum=ssq.t[:, 0:1])
        kb.ts("dve", ssq.t[:, 1:2], ssq.t[:, 0:1], 1.0 / D, EPS, ALU.mult, ALU.add, [ssq], [ssq])
        kb.act(ssq.t[:, 1:2], ssq.t[:, 1:2], AF.Sqrt, [ssq], [ssq])
        kb.recip(ssq.t[:, 2:3], ssq.t[:, 1:2], [ssq], [ssq])
        kb.stt("dve", h32.t[:], xt.t[:], ssq.t[:, 2:3], rows["A" + nm].t[:], ALU.mult, ALU.mult, [xt, ssq, rows["A" + nm]], [h32])
        if full32:
            kb.tt("pool", h32.t[:], h32.t[:], rows["S" + nm].t[:], ALU.add, [h32, rows["S" + nm]], [h32])
            kb.cp("dve", hb.t[:], h32.t[:], [h32], [hb])
        else:
            kb.tt("pool", hb.t[:], h32.t[:], rows["S" + nm].t[:], ALU.add, [h32, rows["S" + nm]], [hb])

    def phase_a(self, l):
        kb, P, W, C, Dr, Dk = self.kb, self.P, self.W, self.C, self.Dr, self.Dk
        with contextlib.ExitStack() as st:
            rows = self.mod_rows(st, l, 1, names=("A", "S"))
            win = kb.sb(st, "win", [128, 8, INW], BF16)
            for kc in range(8):
                kb.dma("pool", win.t[:, kc, :], W["w_in"][l][kc * 128:(kc + 1) * 128, :], [], [win])
            wuq32 = kb.sb(st, "wuq32", [128, 2, 768], F32)
            wuq = kb.sb(st, "wuq", [128, 2, 768], BF16)
            gq = kb.sb(st, "gq", [128, 2], F32)
            wkv32 = kb.sb(st, "wkv32", [128, 1024], F32)
            wkv = kb.sb(st, "wkv", [128, 1024], BF16)
            gkv = kb.sb(st, "gkv", [128, 1], F32)
            wg = kb.sb(st, "wg", [17, 2, 256], F32)
            kb.dma("sp", wuq32.t[:], W["mla_w_uq"][l].rearrange("(k p) n -> p k n", p=128), [], [wuq32])
            kb.dma("sp", gq.t[:], W["mla_q_norm_g"][l].rearrange("(k p) -> p k", p=128), [], [gq], slow=True)
            kb.dma("sp", wkv32.t[:], W["mla_w_ukv"][l], [], [wkv32])
            kb.dma("sp", gkv.t[:], W["mla_kv_norm_g"][l].rearrange("(k p) -> p k", p=128), [], [gkv], slow=True)
            kb.dma("sp", wg.t[0:16, 0, :], W["gla_w_gate_f"][l], [], [wg])
            kb.dma("sp", wg.t[0:16, 1, :], W["gla_w_gate_b"][l], [], [wg])
            kb.dma("sp", wg.t[16:17, 0, :], W["gla_b_gate_f"][l].rearrange("(o n) -> o n", o=1), [], [wg])
            kb.dma("sp", wg.t[16:17, 1, :], W["gla_b_gate_b"][l].rearrange("(o n) -> o n", o=1), [], [wg])
            for kc in range(2):
                kb.ts("dve", wuq.t[:, kc, :], wuq32.t[:, kc, :], gq.t[:, kc:kc + 1], None, ALU.mult, None, [wuq32, gq], [wuq])
            kb.ts("dve", wkv.t[:], wkv32.t[:], gkv.t[:, 0:1], None, ALU.mult, None, [wkv32, gkv], [wkv])
            m16 = kb.sb(st, "m16", [128, 16], F32)
            kb.memset("dve", m16.t[:], 0.0, [], [m16])
            NB2 = 1
            def mk(name, shp, dt):
                return [kb.sb(st, "%s%d" % (name, j), shp, dt) for j in range(NB2)]
            def mk2(name, shp, dt):
                return [kb.sb(st, "%s%d" % (name, j), shp, dt) for j in range(2)]
            xt = mk2("xt", [128, D], F32)
            junk = mk("junk", [128, D], F32)
            ssq = mk2("ssq", [128, 8], F32)
            h32 = mk("h32", [128, D], F32)
            hb = mk2("hb", [128, D], BF16)
            hT = mk2("hT", [128, 8, 128], BF16)
            uqn = mk("uqn", [128, 384], BF16)
            kr = mk("kr", [128, 4, 32], F32)
            uT = mk("uT", [128, 3, 128], BF16)
            rq = mk("rq", [128, 2, 256], F32)
            rk = mk("rk", [128, 2, 32], F32)
            qo = mk("qo", [128, 8, 96], BF16)
            ko = mk("ko", [128, 8, 96], BF16)
            vo = mk("vo", [128, 8, 65], BF16)
            qtmp = mk("qtmp", [128, 3, 256], F32)
            sq = mk("sq", [128, 768], F32)
            n2 = mk("n2", [128, 16], F32)
            qkT = mk("qkT", [96, 16, 128], BF16)
            gkvo = mk("gkvo", [128, 1280], BF16)
            gate = mk("gate", [128, 3072], BF16)
            fT = mk("fT", [128, 4, 128], BF16)
            gqkT = mk("gqkT", [64, 8, 128], BF16)
            ugT = mk("ugT", [17, 2, 128], F32)
            lfb = mk("lfb", [128, 512], F32)
            lfe = mk("lfe", [128, 512], F32)
            for j in range(NB2):
                kb.memset("pool", vo[j].t[:], 1.0, [], [vo[j]])
                kb.memset("pool", ugT[j].t[:], 1.0, [], [ugT[j]])
            idb = P["ident_b"]
            for i in range(NT):
                b = i % NB2
                b2 = i % 2
                X, J, SS, H32, HB, HT = xt[b2], junk[b], ssq[b2], h32[b], hb[b2], hT[b2]
                kb.dma("sp", X.t[:], Dr["XR"][i * 128:(i + 1) * 128, :], [Dk["XR"]], [X])
                kb.dma("sp", rq[b].t[:], C["rope_q"][i * 128:(i + 1) * 128], [], [rq[b]])
                kb.dma("sp", rk[b].t[:], C["rope_k"][i * 128:(i + 1) * 128], [], [rk[b]])
                kb.memset("pool", SS.t[:], 0.0, [], [SS])
                self.norm_mod(X, rows, i, J, SS, H32, HB)
                pT = self.psum()
                pTb = pT.t[:].bitcast(BF16)
                for kc in range(8):
                    kb.tr(pTb[:, kc * 128:(kc + 1) * 128], HB.t[:, kc * 128:(kc + 1) * 128], idb.t[:], [HB, idb], [pT])
                kb.cp("act", HT.t[:].rearrange("p k t -> p (k t)"), pTb, [pT], [HT])

                def tm(c0, cw):
                    ps = self.psum()
                    for kc in range(8):
                        kb.mm(ps.t[:, 0:cw], HT.t[:, kc, :], win.t[:, kc, c0:c0 + cw], kc == 0, kc == 7, [HT, win], [ps])
                    return ps

                def fm(c0, cw, ps, o0):
                    for kc in range(8):
                        kb.mm(ps.t[0:cw, o0:o0 + 128], win.t[:, kc, c0:c0 + cw], HT.t[:, kc, :], kc == 0, kc == 7, [HT, win], [ps])

                ps1 = tm(0, 416)
                UQ, KR, UT, QO, KO, VO, QTMP, SQ, N2 = uqn[b], kr[b], uT[b], qo[b], ko[b], vo[b], qtmp[b], sq[b], n2[b]
                kb.act(J.t[:, 0:256], ps1.t[:, 0:256], AF.Square, [ps1], [J, SS], accum=SS.t[:, 3:4])
                kb.act(J.t[:, 256:384], ps1.t[:, 256:384], AF.Square, [ps1], [J, SS], accum=SS.t[:, 4:5])
                kb.ts("dve", SS.t[:, 5:6], SS.t[:, 3:4], 1.0 / 256, EPS, ALU.mult, ALU.add, [SS], [SS])
                kb.ts("dve", SS.t[:, 6:7], SS.t[:, 4:5], 1.0 / 128, EPS, ALU.mult, ALU.add, [SS], [SS])
                kb.act(SS.t[:, 5:7], SS.t[:, 5:7], AF.Sqrt, [SS], [SS])
                kb.recip(SS.t[:, 5:7], SS.t[:, 5:7], [SS], [SS])
                kb.ts("dve", UQ.t[:, 0:256], ps1.t[:, 0:256], SS.t[:, 5:6], None, ALU.mult, None, [ps1, SS], [UQ])
                kb.ts("dve", UQ.t[:, 256:384], ps1.t[:, 256:384], SS.t[:, 6:7], None, ALU.mult, None, [ps1, SS], [UQ])
                kb.cp("act", KR.t[:, 0, :], ps1.t[:, 384:416], [ps1], [KR])
                krv = KR.t[:, 0, :].rearrange("p (a f e) -> p a f e", a=2, f=2)
                krs = KR.t[:, 1, :].rearrange("p (a f e) -> p a f e", a=2, f=2)
                kb.cp("pool", krs[:, :, 0, :], krv[:, :, 1, :], [KR], [KR])
                kb.cp("pool", krs[:, :, 1, :], krv[:, :, 0, :], [KR], [KR])
                kb.tt("dve", KR.t[:, 2, :], KR.t[:, 0, :], rk[b].t[:, 0, :], ALU.mult, [KR, rk[b]], [KR])
                kb.tt("dve", KR.t[:, 3, :], KR.t[:, 1, :], rk[b].t[:, 1, :], ALU.mult, [KR, rk[b]], [KR])
                kb.tt("dve", KR.t[:, 0, :], KR.t[:, 2, :], KR.t[:, 3, :], ALU.add, [KR], [KR])
                p2 = self.psum()
                p2b = p2.t[:].bitcast(BF16)
                for j in range(3):
                    kb.tr(p2b[:, j * 128:(j + 1) * 128], UQ.t[:, j * 128:(j + 1) * 128], idb.t[:], [UQ, idb], [p2])
                kb.cp("act", UT.t[:].rearrange("p k t -> p (k t)"), p2b[:, 0:384], [p2], [UT])
                for hh in range(2):
                    pq = self.psum()
                    for kc in range(2):
                        kb.mm(pq.t[:, 0:384], UT.t[:, kc, :], wuq.t[:, kc, hh * 384:(hh + 1) * 384], kc == 0, kc == 1, [UT, wuq], [pq])
                    pqv = pq.t[:, 0:384].rearrange("p (h d) -> p h d", h=4)
                    qov = QO.t[:, hh * 4:(hh + 1) * 4, :]
                    kb.act(qov[:, :, 0:64], pqv[:, :, 0:64], AF.Identity, [pq], [QO], scale=MLA_SCALE)
                    t0 = QTMP.t[:, 0, hh * 128:(hh + 1) * 128].rearrange("p (h d) -> p h d", h=4)
                    t1 = QTMP.t[:, 1, hh * 128:(hh + 1) * 128].rearrange("p (h d) -> p h d", h=4)
                    kb.cp("act", t0, pqv[:, :, 64:96], [pq], [QTMP])
                    t0v = t0.rearrange("p h (a f e) -> p h a f e", a=2, f=2)
                    t1v = t1.rearrange("p h (a f e) -> p h a f e", a=2, f=2)
                    for a in range(2):
                        kb.cp("pool", t1v[:, :, a, 0, :], t0v[:, :, a, 1, :], [QTMP], [QTMP])
                        kb.cp("pool", t1v[:, :, a, 1, :], t0v[:, :, a, 0, :], [QTMP], [QTMP])
                    rc = rq[b].t[:, 0, hh * 128:(hh + 1) * 128].rearrange("p (h d) -> p h d", h=4)
                    rs = rq[b].t[:, 1, hh * 128:(hh + 1) * 128].rearrange("p (h d) -> p h d", h=4)
                    kb.tt("dve", t0, t0, rc, ALU.mult, [QTMP, rq[b]], [QTMP])
                    kb.tt("dve", t1, t1, rs, ALU.mult, [QTMP, rq[b]], [QTMP])
                    kb.tt("dve", qov[:, :, 64:96], t0, t1, ALU.add, [QTMP], [QO])
                for hh in range(2):
                    pk = self.psum()
                    kb.mm(pk.t[:], UT.t[:, 2, :], wkv.t[:, hh * 512:(hh + 1) * 512], True, True, [UT, wkv], [pk])
                    pkv = pk.t[:].rearrange("p (h d) -> p h d", h=4)
                    kb.cp("act", KO.t[:, hh * 4:(hh + 1) * 4, 0:64], pkv[:, :, 0:64], [pk], [KO])
                    kb.cp("dve", VO.t[:, hh * 4:(hh + 1) * 4, 0:64], pkv[:, :, 64:128], [pk], [VO])
                for h in range(8):
                    kb.cp("pool", KO.t[:, h, 64:96], KR.t[:, 0, :], [KR], [KO])
                kb.tt("dve", SQ.t[:], QO.t[:].rearrange("p h d -> p (h d)"), QO.t[:].rearrange("p h d -> p (h d)"), ALU.mult, [QO], [SQ])
                kb.red("dve", N2.t[:, 8:16], SQ.t[:].rearrange("p (h d) -> p h d", h=8), ALU.add, [SQ], [N2])
                kb.tt("dve", SQ.t[:], KO.t[:].rearrange("p h d -> p (h d)"), KO.t[:].rearrange("p h d -> p (h d)"), ALU.mult, [KO], [SQ])
                kb.red("dve", N2.t[:, 0:8], SQ.t[:].rearrange("p (h d) -> p h d", h=8), ALU.add, [SQ], [N2])
                kb.tt("dve", m16.t[:], m16.t[:], N2.t[:], ALU.max, [N2, m16], [m16])
                QKT = qkT[b]
                for which, src in ((0, KO), (1, QO)):
                    p3 = self.psum()
                    p3b = p3.t[:].bitcast(BF16)
                    for h in range(8):
                        kb.tr(p3b[0:96, h * 128:(h + 1) * 128], src.t[:, h, :], idb.t[:], [src, idb], [p3])
                    kb.cp("act" if which == 0 else "dve", QKT.t[:, which * 8:(which + 1) * 8, :].rearrange("p h t -> p (h t)"),
                          p3b[0:96, :], [p3], [QKT])
                kb.dma("sp", Dr["KT"][:, :, i * 128:(i + 1) * 128].rearrange("h d t -> d h t"), QKT.t[:, 0:8, :], [QKT], [Dk["KT"]])
                kb.dma("sp", Dr["QT"][:, :, i * 128:(i + 1) * 128].rearrange("h d t -> d h t"), QKT.t[:, 8:16, :], [QKT], [Dk["QT"]])
                kb.dma("sp", Dr["VA"][i * 128:(i + 1) * 128], VO.t[:], [VO], [Dk["VA"]])
                G = gkvo[b]
                for gi, (c0, cw) in enumerate(((O_GK, 512), (O_GK + 512, 512), (O_GK + 1024, 256))):
                    ps = tm(c0, cw)
                    o0 = c0 - O_GK
                    if gi == 0:
                        kb.cp("dve", G.t[:, 0:512], ps.t[:, 0:512], [ps], [G])
                    elif gi == 1:
                        kb.cp("dve", G.t[:, 512:768], ps.t[:, 0:256], [ps], [G])
                        kb.act(G.t[:, 768:1024], ps.t[:, 256:512], AF.Silu, [ps], [G])
                    else:
                        kb.act(G.t[:, 1024:1280], ps.t[:, 0:256], AF.Silu, [ps], [G])
                kb.dma("sp", Dr["GKVO"][i * 128:(i + 1) * 128, :], G.t[:], [G], [Dk["GKVO"]])
                GA = gate[b]
                for gi in range(6):
                    ps = tm(O_GATE + gi * 512, 512)
                    kb.act(GA.t[:, gi * 512:(gi + 1) * 512], ps.t[:], AF.Sigmoid, [ps], [GA])
                kb.dma("sp", Dr["GATE"][i * 128:(i + 1) * 128, :], GA.t[:], [GA], [Dk["GATE"]])
                ps = self.psum()
                for g in range(4):
                    fm(O_FN + g * 128, 128, ps, g * 128)
                kb.cp("dve", fT[b].t[:].rearrange("p g t -> p (g t)"), ps.t[:], [ps], [fT[b]])
                kb.dma("sp", Dr["UFT"][:, :, i * 128:(i + 1) * 128].rearrange("g c t -> c g t"), fT[b].t[:], [fT[b]], [Dk["UFT"]])
                psq = self.psum()
                psk = self.psum()
                for h in range(4):
                    fm(O_GQ + h * 64, 64, psq, h * 128)
                    fm(O_GK + h * 64, 64, psk, h * 128)
                kb.act(gqkT[b].t[:, 0:4, :].rearrange("p h t -> p (h t)"), psq.t[0:64, :], AF.Identity, [psq], [gqkT[b]], scale=GLA_SCALE)
                kb.cp("dve", gqkT[b].t[:, 4:8, :].rearrange("p h t -> p (h t)"), psk.t[0:64, :], [psk], [gqkT[b]])
                kb.dma("sp", Dr["GQT"][:, :, i * 128:(i + 1) * 128].rearrange("h d t -> d h t"), gqkT[b].t[:, 0:4, :], [gqkT[b]], [Dk["GQT"]])
                kb.dma("sp", Dr["GKT"][:, :, i * 128:(i + 1) * 128].rearrange("h d t -> d h t"), gqkT[b].t[:, 4:8, :], [gqkT[b]], [Dk["GKT"]])
                psg = self.psum()
                fm(O_GF, 16, psg, 0)
                fm(O_GB, 16, psg, 128)
                kb.cp("act", ugT[b].t[0:16, :, :].rearrange("p d t -> p (d t)"), psg.t[0:16, 0:256], [psg], [ugT[b]])
                psl = self.psum()
                for d in range(2):
                    kb.mm(psl.t[:, d * 256:(d + 1) * 256], ugT[b].t[:, d, :], wg.t[:, d, :], True, True, [ugT[b], wg], [psl])
                kb.act(lfe[b].t[:], psl.t[:], AF.Exp, [psl], [lfe[b]], scale=-1.0)
                kb.act(lfe[b].t[:], lfe[b].t[:], AF.Ln, [lfe[b]], [lfe[b]], bias=1.0)
                kb.ts("dve", lfb[b].t[:], lfe[b].t[:], -1.0 / 16.0, None, ALU.mult, None, [lfe[b]], [lfb[b]])
                kb.dma("sp", Dr["LFB"][i * 128:(i + 1) * 128, :], lfb[b].t[:], [lfb[b]], [Dk["LFB"]])
            pm = self.psum()
            kb.tr(pm.t[0:16, 0:128], m16.t[:], P["ident_f"].t[:], [m16, P["ident_f"]], [pm])
            mcol = kb.sb(st, "mcol", [16, 1], F32)
            mb = kb.sb(st, "mb", [16, 128], F32)
            kb.red("dve", mcol.t[:], pm.t[0:16, 0:128], ALU.max, [pm], [mcol])
            kb.ts("dve", mb.t[:], P["ones_f"].t[0:16, :], mcol.t[:, 0:1], None, ALU.mult, None, [mcol, P["ones_f"]], [mb])
            pm2 = self.psum()
            kb.mm(pm2.t[:, 0:16], mb.t[:], P["ident_f"].t[0:16, 0:16], True, True, [mb, P["ident_f"]], [pm2])
            nbt = kb.sb(st, "nbt", [128, 16], F32)
            kb.cp("act", nbt.t[:], pm2.t[:, 0:16], [pm2], [nbt])
            kb.tt("dve", nbt.t[:, 0:8], nbt.t[:, 0:8], nbt.t[:, 8:16], ALU.mult, [nbt], [nbt])
            kb.act(nbt.t[:, 0:8], nbt.t[:, 0:8], AF.Sqrt, [nbt], [nbt])
            kb.ts("dve", P["nb"].t[:], nbt.t[:, 0:8], -1.0, None, ALU.mult, None, [nbt], [P["nb"]])
            kb.S.flush()

    def phase_attn(self, l):
        kb, P, Dr, Dk = self.kb, self.P, self.Dr, self.Dk
        with contextlib.ExitStack() as st:
            va = kb.sb(st, "va", [128, NT, 520], BF16)
            kb.dma("pool", va.t[:], Dr["VA"].rearrange("(n p) h e -> p n (h e)", p=128), [Dk["VA"]], [va])
            kt_ = [kb.sb(st, "ktb%d" % j, [96, T], BF16) for j in range(2)]
            qt_ = [kb.sb(st, "qtb%d" % j, [96, T], BF16) for j in range(2)]
            pt = [kb.sb(st, "pt%d" % j, [128, 512], BF16) for j in range(4)]
            osb = [kb.sb(st, "osb%d" % j, [65, 512], F32) for j in range(2)]
            rec = [kb.sb(st, "rec%d" % j, [64, 512], F32) for j in range(2)]
            om = [kb.sb(st, "om%d" % j, [64, 512], BF16) for j in range(2)]
            ones = P["ones_f"]
            nb = P["nb"]
            cnt = 0
            blk = 0
            gla = self.gla_gen(l, st)

            def tick():
                try:
                    next(gla)
                except StopIteration:
                    pass
            for h in range(8):
                KT, QT = kt_[h % 2], qt_[h % 2]
                kb.dma("sp", KT.t[:], Dr["KT"][h], [Dk["KT"]], [KT])
                kb.dma("sp", QT.t[:], Dr["QT"][h], [Dk["QT"]], [QT])
                blocks = [(0, 256, [0, 1])] + [(CTX + qb * 512, 512, list(range(NT))) for qb in range(8)]
                for (q0, qn, keys) in blocks:
                    po = self.psum(pin=True)
                    pend = None
                    seq = []
                    for kt in keys:
                        if kt in (0, 11, 22) or (len(keys) == 2 and kt == 1):
                            tick()
                        ps = self.psum()
                        kb.mm(ps.t[:, 0:qn], KT.t[:, kt * 128:(kt + 1) * 128], QT.t[:, q0:q0 + qn], True, True, [KT, QT], [ps])
                        seq.append((kt, ps))
                        if len(seq) >= 2:
                            self._attn_pv(seq.pop(0), keys, po, pt, va, nb, h, qn, cnt)
                            cnt += 1
                    while seq:
                        self._attn_pv(seq.pop(0), keys, po, pt, va, nb, h, qn, cnt)
                        cnt += 1
                    O, R, OM = osb[blk % 2], rec[blk % 2], om[blk % 2]
                    blk += 1
                    kb.cp("act", O.t[:, 0:qn], po.t[0:65, 0:qn], [po], [O])
                    self.unpin(po)
                    pb = self.psum()
                    kb.mm(pb.t[0:64, 0:qn], ones.t[64:65, 0:64], O.t[64:65, 0:qn], True, True, [ones, O], [pb])
                    kb.recip(R.t[:, 0:qn], pb.t[0:64, 0:qn], [pb], [R])
                    kb.tt("pool", OM.t[:, 0:qn], O.t[0:64, 0:qn], R.t[:, 0:qn], ALU.mult, [O, R], [OM])
                    kb.dma("sp", Dr["OMT"][h, :, q0:q0 + qn], OM.t[:, 0:qn], [OM], [Dk["OMT"]])
            for _ in gla:
                pass
            self.gla_out(l, st)
            kb.S.flush()

    def _attn_pv(self, item, keys, po, pt, va, nb, h, qn, cnt):
        kb = self.kb
        kt, ps = item
        PT = pt[cnt % 4]
        kb.act(PT.t[:, 0:qn], ps.t[:, 0:qn], AF.Exp, [ps, nb], [PT], bias=nb.t[:, h:h + 1], scale=1.0)
        kb.mm(po.t[0:65, 0:qn], va.t[:, kt, h * 65:(h + 1) * 65], PT.t[:, 0:qn], kt == keys[0], kt == keys[-1], [va, PT], [po])

    def phase_fnet(self, l):
        kb, P, C, Dr, Dk = self.kb, self.P, self.C, self.Dr, self.Dk
        with contextlib.ExitStack() as st:
            A = kb.sb(st, "fA", [128, NT, 512], BF16)
            Bm = kb.sb(st, "fB", [128, NT, 512], BF16)
            cs = kb.sb(st, "cs", [128, 256], BF16)
            kb.dma("sp", cs.t[:], C["cs128"], [], [cs])
            uf = [kb.sb(st, "uf%d" % j, [128, 4, 128], BF16) for j in range(2)]
            import os
            for i in range(int(os.environ.get("FN_TILES", NT))):
                U = uf[i % 2]
                kb.dma("sp", U.t[:], Dr["UFT"][:, :, i * 128:(i + 1) * 128].rearrange("g c t -> c g t"), [Dk["UFT"]], [U])
                for half in range(2):
                    ps = self.psum()
                    for g2 in range(2):
                        g = half * 2 + g2
                        kb.mm(ps.t[:, g2 * 256:(g2 + 1) * 256], U.t[:, g, :], cs.t[:], True, True, [U, cs], [ps])
                    psv = ps.t[:].rearrange("p (g x m) -> p g x m", g=2, x=2)
                    kb.cp("act", A.t[:, i, half * 256:(half + 1) * 256].rearrange("p (g m) -> p g m", g=2), psv[:, :, 0, :], [ps], [A])
                    kb.cp("dve", Bm.t[:, i, half * 256:(half + 1) * 256].rearrange("p (g m) -> p g m", g=2), psv[:, :, 1, :], [ps], [Bm])
            dc = [kb.sb(st, "dc%d" % j, [128, 32, 128], BF16) for j in range(2)]
            ds = [kb.sb(st, "ds%d" % j, [128, 32, 128], BF16) for j in range(2)]
            y = [kb.sb(st, "fy%d" % j, [128, 512], BF16) for j in range(2)]
            oft = [kb.sb(st, "oft%d" % j, [128, 4, 128], BF16) for j in range(2)]
            idb = P["ident_b"]
            jobs = [("c", kt) for kt in range(2)] + [("l", kt) for kt in range(32)]
            import os
            if os.environ.get("FN_PART") == "1":
                jobs = []
            if os.environ.get("FN_PART") == "2":
                jobs = jobs[:2]
            for n, (kind, kt) in enumerate(jobs):
                DC, DS, Y, OF = dc[n % 2], ds[n % 2], y[n % 2], oft[n % 2]
                if kind == "c":
                    ntt, t0, tok0 = 2, 0, kt * 128
                    kb.dma("sp", DC.t[:, 0:2, :], C["dft_c2"][kt], [], [DC])
                    kb.dma("pool", DS.t[:, 0:2, :], C["dft_s2"][kt], [], [DS])
                else:
                    ntt, t0, tok0 = 32, 2, CTX + kt * 128
                    kb.dma("sp", DC.t[:], C["dft_c"][kt], [], [DC])
                    kb.dma("pool", DS.t[:], C["dft_s"][kt], [], [DS])
                ps = self.psum()
                for tt in range(ntt):
                    kb.mm(ps.t[:], DC.t[:, tt, :], A.t[:, t0 + tt, :], tt == 0, False, [DC, A], [ps])
                    kb.mm(ps.t[:], DS.t[:, tt, :], Bm.t[:, t0 + tt, :], False, tt == ntt - 1, [DS, Bm], [ps])
                kb.cp("act", Y.t[:], ps.t[:], [ps], [Y])
                p2 = self.psum()
                p2b = p2.t[:].bitcast(BF16)
                for g in range(4):
                    kb.tr(p2b[:, g * 128:(g + 1) * 128], Y.t[:, g * 128:(g + 1) * 128], idb.t[:], [Y, idb], [p2])
                kb.cp("dve", OF.t[:].rearrange("p g t -> p (g t)"), p2b[:, 0:512], [p2], [OF])
                kb.dma("sp", Dr["OFT"][:, :, tok0:tok0 + 128].rearrange("g m t -> m g t"), OF.t[:], [OF], [Dk["OFT"]])
            kb.S.flush()

    def gla_gen(self, l, st):
        kb, P, C, W, Dr, Dk = self.kb, self.P, self.C, self.W, self.Dr, self.Dk
        tri = kb.sb(st, "tri", [128, 2, 128], F32)
        mask4 = kb.sb(st, "mask4", [128, 2, 512], F32)
        for d in range(2):
            kb.dma("sp", tri.t[:, d, :], C["tri_f"][d], [], [tri])
            kb.dma("sp", mask4.t[:, d, :], C["mask4"][d].rearrange("p h t -> p (h t)"), [], [mask4])
        S32 = [kb.sb(st, "S32_%d" % d, [64, 4, 128], F32) for d in range(2)]
        Sb = [kb.sb(st, "Sb_%d" % d, [64, 4, 128], BF16) for d in range(2)]
        NB2 = 2

        def mk(name, shp, dt):
            return [[kb.sb(st, "%s%d_%d" % (name, d, j), shp, dt) for j in range(NB2)] for d in range(2)]
        qT = mk("gqT", [64, 4, 128], BF16)
        kT = mk("gkT", [64, 4, 128], BF16)
        gk = mk("ggk", [128, 768], BF16)
        lf = mk("glf", [128, 256], F32)
        gtok = mk("gtok", [128, 256], F32)
        e1 = mk("ge1", [128, 256], F32)
        khat = mk("khat", [128, 256], BF16)
        eq = mk("geq", [64, 4, 128], F32)
        ek = mk("gek", [64, 4, 128], F32)
        qtl = mk("qtl", [64, 4, 128], BF16)
        ktl = mk("ktl", [64, 4, 128], BF16)
        at = mk("gat", [128, 512], BF16)
        o32 = mk("go32", [128, 512], F32)
        orders = [list(range(NT)), [1, 0] + list(range(NT - 1, 1, -1))]
        lasts = [127, 0]
        outs = ["OFW", "OBW"]
        for d in range(2):
            kb.memset("dve", S32[d].t[:], 0.0, [], [S32[d]])
            kb.memset("pool", Sb[d].t[:], 0.0, [], [Sb[d]])

        def load(n, d):
            b = n % NB2
            i = orders[d][n]
            sl = slice(i * 128, (i + 1) * 128)
            kb.dma("sp", qT[d][b].t[:], Dr["GQT"][:, :, sl].rearrange("h d t -> d h t"), [Dk["GQT"]], [qT[d][b]])
            kb.dma("sp", kT[d][b].t[:], Dr["GKT"][:, :, sl].rearrange("h d t -> d h t"), [Dk["GKT"]], [kT[d][b]])
            kb.dma("sp", gk[d][b].t[:], Dr["GKVO"][sl, 0:768], [Dk["GKVO"]], [gk[d][b]])
            kb.dma("sp", lf[d][b].t[:], Dr["LFB"][sl, d * 256:(d + 1) * 256], [Dk["LFB"]], [lf[d][b]])
        load(0, 0)
        load(0, 1)
        for n in range(NT):
            for d in range(2):
                b = n % NB2
                i = orders[d][n]
                sl = slice(i * 128, (i + 1) * 128)
                last = lasts[d]
                if n + 1 < NT:
                    load(n + 1, d)
                LF, GK = lf[d][b], gk[d][b]
                GT, E1, KH, EQ, EK, QTL, KTL, AT, O32 = gtok[d][b], e1[d][b], khat[d][b], eq[d][b], ek[d][b], qtl[d][b], ktl[d][b], at[d][b], o32[d][b]
                pg = self.psum()
                kb.mm(pg.t[:, 0:256], tri.t[:, d, :], LF.t[:], True, True, [tri, LF], [pg])
                kb.mm(pg.t[:, 256:512], P["ones_f"].t[:], LF.t[:], True, True, [P["ones_f"], LF], [pg])
                pf = self.psum()
                for h in range(4):
                    kb.mm(pf.t[0:64, h * 128:(h + 1) * 128], LF.t[:, h * 64:(h + 1) * 64], tri.t[:, d, :], True, True, [LF, tri], [pf])
                kb.cp("act", GT.t[:], pg.t[:, 0:256], [pg], [GT])
                kb.tt("dve", E1.t[:], pg.t[:, 256:512], GT.t[:], ALU.subtract, [pg, GT], [E1])
                kb.act(E1.t[:], E1.t[:], AF.Exp, [E1], [E1])
                kb.tt("dve", KH.t[:], GK.t[:, 0:256], E1.t[:], ALU.mult, [GK, E1], [KH])
                kb.act(EQ.t[:].rearrange("p h t -> p (h t)"), pf.t[0:64, :], AF.Exp, [pf], [EQ])
                kb.act(EK.t[:].rearrange("p h t -> p (h t)"), pf.t[0:64, :], AF.Exp, [pf], [EK], scale=-1.0)
                kb.tt("dve", QTL.t[:], qT[d][b].t[:], EQ.t[:], ALU.mult, [qT[d][b], EQ], [QTL])
                kb.tt("pool", KTL.t[:], kT[d][b].t[:], EK.t[:], ALU.mult, [kT[d][b], EK], [KTL])
                yield
                pa = self.psum()
                for h in range(4):
                    kb.mm(pa.t[:, h * 128:(h + 1) * 128], KTL.t[:, h, :], QTL.t[:, h, :], True, True, [KTL, QTL], [pa])
                kb.tt("dve", AT.t[:], pa.t[:], mask4.t[:, d, :], ALU.mult, [pa, mask4], [AT])
                yield
                po = self.psum()
                for h in range(4):
                    kb.mm(po.t[:, h * 128:(h + 1) * 128], QTL.t[:, h, :], Sb[d].t[:, h, :], True, False, [QTL, Sb[d]], [po])
                    kb.mm(po.t[:, h * 128:(h + 1) * 128], AT.t[:, h * 128:(h + 1) * 128],
                          GK.t[:, 256 + h * 128:256 + (h + 1) * 128], False, True, [AT, GK], [po])
                pS = self.psum()
                for h in range(4):
                    kb.mm(pS.t[0:64, h * 128:(h + 1) * 128], KH.t[:, h * 64:(h + 1) * 64],
                          GK.t[:, 256 + h * 128:256 + (h + 1) * 128], True, True, [KH, GK], [pS])
                for h in range(4):
                    kb.stt("dve", S32[d].t[:, h, :], S32[d].t[:, h, :], EQ.t[:, h, last:last + 1], pS.t[0:64, h * 128:(h + 1) * 128],
                           ALU.mult, ALU.add, [S32[d], EQ, pS], [S32[d]])
                kb.cp("pool", Sb[d].t[:], S32[d].t[:], [S32[d]], [Sb[d]])
                kb.cp("act", O32.t[:], po.t[:], [po], [O32])
                kb.dma("sp", Dr[outs[d]][sl, :], O32.t[:], [O32], [Dk[outs[d]]])
                yield

    def gla_out(self, l, st):
        kb, P, W, Dr, Dk = self.kb, self.P, self.W, self.Dr, self.Dk
        idb = P["ident_b"]
        gn = kb.sb(st, "gn", [128, 512], F32)
        kb.dma("sp", gn.t[:], W["gla_norm_g"][l].partition_broadcast(128), [], [gn])
        NB2 = 2

        def mk(name, shp, dt):
            return [kb.sb(st, "%s%d" % (name, j), shp, dt) for j in range(NB2)]
        ofw = mk("oofw", [128, 512], F32)
        obw = mk("oobw", [128, 512], F32)
        og = mk("oog", [128, 512], BF16)
        sqo = mk("osq", [128, 512], F32)
        st4 = mk("ost4", [128, 8], F32)
        ob = mk("oob", [128, 512], BF16)
        ogt = mk("oogt", [128, 4, 128], BF16)
        for i in range(NT):
            b = i % NB2
            sl = slice(i * 128, (i + 1) * 128)
            kb.dma("sp", ofw[b].t[:], Dr["OFW"][sl, :], [Dk["OFW"]], [ofw[b]])
            kb.dma("sp", obw[b].t[:], Dr["OBW"][sl, :], [Dk["OBW"]], [obw[b]])
            kb.dma("sp", og[b].t[:], Dr["GKVO"][sl, 768:1280], [Dk["GKVO"]], [og[b]])
            O, SQ, S4 = ofw[b], sqo[b], st4[b]
            kb.tt("dve", O.t[:], O.t[:], obw[b].t[:], ALU.add, [O, obw[b]], [O])
            kb.tt("pool", SQ.t[:], O.t[:], O.t[:], ALU.mult, [O], [SQ])
            kb.red("dve", S4.t[:, 0:4], SQ.t[:].rearrange("p (h e) -> p h e", h=4), ALU.add, [SQ], [S4])
            kb.ts("dve", S4.t[:, 0:4], S4.t[:, 0:4], 1.0 / 128, EPS, ALU.mult, ALU.add, [S4], [S4])
            kb.act(S4.t[:, 0:4], S4.t[:, 0:4], AF.Sqrt, [S4], [S4])
            kb.recip(S4.t[:, 4:8], S4.t[:, 0:4], [S4], [S4])
            for h in range(4):
                kb.stt("dve", O.t[:, h * 128:(h + 1) * 128], O.t[:, h * 128:(h + 1) * 128], S4.t[:, 4 + h:5 + h],
                       gn.t[:, h * 128:(h + 1) * 128], ALU.mult, ALU.mult, [O, S4, gn], [O])
            kb.tt("pool", ob[b].t[:], O.t[:], og[b].t[:], ALU.mult, [O, og[b]], [ob[b]])
            p2 = self.psum()
            p2b = p2.t[:].bitcast(BF16)
            for g in range(4):
                kb.tr(p2b[:, g * 128:(g + 1) * 128], ob[b].t[:, g * 128:(g + 1) * 128], idb.t[:], [ob[b], idb], [p2])
            kb.cp("act", ogt[b].t[:].rearrange("p g t -> p (g t)"), p2b[:, 0:512], [p2], [ogt[b]])
            kb.dma("sp", Dr["OGT"][:, :, sl].rearrange("g m t -> m g t"), ogt[b].t[:], [ogt[b]], [Dk["OGT"]])

    def phase_merge(self, l):
        kb, P, W, Dr, Dk = self.kb, self.P, self.W, self.Dr, self.Dk
        with contextlib.ExitStack() as st:
            rows = self.mod_rows(st, l, 1, names=("G",))
            wbm = kb.sb(st, "wbm", [64, 8, D], BF16)
            wbf = kb.sb(st, "wbf", [128, 4, D], BF16)
            wbg = kb.sb(st, "wbg", [128, 4, D], BF16)
            wo = kb.sb(st, "wo", [128, 8, D], BF16)
            kb.dma("pool", wbm.t[:], W["w_br_mla"][l].rearrange("(h d) n -> d h n", d=64), [], [wbm])
            kb.dma("pool", wbf.t[:], W["w_br_fnet"][l].rearrange("(k p) n -> p k n", p=128), [], [wbf])
            kb.dma("pool", wbg.t[:], W["w_br_gla"][l].rearrange("(k p) n -> p k n", p=128), [], [wbg])
            kb.dma("pool", wo.t[:], W["w_o"][l].rearrange("(k p) n -> p k n", p=128), [], [wo])
            NB2 = 2
            def mk(name, shp, dt):
                return [kb.sb(st, "%s%d" % (name, j), shp, dt) for j in range(NB2)]
            omT = mk("momT", [64, 8, 128], BF16)
            ofT = mk("mofT", [128, 4, 128], BF16)
            ogT = mk("mogT", [128, 4, 128], BF16)
            ga = mk("mga", [128, 3072], BF16)
            xt = mk("mxt", [128, D], F32)
            y32 = mk("my32", [128, D], F32)
            t32 = mk("mt32", [128, D], F32)
            yb = mk("myb", [128, D], BF16)
            yT = mk("myT", [128, 8, 128], BF16)
            xn = mk("mxn", [128, D], F32)
            idb = P["ident_b"]
            for i in range(NT):
                b = i % NB2
                sl = slice(i * 128, (i + 1) * 128)
                G = rows["Gc" if i < 2 else "Gl"]
                kb.dma("sp", omT[b].t[:], Dr["OMT"][:, :, sl].rearrange("h d t -> d h t"), [Dk["OMT"]], [omT[b]])
                kb.dma("sp", ofT[b].t[:], Dr["OFT"][:, :, sl].rearrange("g m t -> m g t"), [Dk["OFT"]], [ofT[b]])
                kb.dma("sp", ogT[b].t[:], Dr["OGT"][:, :, sl].rearrange("g m t -> m g t"), [Dk["OGT"]], [ogT[b]])
                kb.dma("sp", ga[b].t[:], Dr["GATE"][sl, :], [Dk["GATE"]], [ga[b]])
                kb.dma("sp", xt[b].t[:], Dr["XR"][sl, :], [Dk["XR"]], [xt[b]])
                for half in range(2):
                    cs_ = slice(half * 512, (half + 1) * 512)
                    pm = self.psum()
                    for h in range(8):
                        kb.mm(pm.t[:], omT[b].t[:, h, :], wbm.t[:, h, cs_], h == 0, h == 7, [omT[b], wbm], [pm])
                    pf = self.psum()
                    for k in range(4):
                        kb.mm(pf.t[:], ofT[b].t[:, k, :], wbf.t[:, k, cs_], k == 0, k == 3, [ofT[b], wbf], [pf])
                    pg = self.psum()
                    for k in range(4):
                        kb.mm(pg.t[:], ogT[b].t[:, k, :], wbg.t[:, k, cs_], k == 0, k == 3, [ogT[b], wbg], [pg])
                    Y, T32 = y32[b], t32[b]
                    kb.tt("dve", Y.t[:, cs_], pm.t[:], ga[b].t[:, half * 512:(half + 1) * 512], ALU.mult, [pm, ga[b]], [Y])
                    kb.tt("dve", T32.t[:, cs_], pf.t[:], ga[b].t[:, 1024 + half * 512:1024 + (half + 1) * 512], ALU.mult, [pf, ga[b]], [T32])
                    kb.tt("pool", Y.t[:, cs_], Y.t[:, cs_], T32.t[:, cs_], ALU.add, [Y, T32], [Y])
                    kb.tt("dve", T32.t[:, cs_], pg.t[:], ga[b].t[:, 2048 + half * 512:2048 + (half + 1) * 512], ALU.mult, [pg, ga[b]], [T32])
                    kb.tt("pool", yb[b].t[:, cs_], Y.t[:, cs_], T32.t[:, cs_], ALU.add, [Y, T32], [yb[b]])
                p2 = self.psum()
                p2b = p2.t[:].bitcast(BF16)
                for kc in range(8):
                    kb.tr(p2b[:, kc * 128:(kc + 1) * 128], yb[b].t[:, kc * 128:(kc + 1) * 128], idb.t[:], [yb[b], idb], [p2])
                kb.cp("act", yT[b].t[:].rearrange("p k t -> p (k t)"), p2b, [p2], [yT[b]])
                for half in range(2):
                    cs_ = slice(half * 512, (half + 1) * 512)
                    pz = self.psum()
                    for kc in range(8):
                        kb.mm(pz.t[:], yT[b].t[:, kc, :], wo.t[:, kc, cs_], kc == 0, kc == 7, [yT[b], wo], [pz])
                    kb.tt("dve", xn[b].t[:, cs_], pz.t[:], G.t[:, cs_], ALU.mult, [pz, G], [xn[b]])
                    kb.tt("pool", xn[b].t[:, cs_], xn[b].t[:, cs_], xt[b].t[:, cs_], ALU.add, [xn[b], xt[b]], [xn[b]])
                kb.dma("sp", Dr["XR"][sl, :], xn[b].t[:], [xn[b]], [Dk["XR"]])
            kb.S.flush()

    def phase_moe(self, l):
        kb, P, W, C, Dr, Dk = self.kb, self.P, self.W, self.C, self.Dr, self.Dk
        is_last = (l == self.nl - 1)
        idb = P["ident_b"]
        with contextlib.ExitStack() as st0:
            DSTI = kb.sb(st0, "DSTI", [128, NT, 4], I32)
            G4 = kb.sb(st0, "G4", [128, NT, 4], F32)
            IDXW = kb.sb(st0, "IDXW", [128, NBLK, 8], I32)
            EBI = kb.sb(st0, "EBI", [128, NBLK], I32)
            with contextlib.ExitStack() as st:
                rows = self.mod_rows(st, l, 2, names=("A", "S"))
                rw = kb.sb(st, "rw", [128, 8, NE], F32)
                rb = kb.sb(st, "rb", [1, NE], F32)
                trx = kb.sb(st, "trx", [128, 128], BF16)
                jv = kb.sb(st, "jv", [128, NBLK], F32)
                kp = kb.sb(st, "kp", [128, 8], F32)
                kb.dma("sp", rw.t[:], W["router_w"][l].rearrange("(k p) e -> p k e", p=128), [], [rw])
                kb.dma("sp", rb.t[:], W["router_b"][l].rearrange("(o e) -> o e", o=1), [], [rb])
                kb.dma("sp", trx.t[:], C["tri_x"], [], [trx])
                kb.dma("sp", jv.t[:], C["jv"], [], [jv])
                kb.dma("sp", kp.t[:], C["kp"], [], [kp])
                LG = kb.sb(st, "LG", [128, NT, NE], F32)
                GF = kb.sb(st, "GF", [128, NT, NE], F32)
                POS = kb.sb(st, "POS", [128, NT, NE], F32)
                TOP = kb.sb(st, "TOP", [128, NT, 8], F32)
                cnt = kb.sb(st, "cnt", [128, NE], F32)
                kb.memset("dve", cnt.t[:], 0.0, [], [cnt])
                NB2 = 2
                def mk(name, shp, dt):
                    return [kb.sb(st, "%s%d" % (name, j), shp, dt) for j in range(NB2)]
                xt = mk("ext", [128, D], F32)
                junk = kb.sb(st, "ejunk", [128, D], F32)
                ssq = mk("essq", [128, 8], F32)
                h32 = mk("eh32", [128, D], F32)
                hb = mk("ehb", [128, D], BF16)
                h2T = mk("eh2T", [128, 8, 128], F32)
                sm = mk("esm", [128, 4, NE], F32)
                sc = mk("esc", [128, 8], F32)
                mkb = mk("emkb", [128, NE], BF16)
                for i in range(NT):
                    b = i % NB2
                    sl = slice(i * 128, (i + 1) * 128)
                    kb.dma("sp", xt[b].t[:], Dr["XR"][sl, :], [Dk["XR"]], [xt[b]])
                    kb.memset("pool", ssq[b].t[:], 0.0, [], [ssq[b]])
                    self.norm_mod(xt[b], rows, i, junk, ssq[b], h32[b], hb[b], full32=True)
                    kb.dma("sp", Dr["H2B"][sl, :], hb[b].t[:], [hb[b]], [Dk["H2B"]])
                    for half in range(2):
                        pt_ = self.psum()
                        for k4 in range(4):
                            kc = half * 4 + k4
                            kb.tr(pt_.t[:, k4 * 128:(k4 + 1) * 128], h32[b].t[:, kc * 128:(kc + 1) * 128], P["ident_f"].t[:],
                                  [h32[b], P["ident_f"]], [pt_])
                        kb.cp("act" if half == 0 else "dve", h2T[b].t[:, half * 4:(half + 1) * 4, :].rearrange("p k t -> p (k t)"),
                              pt_.t[:], [pt_], [h2T[b]])
                    pl = self.psum()
                    for kc in range(8):
                        kb.mm(pl.t[:, 0:NE], h2T[b].t[:, kc, :], rw.t[:, kc, :], kc == 0, False, [h2T[b], rw], [pl])
                    kb.mm(pl.t[:, 0:NE], P["ones_f"].t[0:1, :], rb.t[0:1, :], False, True, [P["ones_f"], rb], [pl])
                    kb.cp("act", LG.t[:, i, :], pl.t[:, 0:NE], [pl], [LG])
                    kb.op("dve", lambda e, o=TOP.t[:, i, :], a=LG.t[:, i, :]: e.max(out=o, in_=a), [LG], [TOP])
                    SM, SC = sm[b], sc[b]
                    kb.ts("dve", SM.t[:, 0, :], LG.t[:, i, :], TOP.t[:, i, 3:4], None, ALU.is_ge, None, [LG, TOP], [SM])
                    kb.ts("dve", SC.t[:, 0:1], TOP.t[:, i, 0:1], -1.0, None, ALU.mult, None, [TOP], [SC])
                    kb.act(SM.t[:, 1, :], LG.t[:, i, :], AF.Exp, [LG, SC], [SM], bias=SC.t[:, 0:1], scale=1.0)
                    kb.tt("dve", SM.t[:, 2, :], SM.t[:, 1, :], SM.t[:, 0, :], ALU.mult, [SM], [SM])
                    kb.red("dve", SC.t[:, 1:2], SM.t[:, 2, :], ALU.add, [SM], [SC])
                    kb.recip(SC.t[:, 2:3], SC.t[:, 1:2], [SC], [SC])
                    kb.ts("dve", GF.t[:, i, :], SM.t[:, 2, :], SC.t[:, 2:3], None, ALU.mult, None, [SM, SC], [GF])
                    kb.cp("pool", mkb[b].t[:], SM.t[:, 0, :], [SM], [mkb[b]])
                    pp = self.psum()
                    kb.mm(pp.t[:, 0:NE], trx.t[:], mkb[b].t[:], True, True, [trx, mkb[b]], [pp])
                    kb.mm(pp.t[:, NE:2 * NE], P["ones_b"].t[:], mkb[b].t[:], True, True, [P["ones_b"], mkb[b]], [pp])
                    kb.tt("dve", POS.t[:, i, :], pp.t[:, 0:NE], cnt.t[:], ALU.add, [pp, cnt], [POS])
                    kb.tt("dve", cnt.t[:], pp.t[:, NE:2 * NE], cnt.t[:], ALU.add, [pp, cnt], [cnt])
                nbk = kb.sb(st, "nbk", [128, NE], F32)
                pend = kb.sb(st, "pend", [128, NE], F32)
                pstart = kb.sb(st, "pstart", [128, NE], F32)
                eb = kb.sb(st, "eb", [128, NBLK], F32)
                idxf = kb.sb(st, "idxf", [128, NBLK, 8], F32)
                kb.memset("dve", nbk.t[:], 0.0, [], [nbk])
                for j in range(T // BS + 1):
                    kb.stt("dve", nbk.t[:], cnt.t[:], float(j * BS), nbk.t[:], ALU.is_gt, ALU.add, [cnt, nbk], [nbk])
                kb.ts("dve", nbk.t[:], nbk.t[:], float(BS), None, ALU.mult, None, [nbk], [nbk])
                kb.cp("dve", pend.t[:, 0:1], nbk.t[:, 0:1], [nbk], [pend])
                for e_ in range(1, NE):
                    kb.tt("dve", pend.t[:, e_:e_ + 1], pend.t[:, e_ - 1:e_], nbk.t[:, e_:e_ + 1], ALU.add, [pend, nbk], [pend])
                kb.tt("dve", pstart.t[:], pend.t[:], nbk.t[:], ALU.subtract, [pend, nbk], [pstart])
                kb.memset("dve", eb.t[:], 0.0, [], [eb])
                for e_ in range(NE):
                    kb.stt("dve", eb.t[:], jv.t[:], pend.t[:, e_:e_ + 1], eb.t[:], ALU.is_ge, ALU.add, [jv, pend, eb], [eb])
                kb.ts("dve", eb.t[:], eb.t[:], float(NE - 1), None, ALU.min, None, [eb], [eb])
                kb.ts("dve", kp.t[:], kp.t[:], float(l * NE * D), None, ALU.add, None, [kp], [kp])
                for kc in range(8):
                    kb.ts("dve", idxf.t[:, :, kc], eb.t[:], 1024.0, kp.t[:, kc:kc + 1], ALU.mult, ALU.add, [eb, kp], [idxf])
                kb.cp("dve", IDXW.t[:], idxf.t[:], [idxf], [IDXW])
                kb.ts("dve", eb.t[:], eb.t[:], float(l * NE), None, ALU.add, None, [eb], [eb])
                kb.cp("dve", EBI.t[:], eb.t[:], [eb], [EBI])
                dstf = mk("edstf", [128, 4], F32)
                hs = mk("ehs", [128, D], BF16)
                for i in range(NT):
                    b = i % NB2
                    sl = slice(i * 128, (i + 1) * 128)
                    SM = sm[b]
                    kb.tt("dve", SM.t[:, 3, :], POS.t[:, i, :], pstart.t[:], ALU.add, [POS, pstart], [SM])
                    for k in range(4):
                        kb.ts("dve", SM.t[:, 0, :], LG.t[:, i, :], TOP.t[:, i, k:k + 1], None, ALU.is_equal, None, [LG, TOP], [SM])
                        kb.tt("dve", SM.t[:, 1, :], SM.t[:, 0, :], SM.t[:, 3, :], ALU.mult, [SM], [SM])
                        kb.red("dve", dstf[b].t[:, k:k + 1], SM.t[:, 1, :], ALU.add, [SM], [dstf[b]])
                        kb.tt("dve", SM.t[:, 2, :], SM.t[:, 0, :], GF.t[:, i, :], ALU.mult, [SM, GF], [SM])
                        kb.red("dve", G4.t[:, i, k:k + 1], SM.t[:, 2, :], ALU.add, [SM], [G4])
                    kb.cp("dve", DSTI.t[:, i, :], dstf[b].t[:], [dstf[b]], [DSTI])
                    kb.dma("sp", hs[b].t[:], Dr["H2B"][sl, :], [Dk["H2B"]], [hs[b]])
                    for k in range(4):
                        kb.scatter(Dr["XS"], hs[b].t[:], DSTI.t[:, i, k:k + 1], [hs[b], DSTI], [Dk["XS"]], NROWS - 1)
                if "DBG_DST" in self.dbg:
                    kb.dma("sp", Dr["DBG_DST"], DSTI.t[:].rearrange("p n k -> p (n k)"), [DSTI], [Dk["DBG_DST"]])
                    kb.dma("sp", Dr["DBG_G4"], G4.t[:].rearrange("p n k -> p (n k)"), [G4], [Dk["DBG_G4"]])
                    kb.dma("sp", Dr["DBG_EB"], EBI.t[:], [EBI], [Dk["DBG_EB"]])
                    kb.dma("sp", Dr["DBG_LG"], LG.t[:].rearrange("p n k -> p (n k)"), [LG], [Dk["DBG_LG"]])
                    kb.dma("sp", Dr["DBG_CNT"], cnt.t[:], [cnt], [Dk["DBG_CNT"]])
                kb.S.flush()
            with contextlib.ExitStack() as st:
                wup = [kb.sb(st, "wup%d" % j, [128, 8, 2 * D], BF16) for j in range(2)]
                wdn = [kb.sb(st, "wdn%d" % j, [128, 8, D], BF16) for j in range(2)]
                wupk = [[Tok() for _ in range(8)] for _ in range(2)]
                wdnk = [[Tok() for _ in range(8)] for _ in range(2)]
                bu = [kb.sb(st, "bu%d" % j, [2, 2 * D], BF16) for j in range(2)]
                bd = [kb.sb(st, "bd%d" % j, [2, D], BF16) for j in range(2)]
                NB2 = 2
                def mk(name, shp, dt):
                    return [kb.sb(st, "%s%d" % (name, j), shp, dt) for j in range(NB2)]
                xs = mk("xs", [128, D], BF16)
                xT = mk("xT", [128, 8, 128], BF16)
                gl = mk("gl", [128, 512], F32)
                li = mk("li", [128, 512], F32)
                sg = mk("sg", [128, 512], F32)
                ab = mk("ab", [128, D], BF16)
                aT = mk("aT", [128, 8, 128], BF16)
                yb = mk("yb", [128, D], F32)
                wup_src = W["exp_w_up"].rearrange("l e k n -> (l e k) n")
                wdn_src = W["exp_w_down"].rearrange("l e k n -> (l e k) n")
                bup_src = W["exp_b_up"].rearrange("l e n -> (l e) n")
                bdn_src = W["exp_b_down"].rearrange("l e n -> (l e) n")
                nlw = W["exp_w_up"].shape[0]
                ones1 = P["ones_b"].t[0:1, :]

                def load_w(j):
                    jb = j % 2
                    for kc in range(8):
                        kb.gather(wup[jb].t[:, kc, :], wup_src, IDXW.t[:, j, kc:kc + 1], [IDXW], [wupk[jb][kc]], nlw * NE * D - 1)
                    for kc in range(8):
                        kb.gather(wdn[jb].t[:, kc, :], wdn_src, IDXW.t[:, j, kc:kc + 1], [IDXW], [wdnk[jb][kc]], nlw * NE * D - 1)
                    kb.gather(bu[jb].t[:], bup_src, EBI.t[0:2, j:j + 1], [EBI], [bu[jb]], nlw * NE - 1)
                    kb.gather(bd[jb].t[:], bdn_src, EBI.t[0:2, j:j + 1], [EBI], [bd[jb]], nlw * NE - 1)

                def load_x(n):
                    kb.dma("sp", xs[n % NB2].t[:], Dr["XS"][n * 128:(n + 1) * 128, :], [Dk["XS"]], [xs[n % NB2]])

                NSUB = NBLK * SUB

                def stage_a(n):
                    b = n % NB2
                    if n + 1 < NSUB:
                        load_x(n + 1)
                    p2 = self.psum()
                    p2b = p2.t[:].bitcast(BF16)
                    for kc in range(8):
                        kb.tr(p2b[:, kc * 128:(kc + 1) * 128], xs[b].t[:, kc * 128:(kc + 1) * 128], idb.t[:], [xs[b], idb], [p2])
                    kb.cp("act", xT[b].t[:].rearrange("p k t -> p (k t)"), p2b, [p2], [xT[b]])

                def stage_u(n):
                    b = n % NB2
                    jb = (n // SUB) % 2
                    WU, BU = wup[jb], bu[jb]
                    for pair in range(2):
                        pgl = self.psum()
                        pli = self.psum()
                        for (pp_, c0) in ((pgl, pair * 512), (pli, D + pair * 512)):
                            for kc in range(8):
                                kb.mm(pp_.t[:], xT[b].t[:, kc, :], WU.t[:, kc, c0:c0 + 512], kc == 0, False, [xT[b], wupk[jb][kc]], [pp_])
                            kb.mm(pp_.t[:], ones1, BU.t[0:1, c0:c0 + 512], False, True, [P["ones_b"], BU], [pp_])
                        GL, LI, SG = gl[pair], li[pair], sg[pair]
                        kb.ts("dve", GL.t[:], pgl.t[:], 7.0, None, ALU.min, None, [pgl], [GL])
                        kb.act(SG.t[:], GL.t[:], AF.Sigmoid, [GL], [SG], scale=1.702)
                        kb.ts("dve", LI.t[:], pli.t[:], 7.0, -7.0, ALU.min, ALU.max, [pli], [LI])
                        kb.stt("dve", LI.t[:], LI.t[:], 1.0, GL.t[:], ALU.add, ALU.mult, [LI, GL], [LI])
                        kb.tt("dve", ab[b].t[:, pair * 512:(pair + 1) * 512], LI.t[:], SG.t[:], ALU.mult, [LI, SG], [ab[b]])

                def stage_d(n):
                    b = n % NB2
                    jb = (n // SUB) % 2
                    WD, BD = wdn[jb], bd[jb]
                    r0 = n * 128
                    p3 = self.psum()
                    p3b = p3.t[:].bitcast(BF16)
                    for kc in range(8):
                        kb.tr(p3b[:, kc * 128:(kc + 1) * 128], ab[b].t[:, kc * 128:(kc + 1) * 128], idb.t[:], [ab[b], idb], [p3])
                    kb.cp("act", aT[b].t[:].rearrange("p k t -> p (k t)"), p3b, [p3], [aT[b]])
                    for half in range(2):
                        pd = self.psum()
                        for kc in range(8):
                            kb.mm(pd.t[:], aT[b].t[:, kc, :], WD.t[:, kc, half * 512:(half + 1) * 512], kc == 0, False, [aT[b], wdnk[jb][kc]], [pd])
                        kb.mm(pd.t[:], ones1, BD.t[0:1, half * 512:(half + 1) * 512], False, True, [P["ones_b"], BD], [pd])
                        kb.cp("act" if half == 0 else "dve", yb[b].t[:, half * 512:(half + 1) * 512], pd.t[:], [pd], [yb[b]])
                    kb.dma("sp", Dr["YB"][r0:r0 + 128, :], yb[b].t[:], [yb[b]], [Dk["YB"]])

                load_w(0)
                load_w(1)
                load_x(0)
                stage_a(0)
                stage_u(0)
                for n in range(NSUB):
                    if n + 1 < NSUB:
                        stage_a(n + 1)
                        stage_u(n + 1)
                    stage_d(n)
                    if (n + 1) % SUB == 0:
                        j = n // SUB
                        if j + 2 < NBLK:
                            load_w(j + 2)
                kb.S.flush()
            with contextlib.ExitStack() as st:
                rows = self.mod_rows(st, l, 2, names=("G",))
                NB2 = 2
                def mk(name, shp, dt):
                    return [kb.sb(st, "%s%d" % (name, j), shp, dt) for j in range(NB2)]
                xt = mk("cxt", [128, D], F32)
                yk = [kb.sb(st, "cyk%d" % j, [128, D], F32) for j in range(4)]
                acc = mk("cacc", [128, D], F32)
                xn = mk("cxn", [128, D], F32)
                if is_last:
                    fg = kb.sb(st, "fg", [128, D], F32)
                    kb.dma("sp", fg.t[:], W["final_norm_g"].partition_broadcast(128), [], [fg])
                    junk = kb.sb(st, "cjunk", [128, D], F32)
                    ssq = mk("cssq", [128, 8], F32)
                    ot = mk("cot", [128, D], F32)
                for i in range(NT):
                    b = i % NB2
                    sl = slice(i * 128, (i + 1) * 128)
                    G = rows["Gc" if i < 2 else "Gl"]
                    kb.dma("sp", xt[b].t[:], Dr["XR"][sl, :], [Dk["XR"]], [xt[b]])
                    for k in range(4):
                        kb.gather(yk[k].t[:], Dr["YB"], DSTI.t[:, i, k:k + 1], [DSTI, Dk["YB"]], [yk[k]], NROWS - 1)
                    A_ = acc[b]
                    kb.ts("dve", A_.t[:], yk[0].t[:], G4.t[:, i, 0:1], None, ALU.mult, None, [yk[0], G4], [A_])
                    for k in range(1, 4):
                        kb.stt("dve", A_.t[:], yk[k].t[:], G4.t[:, i, k:k + 1], A_.t[:], ALU.mult, ALU.add, [yk[k], G4, A_], [A_])
                    kb.tt("pool", A_.t[:], A_.t[:], G.t[:], ALU.mult, [A_, G], [A_])
                    kb.tt("dve", xn[b].t[:], A_.t[:], xt[b].t[:], ALU.add, [A_, xt[b]], [xn[b]])
                    kb.dma("sp", Dr["XR"][sl, :], xn[b].t[:], [xn[b]], [Dk["XR"]])
                    if is_last and i >= 2:
                        SS = ssq[b]
                        kb.memset("pool", SS.t[:], 0.0, [], [SS])
                        kb.act(junk.t[:], xn[b].t[:], AF.Square, [xn[b]], [junk, SS], accum=SS.t[:, 0:1])
                        kb.ts("dve", SS.t[:, 1:2], SS.t[:, 0:1], 1.0 / D, EPS, ALU.mult, ALU.add, [SS], [SS])
                        kb.act(SS.t[:, 1:2], SS.t[:, 1:2], AF.Sqrt, [SS], [SS])
                        kb.recip(SS.t[:, 2:3], SS.t[:, 1:2], [SS], [SS])
                        kb.stt("dve", ot[b].t[:], xn[b].t[:], SS.t[:, 2:3], fg.t[:], ALU.mult, ALU.mult, [xn[b], SS, fg], [ot[b]])
                        kb.dma("sp", self.out[(i - 2) * 128:(i - 1) * 128, :], ot[b].t[:], [ot[b]], [self.tout])
                kb.S.flush()


_CACHE = {}


def kernel(**inputs):
    n_cores = 8
    if "nc" not in _CACHE:
        pg = Prog(nl=DEPTH)
        _CACHE["nc"] = pg.build()
        _CACHE["consts"] = host_consts()
    nc = _CACHE["nc"]
    consts = _CACHE["consts"]
    shared = {k: np.ascontiguousarray(np.asarray(inputs[k], dtype=np.float32)) for k in WEIGHT_SPECS}
    shared["c_ctx"] = np.ascontiguousarray(np.asarray(inputs["c_ctx"], dtype=np.float32))
    shared.update(consts)
    x = np.asarray(inputs["x"], dtype=np.float32)
    c = np.asarray(inputs["c"], dtype=np.float32)
    ctx = np.asarray(inputs["ctx"], dtype=np.float32)
    in_maps = []
    for b in range(n_cores):
        m = dict(shared)
        m["x"] = np.ascontiguousarray(x[b])
        m["ctx"] = np.ascontiguousarray(ctx[b])
        m["c"] = np.ascontiguousarray(c[b])
        in_maps.append(m)
    res = run_bass_kernel_spmd(nc, in_maps, core_ids=list(range(n_cores)))
    out = np.stack([np.asarray(res.results[b]["out"], dtype=np.float32) for b in range(n_cores)], axis=0)
    return out
```

```python
import contextlib
import math
import numpy as np
import ml_dtypes
import concourse.bass as bass
import concourse.mybir as mybir
from concourse.bass_utils import run_bass_kernel_spmd

F32 = mybir.dt.float32
BF16 = mybir.dt.bfloat16
I32 = mybir.dt.int32
AF = mybir.ActivationFunctionType
ALU = mybir.AluOpType
AX = mybir.AxisListType

D = 1024
SEQ = 4096
CTX = 256
T = SEQ + CTX
NT = T // 128
DEPTH = 4
INW = 5568
NE = 32
BS = 256
SUB = BS // 128
NBLK = (T * 4) // BS + NE
NROWS = NBLK * BS
EPS = 1e-6
MLA_SCALE = 96 ** -0.5
GLA_SCALE = 64 ** -0.5
O_UQ, O_KV, O_FN, O_GQ, O_GK, O_GV, O_OG, O_GF, O_GB, O_GATE = 0, 256, 416, 928, 1184, 1440, 1952, 2464, 2480, 2496


import os as _os
SAME_ENGINE_SYNC = _os.environ.get("NOSAME", "0") != "1"


class Tok:
    __slots__ = ("w", "rs")

    def __init__(self):
        self.w = None
        self.rs = {}


class Sched:
    NDMA = 32

    def __init__(self, nc, st):
        self.nc = nc
        self.engs = ["pe", "act", "dve", "pool", "sp"]
        self.ops = {k: [] for k in self.engs}
        self.cnt = {k: 0 for k in self.engs}
        self.seen = {k: {} for k in self.engs}
        self.dma_k = {"d": 0, "g": 0}
        self.dma_last = {}
        self.n_ops = 0
        self.sems = {}
        for k in ["e_" + e for e in self.engs] + ["d%d" % i for i in range(self.NDMA)] + ["g%d" % i for i in range(self.NDMA)]:
            self.sems[k] = st.enter_context(nc.semaphore(k))

    def _need(self, eng, ev, waits):
        if ev is None:
            return
        src, key, val = ev
        if src == eng and (eng == "pe" or not SAME_ENGINE_SYNC):
            return
        if self.seen[eng].get(key, 0) >= val:
            return
        self.seen[eng][key] = val
        waits.append((key, val))

    def op(self, eng, fn, r=(), w=(), dma=False):
        waits = []
        for t in r:
            self._need(eng, t.w, waits)
        for t in w:
            self._need(eng, t.w, waits)
            for ev in t.rs.values():
                self._need(eng, ev, waits)
        if dma:
            pre = "g" if eng == "pool" else "d"
            k = self.dma_k[pre]
            self.dma_k[pre] += 1
            key = "%s%d" % (pre, k % self.NDMA)
            val = 16 * (k // self.NDMA + 1)
            if val > 16:
                self._need(eng, ("dma", key, val - 16), waits)
            ev = ("dma", key, val)
            inc = (key, 16)
            self.dma_last[key] = val
        else:
            self.cnt[eng] += 1
            key = "e_" + eng
            ev = (eng, key, self.cnt[eng])
            inc = (key, 1)
        self.ops[eng].append((waits, fn, inc))
        self.n_ops += 1
        for t in w:
            t.w = ev
            t.rs = {}
        for t in r:
            if t not in w:
                t.rs[ev[1]] = ev
        return ev

    def barrier(self):
        evs = [(e, "e_" + e, self.cnt[e]) for e in self.engs if self.cnt[e] > 0]
        evs += [("dma", k, v) for k, v in self.dma_last.items()]
        for eng in self.engs:
            waits = []
            for ev in evs:
                if ev[0] == eng:
                    continue
                self._need(eng, ev, waits)
            if waits:
                self.ops[eng].append((waits, None, None))

    def flush(self):
        self.barrier()
        nc = self.nc
        sems = self.sems
        ops = self.ops
        with nc.Block() as block:
            def mk(engname):
                def body(e):
                    for waits, fn, inc in ops[engname]:
                        for key, val in waits:
                            e.wait_ge(sems[key], val)
                        if fn is not None:
                            fn(e).then_inc(sems[inc[0]], inc[1])
                return body
            block.tensor(mk("pe"))
            block.scalar(mk("act"))
            block.vector(mk("dve"))
            block.gpsimd(mk("pool"))
            block.sync(mk("sp"))
        self.ops = {k: [] for k in self.engs}


class B:
    __slots__ = ("t", "k", "psum")

    def __init__(self, t, psum=False):
        self.t = t
        self.k = Tok()
        self.psum = psum


def _toks(xs):
    return [x.k if isinstance(x, B) else x for x in xs]


class KB:
    def __init__(self, nc, st):
        self.nc = nc
        self.S = Sched(nc, st)
        self.dq = 0
        self.bregs = {}

    def sb(self, st, name, shape, dt):
        self.dq += 1
        return B(st.enter_context(self.nc.sbuf_tensor("%s_u%d" % (name, self.dq), shape, dt)))

    def op(self, eng, fn, r, w, dma=False):
        r2 = [x for x in r if not (isinstance(x, B) and x.psum)]
        w2 = list(w) + [x for x in r if isinstance(x, B) and x.psum and x not in w]
        return self.S.op(eng, fn, r=_toks(r2), w=_toks(w2), dma=dma)

    def mm(self, out, lhsT, rhs, start, stop, r, w):
        self.op("pe", lambda e: e.matmul(out, lhsT, rhs, start=start, stop=stop), r, w)

    def tr(self, out, in_, ident, r, w):
        self.op("pe", lambda e: e.transpose(out, in_, ident), r, w)

    def act(self, out, in_, func, r, w, bias=None, scale=None, accum=None):
        kw = {}
        if bias is not None:
            kw["bias"] = bias
        if scale is not None:
            kw["scale"] = scale
        if accum is not None:
            kw["accum_out"] = accum
        self.op("act", lambda e: e.activation(out=out, in_=in_, func=func, **kw), r, w)

    def cp(self, eng, out, in_, r, w):
        if eng == "act":
            self.op("act", lambda e: e.copy(out=out, in_=in_), r, w)
        else:
            self.op(eng, lambda e: e.tensor_copy(out=out, in_=in_), r, w)

    def tt(self, eng, out, in0, in1, op, r, w):
        self.op(eng, lambda e: e.tensor_tensor(out=out, in0=in0, in1=in1, op=op), r, w)

    def ts(self, eng, out, in0, s1, s2, op0, op1, r, w):
        if op1 is None:
            self.op(eng, lambda e: e.tensor_scalar(out=out, in0=in0, scalar1=s1, scalar2=None, op0=op0), r, w)
        else:
            self.op(eng, lambda e: e.tensor_scalar(out=out, in0=in0, scalar1=s1, scalar2=s2, op0=op0, op1=op1), r, w)

    def stt(self, eng, out, in0, scalar, in1, op0, op1, r, w):
        self.op(eng, lambda e: e.scalar_tensor_tensor(out=out, in0=in0, scalar=scalar, in1=in1, op0=op0, op1=op1), r, w)

    def red(self, eng, out, in_, op, r, w, axis=AX.X):
        self.op(eng, lambda e: e.tensor_reduce(out=out, in_=in_, axis=axis, op=op), r, w)

    def recip(self, out, in_, r, w):
        self.op("dve", lambda e: e.reciprocal(out=out, in_=in_), r, w)

    def memset(self, eng, out, val, r, w):
        self.op(eng, lambda e: e.memset(out, val), r, w)

    def dma(self, eng, out, in_, r, w, slow=False):
        if slow:
            self.op(eng, lambda e: e.dma_start(out=out, in_=in_, allow_slow_non_contiguous=True), r, w, dma=True)
        else:
            self.op(eng, lambda e: e.dma_start(out=out, in_=in_), r, w, dma=True)

    def _breg(self, e, bound):
        if bound not in self.bregs:
            self.bregs[bound] = e.to_reg(bound)
        return self.bregs[bound]

    def gather(self, out, in_, idx, r, w, bound):
        self.op("pool", lambda e: e.indirect_dma_start(
            out=out, out_offset=None, in_=in_, in_offset=bass.IndirectOffsetOnAxis(ap=idx, axis=0),
            bounds_check=self._breg(e, bound), oob_is_err=False), r, w, dma=True)

    def scatter(self, out, in_, idx, r, w, bound):
        self.op("pool", lambda e: e.indirect_dma_start(
            out=out, out_offset=bass.IndirectOffsetOnAxis(ap=idx, axis=0), in_=in_, in_offset=None,
            bounds_check=self._breg(e, bound), oob_is_err=False), r, w, dma=True)


def host_consts():
    c = {}
    bf = ml_dtypes.bfloat16
    i = np.arange(128)
    c["ident_f"] = np.eye(128, dtype=np.float32)
    c["ident_b"] = np.eye(128).astype(bf)
    trif = (i[:, None] <= i[None, :]).astype(np.float32)
    trib = (i[:, None] >= i[None, :]).astype(np.float32)
    c["tri_f"] = np.stack([trif, trib], 0)
    c["mask4"] = np.stack([np.repeat(trif[:, None, :], 4, 1), np.repeat(trib[:, None, :], 4, 1)], 0).astype(np.float32)
    c["tri_x"] = (i[:, None] < i[None, :]).astype(bf)
    c["ones_f"] = np.ones((128, 128), np.float32)
    c["ones_b"] = np.ones((128, 128)).astype(bf)
    inv = 10000.0 ** (-np.arange(0, 16, 2, dtype=np.float32) / 16)
    row = np.repeat(np.arange(64, dtype=np.float32), 64)
    col = np.tile(np.arange(64, dtype=np.float32), 64)
    ar = row[:, None] * inv
    ac = col[:, None] * inv
    cr, sr, cc, sc = (np.cos(ar).astype(np.float32), np.sin(ar).astype(np.float32),
                      np.cos(ac).astype(np.float32), np.sin(ac).astype(np.float32))
    cos32 = np.concatenate([cr, cr, cc, cc], 1)
    sin32 = np.concatenate([-sr, sr, -sc, sc], 1)
    cos32 = np.concatenate([np.ones((CTX, 32), np.float32), cos32], 0)
    sin32 = np.concatenate([np.zeros((CTX, 32), np.float32), sin32], 0)
    c["rope_k"] = np.stack([cos32, sin32], 1).astype(np.float32)
    rq = np.stack([np.tile(cos32, (1, 8)), np.tile(sin32, (1, 8))], 1) * np.float32(MLA_SCALE)
    c["rope_q"] = rq.astype(np.float32)
    ang = 2 * np.pi * np.outer(i, i) / 128.0
    c["cs128"] = (np.concatenate([np.cos(ang), np.sin(ang)], 1) / math.sqrt(128)).astype(bf)
    n = np.arange(SEQ, dtype=np.int64)
    kt = (np.outer(n, n) % SEQ).astype(np.float64) * (2 * np.pi / SEQ)
    ctm = (np.cos(kt) / 64.0).astype(np.float32).reshape(32, 128, 32, 128)
    stm = (-np.sin(kt) / 64.0).astype(np.float32).reshape(32, 128, 32, 128)
    c["dft_c"] = np.ascontiguousarray(ctm.transpose(2, 1, 0, 3)).astype(bf)
    c["dft_s"] = np.ascontiguousarray(stm.transpose(2, 1, 0, 3)).astype(bf)
    del kt, ctm, stm
    m = np.arange(CTX, dtype=np.int64)
    k2 = (np.outer(m, m) % CTX).astype(np.float64) * (2 * np.pi / CTX)
    c2 = (np.cos(k2) / 16.0).astype(np.float32).reshape(2, 128, 2, 128)
    s2 = (-np.sin(k2) / 16.0).astype(np.float32).reshape(2, 128, 2, 128)
    c["dft_c2"] = np.ascontiguousarray(c2.transpose(2, 1, 0, 3)).astype(bf)
    c["dft_s2"] = np.ascontiguousarray(s2.transpose(2, 1, 0, 3)).astype(bf)
    c["jv"] = np.tile((np.arange(NBLK, dtype=np.float32) * BS)[None, :], (128, 1))
    c["kp"] = (np.arange(8, dtype=np.float32)[None, :] * 128 + i[:, None]).astype(np.float32)
    return c


CONST_SPECS = {
    "ident_f": ([128, 128], F32), "ident_b": ([128, 128], BF16), "tri_f": ([2, 128, 128], F32),
    "mask4": ([2, 128, 4, 128], F32), "tri_x": ([128, 128], BF16), "ones_f": ([128, 128], F32),
    "ones_b": ([128, 128], BF16), "rope_k": ([T, 2, 32], F32), "rope_q": ([T, 2, 256], F32),
    "cs128": ([128, 256], BF16), "dft_c": ([32, 128, 32, 128], BF16), "dft_s": ([32, 128, 32, 128], BF16),
    "dft_c2": ([2, 128, 2, 128], BF16), "dft_s2": ([2, 128, 2, 128], BF16),
    "jv": ([128, NBLK], F32), "kp": ([128, 8], F32),
}

WEIGHT_SPECS = {
    "w_mod": [DEPTH, D, 6 * D], "b_mod": [DEPTH, 6 * D], "norm1_g": [DEPTH, D], "w_in": [DEPTH, D, INW],
    "mla_q_norm_g": [DEPTH, 256], "mla_w_uq": [DEPTH, 256, 768], "mla_kv_norm_g": [DEPTH, 128],
    "mla_w_ukv": [DEPTH, 128, 1024], "gla_w_gate_f": [DEPTH, 16, 256], "gla_b_gate_f": [DEPTH, 256],
    "gla_w_gate_b": [DEPTH, 16, 256], "gla_b_gate_b": [DEPTH, 256], "gla_norm_g": [DEPTH, 512],
    "w_br_mla": [DEPTH, 512, D], "w_br_fnet": [DEPTH, 512, D], "w_br_gla": [DEPTH, 512, D],
    "w_o": [DEPTH, D, D], "norm2_g": [DEPTH, D], "router_w": [DEPTH, D, NE], "router_b": [DEPTH, NE],
    "exp_w_up": [DEPTH, NE, D, 2 * D], "exp_b_up": [DEPTH, NE, 2 * D], "exp_w_down": [DEPTH, NE, D, D],
    "exp_b_down": [DEPTH, NE, D], "final_norm_g": [D],
}


class Prog:
    def __init__(self, nl=DEPTH, dbg=(), stop_after=None, wdepth=DEPTH):
        self.nl = nl
        self.dbg = set(dbg)
        self.stop_after = stop_after
        self.nc = bass.Bass("TRN2", target_bir_lowering=False)
        self.st = contextlib.ExitStack()
        self.kb = KB(self.nc, self.st)
        nc = self.nc
        self.x_in = nc.dram_tensor("x", [SEQ, D], F32, kind="ExternalInput").ap()
        self.ctx_in = nc.dram_tensor("ctx", [CTX, D], F32, kind="ExternalInput").ap()
        self.c_in = nc.dram_tensor("c", [D], F32, kind="ExternalInput").ap()
        self.cc_in = nc.dram_tensor("c_ctx", [D], F32, kind="ExternalInput").ap()
        self.W = {k: nc.dram_tensor(k, ([wdepth] + shp[1:]) if (len(shp) > 1 or k in ('norm1_g',)) and shp[0] == DEPTH and k != 'final_norm_g' else shp, F32, kind="ExternalInput").ap() for k, shp in WEIGHT_SPECS.items()}
        self.C = {k: nc.dram_tensor(k, shp, dt, kind="ExternalInput").ap() for k, (shp, dt) in CONST_SPECS.items()}
        self.out = nc.dram_tensor("out", [SEQ, D], F32, kind="ExternalOutput").ap()
        self.tout = Tok()
        self.Dr = {}
        self.Dk = {}
        for name, shp, dt in [
            ("XR", [T, D], F32), ("KT", [8, 96, T], BF16), ("QT", [8, 96, T], BF16), ("VA", [T, 8, 65], BF16),
            ("UFT", [4, 128, T], BF16), ("GQT", [4, 64, T], BF16), ("GKT", [4, 64, T], BF16),
            ("GKVO", [T, 1280], BF16), ("LFB", [T, 512], F32), ("GATE", [T, 3072], BF16),
            ("OMT", [8, 64, T], BF16), ("OFT", [4, 128, T], BF16), ("OGT", [4, 128, T], BF16),
            ("OFW", [T, 512], F32), ("OBW", [T, 512], F32), ("XS", [NROWS, D], BF16), ("YB", [NROWS, D], F32),
            ("H2B", [T, D], BF16), ("R_LG", [128, NT * NE], F32), ("R_GF", [128, NT * NE], F32), ("R_POS", [128, NT * NE], F32), ("R_TOP", [128, NT * 8], F32), ("R_CNT", [128, NE], F32), ("DBG_DST", [128, NT * 4], I32), ("DBG_G4", [128, NT * 4], F32),
            ("DBG_EB", [128, NBLK], I32), ("DBG_LG", [128, NT * NE], F32), ("DBG_CNT", [128, NE], F32),
        ]:
            kind = "ExternalOutput" if name in self.dbg else "Internal"
            self.Dr[name] = nc.dram_tensor(name, shp, dt, kind=kind).ap()
            self.Dk[name] = Tok()

    def build(self):
        kb = self.kb
        nc = self.nc
        with self.st:
            P = self.P = {}
            for name, shp, dt in [
                ("ident_f", [128, 128], F32), ("ident_b", [128, 128], BF16), ("ones_f", [128, 128], F32),
                ("ones_b", [128, 128], BF16), ("modT", [128, 48, 2], F32), ("nb", [128, 8], F32),
                ("scT", [128, 8, 2], F32),
            ]:
                P[name] = kb.sb(self.st, "p_" + name, shp, dt)
            self.ps = [B(self.st.enter_context(nc.psum_tensor("ps%d" % i, [128, 512], F32)), psum=True) for i in range(8)]
            self.psi = 0
            self.pinned = set()
            for nm in ["ident_f", "ident_b", "ones_f", "ones_b"]:
                kb.dma("sp", P[nm].t[:], self.C[nm], [], [P[nm]])
            kb.dma("sp", self.Dr["XR"][0:CTX, :], self.ctx_in, [], [self.Dk["XR"]])
            kb.dma("pool", self.Dr["XR"][CTX:T, :], self.x_in, [], [self.Dk["XR"]])
            kb.S.flush()
            for l in range(self.nl):
                import os
                skip = os.environ.get("SKIP_PH", "").split(",")
                for ph in [self.phase_mod, self.phase_a, self.phase_attn, self.phase_fnet,
                           self.phase_merge, self.phase_moe]:
                    if ph.__name__ in skip:
                        continue
                    ph(l)
                    kb.S.flush()
                    if self.stop_after == (l, ph.__name__):
                        break
                else:
                    continue
                break
            kb.S.flush()
        return nc

    def psum(self, pin=False):
        while True:
            idx = self.psi % 8
            self.psi += 1
            if idx not in self.pinned:
                break
        if pin:
            self.pinned.add(idx)
        return self.ps[idx]

    def unpin(self, p):
        self.pinned.discard(self.ps.index(p))

    def phase_mod(self, l):
        kb, P, W = self.kb, self.P, self.W
        with contextlib.ExitStack() as st:
            cT = kb.sb(st, "cT", [128, 8, 2], F32)
            sT = kb.sb(st, "sT", [128, 8, 2], F32)
            bT = kb.sb(st, "bT", [128, 48], F32)
            wm = [kb.sb(st, "wm%d" % i, [128, 8, 1024], F32) for i in range(2)]
            kb.dma("sp", cT.t[:, :, 0], self.c_in.rearrange("(k p) -> p k", p=128), [], [cT], slow=True)
            kb.dma("sp", cT.t[:, :, 1], self.cc_in.rearrange("(k p) -> p k", p=128), [], [cT], slow=True)
            kb.dma("sp", bT.t[:], W["b_mod"][l].rearrange("(j p) -> p j", p=128), [], [bT], slow=True)
            kb.act(sT.t[:], cT.t[:], AF.Silu, [cT], [sT])
            for sec in range(6):
                w = wm[sec % 2]
                kb.dma("sp" if sec % 2 == 0 else "pool", w.t[:],
                       W["w_mod"][l][:, sec * 1024:(sec + 1) * 1024].rearrange("(k p) n -> p k n", p=128), [], [w])
                ps = self.psum()
                for j in range(8):
                    for kc in range(8):
                        kb.mm(ps.t[:, j * 2:j * 2 + 2], w.t[:, kc, j * 128:(j + 1) * 128], sT.t[:, kc, :],
                              kc == 0, kc == 7, [w, sT], [ps])
                for j in range(8):
                    jj = sec * 8 + j
                    kb.ts("dve", P["modT"].t[:, jj, :], ps.t[:, j * 2:j * 2 + 2], bT.t[:, jj:jj + 1], None, ALU.add, None,
                          [ps, bT], [P["modT"]])
            kb.S.flush()

    def bcast_rows(self, st, name, colT_ap_fn, r):
        kb, P = self.kb, self.P
        out = kb.sb(st, name, [128, 1024], F32)
        tmp = kb.sb(st, name + "_t", [128, 128], F32)
        for half in range(2):
            ps = self.psum()
            for k4 in range(4):
                kc = half * 4 + k4
                kb.ts("dve", tmp.t[:], P["ones_f"].t[:], colT_ap_fn(kc), None, ALU.mult, None, r + [P["ones_f"]], [tmp])
                kb.mm(ps.t[:, k4 * 128:(k4 + 1) * 128], tmp.t[:], P["ident_f"].t[:], True, True, [tmp, P["ident_f"]], [ps])
            kb.cp("act", out.t[:, half * 512:(half + 1) * 512], ps.t[:], [ps], [out])
        return out

    def mod_rows(self, st, l, which, names=("A", "S", "G")):
        kb, P, W = self.kb, self.P, self.W
        sec_sh, sec_sc, sec_g = (0, 1, 2) if which == 1 else (3, 4, 5)
        gT = kb.sb(st, "gT", [128, 8], F32)
        kb.dma("sp", gT.t[:], W["norm1_g" if which == 1 else "norm2_g"][l].rearrange("(k p) -> p k", p=128), [], [gT], slow=True)
        aT = kb.sb(st, "aT", [128, 8, 2], F32)
        m = P["modT"]
        for v in range(2):
            kb.stt("dve", aT.t[:, :, v], m.t[:, sec_sc * 8:(sec_sc + 1) * 8, v], 1.0, gT.t[:], ALU.add, ALU.mult, [m, gT], [aT])
        rows = {}
        for v, nm in ((0, "l"), (1, "c")):
            if "A" in names:
                rows["A" + nm] = self.bcast_rows(st, "rA" + nm, lambda kc, v=v: aT.t[:, kc, v:v + 1], [aT])
            if "S" in names:
                rows["S" + nm] = self.bcast_rows(st, "rS" + nm, lambda kc, v=v: m.t[:, sec_sh * 8 + kc, v:v + 1], [m])
            if "G" in names:
                rows["G" + nm] = self.bcast_rows(st, "rG" + nm, lambda kc, v=v: m.t[:, sec_g * 8 + kc, v:v + 1], [m])
        return rows

    def norm_mod(self, xt, rows, i, junk, ssq, h32, hb, full32=False):
        kb = self.kb
        nm = "c" if i < 2 else "l"
        kb.act(junk.t[:], xt.t[:], AF.Square, [xt], [junk, ssq], accum=ssq.t[:, 0:1])
        kb.ts("dve", ssq.t[:, 1:2], ssq.t[:, 0:1], 1.0 / D, EPS, ALU.mult, ALU.add, [ssq], [ssq])
        kb.act(ssq.t[:, 1:2], ssq.t[:, 1:2], AF.Sqrt, [ssq], [ssq])
        kb.recip(ssq.t[:, 2:3], ssq.t[:, 1:2], [ssq], [ssq])
        kb.stt("dve", h32.t[:], xt.t[:], ssq.t[:, 2:3], rows["A" + nm].t[:], ALU.mult, ALU.mult, [xt, ssq, rows["A" + nm]], [h32])
        if full32:
            kb.tt("pool", h32.t[:], h32.t[:], rows["S" + nm].t[:], ALU.add, [h32, rows["S" + nm]], [h32])
            kb.cp("dve", hb.t[:], h32.t[:], [h32], [hb])
        else:
            kb.tt("pool", hb.t[:], h32.t[:], rows["S" + nm].t[:], ALU.add, [h32, rows["S" + nm]], [hb])

    def phase_a(self, l):
        kb, P, W, C, Dr, Dk = self.kb, self.P, self.W, self.C, self.Dr, self.Dk
        with contextlib.ExitStack() as st:
            rows = self.mod_rows(st, l, 1, names=("A", "S"))
            win = kb.sb(st, "win", [128, 8, INW], BF16)
            for kc in range(8):
                kb.dma("pool", win.t[:, kc, :], W["w_in"][l][kc * 128:(kc + 1) * 128, :], [], [win])
            wuq32 = kb.sb(st, "wuq32", [128, 2, 768], F32)
            wuq = kb.sb(st, "wuq", [128, 2, 768], BF16)
            gq = kb.sb(st, "gq", [128, 2], F32)
            wkv32 = kb.sb(st, "wkv32", [128, 1024], F32)
            wkv = kb.sb(st, "wkv", [128, 1024], BF16)
            gkv = kb.sb(st, "gkv", [128, 1], F32)
            wg = kb.sb(st, "wg", [17, 2, 256], F32)
            kb.dma("sp", wuq32.t[:], W["mla_w_uq"][l].rearrange("(k p) n -> p k n", p=128), [], [wuq32])
            kb.dma("sp", gq.t[:], W["mla_q_norm_g"][l].rearrange("(k p) -> p k", p=128), [], [gq], slow=True)
            kb.dma("sp", wkv32.t[:], W["mla_w_ukv"][l], [], [wkv32])
            kb.dma("sp", gkv.t[:], W["mla_kv_norm_g"][l].rearrange("(k p) -> p k", p=128), [], [gkv], slow=True)
            kb.dma("sp", wg.t[0:16, 0, :], W["gla_w_gate_f"][l], [], [wg])
            kb.dma("sp", wg.t[0:16, 1, :], W["gla_w_gate_b"][l], [], [wg])
            kb.dma("sp", wg.t[16:17, 0, :], W["gla_b_gate_f"][l].rearrange("(o n) -> o n", o=1), [], [wg])
            kb.dma("sp", wg.t[16:17, 1, :], W["gla_b_gate_b"][l].rearrange("(o n) -> o n", o=1), [], [wg])
            for kc in range(2):
                kb.ts("dve", wuq.t[:, kc, :], wuq32.t[:, kc, :], gq.t[:, kc:kc + 1], None, ALU.mult, None, [wuq32, gq], [wuq])
            kb.ts("dve", wkv.t[:], wkv32.t[:], gkv.t[:, 0:1], None, ALU.mult, None, [wkv32, gkv], [wkv])
            m16 = kb.sb(st, "m16", [128, 16], F32)
            kb.memset("dve", m16.t[:], 0.0, [], [m16])
            NB2 = 1
            def mk(name, shp, dt):
                return [kb.sb(st, "%s%d" % (name, j), shp, dt) for j in range(NB2)]
            def mk2(name, shp, dt):
                return [kb.sb(st, "%s%d" % (name, j), shp, dt) for j in range(2)]
            xt = mk2("xt", [128, D], F32)
            junk = mk("junk", [128, D], F32)
            ssq = mk2("ssq", [128, 8], F32)
            h32 = mk("h32", [128, D], F32)
            hb = mk2("hb", [128, D], BF16)
            hT = mk2("hT", [128, 8, 128], BF16)
            uqn = mk("uqn", [128, 384], BF16)
            kr = mk("kr", [128, 4, 32], F32)
            uT = mk("uT", [128, 3, 128], BF16)
            rq = mk("rq", [128, 2, 256], F32)
            rk = mk("rk", [128, 2, 32], F32)
            qo = mk("qo", [128, 8, 96], BF16)
            ko = mk("ko", [128, 8, 96], BF16)
            vo = mk("vo", [128, 8, 65], BF16)
            qtmp = mk("qtmp", [128, 3, 256], F32)
            sq = mk("sq", [128, 768], F32)
            n2 = mk("n2", [128, 16], F32)
            qkT = mk("qkT", [96, 16, 128], BF16)
            gkvo = mk("gkvo", [128, 1280], BF16)
            gate = mk("gate", [128, 3072], BF16)
            fT = mk("fT", [128, 4, 128], BF16)
            gqkT = mk("gqkT", [64, 8, 128], BF16)
            ugT = mk("ugT", [17, 2, 128], F32)
            lfb = mk("lfb", [128, 512], F32)
            lfe = mk("lfe", [128, 512], F32)
            for j in range(NB2):
                kb.memset("pool", vo[j].t[:], 1.0, [], [vo[j]])
                kb.memset("pool", ugT[j].t[:], 1.0, [], [ugT[j]])
            idb = P["ident_b"]
            for i in range(NT):
                b = i % NB2
                b2 = i % 2
                X, J, SS, H32, HB, HT = xt[b2], junk[b], ssq[b2], h32[b], hb[b2], hT[b2]
                kb.dma("sp", X.t[:], Dr["XR"][i * 128:(i + 1) * 128, :], [Dk["XR"]], [X])
                kb.dma("sp", rq[b].t[:], C["rope_q"][i * 128:(i + 1) * 128], [], [rq[b]])
                kb.dma("sp", rk[b].t[:], C["rope_k"][i * 128:(i + 1) * 128], [], [rk[b]])
                kb.memset("pool", SS.t[:], 0.0, [], [SS])
                self.norm_mod(X, rows, i, J, SS, H32, HB)
                pT = self.psum()
                pTb = pT.t[:].bitcast(BF16)
                for kc in range(8):
                    kb.tr(pTb[:, kc * 128:(kc + 1) * 128], HB.t[:, kc * 128:(kc + 1) * 128], idb.t[:], [HB, idb], [pT])
                kb.cp("act", HT.t[:].rearrange("p k t -> p (k t)"), pTb, [pT], [HT])

                def tm(c0, cw):
                    ps = self.psum()
                    for kc in range(8):
                        kb.mm(ps.t[:, 0:cw], HT.t[:, kc, :], win.t[:, kc, c0:c0 + cw], kc == 0, kc == 7, [HT, win], [ps])
                    return ps

                def fm(c0, cw, ps, o0):
                    for kc in range(8):
                        kb.mm(ps.t[0:cw, o0:o0 + 128], win.t[:, kc, c0:c0 + cw], HT.t[:, kc, :], kc == 0, kc == 7, [HT, win], [ps])

                ps1 = tm(0, 416)
                UQ, KR, UT, QO, KO, VO, QTMP, SQ, N2 = uqn[b], kr[b], uT[b], qo[b], ko[b], vo[b], qtmp[b], sq[b], n2[b]
                kb.act(J.t[:, 0:256], ps1.t[:, 0:256], AF.Square, [ps1], [J, SS], accum=SS.t[:, 3:4])
                kb.act(J.t[:, 256:384], ps1.t[:, 256:384], AF.Square, [ps1], [J, SS], accum=SS.t[:, 4:5])
                kb.ts("dve", SS.t[:, 5:6], SS.t[:, 3:4], 1.0 / 256, EPS, ALU.mult, ALU.add, [SS], [SS])
                kb.ts("dve", SS.t[:, 6:7], SS.t[:, 4:5], 1.0 / 128, EPS, ALU.mult, ALU.add, [SS], [SS])
                kb.act(SS.t[:, 5:7], SS.t[:, 5:7], AF.Sqrt, [SS], [SS])
                kb.recip(SS.t[:, 5:7], SS.t[:, 5:7], [SS], [SS])
                kb.ts("dve", UQ.t[:, 0:256], ps1.t[:, 0:256], SS.t[:, 5:6], None, ALU.mult, None, [ps1, SS], [UQ])
                kb.ts("dve", UQ.t[:, 256:384], ps1.t[:, 256:384], SS.t[:, 6:7], None, ALU.mult, None, [ps1, SS], [UQ])
                kb.cp("act", KR.t[:, 0, :], ps1.t[:, 384:416], [ps1], [KR])
                krv = KR.t[:, 0, :].rearrange("p (a f e) -> p a f e", a=2, f=2)
                krs = KR.t[:, 1, :].rearrange("p (a f e) -> p a f e", a=2, f=2)
                kb.cp("pool", krs[:, :, 0, :], krv[:, :, 1, :], [KR], [KR])
                kb.cp("pool", krs[:, :, 1, :], krv[:, :, 0, :], [KR], [KR])
                kb.tt("dve", KR.t[:, 2, :], KR.t[:, 0, :], rk[b].t[:, 0, :], ALU.mult, [KR, rk[b]], [KR])
                kb.tt("dve", KR.t[:, 3, :], KR.t[:, 1, :], rk[b].t[:, 1, :], ALU.mult, [KR, rk[b]], [KR])
                kb.tt("dve", KR.t[:, 0, :], KR.t[:, 2, :], KR.t[:, 3, :], ALU.add, [KR], [KR])
                p2 = self.psum()
                p2b = p2.t[:].bitcast(BF16)
                for j in range(3):
                    kb.tr(p2b[:, j * 128:(j + 1) * 128], UQ.t[:, j * 128:(j + 1) * 128], idb.t[:], [UQ, idb], [p2])
                kb.cp("act", UT.t[:].rearrange("p k t -> p (k t)"), p2b[:, 0:384], [p2], [UT])
                for hh in range(2):
                    pq = self.psum()
                    for kc in range(2):
                        kb.mm(pq.t[:, 0:384], UT.t[:, kc, :], wuq.t[:, kc, hh * 384:(hh + 1) * 384], kc == 0, kc == 1, [UT, wuq], [pq])
                    pqv = pq.t[:, 0:384].rearrange("p (h d) -> p h d", h=4)
                    qov = QO.t[:, hh * 4:(hh + 1) * 4, :]
                    kb.act(qov[:, :, 0:64], pqv[:, :, 0:64], AF.Identity, [pq], [QO], scale=MLA_SCALE)
                    t0 = QTMP.t[:, 0, hh * 128:(hh + 1) * 128].rearrange("p (h d) -> p h d", h=4)
                    t1 = QTMP.t[:, 1, hh * 128:(hh + 1) * 128].rearrange("p (h d) -> p h d", h=4)
                    kb.cp("act", t0, pqv[:, :, 64:96], [pq], [QTMP])
                    t0v = t0.rearrange("p h (a f e) -> p h a f e", a=2, f=2)
                    t1v = t1.rearrange("p h (a f e) -> p h a f e", a=2, f=2)
                    for a in range(2):
                        kb.cp("pool", t1v[:, :, a, 0, :], t0v[:, :, a, 1, :], [QTMP], [QTMP])
                        kb.cp("pool", t1v[:, :, a, 1, :], t0v[:, :, a, 0, :], [QTMP], [QTMP])
                    rc = rq[b].t[:, 0, hh * 128:(hh + 1) * 128].rearrange("p (h d) -> p h d", h=4)
                    rs = rq[b].t[:, 1, hh * 128:(hh + 1) * 128].rearrange("p (h d) -> p h d", h=4)
                    kb.tt("dve", t0, t0, rc, ALU.mult, [QTMP, rq[b]], [QTMP])
                    kb.tt("dve", t1, t1, rs, ALU.mult, [QTMP, rq[b]], [QTMP])
                    kb.tt("dve", qov[:, :, 64:96], t0, t1, ALU.add, [QTMP], [QO])
                for hh in range(2):
                    pk = self.psum()
                    kb.mm(pk.t[:], UT.t[:, 2, :], wkv.t[:, hh * 512:(hh + 1) * 512], True, True, [UT, wkv], [pk])
                    pkv = pk.t[:].rearrange("p (h d) -> p h d", h=4)
                    kb.cp("act", KO.t[:, hh * 4:(hh + 1) * 4, 0:64], pkv[:, :, 0:64], [pk], [KO])
                    kb.cp("dve", VO.t[:, hh * 4:(hh + 1) * 4, 0:64], pkv[:, :, 64:128], [pk], [VO])
                for h in range(8):
                    kb.cp("pool", KO.t[:, h, 64:96], KR.t[:, 0, :], [KR], [KO])
                kb.tt("dve", SQ.t[:], QO.t[:].rearrange("p h d -> p (h d)"), QO.t[:].rearrange("p h d -> p (h d)"), ALU.mult, [QO], [SQ])
                kb.red("dve", N2.t[:, 8:16], SQ.t[:].rearrange("p (h d) -> p h d", h=8), ALU.add, [SQ], [N2])
                kb.tt("dve", SQ.t[:], KO.t[:].rearrange("p h d -> p (h d)"), KO.t[:].rearrange("p h d -> p (h d)"), ALU.mult, [KO], [SQ])
                kb.red("dve", N2.t[:, 0:8], SQ.t[:].rearrange("p (h d) -> p h d", h=8), ALU.add, [SQ], [N2])
                kb.tt("dve", m16.t[:], m16.t[:], N2.t[:], ALU.max, [N2, m16], [m16])
                QKT = qkT[b]
                for which, src in ((0, KO), (1, QO)):
                    p3 = self.psum()
                    p3b = p3.t[:].bitcast(BF16)
                    for h in range(8):
                        kb.tr(p3b[0:96, h * 128:(h + 1) * 128], src.t[:, h, :], idb.t[:], [src, idb], [p3])
                    kb.cp("act" if which == 0 else "dve", QKT.t[:, which * 8:(which + 1) * 8, :].rearrange("p h t -> p (h t)"),
                          p3b[0:96, :], [p3], [QKT])
                kb.dma("sp", Dr["KT"][:, :, i * 128:(i + 1) * 128].rearrange("h d t -> d h t"), QKT.t[:, 0:8, :], [QKT], [Dk["KT"]])
                kb.dma("sp", Dr["QT"][:, :, i * 128:(i + 1) * 128].rearrange("h d t -> d h t"), QKT.t[:, 8:16, :], [QKT], [Dk["QT"]])
                kb.dma("sp", Dr["VA"][i * 128:(i + 1) * 128], VO.t[:], [VO], [Dk["VA"]])
                G = gkvo[b]
                for gi, (c0, cw) in enumerate(((O_GK, 512), (O_GK + 512, 512), (O_GK + 1024, 256))):
                    ps = tm(c0, cw)
                    o0 = c0 - O_GK
                    if gi == 0:
                        kb.cp("dve", G.t[:, 0:512], ps.t[:, 0:512], [ps], [G])
                    elif gi == 1:
                        kb.cp("dve", G.t[:, 512:768], ps.t[:, 0:256], [ps], [G])
                        kb.act(G.t[:, 768:1024], ps.t[:, 256:512], AF.Silu, [ps], [G])
                    else:
                        kb.act(G.t[:, 1024:1280], ps.t[:, 0:256], AF.Silu, [ps], [G])
                kb.dma("sp", Dr["GKVO"][i * 128:(i + 1) * 128, :], G.t[:], [G], [Dk["GKVO"]])
                GA = gate[b]
                for gi in range(6):
                    ps = tm(O_GATE + gi * 512, 512)
                    kb.act(GA.t[:, gi * 512:(gi + 1) * 512], ps.t[:], AF.Sigmoid, [ps], [GA])
                kb.dma("sp", Dr["GATE"][i * 128:(i + 1) * 128, :], GA.t[:], [GA], [Dk["GATE"]])
                ps = self.psum()
                for g in range(4):
                    fm(O_FN + g * 128, 128, ps, g * 128)
                kb.cp("dve", fT[b].t[:].rearrange("p g t -> p (g t)"), ps.t[:], [ps], [fT[b]])
                kb.dma("sp", Dr["UFT"][:, :, i * 128:(i + 1) * 128].rearrange("g c t -> c g t"), fT[b].t[:], [fT[b]], [Dk["UFT"]])
                psq = self.psum()
                psk = self.psum()
                for h in range(4):
                    fm(O_GQ + h * 64, 64, psq, h * 128)
                    fm(O_GK + h * 64, 64, psk, h * 128)
                kb.act(gqkT[b].t[:, 0:4, :].rearrange("p h t -> p (h t)"), psq.t[0:64, :], AF.Identity, [psq], [gqkT[b]], scale=GLA_SCALE)
                kb.cp("dve", gqkT[b].t[:, 4:8, :].rearrange("p h t -> p (h t)"), psk.t[0:64, :], [psk], [gqkT[b]])
                kb.dma("sp", Dr["GQT"][:, :, i * 128:(i + 1) * 128].rearrange("h d t -> d h t"), gqkT[b].t[:, 0:4, :], [gqkT[b]], [Dk["GQT"]])
                kb.dma("sp", Dr["GKT"][:, :, i * 128:(i + 1) * 128].rearrange("h d t -> d h t"), gqkT[b].t[:, 4:8, :], [gqkT[b]], [Dk["GKT"]])
                psg = self.psum()
                fm(O_GF, 16, psg, 0)
                fm(O_GB, 16, psg, 128)
                kb.cp("act", ugT[b].t[0:16, :, :].rearrange("p d t -> p (d t)"), psg.t[0:16, 0:256], [psg], [ugT[b]])
                psl = self.psum()
                for d in range(2):
                    kb.mm(psl.t[:, d * 256:(d + 1) * 256], ugT[b].t[:, d, :], wg.t[:, d, :], True, True, [ugT[b], wg], [psl])
                kb.act(lfe[b].t[:], psl.t[:], AF.Exp, [psl], [lfe[b]], scale=-1.0)
                kb.act(lfe[b].t[:], lfe[b].t[:], AF.Ln, [lfe[b]], [lfe[b]], bias=1.0)
                kb.ts("dve", lfb[b].t[:], lfe[b].t[:], -1.0 / 16.0, None, ALU.mult, None, [lfe[b]], [lfb[b]])
                kb.dma("sp", Dr["LFB"][i * 128:(i + 1) * 128, :], lfb[b].t[:], [lfb[b]], [Dk["LFB"]])
            pm = self.psum()
            kb.tr(pm.t[0:16, 0:128], m16.t[:], P["ident_f"].t[:], [m16, P["ident_f"]], [pm])
            mcol = kb.sb(st, "mcol", [16, 1], F32)
            mb = kb.sb(st, "mb", [16, 128], F32)
            kb.red("dve", mcol.t[:], pm.t[0:16, 0:128], ALU.max, [pm], [mcol])
            kb.ts("dve", mb.t[:], P["ones_f"].t[0:16, :], mcol.t[:, 0:1], None, ALU.mult, None, [mcol, P["ones_f"]], [mb])
            pm2 = self.psum()
            kb.mm(pm2.t[:, 0:16], mb.t[:], P["ident_f"].t[0:16, 0:16], True, True, [mb, P["ident_f"]], [pm2])
            nbt = kb.sb(st, "nbt", [128, 16], F32)
            kb.cp("act", nbt.t[:], pm2.t[:, 0:16], [pm2], [nbt])
            kb.tt("dve", nbt.t[:, 0:8], nbt.t[:, 0:8], nbt.t[:, 8:16], ALU.mult, [nbt], [nbt])
            kb.act(nbt.t[:, 0:8], nbt.t[:, 0:8], AF.Sqrt, [nbt], [nbt])
            kb.ts("dve", P["nb"].t[:], nbt.t[:, 0:8], -1.0, None, ALU.mult, None, [nbt], [P["nb"]])
            kb.S.flush()

    def phase_attn(self, l):
        kb, P, Dr, Dk = self.kb, self.P, self.Dr, self.Dk
        with contextlib.ExitStack() as st:
            va = kb.sb(st, "va", [128, NT, 520], BF16)
            kb.dma("pool", va.t[:], Dr["VA"].rearrange("(n p) h e -> p n (h e)", p=128), [Dk["VA"]], [va])
            kt_ = [kb.sb(st, "ktb%d" % j, [96, T], BF16) for j in range(2)]
            qt_ = [kb.sb(st, "qtb%d" % j, [96, T], BF16) for j in range(2)]
            pt = [kb.sb(st, "pt%d" % j, [128, 512], BF16) for j in range(4)]
            osb = [kb.sb(st, "osb%d" % j, [65, 512], F32) for j in range(2)]
            rec = [kb.sb(st, "rec%d" % j, [64, 512], F32) for j in range(2)]
            om = [kb.sb(st, "om%d" % j, [64, 512], BF16) for j in range(2)]
            ones = P["ones_f"]
            nb = P["nb"]
            cnt = 0
            blk = 0
            gla = self.gla_gen(l, st)

            def tick():
                try:
                    next(gla)
                except StopIteration:
                    pass
            for h in range(8):
                KT, QT = kt_[h % 2], qt_[h % 2]
                kb.dma("sp", KT.t[:], Dr["KT"][h], [Dk["KT"]], [KT])
                kb.dma("sp", QT.t[:], Dr["QT"][h], [Dk["QT"]], [QT])
                blocks = [(0, 256, [0, 1])] + [(CTX + qb * 512, 512, list(range(NT))) for qb in range(8)]
                for (q0, qn, keys) in blocks:
                    po = self.psum(pin=True)
                    pend = None
                    seq = []
                    for kt in keys:
                        if kt in (0, 11, 22) or (len(keys) == 2 and kt == 1):
                            tick()
                        ps = self.psum()
                        kb.mm(ps.t[:, 0:qn], KT.t[:, kt * 128:(kt + 1) * 128], QT.t[:, q0:q0 + qn], True, True, [KT, QT], [ps])
                        seq.append((kt, ps))
                        if len(seq) >= 2:
                            self._attn_pv(seq.pop(0), keys, po, pt, va, nb, h, qn, cnt)
                            cnt += 1
                    while seq:
                        self._attn_pv(seq.pop(0), keys, po, pt, va, nb, h, qn, cnt)
                        cnt += 1
                    O, R, OM = osb[blk % 2], rec[blk % 2], om[blk % 2]
                    blk += 1
                    kb.cp("act", O.t[:, 0:qn], po.t[0:65, 0:qn], [po], [O])
                    self.unpin(po)
                    pb = self.psum()
                    kb.mm(pb.t[0:64, 0:qn], ones.t[64:65, 0:64], O.t[64:65, 0:qn], True, True, [ones, O], [pb])
                    kb.recip(R.t[:, 0:qn], pb.t[0:64, 0:qn], [pb], [R])
                    kb.tt("pool", OM.t[:, 0:qn], O.t[0:64, 0:qn], R.t[:, 0:qn], ALU.mult, [O, R], [OM])
                    kb.dma("sp", Dr["OMT"][h, :, q0:q0 + qn], OM.t[:, 0:qn], [OM], [Dk["OMT"]])
            for _ in gla:
                pass
            self.gla_out(l, st)
            kb.S.flush()

    def _attn_pv(self, item, keys, po, pt, va, nb, h, qn, cnt):
        kb = self.kb
        kt, ps = item
        PT = pt[cnt % 4]
        kb.act(PT.t[:, 0:qn], ps.t[:, 0:qn], AF.Exp, [ps, nb], [PT], bias=nb.t[:, h:h + 1], scale=1.0)
        kb.mm(po.t[0:65, 0:qn], va.t[:, kt, h * 65:(h + 1) * 65], PT.t[:, 0:qn], kt == keys[0], kt == keys[-1], [va, PT], [po])

    def phase_fnet(self, l):
        kb, P, C, Dr, Dk = self.kb, self.P, self.C, self.Dr, self.Dk
        with contextlib.ExitStack() as st:
            A = kb.sb(st, "fA", [128, NT, 512], BF16)
            Bm = kb.sb(st, "fB", [128, NT, 512], BF16)
            cs = kb.sb(st, "cs", [128, 256], BF16)
            kb.dma("sp", cs.t[:], C["cs128"], [], [cs])
            uf = [kb.sb(st, "uf%d" % j, [128, 4, 128], BF16) for j in range(2)]
            import os
            for i in range(int(os.environ.get("FN_TILES", NT))):
                U = uf[i % 2]
                kb.dma("sp", U.t[:], Dr["UFT"][:, :, i * 128:(i + 1) * 128].rearrange("g c t -> c g t"), [Dk["UFT"]], [U])
                for half in range(2):
                    ps = self.psum()
                    for g2 in range(2):
                        g = half * 2 + g2
                        kb.mm(ps.t[:, g2 * 256:(g2 + 1) * 256], U.t[:, g, :], cs.t[:], True, True, [U, cs], [ps])
                    psv = ps.t[:].rearrange("p (g x m) -> p g x m", g=2, x=2)
                    kb.cp("act", A.t[:, i, half * 256:(half + 1) * 256].rearrange("p (g m) -> p g m", g=2), psv[:, :, 0, :], [ps], [A])
                    kb.cp("dve", Bm.t[:, i, half * 256:(half + 1) * 256].rearrange("p (g m) -> p g m", g=2), psv[:, :, 1, :], [ps], [Bm])
            dc = [kb.sb(st, "dc%d" % j, [128, 32, 128], BF16) for j in range(2)]
            ds = [kb.sb(st, "ds%d" % j, [128, 32, 128], BF16) for j in range(2)]
            y = [kb.sb(st, "fy%d" % j, [128, 512], BF16) for j in range(2)]
            oft = [kb.sb(st, "oft%d" % j, [128, 4, 128], BF16) for j in range(2)]
            idb = P["ident_b"]
            jobs = [("c", kt) for kt in range(2)] + [("l", kt) for kt in range(32)]
            import os
            if os.environ.get("FN_PART") == "1":
                jobs = []
            if os.environ.get("FN_PART") == "2":
                jobs = jobs[:2]
            for n, (kind, kt) in enumerate(jobs):
                DC, DS, Y, OF = dc[n % 2], ds[n % 2], y[n % 2], oft[n % 2]
                if kind == "c":
                    ntt, t0, tok0 = 2, 0, kt * 128
                    kb.dma("sp", DC.t[:, 0:2, :], C["dft_c2"][kt], [], [DC])
                    kb.dma("pool", DS.t[:, 0:2, :], C["dft_s2"][kt], [], [DS])
                else:
                    ntt, t0, tok0 = 32, 2, CTX + kt * 128
                    kb.dma("sp", DC.t[:], C["dft_c"][kt], [], [DC])
                    kb.dma("pool", DS.t[:], C["dft_s"][kt], [], [DS])
                ps = self.psum()
                for tt in range(ntt):
                    kb.mm(ps.t[:], DC.t[:, tt, :], A.t[:, t0 + tt, :], tt == 0, False, [DC, A], [ps])
                    kb.mm(ps.t[:], DS.t[:, tt, :], Bm.t[:, t0 + tt, :], False, tt == ntt - 1, [DS, Bm], [ps])
                kb.cp("act", Y.t[:], ps.t[:], [ps], [Y])
                p2 = self.psum()
                p2b = p2.t[:].bitcast(BF16)
                for g in range(4):
                    kb.tr(p2b[:, g * 128:(g + 1) * 128], Y.t[:, g * 128:(g + 1) * 128], idb.t[:], [Y, idb], [p2])
                kb.cp("dve", OF.t[:].rearrange("p g t -> p (g t)"), p2b[:, 0:512], [p2], [OF])
                kb.dma("sp", Dr["OFT"][:, :, tok0:tok0 + 128].rearrange("g m t -> m g t"), OF.t[:], [OF], [Dk["OFT"]])
            kb.S.flush()

    def gla_gen(self, l, st):
        kb, P, C, W, Dr, Dk = self.kb, self.P, self.C, self.W, self.Dr, self.Dk
        tri = kb.sb(st, "tri", [128, 2, 128], F32)
        mask4 = kb.sb(st, "mask4", [128, 2, 512], F32)
        for d in range(2):
            kb.dma("sp", tri.t[:, d, :], C["tri_f"][d], [], [tri])
            kb.dma("sp", mask4.t[:, d, :], C["mask4"][d].rearrange("p h t -> p (h t)"), [], [mask4])
        S32 = [kb.sb(st, "S32_%d" % d, [64, 4, 128], F32) for d in range(2)]
        Sb = [kb.sb(st, "Sb_%d" % d, [64, 4, 128], BF16) for d in range(2)]
        NB2 = 2

        def mk(name, shp, dt):
            return [[kb.sb(st, "%s%d_%d" % (name, d, j), shp, dt) for j in range(NB2)] for d in range(2)]
        qT = mk("gqT", [64, 4, 128], BF16)
        kT = mk("gkT", [64, 4, 128], BF16)
        gk = mk("ggk", [128, 768], BF16)
        lf = mk("glf", [128, 256], F32)
        gtok = mk("gtok", [128, 256], F32)
        e1 = mk("ge1", [128, 256], F32)
        khat = mk("khat", [128, 256], BF16)
        eq = mk("geq", [64, 4, 128], F32)
        ek = mk("gek", [64, 4, 128], F32)
        qtl = mk("qtl", [64, 4, 128], BF16)
        ktl = mk("ktl", [64, 4, 128], BF16)
        at = mk("gat", [128, 512], BF16)
        o32 = mk("go32", [128, 512], F32)
        orders = [list(range(NT)), [1, 0] + list(range(NT - 1, 1, -1))]
        lasts = [127, 0]
        outs = ["OFW", "OBW"]
        for d in range(2):
            kb.memset("dve", S32[d].t[:], 0.0, [], [S32[d]])
            kb.memset("pool", Sb[d].t[:], 0.0, [], [Sb[d]])

        def load(n, d):
            b = n % NB2
            i = orders[d][n]
            sl = slice(i * 128, (i + 1) * 128)
            kb.dma("sp", qT[d][b].t[:], Dr["GQT"][:, :, sl].rearrange("h d t -> d h t"), [Dk["GQT"]], [qT[d][b]])
            kb.dma("sp", kT[d][b].t[:], Dr["GKT"][:, :, sl].rearrange("h d t -> d h t"), [Dk["GKT"]], [kT[d][b]])
            kb.dma("sp", gk[d][b].t[:], Dr["GKVO"][sl, 0:768], [Dk["GKVO"]], [gk[d][b]])
            kb.dma("sp", lf[d][b].t[:], Dr["LFB"][sl, d * 256:(d + 1) * 256], [Dk["LFB"]], [lf[d][b]])
        load(0, 0)
        load(0, 1)
        for n in range(NT):
            for d in range(2):
                b = n % NB2
                i = orders[d][n]
                sl = slice(i * 128, (i + 1) * 128)
                last = lasts[d]
                if n + 1 < NT:
                    load(n + 1, d)
                LF, GK = lf[d][b], gk[d][b]
                GT, E1, KH, EQ, EK, QTL, KTL, AT, O32 = gtok[d][b], e1[d][b], khat[d][b], eq[d][b], ek[d][b], qtl[d][b], ktl[d][b], at[d][b], o32[d][b]
                pg = self.psum()
                kb.mm(pg.t[:, 0:256], tri.t[:, d, :], LF.t[:], True, True, [tri, LF], [pg])
                kb.mm(pg.t[:, 256:512], P["ones_f"].t[:], LF.t[:], True, True, [P["ones_f"], LF], [pg])
                pf = self.psum()
                for h in range(4):
                    kb.mm(pf.t[0:64, h * 128:(h + 1) * 128], LF.t[:, h * 64:(h + 1) * 64], tri.t[:, d, :], True, True, [LF, tri], [pf])
                kb.cp("act", GT.t[:], pg.t[:, 0:256], [pg], [GT])
                kb.tt("dve", E1.t[:], pg.t[:, 256:512], GT.t[:], ALU.subtract, [pg, GT], [E1])
                kb.act(E1.t[:], E1.t[:], AF.Exp, [E1], [E1])
                kb.tt("dve", KH.t[:], GK.t[:, 0:256], E1.t[:], ALU.mult, [GK, E1], [KH])
                kb.act(EQ.t[:].rearrange("p h t -> p (h t)"), pf.t[0:64, :], AF.Exp, [pf], [EQ])
                kb.act(EK.t[:].rearrange("p h t -> p (h t)"), pf.t[0:64, :], AF.Exp, [pf], [EK], scale=-1.0)
                kb.tt("dve", QTL.t[:], qT[d][b].t[:], EQ.t[:], ALU.mult, [qT[d][b], EQ], [QTL])
                kb.tt("pool", KTL.t[:], kT[d][b].t[:], EK.t[:], ALU.mult, [kT[d][b], EK], [KTL])
                yield
                pa = self.psum()
                for h in range(4):
                    kb.mm(pa.t[:, h * 128:(h + 1) * 128], KTL.t[:, h, :], QTL.t[:, h, :], True, True, [KTL, QTL], [pa])
                kb.tt("dve", AT.t[:], pa.t[:], mask4.t[:, d, :], ALU.mult, [pa, mask4], [AT])
                yield
                po = self.psum()
                for h in range(4):
                    kb.mm(po.t[:, h * 128:(h + 1) * 128], QTL.t[:, h, :], Sb[d].t[:, h, :], True, False, [QTL, Sb[d]], [po])
                    kb.mm(po.t[:, h * 128:(h + 1) * 128], AT.t[:, h * 128:(h + 1) * 128],
                          GK.t[:, 256 + h * 128:256 + (h + 1) * 128], False, True, [AT, GK], [po])
                pS = self.psum()
                for h in range(4):
                    kb.mm(pS.t[0:64, h * 128:(h + 1) * 128], KH.t[:, h * 64:(h + 1) * 64],
                          GK.t[:, 256 + h * 128:256 + (h + 1) * 128], True, True, [KH, GK], [pS])
                for h in range(4):
                    kb.stt("dve", S32[d].t[:, h, :], S32[d].t[:, h, :], EQ.t[:, h, last:last + 1], pS.t[0:64, h * 128:(h + 1) * 128],
                           ALU.mult, ALU.add, [S32[d], EQ, pS], [S32[d]])
                kb.cp("pool", Sb[d].t[:], S32[d].t[:], [S32[d]], [Sb[d]])
                kb.cp("act", O32.t[:], po.t[:], [po], [O32])
                kb.dma("sp", Dr[outs[d]][sl, :], O32.t[:], [O32], [Dk[outs[d]]])
                yield

    def gla_out(self, l, st):
        kb, P, W, Dr, Dk = self.kb, self.P, self.W, self.Dr, self.Dk
        idb = P["ident_b"]
        gn = kb.sb(st, "gn", [128, 512], F32)
        kb.dma("sp", gn.t[:], W["gla_norm_g"][l].partition_broadcast(128), [], [gn])
        NB2 = 2

        def mk(name, shp, dt):
            return [kb.sb(st, "%s%d" % (name, j), shp, dt) for j in range(NB2)]
        ofw = mk("oofw", [128, 512], F32)
        obw = mk("oobw", [128, 512], F32)
        og = mk("oog", [128, 512], BF16)
        sqo = mk("osq", [128, 512], F32)
        st4 = mk("ost4", [128, 8], F32)
        ob = mk("oob", [128, 512], BF16)
        ogt = mk("oogt", [128, 4, 128], BF16)
        for i in range(NT):
            b = i % NB2
            sl = slice(i * 128, (i + 1) * 128)
            kb.dma("sp", ofw[b].t[:], Dr["OFW"][sl, :], [Dk["OFW"]], [ofw[b]])
            kb.dma("sp", obw[b].t[:], Dr["OBW"][sl, :], [Dk["OBW"]], [obw[b]])
            kb.dma("sp", og[b].t[:], Dr["GKVO"][sl, 768:1280], [Dk["GKVO"]], [og[b]])
            O, SQ, S4 = ofw[b], sqo[b], st4[b]
            kb.tt("dve", O.t[:], O.t[:], obw[b].t[:], ALU.add, [O, obw[b]], [O])
            kb.tt("pool", SQ.t[:], O.t[:], O.t[:], ALU.mult, [O], [SQ])
            kb.red("dve", S4.t[:, 0:4], SQ.t[:].rearrange("p (h e) -> p h e", h=4), ALU.add, [SQ], [S4])
            kb.ts("dve", S4.t[:, 0:4], S4.t[:, 0:4], 1.0 / 128, EPS, ALU.mult, ALU.add, [S4], [S4])
            kb.act(S4.t[:, 0:4], S4.t[:, 0:4], AF.Sqrt, [S4], [S4])
            kb.recip(S4.t[:, 4:8], S4.t[:, 0:4], [S4], [S4])
            for h in range(4):
                kb.stt("dve", O.t[:, h * 128:(h + 1) * 128], O.t[:, h * 128:(h + 1) * 128], S4.t[:, 4 + h:5 + h],
                       gn.t[:, h * 128:(h + 1) * 128], ALU.mult, ALU.mult, [O, S4, gn], [O])
            kb.tt("pool", ob[b].t[:], O.t[:], og[b].t[:], ALU.mult, [O, og[b]], [ob[b]])
            p2 = self.psum()
            p2b = p2.t[:].bitcast(BF16)
            for g in range(4):
                kb.tr(p2b[:, g * 128:(g + 1) * 128], ob[b].t[:, g * 128:(g + 1) * 128], idb.t[:], [ob[b], idb], [p2])
            kb.cp("act", ogt[b].t[:].rearrange("p g t -> p (g t)"), p2b[:, 0:512], [p2], [ogt[b]])
            kb.dma("sp", Dr["OGT"][:, :, sl].rearrange("g m t -> m g t"), ogt[b].t[:], [ogt[b]], [Dk["OGT"]])

    def phase_merge(self, l):
        kb, P, W, Dr, Dk = self.kb, self.P, self.W, self.Dr, self.Dk
        with contextlib.ExitStack() as st:
            rows = self.mod_rows(st, l, 1, names=("G",))
            wbm = kb.sb(st, "wbm", [64, 8, D], BF16)
            wbf = kb.sb(st, "wbf", [128, 4, D], BF16)
            wbg = kb.sb(st, "wbg", [128, 4, D], BF16)
            wo = kb.sb(st, "wo", [128, 8, D], BF16)
            kb.dma("pool", wbm.t[:], W["w_br_mla"][l].rearrange("(h d) n -> d h n", d=64), [], [wbm])
            kb.dma("pool", wbf.t[:], W["w_br_fnet"][l].rearrange("(k p) n -> p k n", p=128), [], [wbf])
            kb.dma("pool", wbg.t[:], W["w_br_gla"][l].rearrange("(k p) n -> p k n", p=128), [], [wbg])
            kb.dma("pool", wo.t[:], W["w_o"][l].rearrange("(k p) n -> p k n", p=128), [], [wo])
            NB2 = 2
            def mk(name, shp, dt):
                return [kb.sb(st, "%s%d" % (name, j), shp, dt) for j in range(NB2)]
            omT = mk("momT", [64, 8, 128], BF16)
            ofT = mk("mofT", [128, 4, 128], BF16)
            ogT = mk("mogT", [128, 4, 128], BF16)
            ga = mk("mga", [128, 3072], BF16)
            xt = mk("mxt", [128, D], F32)
            y32 = mk("my32", [128, D], F32)
            t32 = mk("mt32", [128, D], F32)
            yb = mk("myb", [128, D], BF16)
            yT = mk("myT", [128, 8, 128], BF16)
            xn = mk("mxn", [128, D], F32)
            idb = P["ident_b"]
            R = self.router_setup(st, l)
            for i in range(NT):
                b = i % NB2
                sl = slice(i * 128, (i + 1) * 128)
                G = rows["Gc" if i < 2 else "Gl"]
                kb.dma("sp", omT[b].t[:], Dr["OMT"][:, :, sl].rearrange("h d t -> d h t"), [Dk["OMT"]], [omT[b]])
                kb.dma("sp", ofT[b].t[:], Dr["OFT"][:, :, sl].rearrange("g m t -> m g t"), [Dk["OFT"]], [ofT[b]])
                kb.dma("sp", ogT[b].t[:], Dr["OGT"][:, :, sl].rearrange("g m t -> m g t"), [Dk["OGT"]], [ogT[b]])
                kb.dma("sp", ga[b].t[:], Dr["GATE"][sl, :], [Dk["GATE"]], [ga[b]])
                kb.dma("sp", xt[b].t[:], Dr["XR"][sl, :], [Dk["XR"]], [xt[b]])
                for half in range(2):
                    cs_ = slice(half * 512, (half + 1) * 512)
                    pm = self.psum()
                    for h in range(8):
                        kb.mm(pm.t[:], omT[b].t[:, h, :], wbm.t[:, h, cs_], h == 0, h == 7, [omT[b], wbm], [pm])
                    pf = self.psum()
                    for k in range(4):
                        kb.mm(pf.t[:], ofT[b].t[:, k, :], wbf.t[:, k, cs_], k == 0, k == 3, [ofT[b], wbf], [pf])
                    pg = self.psum()
                    for k in range(4):
                        kb.mm(pg.t[:], ogT[b].t[:, k, :], wbg.t[:, k, cs_], k == 0, k == 3, [ogT[b], wbg], [pg])
                    Y, T32 = y32[b], t32[b]
                    kb.tt("dve", Y.t[:, cs_], pm.t[:], ga[b].t[:, half * 512:(half + 1) * 512], ALU.mult, [pm, ga[b]], [Y])
                    kb.tt("dve", T32.t[:, cs_], pf.t[:], ga[b].t[:, 1024 + half * 512:1024 + (half + 1) * 512], ALU.mult, [pf, ga[b]], [T32])
                    kb.tt("pool", Y.t[:, cs_], Y.t[:, cs_], T32.t[:, cs_], ALU.add, [Y, T32], [Y])
                    kb.tt("dve", T32.t[:, cs_], pg.t[:], ga[b].t[:, 2048 + half * 512:2048 + (half + 1) * 512], ALU.mult, [pg, ga[b]], [T32])
                    kb.tt("pool", yb[b].t[:, cs_], Y.t[:, cs_], T32.t[:, cs_], ALU.add, [Y, T32], [yb[b]])
                p2 = self.psum()
                p2b = p2.t[:].bitcast(BF16)
                for kc in range(8):
                    kb.tr(p2b[:, kc * 128:(kc + 1) * 128], yb[b].t[:, kc * 128:(kc + 1) * 128], idb.t[:], [yb[b], idb], [p2])
                kb.cp("act", yT[b].t[:].rearrange("p k t -> p (k t)"), p2b, [p2], [yT[b]])
                for half in range(2):
                    cs_ = slice(half * 512, (half + 1) * 512)
                    pz = self.psum()
                    for kc in range(8):
                        kb.mm(pz.t[:], yT[b].t[:, kc, :], wo.t[:, kc, cs_], kc == 0, kc == 7, [yT[b], wo], [pz])
                    kb.tt("dve", xn[b].t[:, cs_], pz.t[:], G.t[:, cs_], ALU.mult, [pz, G], [xn[b]])
                    kb.tt("pool", xn[b].t[:, cs_], xn[b].t[:, cs_], xt[b].t[:, cs_], ALU.add, [xn[b], xt[b]], [xn[b]])
                kb.dma("sp", Dr["XR"][sl, :], xn[b].t[:], [xn[b]], [Dk["XR"]])
                self.router_tile(R, i, xn[b])
            self.router_save(R)
            kb.S.flush()

    def router_setup(self, st, l):
        kb, P, W, C = self.kb, self.P, self.W, self.C
        R = {}
        R["rows"] = self.mod_rows(st, l, 2, names=("A", "S"))
        rw = R["rw"] = kb.sb(st, "rw", [128, 8, NE], F32)
        rb = R["rb"] = kb.sb(st, "rb", [1, NE], F32)
        trx = R["trx"] = kb.sb(st, "trx", [128, 128], BF16)
        kb.dma("sp", rw.t[:], W["router_w"][l].rearrange("(k p) e -> p k e", p=128), [], [rw])
        kb.dma("sp", rb.t[:], W["router_b"][l].rearrange("(o e) -> o e", o=1), [], [rb])
        kb.dma("sp", trx.t[:], C["tri_x"], [], [trx])
        for nm_ in ("LG", "GF", "POS"):
            R[nm_] = kb.sb(st, "r" + nm_, [128, NT, NE], F32)
        R["TOP"] = kb.sb(st, "rTOP", [128, NT, 8], F32)
        R["cnt"] = kb.sb(st, "rcnt", [128, NE], F32)
        kb.memset("dve", R["cnt"].t[:], 0.0, [], [R["cnt"]])

        def mk(name, shp, dt):
            return [kb.sb(st, "%s%d" % (name, j), shp, dt) for j in range(2)]
        R["junk"] = kb.sb(st, "ejunk", [128, D], F32)
        R["ssq"] = mk("essq", [128, 8], F32)
        R["h32"] = mk("eh32", [128, D], F32)
        R["hb"] = mk("ehb", [128, D], BF16)
        R["h2T"] = mk("eh2T", [128, 8, 128], F32)
        R["sm"] = mk("rsm", [128, 4, NE], F32)
        R["sc"] = mk("rsc", [128, 8], F32)
        R["mkb"] = mk("rmkb", [128, NE], BF16)
        return R

    def router_tile(self, R, i, xtile):
        kb, P, Dr, Dk = self.kb, self.P, self.Dr, self.Dk
        b = i % 2
        sl = slice(i * 128, (i + 1) * 128)
        rows, rw, rb, trx, junk = R["rows"], R["rw"], R["rb"], R["trx"], R["junk"]
        LG, GF, POS, TOP, cnt = R["LG"], R["GF"], R["POS"], R["TOP"], R["cnt"]
        ssq, h32, hb, h2T, sm, sc, mkb = R["ssq"], R["h32"], R["hb"], R["h2T"], R["sm"], R["sc"], R["mkb"]
        kb.memset("pool", ssq[b].t[:], 0.0, [], [ssq[b]])
        self.norm_mod(xtile, rows, i, junk, ssq[b], h32[b], hb[b], full32=True)
        kb.dma("sp", Dr["H2B"][sl, :], hb[b].t[:], [hb[b]], [Dk["H2B"]])
        for half in range(2):
            pt_ = self.psum()
            for k4 in range(4):
                kc = half * 4 + k4
                kb.tr(pt_.t[:, k4 * 128:(k4 + 1) * 128], h32[b].t[:, kc * 128:(kc + 1) * 128], P["ident_f"].t[:],
                      [h32[b], P["ident_f"]], [pt_])
            kb.cp("act" if half == 0 else "dve", h2T[b].t[:, half * 4:(half + 1) * 4, :].rearrange("p k t -> p (k t)"),
                  pt_.t[:], [pt_], [h2T[b]])
        pl = self.psum()
        for kc in range(8):
            kb.mm(pl.t[:, 0:NE], h2T[b].t[:, kc, :], rw.t[:, kc, :], kc == 0, False, [h2T[b], rw], [pl])
        kb.mm(pl.t[:, 0:NE], P["ones_f"].t[0:1, :], rb.t[0:1, :], False, True, [P["ones_f"], rb], [pl])
        kb.cp("act", LG.t[:, i, :], pl.t[:, 0:NE], [pl], [LG])
        kb.op("dve", lambda e, o=TOP.t[:, i, :], a=LG.t[:, i, :]: e.max(out=o, in_=a), [LG], [TOP])
        SM, SC = sm[b], sc[b]
        kb.ts("dve", SM.t[:, 0, :], LG.t[:, i, :], TOP.t[:, i, 3:4], None, ALU.is_ge, None, [LG, TOP], [SM])
        kb.ts("dve", SC.t[:, 0:1], TOP.t[:, i, 0:1], -1.0, None, ALU.mult, None, [TOP], [SC])
        kb.act(SM.t[:, 1, :], LG.t[:, i, :], AF.Exp, [LG, SC], [SM], bias=SC.t[:, 0:1], scale=1.0)
        kb.tt("dve", SM.t[:, 2, :], SM.t[:, 1, :], SM.t[:, 0, :], ALU.mult, [SM], [SM])
        kb.red("dve", SC.t[:, 1:2], SM.t[:, 2, :], ALU.add, [SM], [SC])
        kb.recip(SC.t[:, 2:3], SC.t[:, 1:2], [SC], [SC])
        kb.ts("dve", GF.t[:, i, :], SM.t[:, 2, :], SC.t[:, 2:3], None, ALU.mult, None, [SM, SC], [GF])
        kb.cp("pool", mkb[b].t[:], SM.t[:, 0, :], [SM], [mkb[b]])
        pp = self.psum()
        kb.mm(pp.t[:, 0:NE], trx.t[:], mkb[b].t[:], True, True, [trx, mkb[b]], [pp])
        kb.mm(pp.t[:, NE:2 * NE], P["ones_b"].t[:], mkb[b].t[:], True, True, [P["ones_b"], mkb[b]], [pp])
        kb.tt("dve", POS.t[:, i, :], pp.t[:, 0:NE], cnt.t[:], ALU.add, [pp, cnt], [POS])
        kb.tt("dve", cnt.t[:], pp.t[:, NE:2 * NE], cnt.t[:], ALU.add, [pp, cnt], [cnt])

    def router_save(self, R):
        kb, Dr, Dk = self.kb, self.Dr, self.Dk
        for nm_, key in (("R_LG", "LG"), ("R_GF", "GF"), ("R_POS", "POS"), ("R_TOP", "TOP")):
            kb.dma("sp", Dr[nm_], R[key].t[:].rearrange("p n k -> p (n k)"), [R[key]], [Dk[nm_]])
        kb.dma("sp", Dr["R_CNT"], R["cnt"].t[:], [R["cnt"]], [Dk["R_CNT"]])

    def phase_moe(self, l):
        kb, P, W, C, Dr, Dk = self.kb, self.P, self.W, self.C, self.Dr, self.Dk
        is_last = (l == self.nl - 1)
        idb = P["ident_b"]
        with contextlib.ExitStack() as st0:
            DSTI = kb.sb(st0, "DSTI", [128, NT, 4], I32)
            G4 = kb.sb(st0, "G4", [128, NT, 4], F32)
            IDXW = kb.sb(st0, "IDXW", [128, NBLK, 8], I32)
            EBI = kb.sb(st0, "EBI", [128, NBLK], I32)
            with contextlib.ExitStack() as st:
                jv = kb.sb(st, "jv", [128, NBLK], F32)
                kp = kb.sb(st, "kp", [128, 8], F32)
                kb.dma("sp", jv.t[:], C["jv"], [], [jv])
                kb.dma("sp", kp.t[:], C["kp"], [], [kp])
                LG = kb.sb(st, "LG", [128, NT, NE], F32)
                GF = kb.sb(st, "GF", [128, NT, NE], F32)
                POS = kb.sb(st, "POS", [128, NT, NE], F32)
                TOP = kb.sb(st, "TOP", [128, NT, 8], F32)
                cnt = kb.sb(st, "cnt", [128, NE], F32)
                NB2 = 2
                def mk(name, shp, dt):
                    return [kb.sb(st, "%s%d" % (name, j), shp, dt) for j in range(NB2)]
                sm = mk("esm", [128, 4, NE], F32)
                for nm_, t_ in (("R_LG", LG), ("R_GF", GF), ("R_POS", POS), ("R_TOP", TOP)):
                    kb.dma("sp", t_.t[:].rearrange("p n k -> p (n k)"), Dr[nm_], [Dk[nm_]], [t_])
                kb.dma("sp", cnt.t[:], Dr["R_CNT"], [Dk["R_CNT"]], [cnt])
                nbk = kb.sb(st, "nbk", [128, NE], F32)
                pend = kb.sb(st, "pend", [128, NE], F32)
                pstart = kb.sb(st, "pstart", [128, NE], F32)
                eb = kb.sb(st, "eb", [128, NBLK], F32)
                idxf = kb.sb(st, "idxf", [128, NBLK, 8], F32)
                kb.memset("dve", nbk.t[:], 0.0, [], [nbk])
                for j in range(T // BS + 1):
                    kb.stt("dve", nbk.t[:], cnt.t[:], float(j * BS), nbk.t[:], ALU.is_gt, ALU.add, [cnt, nbk], [nbk])
                kb.ts("dve", nbk.t[:], nbk.t[:], float(BS), None, ALU.mult, None, [nbk], [nbk])
                kb.cp("dve", pend.t[:, 0:1], nbk.t[:, 0:1], [nbk], [pend])
                for e_ in range(1, NE):
                    kb.tt("dve", pend.t[:, e_:e_ + 1], pend.t[:, e_ - 1:e_], nbk.t[:, e_:e_ + 1], ALU.add, [pend, nbk], [pend])
                kb.tt("dve", pstart.t[:], pend.t[:], nbk.t[:], ALU.subtract, [pend, nbk], [pstart])
                kb.memset("dve", eb.t[:], 0.0, [], [eb])
                for e_ in range(NE):
                    kb.stt("dve", eb.t[:], jv.t[:], pend.t[:, e_:e_ + 1], eb.t[:], ALU.is_ge, ALU.add, [jv, pend, eb], [eb])
                kb.ts("dve", eb.t[:], eb.t[:], float(NE - 1), None, ALU.min, None, [eb], [eb])
                kb.ts("dve", kp.t[:], kp.t[:], float(l * NE * D), None, ALU.add, None, [kp], [kp])
                for kc in range(8):
                    kb.ts("dve", idxf.t[:, :, kc], eb.t[:], 1024.0, kp.t[:, kc:kc + 1], ALU.mult, ALU.add, [eb, kp], [idxf])
                kb.cp("dve", IDXW.t[:], idxf.t[:], [idxf], [IDXW])
                kb.ts("dve", eb.t[:], eb.t[:], float(l * NE), None, ALU.add, None, [eb], [eb])
                kb.cp("dve", EBI.t[:], eb.t[:], [eb], [EBI])
                dstf = mk("edstf", [128, 4], F32)
                hs = mk("ehs", [128, D], BF16)
                for i in range(NT):
                    b = i % NB2
                    sl = slice(i * 128, (i + 1) * 128)
                    SM = sm[b]
                    kb.tt("dve", SM.t[:, 3, :], POS.t[:, i, :], pstart.t[:], ALU.add, [POS, pstart], [SM])
                    for k in range(4):
                        kb.ts("dve", SM.t[:, 0, :], LG.t[:, i, :], TOP.t[:, i, k:k + 1], None, ALU.is_equal, None, [LG, TOP], [SM])
                        kb.tt("dve", SM.t[:, 1, :], SM.t[:, 0, :], SM.t[:, 3, :], ALU.mult, [SM], [SM])
                        kb.red("dve", dstf[b].t[:, k:k + 1], SM.t[:, 1, :], ALU.add, [SM], [dstf[b]])
                        kb.tt("dve", SM.t[:, 2, :], SM.t[:, 0, :], GF.t[:, i, :], ALU.mult, [SM, GF], [SM])
                        kb.red("dve", G4.t[:, i, k:k + 1], SM.t[:, 2, :], ALU.add, [SM], [G4])
                    kb.cp("dve", DSTI.t[:, i, :], dstf[b].t[:], [dstf[b]], [DSTI])
                    kb.dma("sp", hs[b].t[:], Dr["H2B"][sl, :], [Dk["H2B"]], [hs[b]])
                    for k in range(4):
                        kb.scatter(Dr["XS"], hs[b].t[:], DSTI.t[:, i, k:k + 1], [hs[b], DSTI], [Dk["XS"]], NROWS - 1)
                if "DBG_DST" in self.dbg:
                    kb.dma("sp", Dr["DBG_DST"], DSTI.t[:].rearrange("p n k -> p (n k)"), [DSTI], [Dk["DBG_DST"]])
                    kb.dma("sp", Dr["DBG_G4"], G4.t[:].rearrange("p n k -> p (n k)"), [G4], [Dk["DBG_G4"]])
                    kb.dma("sp", Dr["DBG_EB"], EBI.t[:], [EBI], [Dk["DBG_EB"]])
                    kb.dma("sp", Dr["DBG_LG"], LG.t[:].rearrange("p n k -> p (n k)"), [LG], [Dk["DBG_LG"]])
                    kb.dma("sp", Dr["DBG_CNT"], cnt.t[:], [cnt], [Dk["DBG_CNT"]])
                kb.S.flush()
            with contextlib.ExitStack() as st:
                wup = [kb.sb(st, "wup%d" % j, [128, 8, 2 * D], BF16) for j in range(2)]
                wdn = [kb.sb(st, "wdn%d" % j, [128, 8, D], BF16) for j in range(2)]
                wupk = [[Tok() for _ in range(8)] for _ in range(2)]
                wdnk = [[Tok() for _ in range(8)] for _ in range(2)]
                bu = [kb.sb(st, "bu%d" % j, [2, 2 * D], BF16) for j in range(2)]
                bd = [kb.sb(st, "bd%d" % j, [2, D], BF16) for j in range(2)]
                NB2 = 2
                def mk(name, shp, dt):
                    return [kb.sb(st, "%s%d" % (name, j), shp, dt) for j in range(NB2)]
                xs = mk("xs", [128, D], BF16)
                xT = mk("xT", [128, 8, 128], BF16)
                gl = mk("gl", [128, 512], F32)
                li = mk("li", [128, 512], F32)
                sg = mk("sg", [128, 512], F32)
                ab = mk("ab", [128, D], BF16)
                aT = mk("aT", [128, 8, 128], BF16)
                yb = mk("yb", [128, D], F32)
                wup_src = W["exp_w_up"].rearrange("l e k n -> (l e k) n")
                wdn_src = W["exp_w_down"].rearrange("l e k n -> (l e k) n")
                bup_src = W["exp_b_up"].rearrange("l e n -> (l e) n")
                bdn_src = W["exp_b_down"].rearrange("l e n -> (l e) n")
                nlw = W["exp_w_up"].shape[0]
                ones1 = P["ones_b"].t[0:1, :]

                def load_w(j):
                    jb = j % 2
                    for kc in range(8):
                        kb.gather(wup[jb].t[:, kc, :], wup_src, IDXW.t[:, j, kc:kc + 1], [IDXW], [wupk[jb][kc]], nlw * NE * D - 1)
                    for kc in range(8):
                        kb.gather(wdn[jb].t[:, kc, :], wdn_src, IDXW.t[:, j, kc:kc + 1], [IDXW], [wdnk[jb][kc]], nlw * NE * D - 1)
                    kb.gather(bu[jb].t[:], bup_src, EBI.t[0:2, j:j + 1], [EBI], [bu[jb]], nlw * NE - 1)
                    kb.gather(bd[jb].t[:], bdn_src, EBI.t[0:2, j:j + 1], [EBI], [bd[jb]], nlw * NE - 1)

                def load_x(n):
                    kb.dma("sp", xs[n % NB2].t[:], Dr["XS"][n * 128:(n + 1) * 128, :], [Dk["XS"]], [xs[n % NB2]])

                NSUB = NBLK * SUB

                def stage_a(n):
                    b = n % NB2
                    if n + 1 < NSUB:
                        load_x(n + 1)
                    p2 = self.psum()
                    p2b = p2.t[:].bitcast(BF16)
                    for kc in range(8):
                        kb.tr(p2b[:, kc * 128:(kc + 1) * 128], xs[b].t[:, kc * 128:(kc + 1) * 128], idb.t[:], [xs[b], idb], [p2])
                    kb.cp("act", xT[b].t[:].rearrange("p k t -> p (k t)"), p2b, [p2], [xT[b]])

                def stage_u(n):
                    b = n % NB2
                    jb = (n // SUB) % 2
                    WU, BU = wup[jb], bu[jb]
                    for pair in range(2):
                        pgl = self.psum()
                        pli = self.psum()
                        for (pp_, c0) in ((pgl, pair * 512), (pli, D + pair * 512)):
                            for kc in range(8):
                                kb.mm(pp_.t[:], xT[b].t[:, kc, :], WU.t[:, kc, c0:c0 + 512], kc == 0, False, [xT[b], wupk[jb][kc]], [pp_])
                            kb.mm(pp_.t[:], ones1, BU.t[0:1, c0:c0 + 512], False, True, [P["ones_b"], BU], [pp_])
                        GL, LI, SG = gl[pair], li[pair], sg[pair]
                        kb.ts("dve", GL.t[:], pgl.t[:], 7.0, None, ALU.min, None, [pgl], [GL])
                        kb.act(SG.t[:], GL.t[:], AF.Sigmoid, [GL], [SG], scale=1.702)
                        kb.ts("dve", LI.t[:], pli.t[:], 7.0, -7.0, ALU.min, ALU.max, [pli], [LI])
                        kb.stt("dve", LI.t[:], LI.t[:], 1.0, GL.t[:], ALU.add, ALU.mult, [LI, GL], [LI])
                        kb.tt("dve", ab[b].t[:, pair * 512:(pair + 1) * 512], LI.t[:], SG.t[:], ALU.mult, [LI, SG], [ab[b]])

                def stage_d(n):
                    b = n % NB2
                    jb = (n // SUB) % 2
                    WD, BD = wdn[jb], bd[jb]
                    r0 = n * 128
                    p3 = self.psum()
                    p3b = p3.t[:].bitcast(BF16)
                    for kc in range(8):
                        kb.tr(p3b[:, kc * 128:(kc + 1) * 128], ab[b].t[:, kc * 128:(kc + 1) * 128], idb.t[:], [ab[b], idb], [p3])
                    kb.cp("act", aT[b].t[:].rearrange("p k t -> p (k t)"), p3b, [p3], [aT[b]])
                    for half in range(2):
                        pd = self.psum()
                        for kc in range(8):
                            kb.mm(pd.t[:], aT[b].t[:, kc, :], WD.t[:, kc, half * 512:(half + 1) * 512], kc == 0, False, [aT[b], wdnk[jb][kc]], [pd])
                        kb.mm(pd.t[:], ones1, BD.t[0:1, half * 512:(half + 1) * 512], False, True, [P["ones_b"], BD], [pd])
                        kb.cp("act" if half == 0 else "dve", yb[b].t[:, half * 512:(half + 1) * 512], pd.t[:], [pd], [yb[b]])
                    kb.dma("sp", Dr["YB"][r0:r0 + 128, :], yb[b].t[:], [yb[b]], [Dk["YB"]])

                load_w(0)
                load_w(1)
                load_x(0)
                stage_a(0)
                stage_u(0)
                for n in range(NSUB):
                    if n + 1 < NSUB:
                        stage_a(n + 1)
                        stage_u(n + 1)
                    stage_d(n)
                    if (n + 1) % SUB == 0:
                        j = n // SUB
                        if j + 2 < NBLK:
                            load_w(j + 2)
                kb.S.flush()
            with contextlib.ExitStack() as st:
                rows = self.mod_rows(st, l, 2, names=("G",))
                NB2 = 2
                def mk(name, shp, dt):
                    return [kb.sb(st, "%s%d" % (name, j), shp, dt) for j in range(NB2)]
                xt = mk("cxt", [128, D], F32)
                yk = [kb.sb(st, "cyk%d" % j, [128, D], F32) for j in range(4)]
                acc = mk("cacc", [128, D], F32)
                xn = mk("cxn", [128, D], F32)
                if is_last:
                    fg = kb.sb(st, "fg", [128, D], F32)
                    kb.dma("sp", fg.t[:], W["final_norm_g"].partition_broadcast(128), [], [fg])
                    junk = kb.sb(st, "cjunk", [128, D], F32)
                    ssq = mk("cssq", [128, 8], F32)
                    ot = mk("cot", [128, D], F32)
                for i in range(NT):
                    b = i % NB2
                    sl = slice(i * 128, (i + 1) * 128)
                    G = rows["Gc" if i < 2 else "Gl"]
                    kb.dma("sp", xt[b].t[:], Dr["XR"][sl, :], [Dk["XR"]], [xt[b]])
                    for k in range(4):
                        kb.gather(yk[k].t[:], Dr["YB"], DSTI.t[:, i, k:k + 1], [DSTI, Dk["YB"]], [yk[k]], NROWS - 1)
                    A_ = acc[b]
                    kb.ts("dve", A_.t[:], yk[0].t[:], G4.t[:, i, 0:1], None, ALU.mult, None, [yk[0], G4], [A_])
                    for k in range(1, 4):
                        kb.stt("dve", A_.t[:], yk[k].t[:], G4.t[:, i, k:k + 1], A_.t[:], ALU.mult, ALU.add, [yk[k], G4, A_], [A_])
                    kb.tt("pool", A_.t[:], A_.t[:], G.t[:], ALU.mult, [A_, G], [A_])
                    kb.tt("dve", xn[b].t[:], A_.t[:], xt[b].t[:], ALU.add, [A_, xt[b]], [xn[b]])
                    kb.dma("sp", Dr["XR"][sl, :], xn[b].t[:], [xn[b]], [Dk["XR"]])
                    if is_last and i >= 2:
                        SS = ssq[b]
                        kb.memset("pool", SS.t[:], 0.0, [], [SS])
                        kb.act(junk.t[:], xn[b].t[:], AF.Square, [xn[b]], [junk, SS], accum=SS.t[:, 0:1])
                        kb.ts("dve", SS.t[:, 1:2], SS.t[:, 0:1], 1.0 / D, EPS, ALU.mult, ALU.add, [SS], [SS])
                        kb.act(SS.t[:, 1:2], SS.t[:, 1:2], AF.Sqrt, [SS], [SS])
                        kb.recip(SS.t[:, 2:3], SS.t[:, 1:2], [SS], [SS])
                        kb.stt("dve", ot[b].t[:], xn[b].t[:], SS.t[:, 2:3], fg.t[:], ALU.mult, ALU.mult, [xn[b], SS, fg], [ot[b]])
                        kb.dma("sp", self.out[(i - 2) * 128:(i - 1) * 128, :], ot[b].t[:], [ot[b]], [self.tout])
                kb.S.flush()


_CACHE = {}


def kernel(**inputs):
    n_cores = 8
    if "nc" not in _CACHE:
        pg = Prog(nl=DEPTH)
        _CACHE["nc"] = pg.build()
        _CACHE["consts"] = host_consts()
    nc = _CACHE["nc"]
    consts = _CACHE["consts"]
    shared = {k: np.ascontiguousarray(np.asarray(inputs[k], dtype=np.float32)) for k in WEIGHT_SPECS}
    shared["c_ctx"] = np.ascontiguousarray(np.asarray(inputs["c_ctx"], dtype=np.float32))
    shared.update(consts)
    x = np.asarray(inputs["x"], dtype=np.float32)
    c = np.asarray(inputs["c"], dtype=np.float32)
    ctx = np.asarray(inputs["ctx"], dtype=np.float32)
    in_maps = []
    for b in range(n_cores):
        m = dict(shared)
        m["x"] = np.ascontiguousarray(x[b])
        m["ctx"] = np.ascontiguousarray(ctx[b])
        m["c"] = np.ascontiguousarray(c[b])
        in_maps.append(m)
    res = run_bass_kernel_spmd(nc, in_maps, core_ids=list(range(n_cores)))
    out = np.stack([np.asarray(res.results[b]["out"], dtype=np.float32) for b in range(n_cores)], axis=0)
    return out
```

```python
import contextlib
import math
import numpy as np
import ml_dtypes
import concourse.bass as bass
import concourse.mybir as mybir
from concourse.bass_utils import run_bass_kernel_spmd

F32 = mybir.dt.float32
BF16 = mybir.dt.bfloat16
I32 = mybir.dt.int32
AF = mybir.ActivationFunctionType
ALU = mybir.AluOpType
AX = mybir.AxisListType

D = 1024
SEQ = 4096
CTX = 256
T = SEQ + CTX
NT = T // 128
DEPTH = 4
INW = 5568
NE = 32
BS = 256
SUB = BS // 128
NBLK = (T * 4) // BS + NE
NROWS = NBLK * BS
EPS = 1e-6
MLA_SCALE = 96 ** -0.5
GLA_SCALE = 64 ** -0.5
O_UQ, O_KV, O_FN, O_GQ, O_GK, O_GV, O_OG, O_GF, O_GB, O_GATE = 0, 256, 416, 928, 1184, 1440, 1952, 2464, 2480, 2496


import os as _os
SAME_ENGINE_SYNC = _os.environ.get("NOSAME", "0") != "1"


class Tok:
    __slots__ = ("w", "rs")

    def __init__(self):
        self.w = None
        self.rs = {}


class Sched:
    NDMA = 32

    def __init__(self, nc, st):
        self.nc = nc
        self.engs = ["pe", "act", "dve", "pool", "sp"]
        self.ops = {k: [] for k in self.engs}
        self.cnt = {k: 0 for k in self.engs}
        self.seen = {k: {} for k in self.engs}
        self.dma_k = {"d": 0, "g": 0}
        self.dma_last = {}
        self.n_ops = 0
        self.sems = {}
        for k in ["e_" + e for e in self.engs] + ["d%d" % i for i in range(self.NDMA)] + ["g%d" % i for i in range(self.NDMA)]:
            self.sems[k] = st.enter_context(nc.semaphore(k))

    def _need(self, eng, ev, waits):
        if ev is None:
            return
        src, key, val = ev
        if src == eng and (eng == "pe" or not SAME_ENGINE_SYNC):
            return
        if self.seen[eng].get(key, 0) >= val:
            return
        self.seen[eng][key] = val
        waits.append((key, val))

    def op(self, eng, fn, r=(), w=(), dma=False):
        waits = []
        for t in r:
            self._need(eng, t.w, waits)
        for t in w:
            self._need(eng, t.w, waits)
            for ev in t.rs.values():
                self._need(eng, ev, waits)
        if dma:
            pre = "g" if eng == "pool" else "d"
            k = self.dma_k[pre]
            self.dma_k[pre] += 1
            key = "%s%d" % (pre, k % self.NDMA)
            val = 16 * (k // self.NDMA + 1)
            if val > 16:
                self._need(eng, ("dma", key, val - 16), waits)
            ev = ("dma", key, val)
            inc = (key, 16)
            self.dma_last[key] = val
        else:
            self.cnt[eng] += 1
            key = "e_" + eng
            ev = (eng, key, self.cnt[eng])
            inc = (key, 1)
        self.ops[eng].append((waits, fn, inc))
        self.n_ops += 1
        for t in w:
            t.w = ev
            t.rs = {}
        for t in r:
            if t not in w:
                t.rs[ev[1]] = ev
        return ev

    def barrier(self):
        evs = [(e, "e_" + e, self.cnt[e]) for e in self.engs if self.cnt[e] > 0]
        evs += [("dma", k, v) for k, v in self.dma_last.items()]
        for eng in self.engs:
            waits = []
            for ev in evs:
                if ev[0] == eng:
                    continue
                self._need(eng, ev, waits)
            if waits:
                self.ops[eng].append((waits, None, None))

    def flush(self):
        self.barrier()
        nc = self.nc
        sems = self.sems
        ops = self.ops
        with nc.Block() as block:
            def mk(engname):
                def body(e):
                    for waits, fn, inc in ops[engname]:
                        for key, val in waits:
                            e.wait_ge(sems[key], val)
                        if fn is not None:
                            fn(e).then_inc(sems[inc[0]], inc[1])
                return body
            block.tensor(mk("pe"))
            block.scalar(mk("act"))
            block.vector(mk("dve"))
            block.gpsimd(mk("pool"))
            block.sync(mk("sp"))
        self.ops = {k: [] for k in self.engs}


class B:
    __slots__ = ("t", "k", "psum")

    def __init__(self, t, psum=False):
        self.t = t
        self.k = Tok()
        self.psum = psum


def _toks(xs):
    return [x.k if isinstance(x, B) else x for x in xs]


class KB:
    def __init__(self, nc, st):
        self.nc = nc
        self.S = Sched(nc, st)
        self.dq = 0
        self.bregs = {}

    def sb(self, st, name, shape, dt):
        self.dq += 1
        return B(st.enter_context(self.nc.sbuf_tensor("%s_u%d" % (name, self.dq), shape, dt)))

    def op(self, eng, fn, r, w, dma=False):
        r2 = [x for x in r if not (isinstance(x, B) and x.psum)]
        w2 = list(w) + [x for x in r if isinstance(x, B) and x.psum and x not in w]
        return self.S.op(eng, fn, r=_toks(r2), w=_toks(w2), dma=dma)

    def mm(self, out, lhsT, rhs, start, stop, r, w):
        self.op("pe", lambda e: e.matmul(out, lhsT, rhs, start=start, stop=stop), r, w)

    def tr(self, out, in_, ident, r, w):
        self.op("pe", lambda e: e.transpose(out, in_, ident), r, w)

    def act(self, out, in_, func, r, w, bias=None, scale=None, accum=None):
        kw = {}
        if bias is not None:
            kw["bias"] = bias
        if scale is not None:
            kw["scale"] = scale
        if accum is not None:
            kw["accum_out"] = accum
        self.op("act", lambda e: e.activation(out=out, in_=in_, func=func, **kw), r, w)

    def cp(self, eng, out, in_, r, w):
        if eng == "act":
            self.op("act", lambda e: e.copy(out=out, in_=in_), r, w)
        else:
            self.op(eng, lambda e: e.tensor_copy(out=out, in_=in_), r, w)

    def tt(self, eng, out, in0, in1, op, r, w):
        self.op(eng, lambda e: e.tensor_tensor(out=out, in0=in0, in1=in1, op=op), r, w)

    def ts(self, eng, out, in0, s1, s2, op0, op1, r, w):
        if op1 is None:
            self.op(eng, lambda e: e.tensor_scalar(out=out, in0=in0, scalar1=s1, scalar2=None, op0=op0), r, w)
        else:
            self.op(eng, lambda e: e.tensor_scalar(out=out, in0=in0, scalar1=s1, scalar2=s2, op0=op0, op1=op1), r, w)

    def stt(self, eng, out, in0, scalar, in1, op0, op1, r, w):
        self.op(eng, lambda e: e.scalar_tensor_tensor(out=out, in0=in0, scalar=scalar, in1=in1, op0=op0, op1=op1), r, w)

    def red(self, eng, out, in_, op, r, w, axis=AX.X):
        self.op(eng, lambda e: e.tensor_reduce(out=out, in_=in_, axis=axis, op=op), r, w)

    def recip(self, out, in_, r, w):
        self.op("dve", lambda e: e.reciprocal(out=out, in_=in_), r, w)

    def memset(self, eng, out, val, r, w):
        self.op(eng, lambda e: e.memset(out, val), r, w)

    def dma(self, eng, out, in_, r, w, slow=False):
        if slow:
            self.op(eng, lambda e: e.dma_start(out=out, in_=in_, allow_slow_non_contiguous=True), r, w, dma=True)
        else:
            self.op(eng, lambda e: e.dma_start(out=out, in_=in_), r, w, dma=True)

    def _breg(self, e, bound):
        if bound not in self.bregs:
            self.bregs[bound] = e.to_reg(bound)
        return self.bregs[bound]

    def gather(self, out, in_, idx, r, w, bound):
        self.op("pool", lambda e: e.indirect_dma_start(
            out=out, out_offset=None, in_=in_, in_offset=bass.IndirectOffsetOnAxis(ap=idx, axis=0),
            bounds_check=self._breg(e, bound), oob_is_err=False), r, w, dma=True)

    def scatter(self, out, in_, idx, r, w, bound):
        self.op("pool", lambda e: e.indirect_dma_start(
            out=out, out_offset=bass.IndirectOffsetOnAxis(ap=idx, axis=0), in_=in_, in_offset=None,
            bounds_check=self._breg(e, bound), oob_is_err=False), r, w, dma=True)


def host_consts():
    c = {}
    bf = ml_dtypes.bfloat16
    i = np.arange(128)
    c["ident_f"] = np.eye(128, dtype=np.float32)
    c["ident_b"] = np.eye(128).astype(bf)
    trif = (i[:, None] <= i[None, :]).astype(np.float32)
    trib = (i[:, None] >= i[None, :]).astype(np.float32)
    c["tri_f"] = np.stack([trif, trib], 0)
    c["mask4"] = np.stack([np.repeat(trif[:, None, :], 4, 1), np.repeat(trib[:, None, :], 4, 1)], 0).astype(np.float32)
    c["tri_x"] = (i[:, None] < i[None, :]).astype(bf)
    c["ones_f"] = np.ones((128, 128), np.float32)
    c["ones_b"] = np.ones((128, 128)).astype(bf)
    inv = 10000.0 ** (-np.arange(0, 16, 2, dtype=np.float32) / 16)
    row = np.repeat(np.arange(64, dtype=np.float32), 64)
    col = np.tile(np.arange(64, dtype=np.float32), 64)
    ar = row[:, None] * inv
    ac = col[:, None] * inv
    cr, sr, cc, sc = (np.cos(ar).astype(np.float32), np.sin(ar).astype(np.float32),
                      np.cos(ac).astype(np.float32), np.sin(ac).astype(np.float32))
    cos32 = np.concatenate([cr, cr, cc, cc], 1)
    sin32 = np.concatenate([-sr, sr, -sc, sc], 1)
    cos32 = np.concatenate([np.ones((CTX, 32), np.float32), cos32], 0)
    sin32 = np.concatenate([np.zeros((CTX, 32), np.float32), sin32], 0)
    c["rope_k"] = np.stack([cos32, sin32], 1).astype(np.float32)
    rq = np.stack([np.tile(cos32, (1, 8)), np.tile(sin32, (1, 8))], 1) * np.float32(MLA_SCALE)
    c["rope_q"] = rq.astype(np.float32)
    ang = 2 * np.pi * np.outer(i, i) / 128.0
    c["cs128"] = (np.concatenate([np.cos(ang), np.sin(ang)], 1) / math.sqrt(128)).astype(bf)
    n = np.arange(SEQ, dtype=np.int64)
    kt = (np.outer(n, n) % SEQ).astype(np.float64) * (2 * np.pi / SEQ)
    ctm = (np.cos(kt) / 64.0).astype(np.float32).reshape(32, 128, 32, 128)
    stm = (-np.sin(kt) / 64.0).astype(np.float32).reshape(32, 128, 32, 128)
    c["dft_c"] = np.ascontiguousarray(ctm.transpose(2, 1, 0, 3)).astype(bf)
    c["dft_s"] = np.ascontiguousarray(stm.transpose(2, 1, 0, 3)).astype(bf)
    del kt, ctm, stm
    m = np.arange(CTX, dtype=np.int64)
    k2 = (np.outer(m, m) % CTX).astype(np.float64) * (2 * np.pi / CTX)
    c2 = (np.cos(k2) / 16.0).astype(np.float32).reshape(2, 128, 2, 128)
    s2 = (-np.sin(k2) / 16.0).astype(np.float32).reshape(2, 128, 2, 128)
    c["dft_c2"] = np.ascontiguousarray(c2.transpose(2, 1, 0, 3)).astype(bf)
    c["dft_s2"] = np.ascontiguousarray(s2.transpose(2, 1, 0, 3)).astype(bf)
    c["jv"] = np.tile((np.arange(NBLK, dtype=np.float32) * BS)[None, :], (128, 1))
    c["kp"] = (np.arange(8, dtype=np.float32)[None, :] * 128 + i[:, None]).astype(np.float32)
    return c


CONST_SPECS = {
    "ident_f": ([128, 128], F32), "ident_b": ([128, 128], BF16), "tri_f": ([2, 128, 128], F32),
    "mask4": ([2, 128, 4, 128], F32), "tri_x": ([128, 128], BF16), "ones_f": ([128, 128], F32),
    "ones_b": ([128, 128], BF16), "rope_k": ([T, 2, 32], F32), "rope_q": ([T, 2, 256], F32),
    "cs128": ([128, 256], BF16), "dft_c": ([32, 128, 32, 128], BF16), "dft_s": ([32, 128, 32, 128], BF16),
    "dft_c2": ([2, 128, 2, 128], BF16), "dft_s2": ([2, 128, 2, 128], BF16),
    "jv": ([128, NBLK], F32), "kp": ([128, 8], F32),
}

WEIGHT_SPECS = {
    "w_mod": [DEPTH, D, 6 * D], "b_mod": [DEPTH, 6 * D], "norm1_g": [DEPTH, D], "w_in": [DEPTH, D, INW],
    "mla_q_norm_g": [DEPTH, 256], "mla_w_uq": [DEPTH, 256, 768], "mla_kv_norm_g": [DEPTH, 128],
    "mla_w_ukv": [DEPTH, 128, 1024], "gla_w_gate_f": [DEPTH, 16, 256], "gla_b_gate_f": [DEPTH, 256],
    "gla_w_gate_b": [DEPTH, 16, 256], "gla_b_gate_b": [DEPTH, 256], "gla_norm_g": [DEPTH, 512],
    "w_br_mla": [DEPTH, 512, D], "w_br_fnet": [DEPTH, 512, D], "w_br_gla": [DEPTH, 512, D],
    "w_o": [DEPTH, D, D], "norm2_g": [DEPTH, D], "router_w": [DEPTH, D, NE], "router_b": [DEPTH, NE],
    "exp_w_up": [DEPTH, NE, D, 2 * D], "exp_b_up": [DEPTH, NE, 2 * D], "exp_w_down": [DEPTH, NE, D, D],
    "exp_b_down": [DEPTH, NE, D], "final_norm_g": [D],
}


class Prog:
    def __init__(self, nl=DEPTH, dbg=(), stop_after=None, wdepth=DEPTH):
        self.nl = nl
        self.dbg = set(dbg)
        self.stop_after = stop_after
        self.nc = bass.Bass("TRN2", target_bir_lowering=False)
        self.st = contextlib.ExitStack()
        self.kb = KB(self.nc, self.st)
        nc = self.nc
        self.x_in = nc.dram_tensor("x", [SEQ, D], F32, kind="ExternalInput").ap()
        self.ctx_in = nc.dram_tensor("ctx", [CTX, D], F32, kind="ExternalInput").ap()
        self.c_in = nc.dram_tensor("c", [D], F32, kind="ExternalInput").ap()
        self.cc_in = nc.dram_tensor("c_ctx", [D], F32, kind="ExternalInput").ap()
        self.W = {k: nc.dram_tensor(k, ([wdepth] + shp[1:]) if (len(shp) > 1 or k in ('norm1_g',)) and shp[0] == DEPTH and k != 'final_norm_g' else shp, F32, kind="ExternalInput").ap() for k, shp in WEIGHT_SPECS.items()}
        self.C = {k: nc.dram_tensor(k, shp, dt, kind="ExternalInput").ap() for k, (shp, dt) in CONST_SPECS.items()}
        self.out = nc.dram_tensor("out", [SEQ, D], F32, kind="ExternalOutput").ap()
        self.tout = Tok()
        self.Dr = {}
        self.Dk = {}
        for name, shp, dt in [
            ("XR", [T, D], F32), ("KT", [8, 96, T], BF16), ("QT", [8, 96, T], BF16), ("VA", [T, 8, 65], BF16),
            ("UFT", [4, 128, T], BF16), ("GQT", [4, 64, T], BF16), ("GKT", [4, 64, T], BF16),
            ("GKVO", [T, 1280], BF16), ("LFB", [T, 512], F32), ("GATE", [T, 3072], BF16),
            ("OMT", [8, 64, T], BF16), ("OFT", [4, 128, T], BF16), ("OGT", [4, 128, T], BF16),
            ("OFW", [T, 512], F32), ("OBW", [T, 512], F32), ("XS", [NROWS, D], BF16), ("YB", [NROWS, D], F32),
            ("H2B", [T, D], BF16), ("DBG_DST", [128, NT * 4], I32), ("DBG_G4", [128, NT * 4], F32),
            ("DBG_EB", [128, NBLK], I32), ("DBG_LG", [128, NT * NE], F32), ("DBG_CNT", [128, NE], F32),
        ]:
            kind = "ExternalOutput" if name in self.dbg else "Internal"
            self.Dr[name] = nc.dram_tensor(name, shp, dt, kind=kind).ap()
            self.Dk[name] = Tok()

    def build(self):
        kb = self.kb
        nc = self.nc
        with self.st:
            P = self.P = {}
            for name, shp, dt in [
                ("ident_f", [128, 128], F32), ("ident_b", [128, 128], BF16), ("ones_f", [128, 128], F32),
                ("ones_b", [128, 128], BF16), ("modT", [128, 48, 2], F32), ("nb", [128, 8], F32),
                ("scT", [128, 8, 2], F32),
            ]:
                P[name] = kb.sb(self.st, "p_" + name, shp, dt)
            self.ps = [B(self.st.enter_context(nc.psum_tensor("ps%d" % i, [128, 512], F32)), psum=True) for i in range(8)]
            self.psi = 0
            self.pinned = set()
            for nm in ["ident_f", "ident_b", "ones_f", "ones_b"]:
                kb.dma("sp", P[nm].t[:], self.C[nm], [], [P[nm]])
            kb.dma("sp", self.Dr["XR"][0:CTX, :], self.ctx_in, [], [self.Dk["XR"]])
            kb.dma("pool", self.Dr["XR"][CTX:T, :], self.x_in, [], [self.Dk["XR"]])
            kb.S.flush()
            for l in range(self.nl):
                import os
                skip = os.environ.get("SKIP_PH", "").split(",")
                for ph in [self.phase_mod, self.phase_a, self.phase_attn, self.phase_fnet,
                           self.phase_merge, self.phase_moe]:
                    if ph.__name__ in skip:
                        continue
                    ph(l)
                    kb.S.flush()
                    if self.stop_after == (l, ph.__name__):
                        break
                else:
                    continue
                break
            kb.S.flush()
        return nc

    def psum(self, pin=False):
        while True:
            idx = self.psi % 8
            self.psi += 1
            if idx not in self.pinned:
                break
        if pin:
            self.pinned.add(idx)
        return self.ps[idx]

    def unpin(self, p):
        self.pinned.discard(self.ps.index(p))

    def phase_mod(self, l):
        kb, P, W = self.kb, self.P, self.W
        with contextlib.ExitStack() as st:
            cT = kb.sb(st, "cT", [128, 8, 2], F32)
            sT = kb.sb(st, "sT", [128, 8, 2], F32)
            bT = kb.sb(st, "bT", [128, 48], F32)
            wm = [kb.sb(st, "wm%d" % i, [128, 8, 1024], F32) for i in range(2)]
            kb.dma("sp", cT.t[:, :, 0], self.c_in.rearrange("(k p) -> p k", p=128), [], [cT], slow=True)
            kb.dma("sp", cT.t[:, :, 1], self.cc_in.rearrange("(k p) -> p k", p=128), [], [cT], slow=True)
            kb.dma("sp", bT.t[:], W["b_mod"][l].rearrange("(j p) -> p j", p=128), [], [bT], slow=True)
            kb.act(sT.t[:], cT.t[:], AF.Silu, [cT], [sT])
            for sec in range(6):
                w = wm[sec % 2]
                kb.dma("sp" if sec % 2 == 0 else "pool", w.t[:],
                       W["w_mod"][l][:, sec * 1024:(sec + 1) * 1024].rearrange("(k p) n -> p k n", p=128), [], [w])
                ps = self.psum()
                for j in range(8):
                    for kc in range(8):
                        kb.mm(ps.t[:, j * 2:j * 2 + 2], w.t[:, kc, j * 128:(j + 1) * 128], sT.t[:, kc, :],
                              kc == 0, kc == 7, [w, sT], [ps])
                for j in range(8):
                    jj = sec * 8 + j
                    kb.ts("dve", P["modT"].t[:, jj, :], ps.t[:, j * 2:j * 2 + 2], bT.t[:, jj:jj + 1], None, ALU.add, None,
                          [ps, bT], [P["modT"]])
            kb.S.flush()

    def bcast_rows(self, st, name, colT_ap_fn, r):
        kb, P = self.kb, self.P
        out = kb.sb(st, name, [128, 1024], F32)
        tmp = kb.sb(st, name + "_t", [128, 128], F32)
        for half in range(2):
            ps = self.psum()
            for k4 in range(4):
                kc = half * 4 + k4
                kb.ts("dve", tmp.t[:], P["ones_f"].t[:], colT_ap_fn(kc), None, ALU.mult, None, r + [P["ones_f"]], [tmp])
                kb.mm(ps.t[:, k4 * 128:(k4 + 1) * 128], tmp.t[:], P["ident_f"].t[:], True, True, [tmp, P["ident_f"]], [ps])
            kb.cp("act", out.t[:, half * 512:(half + 1) * 512], ps.t[:], [ps], [out])
        return out

    def mod_rows(self, st, l, which, names=("A", "S", "G")):
        kb, P, W = self.kb, self.P, self.W
        sec_sh, sec_sc, sec_g = (0, 1, 2) if which == 1 else (3, 4, 5)
        gT = kb.sb(st, "gT", [128, 8], F32)
        kb.dma("sp", gT.t[:], W["norm1_g" if which == 1 else "norm2_g"][l].rearrange("(k p) -> p k", p=128), [], [gT], slow=True)
        aT = kb.sb(st, "aT", [128, 8, 2], F32)
        m = P["modT"]
        for v in range(2):
            kb.stt("dve", aT.t[:, :, v], m.t[:, sec_sc * 8:(sec_sc + 1) * 8, v], 1.0, gT.t[:], ALU.add, ALU.mult, [m, gT], [aT])
        rows = {}
        for v, nm in ((0, "l"), (1, "c")):
            if "A" in names:
                rows["A" + nm] = self.bcast_rows(st, "rA" + nm, lambda kc, v=v: aT.t[:, kc, v:v + 1], [aT])
            if "S" in names:
                rows["S" + nm] = self.bcast_rows(st, "rS" + nm, lambda kc, v=v: m.t[:, sec_sh * 8 + kc, v:v + 1], [m])
            if "G" in names:
                rows["G" + nm] = self.bcast_rows(st, "rG" + nm, lambda kc, v=v: m.t[:, sec_g * 8 + kc, v:v + 1], [m])
        return rows

    def norm_mod(self, xt, rows, i, junk, ssq, h32, hb, full32=False):
        kb = self.kb
        nm = "c" if i < 2 else "l"
        kb.act(junk.t[:], xt.t[:], AF.Square, [xt], [junk, ssq], accum=ssq.t[:, 0:1])
        kb.ts("dve", ssq.t[:, 1:2], ssq.t[:, 0:1], 1.0 / D, EPS, ALU.mult, ALU.add, [ssq], [ssq])
        kb.act(ssq.t[:, 1:2], ssq.t[:, 1:2], AF.Sqrt, [ssq], [ssq])
        kb.recip(ssq.t[:, 2:3], ssq.t[:, 1:2], [ssq], [ssq])
        kb.stt("dve", h32.t[:], xt.t[:], ssq.t[:, 2:3], rows["A" + nm].t[:], ALU.mult, ALU.mult, [xt, ssq, rows["A" + nm]], [h32])
        if full32:
            kb.tt("pool", h32.t[:], h32.t[:], rows["S" + nm].t[:], ALU.add, [h32, rows["S" + nm]], [h32])
            kb.cp("dve", hb.t[:], h32.t[:], [h32], [hb])
        else:
            kb.tt("pool", hb.t[:], h32.t[:], rows["S" + nm].t[:], ALU.add, [h32, rows["S" + nm]], [hb])

    def phase_a(self, l):
        kb, P, W, C, Dr, Dk = self.kb, self.P, self.W, self.C, self.Dr, self.Dk
        with contextlib.ExitStack() as st:
            rows = self.mod_rows(st, l, 1, names=("A", "S"))
            win = kb.sb(st, "win", [128, 8, INW], BF16)
            for kc in range(8):
                kb.dma("pool", win.t[:, kc, :], W["w_in"][l][kc * 128:(kc + 1) * 128, :], [], [win])
            wuq32 = kb.sb(st, "wuq32", [128, 2, 768], F32)
            wuq = kb.sb(st, "wuq", [128, 2, 768], BF16)
            gq = kb.sb(st, "gq", [128, 2], F32)
            wkv32 = kb.sb(st, "wkv32", [128, 1024], F32)
            wkv = kb.sb(st, "wkv", [128, 1024], BF16)
            gkv = kb.sb(st, "gkv", [128, 1], F32)
            wg = kb.sb(st, "wg", [17, 2, 256], F32)
            kb.dma("sp", wuq32.t[:], W["mla_w_uq"][l].rearrange("(k p) n -> p k n", p=128), [], [wuq32])
            kb.dma("sp", gq.t[:], W["mla_q_norm_g"][l].rearrange("(k p) -> p k", p=128), [], [gq], slow=True)
            kb.dma("sp", wkv32.t[:], W["mla_w_ukv"][l], [], [wkv32])
            kb.dma("sp", gkv.t[:], W["mla_kv_norm_g"][l].rearrange("(k p) -> p k", p=128), [], [gkv], slow=True)
            kb.dma("sp", wg.t[0:16, 0, :], W["gla_w_gate_f"][l], [], [wg])
            kb.dma("sp", wg.t[0:16, 1, :], W["gla_w_gate_b"][l], [], [wg])
            kb.dma("sp", wg.t[16:17, 0, :], W["gla_b_gate_f"][l].rearrange("(o n) -> o n", o=1), [], [wg])
            kb.dma("sp", wg.t[16:17, 1, :], W["gla_b_gate_b"][l].rearrange("(o n) -> o n", o=1), [], [wg])
            for kc in range(2):
                kb.ts("dve", wuq.t[:, kc, :], wuq32.t[:, kc, :], gq.t[:, kc:kc + 1], None, ALU.mult, None, [wuq32, gq], [wuq])
            kb.ts("dve", wkv.t[:], wkv32.t[:], gkv.t[:, 0:1], None, ALU.mult, None, [wkv32, gkv], [wkv])
            m16 = kb.sb(st, "m16", [128, 16], F32)
            kb.memset("dve", m16.t[:], 0.0, [], [m16])
            NB2 = 1
            def mk(name, shp, dt):
                return [kb.sb(st, "%s%d" % (name, j), shp, dt) for j in range(NB2)]
            def mk2(name, shp, dt):
                return [kb.sb(st, "%s%d" % (name, j), shp, dt) for j in range(2)]
            xt = mk2("xt", [128, D], F32)
            junk = mk("junk", [128, D], F32)
            ssq = mk2("ssq", [128, 8], F32)
            h32 = mk("h32", [128, D], F32)
            hb = mk2("hb", [128, D], BF16)
            hT = mk2("hT", [128, 8, 128], BF16)
            uqn = mk("uqn", [128, 384], BF16)
            kr = mk("kr", [128, 4, 32], F32)
            uT = mk("uT", [128, 3, 128], BF16)
            rq = mk("rq", [128, 2, 256], F32)
            rk = mk("rk", [128, 2, 32], F32)
            qo = mk("qo", [128, 8, 96], BF16)
            ko = mk("ko", [128, 8, 96], BF16)
            vo = mk("vo", [128, 8, 65], BF16)
            qtmp = mk("qtmp", [128, 3, 256], F32)
            sq = mk("sq", [128, 768], F32)
            n2 = mk("n2", [128, 16], F32)
            qkT = mk("qkT", [96, 16, 128], BF16)
            gkvo = mk("gkvo", [128, 1280], BF16)
            gate = mk("gate", [128, 3072], BF16)
            fT = mk("fT", [128, 4, 128], BF16)
            gqkT = mk("gqkT", [64, 8, 128], BF16)
            ugT = mk("ugT", [17, 2, 128], F32)
            lfb = mk("lfb", [128, 512], F32)
            lfe = mk("lfe", [128, 512], F32)
            for j in range(NB2):
                kb.memset("pool", vo[j].t[:], 1.0, [], [vo[j]])
                kb.memset("pool", ugT[j].t[:], 1.0, [], [ugT[j]])
            idb = P["ident_b"]
            for i in range(NT):
                b = i % NB2
                b2 = i % 2
                X, J, SS, H32, HB, HT = xt[b2], junk[b], ssq[b2], h32[b], hb[b2], hT[b2]
                kb.dma("sp", X.t[:], Dr["XR"][i * 128:(i + 1) * 128, :], [Dk["XR"]], [X])
                kb.dma("sp", rq[b].t[:], C["rope_q"][i * 128:(i + 1) * 128], [], [rq[b]])
                kb.dma("sp", rk[b].t[:], C["rope_k"][i * 128:(i + 1) * 128], [], [rk[b]])
                kb.memset("pool", SS.t[:], 0.0, [], [SS])
                self.norm_mod(X, rows, i, J, SS, H32, HB)
                pT = self.psum()
                pTb = pT.t[:].bitcast(BF16)
                for kc in range(8):
                    kb.tr(pTb[:, kc * 128:(kc + 1) * 128], HB.t[:, kc * 128:(kc + 1) * 128], idb.t[:], [HB, idb], [pT])
                kb.cp("act", HT.t[:].rearrange("p k t -> p (k t)"), pTb, [pT], [HT])

                def tm(c0, cw):
                    ps = self.psum()
                    for kc in range(8):
                        kb.mm(ps.t[:, 0:cw], HT.t[:, kc, :], win.t[:, kc, c0:c0 + cw], kc == 0, kc == 7, [HT, win], [ps])
                    return ps

                def fm(c0, cw, ps, o0):
                    for kc in range(8):
                        kb.mm(ps.t[0:cw, o0:o0 + 128], win.t[:, kc, c0:c0 + cw], HT.t[:, kc, :], kc == 0, kc == 7, [HT, win], [ps])

                ps1 = tm(0, 416)
                UQ, KR, UT, QO, KO, VO, QTMP, SQ, N2 = uqn[b], kr[b], uT[b], qo[b], ko[b], vo[b], qtmp[b], sq[b], n2[b]
                kb.act(J.t[:, 0:256], ps1.t[:, 0:256], AF.Square, [ps1], [J, SS], accum=SS.t[:, 3:4])
                kb.act(J.t[:, 256:384], ps1.t[:, 256:384], AF.Square, [ps1], [J, SS], accum=SS.t[:, 4:5])
                kb.ts("dve", SS.t[:, 5:6], SS.t[:, 3:4], 1.0 / 256, EPS, ALU.mult, ALU.add, [SS], [SS])
                kb.ts("dve", SS.t[:, 6:7], SS.t[:, 4:5], 1.0 / 128, EPS, ALU.mult, ALU.add, [SS], [SS])
                kb.act(SS.t[:, 5:7], SS.t[:, 5:7], AF.Sqrt, [SS], [SS])
                kb.recip(SS.t[:, 5:7], SS.t[:, 5:7], [SS], [SS])
                kb.ts("dve", UQ.t[:, 0:256], ps1.t[:, 0:256], SS.t[:, 5:6], None, ALU.mult, None, [ps1, SS], [UQ])
                kb.ts("dve", UQ.t[:, 256:384], ps1.t[:, 256:384], SS.t[:, 6:7], None, ALU.mult, None, [ps1, SS], [UQ])
                kb.cp("act", KR.t[:, 0, :], ps1.t[:, 384:416], [ps1], [KR])
                krv = KR.t[:, 0, :].rearrange("p (a f e) -> p a f e", a=2, f=2)
                krs = KR.t[:, 1, :].rearrange("p (a f e) -> p a f e", a=2, f=2)
                kb.cp("pool", krs[:, :, 0, :], krv[:, :, 1, :], [KR], [KR])
                kb.cp("pool", krs[:, :, 1, :], krv[:, :, 0, :], [KR], [KR])
                kb.tt("dve", KR.t[:, 2, :], KR.t[:, 0, :], rk[b].t[:, 0, :], ALU.mult, [KR, rk[b]], [KR])
                kb.tt("dve", KR.t[:, 3, :], KR.t[:, 1, :], rk[b].t[:, 1, :], ALU.mult, [KR, rk[b]], [KR])
                kb.tt("dve", KR.t[:, 0, :], KR.t[:, 2, :], KR.t[:, 3, :], ALU.add, [KR], [KR])
                p2 = self.psum()
                p2b = p2.t[:].bitcast(BF16)
                for j in range(3):
                    kb.tr(p2b[:, j * 128:(j + 1) * 128], UQ.t[:, j * 128:(j + 1) * 128], idb.t[:], [UQ, idb], [p2])
                kb.cp("act", UT.t[:].rearrange("p k t -> p (k t)"), p2b[:, 0:384], [p2], [UT])
                for hh in range(2):
                    pq = self.psum()
                    for kc in range(2):
                        kb.mm(pq.t[:, 0:384], UT.t[:, kc, :], wuq.t[:, kc, hh * 384:(hh + 1) * 384], kc == 0, kc == 1, [UT, wuq], [pq])
                    pqv = pq.t[:, 0:384].rearrange("p (h d) -> p h d", h=4)
                    qov = QO.t[:, hh * 4:(hh + 1) * 4, :]
                    kb.act(qov[:, :, 0:64], pqv[:, :, 0:64], AF.Identity, [pq], [QO], scale=MLA_SCALE)
                    t0 = QTMP.t[:, 0, hh * 128:(hh + 1) * 128].rearrange("p (h d) -> p h d", h=4)
                    t1 = QTMP.t[:, 1, hh * 128:(hh + 1) * 128].rearrange("p (h d) -> p h d", h=4)
                    kb.cp("act", t0, pqv[:, :, 64:96], [pq], [QTMP])
                    t0v = t0.rearrange("p h (a f e) -> p h a f e", a=2, f=2)
                    t1v = t1.rearrange("p h (a f e) -> p h a f e", a=2, f=2)
                    for a in range(2):
                        kb.cp("pool", t1v[:, :, a, 0, :], t0v[:, :, a, 1, :], [QTMP], [QTMP])
                        kb.cp("pool", t1v[:, :, a, 1, :], t0v[:, :, a, 0, :], [QTMP], [QTMP])
                    rc = rq[b].t[:, 0, hh * 128:(hh + 1) * 128].rearrange("p (h d) -> p h d", h=4)
                    rs = rq[b].t[:, 1, hh * 128:(hh + 1) * 128].rearrange("p (h d) -> p h d", h=4)
                    kb.tt("dve", t0, t0, rc, ALU.mult, [QTMP, rq[b]], [QTMP])
                    kb.tt("dve", t1, t1, rs, ALU.mult, [QTMP, rq[b]], [QTMP])
                    kb.tt("dve", qov[:, :, 64:96], t0, t1, ALU.add, [QTMP], [QO])
                for hh in range(2):
                    pk = self.psum()
                    kb.mm(pk.t[:], UT.t[:, 2, :], wkv.t[:, hh * 512:(hh + 1) * 512], True, True, [UT, wkv], [pk])
                    pkv = pk.t[:].rearrange("p (h d) -> p h d", h=4)
                    kb.cp("act", KO.t[:, hh * 4:(hh + 1) * 4, 0:64], pkv[:, :, 0:64], [pk], [KO])
                    kb.cp("dve", VO.t[:, hh * 4:(hh + 1) * 4, 0:64], pkv[:, :, 64:128], [pk], [VO])
                for h in range(8):
                    kb.cp("pool", KO.t[:, h, 64:96], KR.t[:, 0, :], [KR], [KO])
                kb.tt("dve", SQ.t[:], QO.t[:].rearrange("p h d -> p (h d)"), QO.t[:].rearrange("p h d -> p (h d)"), ALU.mult, [QO], [SQ])
                kb.red("dve", N2.t[:, 8:16], SQ.t[:].rearrange("p (h d) -> p h d", h=8), ALU.add, [SQ], [N2])
                kb.tt("dve", SQ.t[:], KO.t[:].rearrange("p h d -> p (h d)"), KO.t[:].rearrange("p h d -> p (h d)"), ALU.mult, [KO], [SQ])
                kb.red("dve", N2.t[:, 0:8], SQ.t[:].rearrange("p (h d) -> p h d", h=8), ALU.add, [SQ], [N2])
                kb.tt("dve", m16.t[:], m16.t[:], N2.t[:], ALU.max, [N2, m16], [m16])
                QKT = qkT[b]
                for which, src in ((0, KO), (1, QO)):
                    p3 = self.psum()
                    p3b = p3.t[:].bitcast(BF16)
                    for h in range(8):
                        kb.tr(p3b[0:96, h * 128:(h + 1) * 128], src.t[:, h, :], idb.t[:], [src, idb], [p3])
                    kb.cp("act" if which == 0 else "dve", QKT.t[:, which * 8:(which + 1) * 8, :].rearrange("p h t -> p (h t)"),
                          p3b[0:96, :], [p3], [QKT])
                kb.dma("sp", Dr["KT"][:, :, i * 128:(i + 1) * 128].rearrange("h d t -> d h t"), QKT.t[:, 0:8, :], [QKT], [Dk["KT"]])
                kb.dma("sp", Dr["QT"][:, :, i * 128:(i + 1) * 128].rearrange("h d t -> d h t"), QKT.t[:, 8:16, :], [QKT], [Dk["QT"]])
                kb.dma("sp", Dr["VA"][i * 128:(i + 1) * 128], VO.t[:], [VO], [Dk["VA"]])
                G = gkvo[b]
                for gi, (c0, cw) in enumerate(((O_GK, 512), (O_GK + 512, 512), (O_GK + 1024, 256))):
                    ps = tm(c0, cw)
                    o0 = c0 - O_GK
                    if gi == 0:
                        kb.cp("dve", G.t[:, 0:512], ps.t[:, 0:512], [ps], [G])
                    elif gi == 1:
                        kb.cp("dve", G.t[:, 512:768], ps.t[:, 0:256], [ps], [G])
                        kb.act(G.t[:, 768:1024], ps.t[:, 256:512], AF.Silu, [ps], [G])
                    else:
                        kb.act(G.t[:, 1024:1280], ps.t[:, 0:256], AF.Silu, [ps], [G])
                kb.dma("sp", Dr["GKVO"][i * 128:(i + 1) * 128, :], G.t[:], [G], [Dk["GKVO"]])
                GA = gate[b]
                for gi in range(6):
                    ps = tm(O_GATE + gi * 512, 512)
                    kb.act(GA.t[:, gi * 512:(gi + 1) * 512], ps.t[:], AF.Sigmoid, [ps], [GA])
                kb.dma("sp", Dr["GATE"][i * 128:(i + 1) * 128, :], GA.t[:], [GA], [Dk["GATE"]])
                ps = self.psum()
                for g in range(4):
                    fm(O_FN + g * 128, 128, ps, g * 128)
                kb.cp("dve", fT[b].t[:].rearrange("p g t -> p (g t)"), ps.t[:], [ps], [fT[b]])
                kb.dma("sp", Dr["UFT"][:, :, i * 128:(i + 1) * 128].rearrange("g c t -> c g t"), fT[b].t[:], [fT[b]], [Dk["UFT"]])
                psq = self.psum()
                psk = self.psum()
                for h in range(4):
                    fm(O_GQ + h * 64, 64, psq, h * 128)
                    fm(O_GK + h * 64, 64, psk, h * 128)
                kb.act(gqkT[b].t[:, 0:4, :].rearrange("p h t -> p (h t)"), psq.t[0:64, :], AF.Identity, [psq], [gqkT[b]], scale=GLA_SCALE)
                kb.cp("dve", gqkT[b].t[:, 4:8, :].rearrange("p h t -> p (h t)"), psk.t[0:64, :], [psk], [gqkT[b]])
                kb.dma("sp", Dr["GQT"][:, :, i * 128:(i + 1) * 128].rearrange("h d t -> d h t"), gqkT[b].t[:, 0:4, :], [gqkT[b]], [Dk["GQT"]])
                kb.dma("sp", Dr["GKT"][:, :, i * 128:(i + 1) * 128].rearrange("h d t -> d h t"), gqkT[b].t[:, 4:8, :], [gqkT[b]], [Dk["GKT"]])
                psg = self.psum()
                fm(O_GF, 16, psg, 0)
                fm(O_GB, 16, psg, 128)
                kb.cp("act", ugT[b].t[0:16, :, :].rearrange("p d t -> p (d t)"), psg.t[0:16, 0:256], [psg], [ugT[b]])
                psl = self.psum()
                for d in range(2):
                    kb.mm(psl.t[:, d * 256:(d + 1) * 256], ugT[b].t[:, d, :], wg.t[:, d, :], True, True, [ugT[b], wg], [psl])
                kb.act(lfe[b].t[:], psl.t[:], AF.Exp, [psl], [lfe[b]], scale=-1.0)
                kb.act(lfe[b].t[:], lfe[b].t[:], AF.Ln, [lfe[b]], [lfe[b]], bias=1.0)
                kb.ts("dve", lfb[b].t[:], lfe[b].t[:], -1.0 / 16.0, None, ALU.mult, None, [lfe[b]], [lfb[b]])
                kb.dma("sp", Dr["LFB"][i * 128:(i + 1) * 128, :], lfb[b].t[:], [lfb[b]], [Dk["LFB"]])
            pm = self.psum()
            kb.tr(pm.t[0:16, 0:128], m16.t[:], P["ident_f"].t[:], [m16, P["ident_f"]], [pm])
            mcol = kb.sb(st, "mcol", [16, 1], F32)
            mb = kb.sb(st, "mb", [16, 128], F32)
            kb.red("dve", mcol.t[:], pm.t[0:16, 0:128], ALU.max, [pm], [mcol])
            kb.ts("dve", mb.t[:], P["ones_f"].t[0:16, :], mcol.t[:, 0:1], None, ALU.mult, None, [mcol, P["ones_f"]], [mb])
            pm2 = self.psum()
            kb.mm(pm2.t[:, 0:16], mb.t[:], P["ident_f"].t[0:16, 0:16], True, True, [mb, P["ident_f"]], [pm2])
            nbt = kb.sb(st, "nbt", [128, 16], F32)
            kb.cp("act", nbt.t[:], pm2.t[:, 0:16], [pm2], [nbt])
            kb.tt("dve", nbt.t[:, 0:8], nbt.t[:, 0:8], nbt.t[:, 8:16], ALU.mult, [nbt], [nbt])
            kb.act(nbt.t[:, 0:8], nbt.t[:, 0:8], AF.Sqrt, [nbt], [nbt])
            kb.ts("dve", P["nb"].t[:], nbt.t[:, 0:8], -1.0, None, ALU.mult, None, [nbt], [P["nb"]])
            kb.S.flush()

    def phase_attn(self, l):
        kb, P, Dr, Dk = self.kb, self.P, self.Dr, self.Dk
        with contextlib.ExitStack() as st:
            va = kb.sb(st, "va", [128, NT, 520], BF16)
            kb.dma("pool", va.t[:], Dr["VA"].rearrange("(n p) h e -> p n (h e)", p=128), [Dk["VA"]], [va])
            kt_ = [kb.sb(st, "ktb%d" % j, [96, T], BF16) for j in range(2)]
            qt_ = [kb.sb(st, "qtb%d" % j, [96, T], BF16) for j in range(2)]
            pt = [kb.sb(st, "pt%d" % j, [128, 512], BF16) for j in range(4)]
            osb = [kb.sb(st, "osb%d" % j, [65, 512], F32) for j in range(2)]
            rec = [kb.sb(st, "rec%d" % j, [64, 512], F32) for j in range(2)]
            om = [kb.sb(st, "om%d" % j, [64, 512], BF16) for j in range(2)]
            ones = P["ones_f"]
            nb = P["nb"]
            cnt = 0
            blk = 0
            gla = self.gla_gen(l, st)

            def tick():
                try:
                    next(gla)
                except StopIteration:
                    pass
            for h in range(8):
                KT, QT = kt_[h % 2], qt_[h % 2]
                kb.dma("sp", KT.t[:], Dr["KT"][h], [Dk["KT"]], [KT])
                kb.dma("sp", QT.t[:], Dr["QT"][h], [Dk["QT"]], [QT])
                blocks = [(0, 256, [0, 1])] + [(CTX + qb * 512, 512, list(range(NT))) for qb in range(8)]
                for (q0, qn, keys) in blocks:
                    po = self.psum(pin=True)
                    pend = None
                    seq = []
                    for kt in keys:
                        if kt in (0, 11, 22) or (len(keys) == 2 and kt == 1):
                            tick()
                        ps = self.psum()
                        kb.mm(ps.t[:, 0:qn], KT.t[:, kt * 128:(kt + 1) * 128], QT.t[:, q0:q0 + qn], True, True, [KT, QT], [ps])
                        seq.append((kt, ps))
                        if len(seq) >= 2:
                            self._attn_pv(seq.pop(0), keys, po, pt, va, nb, h, qn, cnt)
                            cnt += 1
                    while seq:
                        self._attn_pv(seq.pop(0), keys, po, pt, va, nb, h, qn, cnt)
                        cnt += 1
                    O, R, OM = osb[blk % 2], rec[blk % 2], om[blk % 2]
                    blk += 1
                    kb.cp("act", O.t[:, 0:qn], po.t[0:65, 0:qn], [po], [O])
                    self.unpin(po)
                    pb = self.psum()
                    kb.mm(pb.t[0:64, 0:qn], ones.t[64:65, 0:64], O.t[64:65, 0:qn], True, True, [ones, O], [pb])
                    kb.recip(R.t[:, 0:qn], pb.t[0:64, 0:qn], [pb], [R])
                    kb.tt("pool", OM.t[:, 0:qn], O.t[0:64, 0:qn], R.t[:, 0:qn], ALU.mult, [O, R], [OM])
                    kb.dma("sp", Dr["OMT"][h, :, q0:q0 + qn], OM.t[:, 0:qn], [OM], [Dk["OMT"]])
            for _ in gla:
                pass
            self.gla_out(l, st)
            kb.S.flush()

    def _attn_pv(self, item, keys, po, pt, va, nb, h, qn, cnt):
        kb = self.kb
        kt, ps = item
        PT = pt[cnt % 4]
        kb.act(PT.t[:, 0:qn], ps.t[:, 0:qn], AF.Exp, [ps, nb], [PT], bias=nb.t[:, h:h + 1], scale=1.0)
        kb.mm(po.t[0:65, 0:qn], va.t[:, kt, h * 65:(h + 1) * 65], PT.t[:, 0:qn], kt == keys[0], kt == keys[-1], [va, PT], [po])

    def phase_fnet(self, l):
        kb, P, C, Dr, Dk = self.kb, self.P, self.C, self.Dr, self.Dk
        with contextlib.ExitStack() as st:
            A = kb.sb(st, "fA", [128, NT, 512], BF16)
            Bm = kb.sb(st, "fB", [128, NT, 512], BF16)
            cs = kb.sb(st, "cs", [128, 256], BF16)
            kb.dma("sp", cs.t[:], C["cs128"], [], [cs])
            uf = [kb.sb(st, "uf%d" % j, [128, 4, 128], BF16) for j in range(2)]
            import os
            for i in range(int(os.environ.get("FN_TILES", NT))):
                U = uf[i % 2]
                kb.dma("sp", U.t[:], Dr["UFT"][:, :, i * 128:(i + 1) * 128].rearrange("g c t -> c g t"), [Dk["UFT"]], [U])
                for half in range(2):
                    ps = self.psum()
                    for g2 in range(2):
                        g = half * 2 + g2
                        kb.mm(ps.t[:, g2 * 256:(g2 + 1) * 256], U.t[:, g, :], cs.t[:], True, True, [U, cs], [ps])
                    psv = ps.t[:].rearrange("p (g x m) -> p g x m", g=2, x=2)
                    kb.cp("act", A.t[:, i, half * 256:(half + 1) * 256].rearrange("p (g m) -> p g m", g=2), psv[:, :, 0, :], [ps], [A])
                    kb.cp("dve", Bm.t[:, i, half * 256:(half + 1) * 256].rearrange("p (g m) -> p g m", g=2), psv[:, :, 1, :], [ps], [Bm])
            dc = [kb.sb(st, "dc%d" % j, [128, 32, 128], BF16) for j in range(2)]
            ds = [kb.sb(st, "ds%d" % j, [128, 32, 128], BF16) for j in range(2)]
            y = [kb.sb(st, "fy%d" % j, [128, 512], BF16) for j in range(2)]
            oft = [kb.sb(st, "oft%d" % j, [128, 4, 128], BF16) for j in range(2)]
            idb = P["ident_b"]
            jobs = [("c", kt) for kt in range(2)] + [("l", kt) for kt in range(32)]
            import os
            if os.environ.get("FN_PART") == "1":
                jobs = []
            if os.environ.get("FN_PART") == "2":
                jobs = jobs[:2]
            for n, (kind, kt) in enumerate(jobs):
                DC, DS, Y, OF = dc[n % 2], ds[n % 2], y[n % 2], oft[n % 2]
                if kind == "c":
                    ntt, t0, tok0 = 2, 0, kt * 128
                    kb.dma("sp", DC.t[:, 0:2, :], C["dft_c2"][kt], [], [DC])
                    kb.dma("pool", DS.t[:, 0:2, :], C["dft_s2"][kt], [], [DS])
                else:
                    ntt, t0, tok0 = 32, 2, CTX + kt * 128
                    kb.dma("sp", DC.t[:], C["dft_c"][kt], [], [DC])
                    kb.dma("pool", DS.t[:], C["dft_s"][kt], [], [DS])
                ps = self.psum()
                for tt in range(ntt):
                    kb.mm(ps.t[:], DC.t[:, tt, :], A.t[:, t0 + tt, :], tt == 0, False, [DC, A], [ps])
                    kb.mm(ps.t[:], DS.t[:, tt, :], Bm.t[:, t0 + tt, :], False, tt == ntt - 1, [DS, Bm], [ps])
                kb.cp("act", Y.t[:], ps.t[:], [ps], [Y])
                p2 = self.psum()
                p2b = p2.t[:].bitcast(BF16)
                for g in range(4):
                    kb.tr(p2b[:, g * 128:(g + 1) * 128], Y.t[:, g * 128:(g + 1) * 128], idb.t[:], [Y, idb], [p2])
                kb.cp("dve", OF.t[:].rearrange("p g t -> p (g t)"), p2b[:, 0:512], [p2], [OF])
                kb.dma("sp", Dr["OFT"][:, :, tok0:tok0 + 128].rearrange("g m t -> m g t"), OF.t[:], [OF], [Dk["OFT"]])
            kb.S.flush()

    def gla_gen(self, l, st):
        kb, P, C, W, Dr, Dk = self.kb, self.P, self.C, self.W, self.Dr, self.Dk
        tri = kb.sb(st, "tri", [128, 2, 128], F32)
        mask4 = kb.sb(st, "mask4", [128, 2, 512], F32)
        for d in range(2):
            kb.dma("sp", tri.t[:, d, :], C["tri_f"][d], [], [tri])
            kb.dma("sp", mask4.t[:, d, :], C["mask4"][d].rearrange("p h t -> p (h t)"), [], [mask4])
        S32 = [kb.sb(st, "S32_%d" % d, [64, 4, 128], F32) for d in range(2)]
        Sb = [kb.sb(st, "Sb_%d" % d, [64, 4, 128], BF16) for d in range(2)]
        NB2 = 2

        def mk(name, shp, dt):
            return [[kb.sb(st, "%s%d_%d" % (name, d, j), shp, dt) for j in range(NB2)] for d in range(2)]
        qT = mk("gqT", [64, 4, 128], BF16)
        kT = mk("gkT", [64, 4, 128], BF16)
        gk = mk("ggk", [128, 768], BF16)
        lf = mk("glf", [128, 256], F32)
        gtok = mk("gtok", [128, 256], F32)
        e1 = mk("ge1", [128, 256], F32)
        khat = mk("khat", [128, 256], BF16)
        eq = mk("geq", [64, 4, 128], F32)
        ek = mk("gek", [64, 4, 128], F32)
        qtl = mk("qtl", [64, 4, 128], BF16)
        ktl = mk("ktl", [64, 4, 128], BF16)
        at = mk("gat", [128, 512], BF16)
        o32 = mk("go32", [128, 512], F32)
        orders = [list(range(NT)), [1, 0] + list(range(NT - 1, 1, -1))]
        lasts = [127, 0]
        outs = ["OFW", "OBW"]
        for d in range(2):
            kb.memset("dve", S32[d].t[:], 0.0, [], [S32[d]])
            kb.memset("pool", Sb[d].t[:], 0.0, [], [Sb[d]])

        def load(n, d):
            b = n % NB2
            i = orders[d][n]
            sl = slice(i * 128, (i + 1) * 128)
            kb.dma("sp", qT[d][b].t[:], Dr["GQT"][:, :, sl].rearrange("h d t -> d h t"), [Dk["GQT"]], [qT[d][b]])
            kb.dma("sp", kT[d][b].t[:], Dr["GKT"][:, :, sl].rearrange("h d t -> d h t"), [Dk["GKT"]], [kT[d][b]])
            kb.dma("sp", gk[d][b].t[:], Dr["GKVO"][sl, 0:768], [Dk["GKVO"]], [gk[d][b]])
            kb.dma("sp", lf[d][b].t[:], Dr["LFB"][sl, d * 256:(d + 1) * 256], [Dk["LFB"]], [lf[d][b]])
        load(0, 0)
        load(0, 1)
        for n in range(NT):
            for d in range(2):
                b = n % NB2
                i = orders[d][n]
                sl = slice(i * 128, (i + 1) * 128)
                last = lasts[d]
                if n + 1 < NT:
                    load(n + 1, d)
                LF, GK = lf[d][b], gk[d][b]
                GT, E1, KH, EQ, EK, QTL, KTL, AT, O32 = gtok[d][b], e1[d][b], khat[d][b], eq[d][b], ek[d][b], qtl[d][b], ktl[d][b], at[d][b], o32[d][b]
                pg = self.psum()
                kb.mm(pg.t[:, 0:256], tri.t[:, d, :], LF.t[:], True, True, [tri, LF], [pg])
                kb.mm(pg.t[:, 256:512], P["ones_f"].t[:], LF.t[:], True, True, [P["ones_f"], LF], [pg])
                pf = self.psum()
                for h in range(4):
                    kb.mm(pf.t[0:64, h * 128:(h + 1) * 128], LF.t[:, h * 64:(h + 1) * 64], tri.t[:, d, :], True, True, [LF, tri], [pf])
                kb.cp("act", GT.t[:], pg.t[:, 0:256], [pg], [GT])
                kb.tt("dve", E1.t[:], pg.t[:, 256:512], GT.t[:], ALU.subtract, [pg, GT], [E1])
                kb.act(E1.t[:], E1.t[:], AF.Exp, [E1], [E1])
                kb.tt("dve", KH.t[:], GK.t[:, 0:256], E1.t[:], ALU.mult, [GK, E1], [KH])
                kb.act(EQ.t[:].rearrange("p h t -> p (h t)"), pf.t[0:64, :], AF.Exp, [pf], [EQ])
                kb.act(EK.t[:].rearrange("p h t -> p (h t)"), pf.t[0:64, :], AF.Exp, [pf], [EK], scale=-1.0)
                kb.tt("dve", QTL.t[:], qT[d][b].t[:], EQ.t[:], ALU.mult, [qT[d][b], EQ], [QTL])
                kb.tt("pool", KTL.t[:], kT[d][b].t[:], EK.t[:], ALU.mult, [kT[d][b], EK], [KTL])
                yield
                pa = self.psum()
                for h in range(4):
                    kb.mm(pa.t[:, h * 128:(h + 1) * 128], KTL.t[:, h, :], QTL.t[:, h, :], True, True, [KTL, QTL], [pa])
                kb.tt("dve", AT.t[:], pa.t[:], mask4.t[:, d, :], ALU.mult, [pa, mask4], [AT])
                yield
                po = self.psum()
                for h in range(4):
                    kb.mm(po.t[:, h * 128:(h + 1) * 128], QTL.t[:, h, :], Sb[d].t[:, h, :], True, False, [QTL, Sb[d]], [po])
                    kb.mm(po.t[:, h * 128:(h + 1) * 128], AT.t[:, h * 128:(h + 1) * 128],
                          GK.t[:, 256 + h * 128:256 + (h + 1) * 128], False, True, [AT, GK], [po])
                pS = self.psum()
                for h in range(4):
                    kb.mm(pS.t[0:64, h * 128:(h + 1) * 128], KH.t[:, h * 64:(h + 1) * 64],
                          GK.t[:, 256 + h * 128:256 + (h + 1) * 128], True, True, [KH, GK], [pS])
                for h in range(4):
                    kb.stt("dve", S32[d].t[:, h, :], S32[d].t[:, h, :], EQ.t[:, h, last:last + 1], pS.t[0:64, h * 128:(h + 1) * 128],
                           ALU.mult, ALU.add, [S32[d], EQ, pS], [S32[d]])
                kb.cp("pool", Sb[d].t[:], S32[d].t[:], [S32[d]], [Sb[d]])
                kb.cp("act", O32.t[:], po.t[:], [po], [O32])
                kb.dma("sp", Dr[outs[d]][sl, :], O32.t[:], [O32], [Dk[outs[d]]])
                yield

    def gla_out(self, l, st):
        kb, P, W, Dr, Dk = self.kb, self.P, self.W, self.Dr, self.Dk
        idb = P["ident_b"]
        gn = kb.sb(st, "gn", [128, 512], F32)
        kb.dma("sp", gn.t[:], W["gla_norm_g"][l].partition_broadcast(128), [], [gn])
        NB2 = 2

        def mk(name, shp, dt):
            return [kb.sb(st, "%s%d" % (name, j), shp, dt) for j in range(NB2)]
        ofw = mk("oofw", [128, 512], F32)
        obw = mk("oobw", [128, 512], F32)
        og = mk("oog", [128, 512], BF16)
        sqo = mk("osq", [128, 512], F32)
        st4 = mk("ost4", [128, 8], F32)
        ob = mk("oob", [128, 512], BF16)
        ogt = mk("oogt", [128, 4, 128], BF16)
        for i in range(NT):
            b = i % NB2
            sl = slice(i * 128, (i + 1) * 128)
            kb.dma("sp", ofw[b].t[:], Dr["OFW"][sl, :], [Dk["OFW"]], [ofw[b]])
            kb.dma("sp", obw[b].t[:], Dr["OBW"][sl, :], [Dk["OBW"]], [obw[b]])
            kb.dma("sp", og[b].t[:], Dr["GKVO"][sl, 768:1280], [Dk["GKVO"]], [og[b]])
            O, SQ, S4 = ofw[b], sqo[b], st4[b]
            kb.tt("dve", O.t[:], O.t[:], obw[b].t[:], ALU.add, [O, obw[b]], [O])
            kb.tt("pool", SQ.t[:], O.t[:], O.t[:], ALU.mult, [O], [SQ])
            kb.red("dve", S4.t[:, 0:4], SQ.t[:].rearrange("p (h e) -> p h e", h=4), ALU.add, [SQ], [S4])
            kb.ts("dve", S4.t[:, 0:4], S4.t[:, 0:4], 1.0 / 128, EPS, ALU.mult, ALU.add, [S4], [S4])
            kb.act(S4.t[:, 0:4], S4.t[:, 0:4], AF.Sqrt, [S4], [S4])
            kb.recip(S4.t[:, 4:8], S4.t[:, 0:4], [S4], [S4])
            for h in range(4):
                kb.stt("dve", O.t[:, h * 128:(h + 1) * 128], O.t[:, h * 128:(h + 1) * 128], S4.t[:, 4 + h:5 + h],
                       gn.t[:, h * 128:(h + 1) * 128], ALU.mult, ALU.mult, [O, S4, gn], [O])
            kb.tt("pool", ob[b].t[:], O.t[:], og[b].t[:], ALU.mult, [O, og[b]], [ob[b]])
            p2 = self.psum()
            p2b = p2.t[:].bitcast(BF16)
            for g in range(4):
                kb.tr(p2b[:, g * 128:(g + 1) * 128], ob[b].t[:, g * 128:(g + 1) * 128], idb.t[:], [ob[b], idb], [p2])
            kb.cp("act", ogt[b].t[:].rearrange("p g t -> p (g t)"), p2b[:, 0:512], [p2], [ogt[b]])
            kb.dma("sp", Dr["OGT"][:, :, sl].rearrange("g m t -> m g t"), ogt[b].t[:], [ogt[b]], [Dk["OGT"]])

    def phase_merge(self, l):
        kb, P, W, Dr, Dk = self.kb, self.P, self.W, self.Dr, self.Dk
        with contextlib.ExitStack() as st:
            rows = self.mod_rows(st, l, 1, names=("G",))
            wbm = kb.sb(st, "wbm", [64, 8, D], BF16)
            wbf = kb.sb(st, "wbf", [128, 4, D], BF16)
            wbg = kb.sb(st, "wbg", [128, 4, D], BF16)
            wo = kb.sb(st, "wo", [128, 8, D], BF16)
            kb.dma("pool", wbm.t[:], W["w_br_mla"][l].rearrange("(h d) n -> d h n", d=64), [], [wbm])
            kb.dma("pool", wbf.t[:], W["w_br_fnet"][l].rearrange("(k p) n -> p k n", p=128), [], [wbf])
            kb.dma("pool", wbg.t[:], W["w_br_gla"][l].rearrange("(k p) n -> p k n", p=128), [], [wbg])
            kb.dma("pool", wo.t[:], W["w_o"][l].rearrange("(k p) n -> p k n", p=128), [], [wo])
            NB2 = 2
            def mk(name, shp, dt):
                return [kb.sb(st, "%s%d" % (name, j), shp, dt) for j in range(NB2)]
            omT = mk("momT", [64, 8, 128], BF16)
            ofT = mk("mofT", [128, 4, 128], BF16)
            ogT = mk("mogT", [128, 4, 128], BF16)
            ga = mk("mga", [128, 3072], BF16)
            xt = mk("mxt", [128, D], F32)
            y32 = mk("my32", [128, D], F32)
            t32 = mk("mt32", [128, D], F32)
            yb = mk("myb", [128, D], BF16)
            yT = mk("myT", [128, 8, 128], BF16)
            xn = mk("mxn", [128, D], F32)
            idb = P["ident_b"]
            for i in range(NT):
                b = i % NB2
                sl = slice(i * 128, (i + 1) * 128)
                G = rows["Gc" if i < 2 else "Gl"]
                kb.dma("sp", omT[b].t[:], Dr["OMT"][:, :, sl].rearrange("h d t -> d h t"), [Dk["OMT"]], [omT[b]])
                kb.dma("sp", ofT[b].t[:], Dr["OFT"][:, :, sl].rearrange("g m t -> m g t"), [Dk["OFT"]], [ofT[b]])
                kb.dma("sp", ogT[b].t[:], Dr["OGT"][:, :, sl].rearrange("g m t -> m g t"), [Dk["OGT"]], [ogT[b]])
                kb.dma("sp", ga[b].t[:], Dr["GATE"][sl, :], [Dk["GATE"]], [ga[b]])
                kb.dma("sp", xt[b].t[:], Dr["XR"][sl, :], [Dk["XR"]], [xt[b]])
                for half in range(2):
                    cs_ = slice(half * 512, (half + 1) * 512)
                    pm = self.psum()
                    for h in range(8):
                        kb.mm(pm.t[:], omT[b].t[:, h, :], wbm.t[:, h, cs_], h == 0, h == 7, [omT[b], wbm], [pm])
                    pf = self.psum()
                    for k in range(4):
                        kb.mm(pf.t[:], ofT[b].t[:, k, :], wbf.t[:, k, cs_], k == 0, k == 3, [ofT[b], wbf], [pf])
                    pg = self.psum()
                    for k in range(4):
                        kb.mm(pg.t[:], ogT[b].t[:, k, :], wbg.t[:, k, cs_], k == 0, k == 3, [ogT[b], wbg], [pg])
                    Y, T32 = y32[b], t32[b]
                    kb.tt("dve", Y.t[:, cs_], pm.t[:], ga[b].t[:, half * 512:(half + 1) * 512], ALU.mult, [pm, ga[b]], [Y])
                    kb.tt("dve", T32.t[:, cs_], pf.t[:], ga[b].t[:, 1024 + half * 512:1024 + (half + 1) * 512], ALU.mult, [pf, ga[b]], [T32])
                    kb.tt("pool", Y.t[:, cs_], Y.t[:, cs_], T32.t[:, cs_], ALU.add, [Y, T32], [Y])
                    kb.tt("dve", T32.t[:, cs_], pg.t[:], ga[b].t[:, 2048 + half * 512:2048 + (half + 1) * 512], ALU.mult, [pg, ga[b]], [T32])
                    kb.tt("pool", yb[b].t[:, cs_], Y.t[:, cs_], T32.t[:, cs_], ALU.add, [Y, T32], [yb[b]])
                p2 = self.psum()
                p2b = p2.t[:].bitcast(BF16)
                for kc in range(8):
                    kb.tr(p2b[:, kc * 128:(kc + 1) * 128], yb[b].t[:, kc * 128:(kc + 1) * 128], idb.t[:], [yb[b], idb], [p2])
                kb.cp("act", yT[b].t[:].rearrange("p k t -> p (k t)"), p2b, [p2], [yT[b]])
                for half in range(2):
                    cs_ = slice(half * 512, (half + 1) * 512)
                    pz = self.psum()
                    for kc in range(8):
                        kb.mm(pz.t[:], yT[b].t[:, kc, :], wo.t[:, kc, cs_], kc == 0, kc == 7, [yT[b], wo], [pz])
                    kb.tt("dve", xn[b].t[:, cs_], pz.t[:], G.t[:, cs_], ALU.mult, [pz, G], [xn[b]])
                    kb.tt("pool", xn[b].t[:, cs_], xn[b].t[:, cs_], xt[b].t[:, cs_], ALU.add, [xn[b], xt[b]], [xn[b]])
                kb.dma("sp", Dr["XR"][sl, :], xn[b].t[:], [xn[b]], [Dk["XR"]])
            kb.S.flush()

    def phase_moe(self, l):
        kb, P, W, C, Dr, Dk = self.kb, self.P, self.W, self.C, self.Dr, self.Dk
        is_last = (l == self.nl - 1)
        idb = P["ident_b"]
        with contextlib.ExitStack() as st0:
            DSTI = kb.sb(st0, "DSTI", [128, NT, 4], I32)
            G4 = kb.sb(st0, "G4", [128, NT, 4], F32)
            IDXW = kb.sb(st0, "IDXW", [128, NBLK, 8], I32)
            EBI = kb.sb(st0, "EBI", [128, NBLK], I32)
            with contextlib.ExitStack() as st:
                rows = self.mod_rows(st, l, 2, names=("A", "S"))
                rw = kb.sb(st, "rw", [128, 8, NE], F32)
                rb = kb.sb(st, "rb", [1, NE], F32)
                trx = kb.sb(st, "trx", [128, 128], BF16)
                jv = kb.sb(st, "jv", [128, NBLK], F32)
                kp = kb.sb(st, "kp", [128, 8], F32)
                kb.dma("sp", rw.t[:], W["router_w"][l].rearrange("(k p) e -> p k e", p=128), [], [rw])
                kb.dma("sp", rb.t[:], W["router_b"][l].rearrange("(o e) -> o e", o=1), [], [rb])
                kb.dma("sp", trx.t[:], C["tri_x"], [], [trx])
                kb.dma("sp", jv.t[:], C["jv"], [], [jv])
                kb.dma("sp", kp.t[:], C["kp"], [], [kp])
                LG = kb.sb(st, "LG", [128, NT, NE], F32)
                GF = kb.sb(st, "GF", [128, NT, NE], F32)
                POS = kb.sb(st, "POS", [128, NT, NE], F32)
                TOP = kb.sb(st, "TOP", [128, NT, 8], F32)
                cnt = kb.sb(st, "cnt", [128, NE], F32)
                kb.memset("dve", cnt.t[:], 0.0, [], [cnt])
                NB2 = 2
                def mk(name, shp, dt):
                    return [kb.sb(st, "%s%d" % (name, j), shp, dt) for j in range(NB2)]
                xt = mk("ext", [128, D], F32)
                junk = kb.sb(st, "ejunk", [128, D], F32)
                ssq = mk("essq", [128, 8], F32)
                h32 = mk("eh32", [128, D], F32)
                hb = mk("ehb", [128, D], BF16)
                h2T = mk("eh2T", [128, 8, 128], F32)
                sm = mk("esm", [128, 4, NE], F32)
                sc = mk("esc", [128, 8], F32)
                mkb = mk("emkb", [128, NE], BF16)
                for i in range(NT):
                    b = i % NB2
                    sl = slice(i * 128, (i + 1) * 128)
                    kb.dma("sp", xt[b].t[:], Dr["XR"][sl, :], [Dk["XR"]], [xt[b]])
                    kb.memset("pool", ssq[b].t[:], 0.0, [], [ssq[b]])
                    self.norm_mod(xt[b], rows, i, junk, ssq[b], h32[b], hb[b], full32=True)
                    kb.dma("sp", Dr["H2B"][sl, :], hb[b].t[:], [hb[b]], [Dk["H2B"]])
                    for half in range(2):
                        pt_ = self.psum()
                        for k4 in range(4):
                            kc = half * 4 + k4
                            kb.tr(pt_.t[:, k4 * 128:(k4 + 1) * 128], h32[b].t[:, kc * 128:(kc + 1) * 128], P["ident_f"].t[:],
                                  [h32[b], P["ident_f"]], [pt_])
                        kb.cp("act" if half == 0 else "dve", h2T[b].t[:, half * 4:(half + 1) * 4, :].rearrange("p k t -> p (k t)"),
                              pt_.t[:], [pt_], [h2T[b]])
                    pl = self.psum()
                    for kc in range(8):
                        kb.mm(pl.t[:, 0:NE], h2T[b].t[:, kc, :], rw.t[:, kc, :], kc == 0, False, [h2T[b], rw], [pl])
                    kb.mm(pl.t[:, 0:NE], P["ones_f"].t[0:1, :], rb.t[0:1, :], False, True, [P["ones_f"], rb], [pl])
                    kb.cp("act", LG.t[:, i, :], pl.t[:, 0:NE], [pl], [LG])
                    kb.op("dve", lambda e, o=TOP.t[:, i, :], a=LG.t[:, i, :]: e.max(out=o, in_=a), [LG], [TOP])
                    SM, SC = sm[b], sc[b]
                    kb.ts("dve", SM.t[:, 0, :], LG.t[:, i, :], TOP.t[:, i, 3:4], None, ALU.is_ge, None, [LG, TOP], [SM])
                    kb.ts("dve", SC.t[:, 0:1], TOP.t[:, i, 0:1], -1.0, None, ALU.mult, None, [TOP], [SC])
                    kb.act(SM.t[:, 1, :], LG.t[:, i, :], AF.Exp, [LG, SC], [SM], bias=SC.t[:, 0:1], scale=1.0)
                    kb.tt("dve", SM.t[:, 2, :], SM.t[:, 1, :], SM.t[:, 0, :], ALU.mult, [SM], [SM])
                    kb.red("dve", SC.t[:, 1:2], SM.t[:, 2, :], ALU.add, [SM], [SC])
                    kb.recip(SC.t[:, 2:3], SC.t[:, 1:2], [SC], [SC])
                    kb.ts("dve", GF.t[:, i, :], SM.t[:, 2, :], SC.t[:, 2:3], None, ALU.mult, None, [SM, SC], [GF])
                    kb.cp("pool", mkb[b].t[:], SM.t[:, 0, :], [SM], [mkb[b]])
                    pp = self.psum()
                    kb.mm(pp.t[:, 0:NE], trx.t[:], mkb[b].t[:], True, True, [trx, mkb[b]], [pp])
                    kb.mm(pp.t[:, NE:2 * NE], P["ones_b"].t[:], mkb[b].t[:], True, True, [P["ones_b"], mkb[b]], [pp])
                    kb.tt("dve", POS.t[:, i, :], pp.t[:, 0:NE], cnt.t[:], ALU.add, [pp, cnt], [POS])
                    kb.tt("dve", cnt.t[:], pp.t[:, NE:2 * NE], cnt.t[:], ALU.add, [pp, cnt], [cnt])
                nbk = kb.sb(st, "nbk", [128, NE], F32)
                pend = kb.sb(st, "pend", [128, NE], F32)
                pstart = kb.sb(st, "pstart", [128, NE], F32)
                eb = kb.sb(st, "eb", [128, NBLK], F32)
                idxf = kb.sb(st, "idxf", [128, NBLK, 8], F32)
                kb.memset("dve", nbk.t[:], 0.0, [], [nbk])
                for j in range(T // BS + 1):
                    kb.stt("dve", nbk.t[:], cnt.t[:], float(j * BS), nbk.t[:], ALU.is_gt, ALU.add, [cnt, nbk], [nbk])
                kb.ts("dve", nbk.t[:], nbk.t[:], float(BS), None, ALU.mult, None, [nbk], [nbk])
                kb.cp("dve", pend.t[:, 0:1], nbk.t[:, 0:1], [nbk], [pend])
                for e_ in range(1, NE):
                    kb.tt("dve", pend.t[:, e_:e_ + 1], pend.t[:, e_ - 1:e_], nbk.t[:, e_:e_ + 1], ALU.add, [pend, nbk], [pend])
                kb.tt("dve", pstart.t[:], pend.t[:], nbk.t[:], ALU.subtract, [pend, nbk], [pstart])
                kb.memset("dve", eb.t[:], 0.0, [], [eb])
                for e_ in range(NE):
                    kb.stt("dve", eb.t[:], jv.t[:], pend.t[:, e_:e_ + 1], eb.t[:], ALU.is_ge, ALU.add, [jv, pend, eb], [eb])
                kb.ts("dve", eb.t[:], eb.t[:], float(NE - 1), None, ALU.min, None, [eb], [eb])
                same2 = kb.sb(st, "same2", [128, NBLK], F32)
                kb.memset("dve", same2.t[:], 0.0, [], [same2])
                kb.tt("dve", same2.t[:, 2:NBLK], eb.t[:, 2:NBLK], eb.t[:, 0:NBLK - 2], ALU.is_equal, [eb], [same2])
                kb.ts("dve", same2.t[:], same2.t[:], float(1 << 20), None, ALU.mult, None, [same2], [same2])
                kb.ts("dve", kp.t[:], kp.t[:], float(l * NE * D), None, ALU.add, None, [kp], [kp])
                for kc in range(8):
                    kb.ts("dve", idxf.t[:, :, kc], eb.t[:], 1024.0, kp.t[:, kc:kc + 1], ALU.mult, ALU.add, [eb, kp], [idxf])
                    kb.tt("dve", idxf.t[:, :, kc], idxf.t[:, :, kc], same2.t[:], ALU.add, [idxf, same2], [idxf])
                kb.cp("dve", IDXW.t[:], idxf.t[:], [idxf], [IDXW])
                kb.ts("dve", eb.t[:], eb.t[:], float(l * NE), None, ALU.add, None, [eb], [eb])
                kb.tt("dve", eb.t[:], eb.t[:], same2.t[:], ALU.add, [eb, same2], [eb])
                kb.cp("dve", EBI.t[:], eb.t[:], [eb], [EBI])
                dstf = mk("edstf", [128, 4], F32)
                hs = mk("ehs", [128, D], BF16)
                for i in range(NT):
                    b = i % NB2
                    sl = slice(i * 128, (i + 1) * 128)
                    SM = sm[b]
                    kb.tt("dve", SM.t[:, 3, :], POS.t[:, i, :], pstart.t[:], ALU.add, [POS, pstart], [SM])
                    for k in range(4):
                        kb.ts("dve", SM.t[:, 0, :], LG.t[:, i, :], TOP.t[:, i, k:k + 1], None, ALU.is_equal, None, [LG, TOP], [SM])
                        kb.tt("dve", SM.t[:, 1, :], SM.t[:, 0, :], SM.t[:, 3, :], ALU.mult, [SM], [SM])
                        kb.red("dve", dstf[b].t[:, k:k + 1], SM.t[:, 1, :], ALU.add, [SM], [dstf[b]])
                        kb.tt("dve", SM.t[:, 2, :], SM.t[:, 0, :], GF.t[:, i, :], ALU.mult, [SM, GF], [SM])
                        kb.red("dve", G4.t[:, i, k:k + 1], SM.t[:, 2, :], ALU.add, [SM], [G4])
                    kb.cp("dve", DSTI.t[:, i, :], dstf[b].t[:], [dstf[b]], [DSTI])
                    kb.dma("sp", hs[b].t[:], Dr["H2B"][sl, :], [Dk["H2B"]], [hs[b]])
                    for k in range(4):
                        kb.scatter(Dr["XS"], hs[b].t[:], DSTI.t[:, i, k:k + 1], [hs[b], DSTI], [Dk["XS"]], NROWS - 1)
                if "DBG_DST" in self.dbg:
                    kb.dma("sp", Dr["DBG_DST"], DSTI.t[:].rearrange("p n k -> p (n k)"), [DSTI], [Dk["DBG_DST"]])
                    kb.dma("sp", Dr["DBG_G4"], G4.t[:].rearrange("p n k -> p (n k)"), [G4], [Dk["DBG_G4"]])
                    kb.dma("sp", Dr["DBG_EB"], EBI.t[:], [EBI], [Dk["DBG_EB"]])
                    kb.dma("sp", Dr["DBG_LG"], LG.t[:].rearrange("p n k -> p (n k)"), [LG], [Dk["DBG_LG"]])
                    kb.dma("sp", Dr["DBG_CNT"], cnt.t[:], [cnt], [Dk["DBG_CNT"]])
                kb.S.flush()
            with contextlib.ExitStack() as st:
                wup = [kb.sb(st, "wup%d" % j, [128, 8, 2 * D], BF16) for j in range(2)]
                wdn = [kb.sb(st, "wdn%d" % j, [128, 8, D], BF16) for j in range(2)]
                wupk = [[Tok() for _ in range(8)] for _ in range(2)]
                wdnk = [[Tok() for _ in range(8)] for _ in range(2)]
                bu = [kb.sb(st, "bu%d" % j, [2, 2 * D], BF16) for j in range(2)]
                bd = [kb.sb(st, "bd%d" % j, [2, D], BF16) for j in range(2)]
                NB2 = 2
                def mk(name, shp, dt):
                    return [kb.sb(st, "%s%d" % (name, j), shp, dt) for j in range(NB2)]
                xs = mk("xs", [128, D], BF16)
                xT = mk("xT", [128, 8, 128], BF16)
                gl = mk("gl", [128, 512], F32)
                li = mk("li", [128, 512], F32)
                sg = mk("sg", [128, 512], F32)
                ab = mk("ab", [128, D], BF16)
                aT = mk("aT", [128, 8, 128], BF16)
                yb = mk("yb", [128, D], F32)
                wup_src = W["exp_w_up"].rearrange("l e k n -> (l e k) n")
                wdn_src = W["exp_w_down"].rearrange("l e k n -> (l e k) n")
                bup_src = W["exp_b_up"].rearrange("l e n -> (l e) n")
                bdn_src = W["exp_b_down"].rearrange("l e n -> (l e) n")
                nlw = W["exp_w_up"].shape[0]
                ones1 = P["ones_b"].t[0:1, :]

                def load_w(j):
                    jb = j % 2
                    for kc in range(8):
                        kb.gather(wup[jb].t[:, kc, :], wup_src, IDXW.t[:, j, kc:kc + 1], [IDXW], [wupk[jb][kc]], nlw * NE * D - 1)
                    for kc in range(8):
                        kb.gather(wdn[jb].t[:, kc, :], wdn_src, IDXW.t[:, j, kc:kc + 1], [IDXW], [wdnk[jb][kc]], nlw * NE * D - 1)
                    kb.gather(bu[jb].t[:], bup_src, EBI.t[0:2, j:j + 1], [EBI], [bu[jb]], nlw * NE - 1)
                    kb.gather(bd[jb].t[:], bdn_src, EBI.t[0:2, j:j + 1], [EBI], [bd[jb]], nlw * NE - 1)

                def load_x(n):
                    kb.dma("sp", xs[n % NB2].t[:], Dr["XS"][n * 128:(n + 1) * 128, :], [Dk["XS"]], [xs[n % NB2]])

                NSUB = NBLK * SUB

                def stage_a(n):
                    b = n % NB2
                    if n + 1 < NSUB:
                        load_x(n + 1)
                    p2 = self.psum()
                    p2b = p2.t[:].bitcast(BF16)
                    for kc in range(8):
                        kb.tr(p2b[:, kc * 128:(kc + 1) * 128], xs[b].t[:, kc * 128:(kc + 1) * 128], idb.t[:], [xs[b], idb], [p2])
                    kb.cp("act", xT[b].t[:].rearrange("p k t -> p (k t)"), p2b, [p2], [xT[b]])

                def stage_u(n):
                    b = n % NB2
                    jb = (n // SUB) % 2
                    WU, BU = wup[jb], bu[jb]
                    for pair in range(2):
                        pgl = self.psum()
                        pli = self.psum()
                        for (pp_, c0) in ((pgl, pair * 512), (pli, D + pair * 512)):
                            for kc in range(8):
                                kb.mm(pp_.t[:], xT[b].t[:, kc, :], WU.t[:, kc, c0:c0 + 512], kc == 0, False, [xT[b], wupk[jb][kc]], [pp_])
                            kb.mm(pp_.t[:], ones1, BU.t[0:1, c0:c0 + 512], False, True, [P["ones_b"], BU], [pp_])
                        GL, LI, SG = gl[pair], li[pair], sg[pair]
                        kb.ts("dve", GL.t[:], pgl.t[:], 7.0, None, ALU.min, None, [pgl], [GL])
                        kb.act(SG.t[:], GL.t[:], AF.Sigmoid, [GL], [SG], scale=1.702)
                        kb.ts("dve", LI.t[:], pli.t[:], 7.0, -7.0, ALU.min, ALU.max, [pli], [LI])
                        kb.stt("dve", LI.t[:], LI.t[:], 1.0, GL.t[:], ALU.add, ALU.mult, [LI, GL], [LI])
                        kb.tt("dve", ab[b].t[:, pair * 512:(pair + 1) * 512], LI.t[:], SG.t[:], ALU.mult, [LI, SG], [ab[b]])

                def stage_d(n):
                    b = n % NB2
                    jb = (n // SUB) % 2
                    WD, BD = wdn[jb], bd[jb]
                    r0 = n * 128
                    p3 = self.psum()
                    p3b = p3.t[:].bitcast(BF16)
                    for kc in range(8):
                        kb.tr(p3b[:, kc * 128:(kc + 1) * 128], ab[b].t[:, kc * 128:(kc + 1) * 128], idb.t[:], [ab[b], idb], [p3])
                    kb.cp("act", aT[b].t[:].rearrange("p k t -> p (k t)"), p3b, [p3], [aT[b]])
                    for half in range(2):
                        pd = self.psum()
                        for kc in range(8):
                            kb.mm(pd.t[:], aT[b].t[:, kc, :], WD.t[:, kc, half * 512:(half + 1) * 512], kc == 0, False, [aT[b], wdnk[jb][kc]], [pd])
                        kb.mm(pd.t[:], ones1, BD.t[0:1, half * 512:(half + 1) * 512], False, True, [P["ones_b"], BD], [pd])
                        kb.cp("act" if half == 0 else "dve", yb[b].t[:, half * 512:(half + 1) * 512], pd.t[:], [pd], [yb[b]])
                    kb.dma("sp", Dr["YB"][r0:r0 + 128, :], yb[b].t[:], [yb[b]], [Dk["YB"]])

                load_w(0)
                load_w(1)
                load_x(0)
                stage_a(0)
                stage_u(0)
                for n in range(NSUB):
                    if n + 1 < NSUB:
                        stage_a(n + 1)
                        stage_u(n + 1)
                    stage_d(n)
                    if (n + 1) % SUB == 0:
                        j = n // SUB
                        if j + 2 < NBLK:
                            load_w(j + 2)
                kb.S.flush()
            with contextlib.ExitStack() as st:
                rows = self.mod_rows(st, l, 2, names=("G",))
                NB2 = 2
                def mk(name, shp, dt):
                    return [kb.sb(st, "%s%d" % (name, j), shp, dt) for j in range(NB2)]
                xt = mk("cxt", [128, D], F32)
                yk = [kb.sb(st, "cyk%d" % j, [128, D], F32) for j in range(4)]
                acc = mk("cacc", [128, D], F32)
                xn = mk("cxn", [128, D], F32)
                if is_last:
                    fg = kb.sb(st, "fg", [128, D], F32)
                    kb.dma("sp", fg.t[:], W["final_norm_g"].partition_broadcast(128), [], [fg])
                    junk = kb.sb(st, "cjunk", [128, D], F32)
                    ssq = mk("cssq", [128, 8], F32)
                    ot = mk("cot", [128, D], F32)
                for i in range(NT):
                    b = i % NB2
                    sl = slice(i * 128, (i + 1) * 128)
                    G = rows["Gc" if i < 2 else "Gl"]
                    kb.dma("sp", xt[b].t[:], Dr["XR"][sl, :], [Dk["XR"]], [xt[b]])
                    for k in range(4):
                        kb.gather(yk[k].t[:], Dr["YB"], DSTI.t[:, i, k:k + 1], [DSTI, Dk["YB"]], [yk[k]], NROWS - 1)
                    A_ = acc[b]
                    kb.ts("dve", A_.t[:], yk[0].t[:], G4.t[:, i, 0:1], None, ALU.mult, None, [yk[0], G4], [A_])
                    for k in range(1, 4):
                        kb.stt("dve", A_.t[:], yk[k].t[:], G4.t[:, i, k:k + 1], A_.t[:], ALU.mult, ALU.add, [yk[k], G4, A_], [A_])
                    kb.tt("pool", A_.t[:], A_.t[:], G.t[:], ALU.mult, [A_, G], [A_])
                    kb.tt("dve", xn[b].t[:], A_.t[:], xt[b].t[:], ALU.add, [A_, xt[b]], [xn[b]])
                    kb.dma("sp", Dr["XR"][sl, :], xn[b].t[:], [xn[b]], [Dk["XR"]])
                    if is_last and i >= 2:
                        SS = ssq[b]
                        kb.memset("pool", SS.t[:], 0.0, [], [SS])
                        kb.act(junk.t[:], xn[b].t[:], AF.Square, [xn[b]], [junk, SS], accum=SS.t[:, 0:1])
                        kb.ts("dve", SS.t[:, 1:2], SS.t[:, 0:1], 1.0 / D, EPS, ALU.mult, ALU.add, [SS], [SS])
                        kb.act(SS.t[:, 1:2], SS.t[:, 1:2], AF.Sqrt, [SS], [SS])
                        kb.recip(SS.t[:, 2:3], SS.t[:, 1:2], [SS], [SS])
                        kb.stt("dve", ot[b].t[:], xn[b].t[:], SS.t[:, 2:3], fg.t[:], ALU.mult, ALU.mult, [xn[b], SS, fg], [ot[b]])
                        kb.dma("sp", self.out[(i - 2) * 128:(i - 1) * 128, :], ot[b].t[:], [ot[b]], [self.tout])
                kb.S.flush()


_CACHE = {}


def kernel(**inputs):
    n_cores = 8
    if "nc" not in _CACHE:
        pg = Prog(nl=DEPTH)
        _CACHE["nc"] = pg.build()
        _CACHE["consts"] = host_consts()
    nc = _CACHE["nc"]
    consts = _CACHE["consts"]
    shared = {k: np.ascontiguousarray(np.asarray(inputs[k], dtype=np.float32)) for k in WEIGHT_SPECS}
    shared["c_ctx"] = np.ascontiguousarray(np.asarray(inputs["c_ctx"], dtype=np.float32))
    shared.update(consts)
    x = np.asarray(inputs["x"], dtype=np.float32)
    c = np.asarray(inputs["c"], dtype=np.float32)
    ctx = np.asarray(inputs["ctx"], dtype=np.float32)
    in_maps = []
    for b in range(n_cores):
        m = dict(shared)
        m["x"] = np.ascontiguousarray(x[b])
        m["ctx"] = np.ascontiguousarray(ctx[b])
        m["c"] = np.ascontiguousarray(c[b])
        in_maps.append(m)
    res = run_bass_kernel_spmd(nc, in_maps, core_ids=list(range(n_cores)))
    out = np.stack([np.asarray(res.results[b]["out"], dtype=np.float32) for b in range(n_cores)], axis=0)
    return out
```
